# Optimizing a Trainium2 kernel written in Bass

```python
import math
import jax, jax.numpy as jnp
from jax import lax
import numpy as np

D_MODEL = 1024
BATCH = 1
SEQ = 16384
DEPTH = 4

N_MIXERS = 4
EPS = 1e-6
Q_BLOCK = 128
D_FF = 4 * D_MODEL

DSA_HEADS = 8
DSA_HEAD_DIM = D_MODEL // DSA_HEADS
IDX_HEADS = 8
IDX_DIM = 64
TOPK_MAX = 256
DSA_IN = 3 * D_MODEL + IDX_HEADS * IDX_DIM + IDX_DIM + IDX_HEADS

CONV_WIDTH = 31

MLA_HEADS = 16
MLA_Q_LORA = 384
MLA_KV_LORA = 256
MLA_NOPE = 64
MLA_ROPE = 32
MLA_V = 64
ROPE_THETA = 10000.0
MLA_IN = MLA_Q_LORA + MLA_KV_LORA + MLA_ROPE

GDN_HEADS = 8
GDN_DK = 128
GDN_DV = 128
GDN_CONV = 4
GDN_CHUNK = 64
GDN_HK = GDN_HEADS * GDN_DK
GDN_HV = GDN_HEADS * GDN_DV
GDN_IN = 2 * GDN_HK + 2 * GDN_HV + 2 * GDN_HEADS

kernel_name = 'hybrid_dsa_conformer_mla_gdn_trunk'


def rmsnorm(x, g):
    xf = x.astype(jnp.float32)
    y = xf * lax.rsqrt(jnp.mean(xf * xf, axis=-1, keepdims=True) + EPS)
    return (y * g.astype(jnp.float32)).astype(x.dtype)


def layernorm(x, g, b):
    xf = x.astype(jnp.float32)
    mu = jnp.mean(xf, axis=-1, keepdims=True)
    var = jnp.mean(jnp.square(xf - mu), axis=-1, keepdims=True)
    y = (xf - mu) * lax.rsqrt(var + EPS) * g.astype(jnp.float32) + b.astype(jnp.float32)
    return y.astype(x.dtype)


def l2norm(x):
    return x * lax.rsqrt(jnp.sum(x * x, axis=-1, keepdims=True) + EPS)


def causal_depthwise_conv(x, w):
    width = w.shape[0]
    xp = jnp.pad(x, ((0, 0), (width - 1, 0), (0, 0)))
    return lax.conv_general_dilated(xp, w[:, None, :].astype(x.dtype), window_strides=(1,), padding='VALID',
                                    dimension_numbers=('NWC', 'WIO', 'NWC'), feature_group_count=x.shape[-1])


def rope_tables(length, dim):
    pos = jnp.arange(length, dtype=jnp.float32)
    inv_freq = ROPE_THETA ** (-jnp.arange(0, dim, 2, dtype=jnp.float32) / dim)
    ang = pos[:, None] * inv_freq[None, :]
    return jnp.cos(ang), jnp.sin(ang)


def apply_rope(x, cos, sin):
    xf = x.astype(jnp.float32)
    half = x.shape[-1] // 2
    x1, x2 = xf[..., :half], xf[..., half:]
    return jnp.concatenate([x1 * cos - x2 * sin, x2 * cos + x1 * sin], axis=-1).astype(x.dtype)


def blocked_queries(fn, *qs):
    b, length = qs[0].shape[0], qs[0].shape[1]
    nb = length // Q_BLOCK
    blks = [jnp.moveaxis(q.reshape(b, nb, Q_BLOCK, *q.shape[2:]), 1, 0) for q in qs]
    t0 = jnp.arange(nb, dtype=jnp.int32) * Q_BLOCK
    out = lax.map(lambda a: fn(*a), (t0, *blks))
    out = jnp.moveaxis(out, 0, 1)
    return out.reshape(b, length, *out.shape[3:])


def dsa_mixer(h, w_in, idx_k_g, idx_k_b, w_out):
    b, length, _ = h.shape
    topk = min(TOPK_MAX, length // 4)
    proj = h @ w_in
    o1 = D_MODEL; o2 = 2 * D_MODEL; o3 = 3 * D_MODEL
    o4 = o3 + IDX_HEADS * IDX_DIM; o5 = o4 + IDX_DIM
    q = proj[..., :o1].reshape(b, length, DSA_HEADS, DSA_HEAD_DIM)
    k = proj[..., o1:o2].reshape(b, length, DSA_HEADS, DSA_HEAD_DIM)
    v = proj[..., o2:o3].reshape(b, length, DSA_HEADS, DSA_HEAD_DIM)
    qi = proj[..., o3:o4].reshape(b, length, IDX_HEADS, IDX_DIM).astype(jnp.float32)
    ki = layernorm(proj[..., o4:o5], idx_k_g, idx_k_b).astype(jnp.float32)
    wi = proj[..., o5:].astype(jnp.float32) * (IDX_HEADS ** -0.5 * IDX_DIM ** -0.5)
    key_pos = jnp.arange(length, dtype=jnp.int32)
    scale = DSA_HEAD_DIM ** -0.5

    def block(t0, qb, qib, wib):
        qpos = t0 + jnp.arange(Q_BLOCK, dtype=jnp.int32)
        causal = key_pos[None, :] <= qpos[:, None]
        s = jax.nn.relu(jnp.einsum('bqhd,bsd->bqhs', qib, ki))
        score = jnp.einsum('bqh,bqhs->bqs', wib, s)
        score = jnp.where(causal[None], score, -jnp.inf)
        _, sel = lax.top_k(score, topk)
        valid = sel <= qpos[None, :, None]
        k_sel = jax.vmap(lambda kk, ii: kk[ii])(k, sel)
        v_sel = jax.vmap(lambda vv, ii: vv[ii])(v, sel)
        logits = jnp.einsum('bqhd,bqkhd->bqhk', qb, k_sel).astype(jnp.float32) * scale
        logits = jnp.where(valid[:, :, None, :], logits, -jnp.inf)
        p = jax.nn.softmax(logits, axis=-1)
        return jnp.einsum('bqhk,bqkhd->bqhd', p.astype(v.dtype), v_sel)

    o = blocked_queries(block, q, qi, wi)
    return o.reshape(b, length, D_MODEL) @ w_out


def conv_mixer(h, w_pw1, b_pw1, w_dw, b_dw, ln_g, ln_b, w_pw2, b_pw2):
    a = h @ w_pw1 + b_pw1
    u = a[..., :D_MODEL] * jax.nn.sigmoid(a[..., D_MODEL:])
    u = causal_depthwise_conv(u, w_dw) + b_dw
    u = jax.nn.silu(layernorm(u, ln_g, ln_b))
    return u @ w_pw2 + b_pw2


def mla_mixer(h, w_in, q_norm_g, w_uq, kv_norm_g, w_ukv, w_out):
    b, length, _ = h.shape
    proj = h @ w_in
    cq = rmsnorm(proj[..., :MLA_Q_LORA], q_norm_g)
    ckv = rmsnorm(proj[..., MLA_Q_LORA:MLA_Q_LORA + MLA_KV_LORA], kv_norm_g)
    kr = proj[..., MLA_Q_LORA + MLA_KV_LORA:]
    q = (cq @ w_uq).reshape(b, length, MLA_HEADS, MLA_NOPE + MLA_ROPE)
    kv = (ckv @ w_ukv).reshape(b, length, MLA_HEADS, MLA_NOPE + MLA_V)
    q_nope, q_rope = q[..., :MLA_NOPE], q[..., MLA_NOPE:]
    k_nope, v = kv[..., :MLA_NOPE], kv[..., MLA_NOPE:]
    cos, sin = rope_tables(length, MLA_ROPE)
    q_rope = apply_rope(q_rope, cos[None, :, None, :], sin[None, :, None, :])
    kr = apply_rope(kr, cos[None], sin[None])
    key_pos = jnp.arange(length, dtype=jnp.int32)
    scale = (MLA_NOPE + MLA_ROPE) ** -0.5

    def block(t0, qn, qr):
        qpos = t0 + jnp.arange(Q_BLOCK, dtype=jnp.int32)
        causal = key_pos[None, :] <= qpos[:, None]
        logits = (jnp.einsum('bqhd,bshd->bhqs', qn, k_nope)
                  + jnp.einsum('bqhr,bsr->bhqs', qr, kr)).astype(jnp.float32) * scale
        logits = jnp.where(causal[None, None], logits, -jnp.inf)
        p = jax.nn.softmax(logits, axis=-1)
        return jnp.einsum('bhqs,bshd->bqhd', p.astype(v.dtype), v)

    o = blocked_queries(block, q_nope, q_rope)
    return o.reshape(b, length, MLA_HEADS * MLA_V) @ w_out


def chunk_gated_delta_rule(q, k, v, g, beta):
    b, length, nh, dk = q.shape
    dv = v.shape[-1]
    c = GDN_CHUNK
    n = length // c

    def to_chunks(x):
        return jnp.moveaxis(x.reshape(b, n, c, nh, *x.shape[3:]), 3, 1)

    q, k, v, g, beta = to_chunks(q), to_chunks(k), to_chunks(v), to_chunks(g), to_chunks(beta)
    gc = jnp.cumsum(g, axis=-1)
    tril = jnp.tril(jnp.ones((c, c), dtype=bool))
    tril_strict = jnp.tril(jnp.ones((c, c), dtype=bool), -1)
    diff = gc[..., :, None] - gc[..., None, :]
    decay_mat = jnp.where(tril, jnp.exp(jnp.where(tril, diff, 0.0)), 0.0)
    kk = jnp.einsum('bhncd,bhnsd->bhncs', k, k)
    a_mat = jnp.where(tril_strict, beta[..., :, None] * kk * decay_mat, 0.0)
    eye = jnp.eye(c, dtype=jnp.float32)
    rhs = jnp.concatenate([v * beta[..., None], k * (beta * jnp.exp(gc))[..., None]], axis=-1)
    sol = lax.linalg.triangular_solve(eye + a_mat, rhs, left_side=True, lower=True, unit_diagonal=True)
    u, w = sol[..., :dv], sol[..., dv:]
    qk = jnp.einsum('bhncd,bhnsd->bhncs', q, k) * decay_mat
    q_dec = q * jnp.exp(gc)[..., None]
    k_dec = k * jnp.exp(gc[..., -1:] - gc)[..., None]
    g_last = jnp.exp(gc[..., -1])

    xs = (jnp.moveaxis(u, 2, 0), jnp.moveaxis(w, 2, 0), jnp.moveaxis(qk, 2, 0),
          jnp.moveaxis(q_dec, 2, 0), jnp.moveaxis(k_dec, 2, 0), jnp.moveaxis(g_last, 2, 0))

    def step(s, inp):
        u_c, w_c, qk_c, qd_c, kd_c, gl_c = inp
        v_new = u_c - jnp.einsum('bhcd,bhdv->bhcv', w_c, s)
        o_c = jnp.einsum('bhcd,bhdv->bhcv', qd_c, s) + jnp.einsum('bhcs,bhsv->bhcv', qk_c, v_new)
        s = s * gl_c[..., None, None] + jnp.einsum('bhcd,bhcv->bhdv', kd_c, v_new)
        return s, o_c

    s0 = jnp.zeros((b, nh, dk, dv), dtype=jnp.float32)
    _, o = lax.scan(step, s0, xs)
    return jnp.transpose(o, (1, 0, 3, 2, 4)).reshape(b, length, nh, dv)


def gdn_mixer(h, w_in, conv_w, a_log, dt_bias, o_norm_g, w_out):
    b, length, _ = h.shape
    proj = h @ w_in
    n_qkv = 2 * GDN_HK + GDN_HV
    qkv = jax.nn.silu(causal_depthwise_conv(proj[..., :n_qkv], conv_w))
    gate = proj[..., n_qkv:n_qkv + GDN_HV].reshape(b, length, GDN_HEADS, GDN_DV).astype(jnp.float32)
    b_raw = proj[..., n_qkv + GDN_HV:n_qkv + GDN_HV + GDN_HEADS].astype(jnp.float32)
    a_raw = proj[..., n_qkv + GDN_HV + GDN_HEADS:].astype(jnp.float32)
    q = l2norm(qkv[..., :GDN_HK].reshape(b, length, GDN_HEADS, GDN_DK).astype(jnp.float32))
    k = l2norm(qkv[..., GDN_HK:2 * GDN_HK].reshape(b, length, GDN_HEADS, GDN_DK).astype(jnp.float32))
    v = qkv[..., 2 * GDN_HK:].reshape(b, length, GDN_HEADS, GDN_DV).astype(jnp.float32)
    beta = jax.nn.sigmoid(b_raw)
    g = -jnp.exp(a_log.astype(jnp.float32)) * jax.nn.softplus(a_raw + dt_bias.astype(jnp.float32))
    o = chunk_gated_delta_rule(q * GDN_DK ** -0.5, k, v, g, beta)
    o = rmsnorm(o, o_norm_g) * jax.nn.silu(gate)
    return o.reshape(b, length, GDN_HV).astype(h.dtype) @ w_out


def sq_relu_mlp(h, w1, w2):
    return jnp.square(jax.nn.relu(h @ w1)) @ w2


def _num_layers_of(m):
    return len(range(m, DEPTH, N_MIXERS))


def setup_inputs(seed: int = 0) -> dict:
    key = jax.random.key(seed)
    keys = jax.random.split(key, 64)
    counter = [0]

    def nxt():
        kk = keys[counter[0]]
        counter[0] += 1
        return kk

    def nrm(shape, fan_in):
        return jax.random.normal(nxt(), shape, jnp.float32) * fan_in ** -0.5

    def gain(shape):
        return 1.0 + 0.02 * jax.random.normal(nxt(), shape, jnp.float32)

    def bias(shape):
        return 0.02 * jax.random.normal(nxt(), shape, jnp.float32)

    na, nb, nc, nd = (_num_layers_of(m) for m in range(N_MIXERS))
    x = jax.random.normal(nxt(), (BATCH, SEQ, D_MODEL), jnp.float32)
    dt = jnp.exp(jax.random.uniform(nxt(), (nd, GDN_HEADS), jnp.float32, math.log(1e-3), math.log(1e-1)))
    return {
        'x': x,
        'norm_mix_g': gain((DEPTH, D_MODEL)),
        'norm_mlp_g': gain((DEPTH, D_MODEL)),
        'final_g': gain((D_MODEL,)),
        'mlp_w1': nrm((DEPTH, D_MODEL, D_FF), D_MODEL),
        'mlp_w2': nrm((DEPTH, D_FF, D_MODEL), D_FF),
        'dsa_w_in': nrm((na, D_MODEL, DSA_IN), D_MODEL),
        'dsa_idx_k_g': gain((na, IDX_DIM)),
        'dsa_idx_k_b': bias((na, IDX_DIM)),
        'dsa_w_out': nrm((na, D_MODEL, D_MODEL), D_MODEL),
        'conv_w_pw1': nrm((nb, D_MODEL, 2 * D_MODEL), D_MODEL),
        'conv_b_pw1': bias((nb, 2 * D_MODEL)),
        'conv_w_dw': nrm((nb, CONV_WIDTH, D_MODEL), CONV_WIDTH),
        'conv_b_dw': bias((nb, D_MODEL)),
        'conv_ln_g': gain((nb, D_MODEL)),
        'conv_ln_b': bias((nb, D_MODEL)),
        'conv_w_pw2': nrm((nb, D_MODEL, D_MODEL), D_MODEL),
        'conv_b_pw2': bias((nb, D_MODEL)),
        'mla_w_in': nrm((nc, D_MODEL, MLA_IN), D_MODEL),
        'mla_q_norm_g': gain((nc, MLA_Q_LORA)),
        'mla_w_uq': nrm((nc, MLA_Q_LORA, MLA_HEADS * (MLA_NOPE + MLA_ROPE)), MLA_Q_LORA),
        'mla_kv_norm_g': gain((nc, MLA_KV_LORA)),
        'mla_w_ukv': nrm((nc, MLA_KV_LORA, MLA_HEADS * (MLA_NOPE + MLA_V)), MLA_KV_LORA),
        'mla_w_out': nrm((nc, MLA_HEADS * MLA_V, D_MODEL), MLA_HEADS * MLA_V),
        'gdn_w_in': nrm((nd, D_MODEL, GDN_IN), D_MODEL),
        'gdn_conv_w': nrm((nd, GDN_CONV, 2 * GDN_HK + GDN_HV), GDN_CONV),
        'gdn_a_log': jnp.log(jax.random.uniform(nxt(), (nd, GDN_HEADS), jnp.float32, 1.0, 16.0)),
        'gdn_dt_bias': dt + jnp.log(-jnp.expm1(-dt)),
        'gdn_o_norm_g': gain((nd, GDN_DV)),
        'gdn_w_out': nrm((nd, GDN_HV, D_MODEL), GDN_HV),
    }


def reference(x, norm_mix_g, norm_mlp_g, final_g, mlp_w1, mlp_w2,
              dsa_w_in, dsa_idx_k_g, dsa_idx_k_b, dsa_w_out,
              conv_w_pw1, conv_b_pw1, conv_w_dw, conv_b_dw, conv_ln_g, conv_ln_b, conv_w_pw2, conv_b_pw2,
              mla_w_in, mla_q_norm_g, mla_w_uq, mla_kv_norm_g, mla_w_ukv, mla_w_out,
              gdn_w_in, gdn_conv_w, gdn_a_log, gdn_dt_bias, gdn_o_norm_g, gdn_w_out):
    h = x
    for i in range(DEPTH):
        m = i % N_MIXERS
        j = i // N_MIXERS
        hn = rmsnorm(h, norm_mix_g[i])
        if m == 0:
            y = dsa_mixer(hn, dsa_w_in[j], dsa_idx_k_g[j], dsa_idx_k_b[j], dsa_w_out[j])
        elif m == 1:
            y = conv_mixer(hn, conv_w_pw1[j], conv_b_pw1[j], conv_w_dw[j], conv_b_dw[j],
                           conv_ln_g[j], conv_ln_b[j], conv_w_pw2[j], conv_b_pw2[j])
        elif m == 2:
            y = mla_mixer(hn, mla_w_in[j], mla_q_norm_g[j], mla_w_uq[j], mla_kv_norm_g[j],
                          mla_w_ukv[j], mla_w_out[j])
        else:
            y = gdn_mixer(hn, gdn_w_in[j], gdn_conv_w[j], gdn_a_log[j], gdn_dt_bias[j],
                          gdn_o_norm_g[j], gdn_w_out[j])
        h = h + y
        h = h + sq_relu_mlp(rmsnorm(h, norm_mlp_g[i]), mlp_w1[i], mlp_w2[i])
    return rmsnorm(h, final_g)
```

```python
import numpy as np
import concourse.bass as bass
import concourse.mybir as mybir
from contextlib import ExitStack
from concourse.bass_utils import run_bass_kernel_spmd

F32 = mybir.dt.float32
BF16 = mybir.dt.bfloat16
U8 = mybir.dt.uint8
I32 = mybir.dt.int32
ALU = mybir.AluOpType
AF = mybir.ActivationFunctionType
AX = mybir.AxisListType

ENGS = ["pe", "act", "dve", "pool", "sp"]
NDMA = 8


class Res:
    __slots__ = ("name", "lastw", "readers")

    def __init__(self, name):
        self.name = name
        self.lastw = None
        self.readers = []


class Tile:
    def __init__(self, t, name, nsub=1):
        self.t = t
        self.name = name
        self.res = [Res(f"{name}.{i}") for i in range(nsub)]

    def __getitem__(self, idx):
        return self.t[idx]

    def r(self, i=0):
        return self.res[i]

    def all(self):
        return list(self.res)


class View:
    def __init__(self, base, off, width, name, share=True):
        self.base = base
        self.off = off
        self.width = width
        self.res = base.res if share else [Res(name)]

    def __getitem__(self, idx):
        rows, cols = idx
        start = cols.start or 0
        stop = self.width if cols.stop is None else cols.stop
        return self.base.t[rows, self.off + start:self.off + stop]

    def r(self, i=0):
        return self.res[0]

    def all(self):
        return list(self.res)


class Prog:
    def __init__(self, nc, stack):
        self.nc = nc
        self.stack = stack
        self.ops = {e: [] for e in ENGS}
        self.cnt = {e: 0 for e in ENGS}
        self.sem = {e: stack.enter_context(nc.semaphore(f"s_{e}")) for e in ENGS if e != "sp"}
        self.dsem = {q: [stack.enter_context(nc.semaphore(f"d_{q}{i}")) for i in range(NDMA)]
                     for q in ("sp", "pool", "act")}
        self.dcnt = {q: 0 for q in ("sp", "pool", "act")}
        self.dtok = {q: [] for q in ("sp", "pool", "act")}
        self.known = {e: {} for e in ENGS}
        self.nops = 0

    def sb(self, name, shape, dtype, nsub=1):
        t = self.stack.enter_context(self.nc.sbuf_tensor("sb_" + name, list(shape), dtype))
        return Tile(t, name, nsub)

    def ps(self, name, shape, dtype=F32, nsub=1):
        t = self.stack.enter_context(self.nc.psum_tensor("ps_" + name, list(shape), dtype))
        return Tile(t, name, nsub)

    def push_scope(self):
        self._saved_stack = self.stack
        self.stack = ExitStack()
        return self.stack

    def pop_scope(self):
        self.barrier()
        self.stack.close()
        self.stack = self._saved_stack

    def barrier(self):
        toks = []
        for e in ENGS:
            if e != "sp" and self.cnt[e] > 0:
                toks.append((("c", e), self.cnt[e], e))
        for q in self.dtok:
            toks += self.dtok[q][-NDMA:]
        self._pending = {e: list(toks) for e in ENGS}

    def _deps(self, eng, reads, writes):
        deps = {}
        for tok in getattr(self, "_pending", {}).get(eng, []):
            if not (tok[2] == eng and eng == "pe"):
                if deps.get(tok[0], (0,))[0] < tok[1]:
                    deps[tok[0]] = (tok[1], tok)
        if getattr(self, "_pending", None):
            self._pending[eng] = []
        def add(tok):
            if tok is None:
                return
            key, val, teng = tok
            if teng == eng and eng == "pe":
                return
            if deps.get(key, (0,))[0] < val:
                deps[key] = (val, tok)
        for r in reads:
            add(r.lastw)
        for w in writes:
            add(w.lastw)
            for t in w.readers:
                add(t)
        out = []
        kn = self.known[eng]
        for key, (val, tok) in deps.items():
            if kn.get(key, 0) >= val:
                continue
            kn[key] = val
            out.append(tok)
        return out

    def _commit(self, tok, reads, writes):
        for r in reads:
            r.readers.append(tok)
        for w in writes:
            w.lastw = tok
            w.readers = []

    def op(self, eng, fn, reads=(), writes=()):
        waits = self._deps(eng, reads, writes)
        self.cnt[eng] += 1
        tok = (("c", eng), self.cnt[eng], eng)
        self.ops[eng].append((waits, fn, tok))
        self._commit(tok, reads, writes)
        self.nops += 1
        return tok

    def dma(self, fn, reads=(), writes=(), q="sp"):
        eng = q
        waits = self._deps(eng, reads, writes)
        i = self.dcnt[q]
        self.dcnt[q] += 1
        slot = i % NDMA
        val = 16 * (i // NDMA + 1)
        if i >= NDMA:
            prev = self.dtok[q][i - NDMA]
            key, pval, _ = prev
            if self.known[eng].get(key, 0) < pval:
                self.known[eng][key] = pval
                waits.append(prev)
        tok = (("d", q, slot), val, "dma")
        self.dtok[q].append(tok)
        self.ops[eng].append((waits, fn, tok))
        self._commit(tok, reads, writes)
        self.nops += 1
        return tok

    def _semof(self, tok):
        key = tok[0]
        if key[0] == "c":
            return self.sem[key[1]]
        return self.dsem[key[1]][key[2]]

    def emit(self, final_waits=()):
        nc = self.nc
        with nc.Block() as block:
            def run(eng, e):
                for waits, fn, tok in self.ops[eng]:
                    for w in waits:
                        e.wait_ge(self._semof(w), w[1])
                    ins = fn(e)
                    if tok[2] == "dma":
                        ins.then_inc(self._semof(tok), 16)
                    else:
                        ins.then_inc(self._semof(tok), 1)
                if eng == "sp":
                    for w in final_waits:
                        e.wait_ge(self._semof(w), w[1])

            @block.tensor
            def _(e):
                run("pe", e)

            @block.scalar
            def _(e):
                run("act", e)

            @block.vector
            def _(e):
                run("dve", e)

            @block.gpsimd
            def _(e):
                run("pool", e)

            @block.sync
            def _(e):
                run("sp", e)


def load_w_bf16(P, w_ap, KC, Fo, name, q="pool"):
    t = P.sb(name, [128, KC, Fo], BF16, nsub=KC)
    wv = w_ap.rearrange("(c p) f -> p c f", p=128)
    for c in range(KC):
        P.dma(lambda e, c=c: e.dma_start(out=t[:, c, :], in_=wv[:, c, :], max_dma_last_dim=8192),
              writes=[t.r(c)], q=q)
    return t


def load_vec_fm(P, v_ap, KC, name):
    t = P.sb(name, [128, KC], F32)
    vv = v_ap.rearrange("(c p) -> p c", p=128)
    P.dma(lambda e: e.dma_start(out=t[:, :], in_=vv, allow_slow_non_contiguous=True), writes=[t.r()])
    return t


class Ctx:
    pass


def make_consts(P):
    C = Ctx()
    C.ones_bf = P.sb("ones_bf", [128, 128], BF16)
    P.op("dve", lambda e: e.memset(C.ones_bf[:, :], 1.0), writes=[C.ones_bf.r()])
    C.eps = P.sb("eps_t", [128, 1], F32)
    P.op("dve", lambda e: e.memset(C.eps[:, :], 1e-6), writes=[C.eps.r()])
    return C


def rms_rstd(P, C, x, KC, T, psum, sq, rstd, dim):
    for c in range(KC):
        P.op("act", lambda e, c=c: e.activation(out=sq[:, c, :], in_=x[:, c, :], func=AF.Square),
             reads=[x.r()], writes=[sq.r(c)])
    for c in range(KC):
        P.op("pe", lambda e, c=c: e.matmul(psum[:, :T], lhsT=C.ones_bf[:, :], rhs=sq[:, c, :],
                                          start=(c == 0), stop=(c == KC - 1)),
             reads=[sq.r(c), C.ones_bf.r()], writes=[psum.r()])
    P.op("act", lambda e: e.activation(out=rstd[:, :], in_=psum[:, :T], func=AF.Sqrt,
                                       bias=C.eps[:, :], scale=1.0 / dim),
         reads=[psum.r(), C.eps.r()], writes=[rstd.r()])
    P.op("dve", lambda e: e.reciprocal(out=rstd[:, :], in_=rstd[:, :]), reads=[rstd.r()], writes=[rstd.r()])


def build_mlp(TOK=2048, T=256, final=False):
    nc = bass.Bass("TRN2", target_bir_lowering=False)
    hT = nc.dram_tensor("hT", [1024, TOK], F32, kind="ExternalInput").ap()
    g = nc.dram_tensor("g", [1024], F32, kind="ExternalInput").ap()
    w1 = nc.dram_tensor("w1", [1024, 4096], F32, kind="ExternalInput").ap()
    w2 = nc.dram_tensor("w2", [4096, 1024], F32, kind="ExternalInput").ap()
    oT = nc.dram_tensor("oT", [1024, TOK], F32, kind="ExternalOutput").ap()
    with ExitStack() as st:
        st.enter_context(nc.allow_low_precision("bf16 matmul operands, fp32 accumulate"))
        P = Prog(nc, st)
        C = make_consts(P)
        gt = load_vec_fm(P, g, 8, "g")
        W1 = load_w_bf16(P, w1, 8, 4096, "W1")
        W2 = load_w_bf16(P, w2, 32, 1024, "W2")
        fgt = None
        if final:
            fg = nc.dram_tensor("fg", [1024], F32, kind="ExternalInput").ap()
            fgt = load_vec_fm(P, fg, 8, "fg")
        mlp_body(P, C, hT, oT, gt, W1, W2, TOK, T, fgt)
        P.emit(final_waits=P.out_toks)
    return nc


def mlp_body(P, C, hT, oT, gt, W1, W2, TOK, T, fgt=None):
    hv = hT.rearrange("(c p) t -> p c t", p=128)
    ov = oT.rearrange("(c p) t -> p c t", p=128)
    NB = 2
    xs = [P.sb(f"x{i}", [128, 8, T], F32) for i in range(NB)]
    sqs = [P.sb(f"sq{i}", [128, 8, T], BF16, nsub=8) for i in range(NB)]
    hns = [P.sb(f"hn{i}", [128, 8, T], BF16, nsub=8) for i in range(NB)]
    rstds = [P.sb(f"rstd{i}", [128, T], F32) for i in range(NB)]
    aT = P.sb("aT", [128, 32, T], BF16, nsub=32)
    rl = [P.sb(f"rl{i}", [128, T], BF16) for i in range(2)]
    ys = [P.sb(f"y{i}", [128, 8, T], F32, nsub=8) for i in range(NB)]
    pss = [P.ps(f"ps{i}", [128, 512], F32) for i in range(8)]
    P.out_toks = []
    pi = 0
    for it in range(TOK // T):
        b = it % NB
        x, sq, hn, rstd, y = xs[b], sqs[b], hns[b], rstds[b], ys[b]
        t0 = it * T
        P.dma(lambda e, x=x, t0=t0: e.dma_start(out=x[:, :, :], in_=hv[:, :, t0:t0 + T]), writes=[x.r()])
        ps = pss[pi % 8]; pi += 1
        rms_rstd(P, C, x, 8, T, ps, sq, rstd, 1024.0)
        for c in range(8):
            P.op("dve", lambda e, c=c, x=x, hn=hn, rstd=rstd: e.scalar_tensor_tensor(
                out=hn[:, c, :], in0=x[:, c, :], scalar=gt[:, c:c + 1], in1=rstd[:, :],
                op0=ALU.mult, op1=ALU.mult), reads=[x.r(), rstd.r(), gt.r()], writes=[hn.r(c)])
        for f in range(32):
            ps = pss[pi % 8]; pi += 1
            for c in range(8):
                P.op("pe", lambda e, c=c, f=f, ps=ps, hn=hn: e.matmul(
                    ps[:, :T], lhsT=W1[:, c, f * 128:(f + 1) * 128], rhs=hn[:, c, :],
                    start=(c == 0), stop=(c == 7)), reads=[W1.r(c), hn.r(c)], writes=[ps.r()])
            r = rl[f % 2]
            P.op("act", lambda e, ps=ps, r=r: e.activation(out=r[:, :], in_=ps[:, :T], func=AF.Relu),
                 reads=[ps.r()], writes=[r.r()])
            eng = "pool" if f % 2 == 0 else "dve"
            P.op(eng, lambda e, f=f, r=r: e.tensor_tensor(out=aT[:, f, :], in0=r[:, :], in1=r[:, :], op=ALU.mult),
                 reads=[r.r()], writes=[aT.r(f)])
        for o in range(8):
            ps = pss[pi % 8]; pi += 1
            for f in range(32):
                P.op("pe", lambda e, o=o, f=f, ps=ps: e.matmul(
                    ps[:, :T], lhsT=W2[:, f, o * 128:(o + 1) * 128], rhs=aT[:, f, :],
                    start=(f == 0), stop=(f == 31)), reads=[W2.r(f), aT.r(f)], writes=[ps.r()])
            P.op("dve", lambda e, o=o, ps=ps, x=x, y=y: e.tensor_tensor(
                out=y[:, o, :], in0=ps[:, :T], in1=x[:, o, :], op=ALU.add),
                reads=[ps.r(), x.r()], writes=[y.r(o)])
        if fgt is not None:
            ps = pss[pi % 8]; pi += 1
            for c in range(8):
                P.op("act", lambda e, c=c, y=y, sq=sq: e.activation(out=sq[:, c, :], in_=y[:, c, :], func=AF.Square),
                     reads=[y.r(c)], writes=[sq.r(c)])
            for c in range(8):
                P.op("pe", lambda e, c=c, ps=ps, sq=sq: e.matmul(ps[:, :T], lhsT=C.ones_bf[:, :], rhs=sq[:, c, :],
                                                              start=(c == 0), stop=(c == 7)),
                     reads=[sq.r(c), C.ones_bf.r()], writes=[ps.r()])
            P.op("act", lambda e, ps=ps, rstd=rstd: e.activation(out=rstd[:, :], in_=ps[:, :T], func=AF.Sqrt,
                                                               bias=C.eps[:, :], scale=1.0 / 1024),
                 reads=[ps.r(), C.eps.r()], writes=[rstd.r()])
            P.op("dve", lambda e, rstd=rstd: e.reciprocal(out=rstd[:, :], in_=rstd[:, :]), reads=[rstd.r()], writes=[rstd.r()])
            for c in range(8):
                P.op("dve", lambda e, c=c, y=y, rstd=rstd: e.scalar_tensor_tensor(
                    out=y[:, c, :], in0=y[:, c, :], scalar=fgt[:, c:c + 1], in1=rstd[:, :], op0=ALU.mult, op1=ALU.mult),
                    reads=[y.r(c), rstd.r(), fgt.r()], writes=[y.r(c)])
        tok = P.dma(lambda e, y=y, t0=t0: e.dma_start(out=ov[:, :, t0:t0 + T], in_=y[:, :, :]),
                    reads=y.all())
        P.out_toks.append(tok)


def next_ps(P):
    i = getattr(P, "_psi", 0)
    P._psi = i + 1
    return P.pss[i % len(P.pss)]


def alloc_ps(P, n=8):
    P.pss = [P.ps(f"ps{i}", [128, 512], F32) for i in range(n)]
    P._psi = 0


def load_const(P, ap, shape, name, dtype=F32, q="sp"):
    t = P.sb(name, shape, dtype)
    P.dma(lambda e: e.dma_start(out=t[tuple(slice(None) for _ in shape)], in_=ap), writes=[t.r()], q=q)
    return t


def build_outproj(TOK=2048, T=512):
    nc = bass.Bass("TRN2", target_bir_lowering=False)
    hT = nc.dram_tensor("hT", [1024, TOK], F32, kind="ExternalInput").ap()
    mT = nc.dram_tensor("mT", [1024, TOK], F32, kind="ExternalInput").ap()
    w = nc.dram_tensor("w", [1024, 1024], F32, kind="ExternalInput").ap()
    oT = nc.dram_tensor("oT", [1024, TOK], F32, kind="ExternalOutput").ap()
    hv = hT.rearrange("(c p) t -> p c t", p=128)
    mv = mT.rearrange("(c p) t -> p c t", p=128)
    ov = oT.rearrange("(c p) t -> p c t", p=128)
    with ExitStack() as st:
        st.enter_context(nc.allow_low_precision("bf16 matmul operands, fp32 accumulate"))
        P = Prog(nc, st)
        alloc_ps(P)
        W = load_w_bf16(P, w, 8, 1024, "W")
        NB = 2
        xs = [P.sb(f"x{i}", [128, 8, T], F32) for i in range(NB)]
        ms = [P.sb(f"m{i}", [128, 8, T], F32) for i in range(NB)]
        mb = [P.sb(f"mb{i}", [128, 8, T], BF16, nsub=8) for i in range(NB)]
        ys = [P.sb(f"y{i}", [128, 8, T], F32, nsub=8) for i in range(NB)]
        outs = []
        for it in range(TOK // T):
            b = it % NB
            x, m, mbb, y = xs[b], ms[b], mb[b], ys[b]
            t0 = it * T
            P.dma(lambda e, x=x, t0=t0: e.dma_start(out=x[:, :, :], in_=hv[:, :, t0:t0 + T]), writes=[x.r()])
            P.dma(lambda e, m=m, t0=t0: e.dma_start(out=m[:, :, :], in_=mv[:, :, t0:t0 + T]), writes=[m.r()], q="act")
            for c in range(8):
                eng = "act" if c % 2 == 0 else "pool"
                if eng == "act":
                    P.op("act", lambda e, c=c, m=m, mbb=mbb: e.copy(out=mbb[:, c, :], in_=m[:, c, :]),
                         reads=[m.r()], writes=[mbb.r(c)])
                else:
                    P.op("pool", lambda e, c=c, m=m, mbb=mbb: e.tensor_copy(out=mbb[:, c, :], in_=m[:, c, :]),
                         reads=[m.r()], writes=[mbb.r(c)])
            for o in range(8):
                ps = next_ps(P)
                for c in range(8):
                    P.op("pe", lambda e, o=o, c=c, ps=ps, mbb=mbb: e.matmul(
                        ps[:, :T], lhsT=W[:, c, o * 128:(o + 1) * 128], rhs=mbb[:, c, :],
                        start=(c == 0), stop=(c == 7)), reads=[W.r(c), mbb.r(c)], writes=[ps.r()])
                P.op("dve", lambda e, o=o, ps=ps, x=x, y=y: e.tensor_tensor(
                    out=y[:, o, :], in0=ps[:, :T], in1=x[:, o, :], op=ALU.add),
                    reads=[ps.r(), x.r()], writes=[y.r(o)])
            outs.append(P.dma(lambda e, y=y, t0=t0: e.dma_start(out=ov[:, :, t0:t0 + T], in_=y[:, :, :]),
                              reads=y.all()))
        P.emit(final_waits=outs)
    return nc


def build_conv(TOK=2048, T=256):
    HALO = 30
    NT = TOK + HALO
    nc = bass.Bass("TRN2", target_bir_lowering=False)
    hT = nc.dram_tensor("hT", [1024, NT], F32, kind="ExternalInput").ap()
    g = nc.dram_tensor("g", [1024], F32, kind="ExternalInput").ap()
    w1 = nc.dram_tensor("w1", [1024, 2048], F32, kind="ExternalInput").ap()
    b1 = nc.dram_tensor("b1", [2048], F32, kind="ExternalInput").ap()
    wdT = nc.dram_tensor("wdT", [1024, 31], F32, kind="ExternalInput").ap()
    bd = nc.dram_tensor("bd", [1024], F32, kind="ExternalInput").ap()
    lg = nc.dram_tensor("lg", [1024], F32, kind="ExternalInput").ap()
    lb = nc.dram_tensor("lb", [1024], F32, kind="ExternalInput").ap()
    w2 = nc.dram_tensor("w2", [1024, 1024], F32, kind="ExternalInput").ap()
    b2 = nc.dram_tensor("b2", [1024], F32, kind="ExternalInput").ap()
    hs = nc.dram_tensor("hs", [128, 1], F32, kind="ExternalInput").ap()
    ident = nc.dram_tensor("ident", [128, 128], F32, kind="ExternalInput").ap()
    oT = nc.dram_tensor("oT", [1024, TOK], F32, kind="ExternalOutput").ap()
    hv = hT.rearrange("(c p) t -> p c t", p=128)
    ov = oT.rearrange("(c p) t -> p c t", p=128)
    with ExitStack() as st:
        st.enter_context(nc.allow_low_precision("bf16 matmul operands, fp32 accumulate"))
        P = Prog(nc, st)
        alloc_ps(P)
        C = make_consts(P)
        ones_f = P.sb("ones_f", [128, 128], F32)
        P.op("dve", lambda e: e.memset(ones_f[:, :], 1.0), writes=[ones_f.r()])
        gt = load_vec_fm(P, g, 8, "g")
        b1t = load_vec_fm(P, b1, 16, "b1")
        bdt = load_vec_fm(P, bd, 8, "bd")
        lgt = load_vec_fm(P, lg, 8, "lg")
        lbt = load_vec_fm(P, lb, 8, "lb")
        b2t = load_vec_fm(P, b2, 8, "b2")
        hst = load_const(P, hs, [128, 1], "hs")
        idf = load_const(P, ident, [128, 128], "idf")
        wd = P.sb("wd", [128, 8, 31], F32)
        P.dma(lambda e: e.dma_start(out=wd[:, :, :], in_=wdT.rearrange("(c p) j -> p c j", p=128)), writes=[wd.r()])
        W1 = load_w_bf16(P, w1, 8, 2048, "W1")
        W2 = load_w_bf16(P, w2, 8, 1024, "W2")
        diag = P.sb("diag", [128, 8 * 31, 128], BF16, nsub=8)
        for c in range(8):
            for j in range(31):
                eng = "pool" if (j % 2 == 0) else "dve"
                P.op(eng, lambda e, c=c, j=j: e.tensor_scalar(
                    out=diag[:, c * 31 + j, :], in0=idf[:, :], scalar1=wd[:, c, j:j + 1], scalar2=None,
                    op0=ALU.mult), reads=[idf.r(), wd.r()], writes=[diag.r(c)])
        uT = P.sb("uT", [128, 8, NT], BF16, nsub=8)
        NB = 2
        xs = [P.sb(f"x{i}", [128, 8, T], F32) for i in range(NB)]
        sqs = [P.sb(f"sq{i}", [128, 8, T], BF16, nsub=8) for i in range(1)] * 2
        hns = [P.sb(f"hn{i}", [128, 8, T], BF16, nsub=8) for i in range(1)] * 2
        rstds = [P.sb(f"rstd{i}", [128, T], F32) for i in range(1)] * 2
        sig = [P.sb(f"sig{i}", [128, T], F32) for i in range(2)]
        segs = [(0, HALO)] + [(HALO + k * T, T) for k in range(TOK // T)]
        for it, (s0, L) in enumerate(segs):
            b = it % NB
            x, sq, hn, rstd = xs[b], sqs[b], hns[b], rstds[b]
            P.dma(lambda e, x=x, s0=s0, L=L: e.dma_start(out=x[:, :, :L], in_=hv[:, :, s0:s0 + L]), writes=[x.r()])
            ps = next_ps(P)
            for c in range(8):
                P.op("act", lambda e, c=c, x=x, sq=sq, L=L: e.activation(out=sq[:, c, :L], in_=x[:, c, :L], func=AF.Square),
                     reads=[x.r()], writes=[sq.r(c)])
            for c in range(8):
                P.op("pe", lambda e, c=c, ps=ps, sq=sq, L=L: e.matmul(ps[:, :L], lhsT=C.ones_bf[:, :], rhs=sq[:, c, :L],
                                                                  start=(c == 0), stop=(c == 7)),
                     reads=[sq.r(c), C.ones_bf.r()], writes=[ps.r()])
            P.op("act", lambda e, ps=ps, rstd=rstd, L=L: e.activation(out=rstd[:, :L], in_=ps[:, :L], func=AF.Sqrt,
                                                               bias=C.eps[:, :], scale=1.0 / 1024),
                 reads=[ps.r(), C.eps.r()], writes=[rstd.r()])
            P.op("dve", lambda e, rstd=rstd, L=L: e.reciprocal(out=rstd[:, :L], in_=rstd[:, :L]),
                 reads=[rstd.r()], writes=[rstd.r()])
            for c in range(8):
                P.op("dve", lambda e, c=c, x=x, hn=hn, rstd=rstd, L=L: e.scalar_tensor_tensor(
                    out=hn[:, c, :L], in0=x[:, c, :L], scalar=gt[:, c:c + 1], in1=rstd[:, :L],
                    op0=ALU.mult, op1=ALU.mult), reads=[x.r(), rstd.r(), gt.r()], writes=[hn.r(c)])
            for j in range(8):
                psa = next_ps(P)
                psg = next_ps(P)
                for c in range(8):
                    P.op("pe", lambda e, c=c, j=j, psa=psa, hn=hn, L=L: e.matmul(
                        psa[:, :L], lhsT=W1[:, c, j * 128:(j + 1) * 128], rhs=hn[:, c, :L],
                        start=(c == 0), stop=(c == 7)), reads=[W1.r(c), hn.r(c)], writes=[psa.r()])
                for c in range(8):
                    P.op("pe", lambda e, c=c, j=j, psg=psg, hn=hn, L=L: e.matmul(
                        psg[:, :L], lhsT=W1[:, c, 1024 + j * 128:1024 + (j + 1) * 128], rhs=hn[:, c, :L],
                        start=(c == 0), stop=(c == 7)), reads=[W1.r(c), hn.r(c)], writes=[psg.r()])
                sg = sig[j % 2]
                P.op("act", lambda e, j=j, psg=psg, sg=sg, L=L: e.activation(
                    out=sg[:, :L], in_=psg[:, :L], func=AF.Sigmoid, bias=b1t[:, 8 + j:9 + j], scale=1.0),
                    reads=[psg.r(), b1t.r()], writes=[sg.r()])
                P.op("dve", lambda e, j=j, psa=psa, sg=sg, s0=s0, L=L: e.scalar_tensor_tensor(
                    out=uT[:, j, s0:s0 + L], in0=psa[:, :L], scalar=b1t[:, j:j + 1], in1=sg[:, :L],
                    op0=ALU.add, op1=ALU.mult), reads=[psa.r(), sg.r(), b1t.r()], writes=[uT.r(j)])
            if it == 0:
                for j in range(8):
                    P.op("dve", lambda e, j=j: e.tensor_scalar(
                        out=uT[:, j, 0:HALO], in0=uT[:, j, 0:HALO], scalar1=hst[:, 0:1], scalar2=None, op0=ALU.mult),
                        reads=[uT.r(j), hst.r()], writes=[uT.r(j)])
        vs = [P.sb(f"v{i}", [128, 8, T], F32, nsub=8) for i in range(1)] * 2
        zs = [P.sb(f"z{i}", [128, 8, T], BF16, nsub=8) for i in range(1)] * 2
        ys = [P.sb(f"y{i}", [128, 8, T], F32, nsub=8) for i in range(1)] * 2
        v2 = P.sb("v2", [128, 8, T], F32, nsub=8)
        mean = P.sb("mean", [128, T], F32)
        msq = P.sb("msq", [128, T], F32)
        lrstd = P.sb("lrstd", [128, T], F32)
        dd = [P.sb(f"dd{i}", [128, T], F32) for i in range(2)]
        outs = []
        for it in range(TOK // T):
            b = it % NB
            x, v, z, y = xs[b], vs[b], zs[b], ys[b]
            tl = it * T
            P.dma(lambda e, x=x, tl=tl: e.dma_start(out=x[:, :, :], in_=hv[:, :, HALO + tl:HALO + tl + T]), writes=[x.r()])
            for c in range(8):
                ps = next_ps(P)
                for j in range(31):
                    P.op("pe", lambda e, c=c, j=j, ps=ps, tl=tl: e.matmul(
                        ps[:, :T], lhsT=diag[:, c * 31 + j, :], rhs=uT[:, c, tl + j:tl + j + T],
                        start=(j == 0), stop=(j == 30)), reads=[diag.r(c), uT.r(c)], writes=[ps.r()])
                P.op("act", lambda e, c=c, ps=ps, v=v: e.activation(
                    out=v[:, c, :], in_=ps[:, :T], func=AF.Identity, bias=bdt[:, c:c + 1], scale=1.0),
                    reads=[ps.r(), bdt.r()], writes=[v.r(c)])
                P.op("pool", lambda e, c=c, v=v: e.tensor_tensor(out=v2[:, c, :], in0=v[:, c, :], in1=v[:, c, :], op=ALU.mult),
                     reads=[v.r(c)], writes=[v2.r(c)])
            ps1 = next_ps(P)
            ps2 = next_ps(P)
            for c in range(8):
                P.op("pe", lambda e, c=c, ps1=ps1, v=v: e.matmul(ps1[:, :T], lhsT=ones_f[:, :], rhs=v[:, c, :],
                                                              start=(c == 0), stop=(c == 7)),
                     reads=[ones_f.r(), v.r(c)], writes=[ps1.r()])
            for c in range(8):
                P.op("pe", lambda e, c=c, ps2=ps2: e.matmul(ps2[:, :T], lhsT=ones_f[:, :], rhs=v2[:, c, :],
                                                         start=(c == 0), stop=(c == 7)),
                     reads=[ones_f.r(), v2.r(c)], writes=[ps2.r()])
            P.op("act", lambda e, ps1=ps1: e.activation(out=mean[:, :], in_=ps1[:, :T], func=AF.Copy, scale=1.0 / 1024),
                 reads=[ps1.r()], writes=[mean.r()])
            P.op("dve", lambda e: e.tensor_tensor(out=msq[:, :], in0=mean[:, :], in1=mean[:, :], op=ALU.mult),
                 reads=[mean.r()], writes=[msq.r()])
            P.op("dve", lambda e, ps2=ps2: e.scalar_tensor_tensor(
                out=lrstd[:, :], in0=ps2[:, :T], scalar=1.0 / 1024, in1=msq[:, :], op0=ALU.mult, op1=ALU.subtract),
                reads=[ps2.r(), msq.r()], writes=[lrstd.r()])
            P.op("act", lambda e: e.activation(out=lrstd[:, :], in_=lrstd[:, :], func=AF.Sqrt, bias=C.eps[:, :], scale=1.0),
                 reads=[lrstd.r(), C.eps.r()], writes=[lrstd.r()])
            P.op("dve", lambda e: e.reciprocal(out=lrstd[:, :], in_=lrstd[:, :]), reads=[lrstd.r()], writes=[lrstd.r()])
            for c in range(8):
                d = dd[c % 2]
                P.op("pool", lambda e, c=c, d=d, v=v: e.tensor_tensor(out=d[:, :], in0=v[:, c, :], in1=mean[:, :], op=ALU.subtract),
                     reads=[v.r(c), mean.r()], writes=[d.r()])
                P.op("dve", lambda e, c=c, d=d: e.scalar_tensor_tensor(
                    out=d[:, :], in0=d[:, :], scalar=lgt[:, c:c + 1], in1=lrstd[:, :], op0=ALU.mult, op1=ALU.mult),
                    reads=[d.r(), lrstd.r(), lgt.r()], writes=[d.r()])
                P.op("act", lambda e, c=c, d=d, z=z: e.activation(
                    out=z[:, c, :], in_=d[:, :], func=AF.Silu, bias=lbt[:, c:c + 1], scale=1.0),
                    reads=[d.r(), lbt.r()], writes=[z.r(c)])
            for o in range(8):
                ps = next_ps(P)
                for c in range(8):
                    P.op("pe", lambda e, o=o, c=c, ps=ps, z=z: e.matmul(
                        ps[:, :T], lhsT=W2[:, c, o * 128:(o + 1) * 128], rhs=z[:, c, :],
                        start=(c == 0), stop=(c == 7)), reads=[W2.r(c), z.r(c)], writes=[ps.r()])
                P.op("dve", lambda e, o=o, ps=ps, x=x, y=y: e.scalar_tensor_tensor(
                    out=y[:, o, :], in0=ps[:, :T], scalar=b2t[:, o:o + 1], in1=x[:, o, :], op0=ALU.add, op1=ALU.add),
                    reads=[ps.r(), x.r(), b2t.r()], writes=[y.r(o)])
            outs.append(P.dma(lambda e, y=y, tl=tl: e.dma_start(out=ov[:, :, tl:tl + T], in_=y[:, :, :]),
                              reads=y.all()))
        P.emit(final_waits=outs)
    return nc


def norm_tile(P, C, x, gt, sq, hn, rstd, L, dim=1024.0, KC=8):
    ps = next_ps(P)
    for c in range(KC):
        P.op("act", lambda e, c=c: e.activation(out=sq[:, c, :L], in_=x[:, c, :L], func=AF.Square),
             reads=[x.r()], writes=[sq.r(c)])
    for c in range(KC):
        P.op("pe", lambda e, c=c: e.matmul(ps[:, :L], lhsT=C.ones_bf[:, :], rhs=sq[:, c, :L],
                                          start=(c == 0), stop=(c == KC - 1)),
             reads=[sq.r(c), C.ones_bf.r()], writes=[ps.r()])
    P.op("act", lambda e: e.activation(out=rstd[:, :L], in_=ps[:, :L], func=AF.Sqrt,
                                       bias=C.eps[:, :], scale=1.0 / dim),
         reads=[ps.r(), C.eps.r()], writes=[rstd.r()])
    P.op("dve", lambda e: e.reciprocal(out=rstd[:, :L], in_=rstd[:, :L]), reads=[rstd.r()], writes=[rstd.r()])
    for c in range(KC):
        P.op("dve", lambda e, c=c: e.scalar_tensor_tensor(
            out=hn[:, c, :L], in0=x[:, c, :L], scalar=gt[:, c:c + 1], in1=rstd[:, :L],
            op0=ALU.mult, op1=ALU.mult), reads=[x.r(), rstd.r(), gt.r()], writes=[hn.r(c)])


def build_mla(L=16384):
    T = 512
    NTI = L // T
    NKB = L // 128
    SCALE = 96.0 ** -0.5
    nc = bass.Bass("TRN2", target_bir_lowering=False)
    hT = nc.dram_tensor("hT", [1024, L], F32, kind="ExternalInput").ap()
    g = nc.dram_tensor("g", [1024], F32, kind="ExternalInput").ap()
    wall = nc.dram_tensor("wall", [1024, 832], F32, kind="ExternalInput").ap()
    gq = nc.dram_tensor("gq", [384], F32, kind="ExternalInput").ap()
    gkv = nc.dram_tensor("gkv", [256], F32, kind="ExternalInput").ap()
    wuq = nc.dram_tensor("wuq", [384, 384], F32, kind="ExternalInput").ap()
    wukv = nc.dram_tensor("wukv", [256, 256], F32, kind="ExternalInput").ap()
    cos2 = nc.dram_tensor("cos2", [96, L], F32, kind="ExternalInput").ap()
    sin2 = nc.dram_tensor("sin2", [96, L], F32, kind="ExternalInput").ap()
    cmask = nc.dram_tensor("cmask", [128, 4, 512], F32, kind="ExternalInput").ap()
    esel = nc.dram_tensor("esel", [65, 64], F32, kind="ExternalInput").ap()
    oT = nc.dram_tensor("oT", [128, L], F32, kind="ExternalOutput").ap()
    qTd = nc.dram_tensor("qTd", [2, 96, L], BF16, kind="Internal").ap()
    hv = hT.rearrange("(c p) t -> p c t", p=128)
    with ExitStack() as st:
        st.enter_context(nc.allow_low_precision("bf16 matmul operands, fp32 accumulate"))
        P = Prog(nc, st)
        alloc_ps(P, 6)
        C = make_consts(P)
        gt = load_vec_fm(P, g, 8, "g")
        gqt = load_vec_fm(P, gq, 3, "gq")
        gkvt = load_vec_fm(P, gkv, 2, "gkv")
        Wall = load_w_bf16(P, wall, 8, 832, "Wall")
        Wuq = load_w_bf16(P, wuq, 3, 384, "Wuq")
        Wukv = load_w_bf16(P, wukv, 2, 256, "Wukv")
        cm_f = load_const(P, cmask, [128, 4, 512], "cm_f")
        cm = P.sb("cm", [128, 4, 512], BF16)
        P.op("dve", lambda e: e.tensor_copy(out=cm[:, :, :], in_=cm_f[:, :, :]), reads=[cm_f.r()], writes=[cm.r()])
        es = load_const(P, esel, [65, 64], "es")
        kT = [P.sb(f"kT{h}", [96, L], BF16, nsub=NTI) for h in range(2)]
        Va = P.sb("Va", [128, NKB, 2, 65], BF16, nsub=NTI)
        P.op("pool", lambda e: e.memset(Va[:, :, :, :], 1.0), writes=Va.all())
        x = P.sb("x", [128, 8, T], F32)
        sq = P.sb("sq", [128, 8, T], BF16, nsub=8)
        hn = P.sb("hn", [128, 8, T], BF16, nsub=8)
        rstd = P.sb("rstd", [128, T], F32)
        cq = P.sb("cq", [128, 5, T], F32, nsub=5)
        cqs = P.sb("cqs", [128, 5, T], BF16, nsub=5)
        cqn = P.sb("cqn", [128, 5, T], BF16, nsub=5)
        rq = P.sb("rq", [128, T], F32)
        rkv = P.sb("rkv", [128, T], F32)
        cs = P.sb("cs", [96, 2, T], F32)
        t1 = P.sb("t1", [96, T], F32)
        t2 = P.sb("t2", [96, T], F32)
        qt = [P.sb(f"qt{h}", [96, T], BF16) for h in range(2)]
        for it in range(NTI):
            t0 = it * T
            P.dma(lambda e, t0=t0: e.dma_start(out=x[:, :, :], in_=hv[:, :, t0:t0 + T]), writes=[x.r()])
            P.dma(lambda e, t0=t0: e.dma_start(out=cs[:, 0, :], in_=cos2[:, t0:t0 + T]), writes=[cs.r()], q="act")
            P.dma(lambda e, t0=t0: e.dma_start(out=cs[:, 1, :], in_=sin2[:, t0:t0 + T]), writes=[cs.r()], q="act")
            norm_tile(P, C, x, gt, sq, hn, rstd, T)
            for j in range(5):
                ps = next_ps(P)
                for c in range(8):
                    P.op("pe", lambda e, c=c, j=j, ps=ps: e.matmul(
                        ps[:, :T], lhsT=Wall[:, c, j * 128:(j + 1) * 128], rhs=hn[:, c, :],
                        start=(c == 0), stop=(c == 7)), reads=[Wall.r(c), hn.r(c)], writes=[ps.r()])
                P.op("act", lambda e, j=j, ps=ps: e.copy(out=cq[:, j, :], in_=ps[:, :T]), reads=[ps.r()], writes=[cq.r(j)])
                P.op("pool", lambda e, j=j: e.tensor_tensor(out=cqs[:, j, :], in0=cq[:, j, :], in1=cq[:, j, :], op=ALU.mult),
                     reads=[cq.r(j)], writes=[cqs.r(j)])
            for (lo, hi, rr, dim, gg) in ((0, 3, rq, 384.0, gqt), (3, 5, rkv, 256.0, gkvt)):
                ps = next_ps(P)
                for j in range(lo, hi):
                    P.op("pe", lambda e, j=j, ps=ps, lo=lo, hi=hi: e.matmul(
                        ps[:, :T], lhsT=C.ones_bf[:, :], rhs=cqs[:, j, :], start=(j == lo), stop=(j == hi - 1)),
                        reads=[cqs.r(j), C.ones_bf.r()], writes=[ps.r()])
                P.op("act", lambda e, ps=ps, rr=rr, dim=dim: e.activation(
                    out=rr[:, :], in_=ps[:, :T], func=AF.Sqrt, bias=C.eps[:, :], scale=1.0 / dim),
                    reads=[ps.r(), C.eps.r()], writes=[rr.r()])
                P.op("dve", lambda e, rr=rr: e.reciprocal(out=rr[:, :], in_=rr[:, :]), reads=[rr.r()], writes=[rr.r()])
                for j in range(lo, hi):
                    P.op("dve", lambda e, j=j, rr=rr, gg=gg, lo=lo: e.scalar_tensor_tensor(
                        out=cqn[:, j, :], in0=cq[:, j, :], scalar=gg[:, j - lo:j - lo + 1], in1=rr[:, :],
                        op0=ALU.mult, op1=ALU.mult), reads=[cq.r(j), rr.r(), gg.r()], writes=[cqn.r(j)])
            pk = next_ps(P)
            pks = next_ps(P)
            for c in range(8):
                P.op("pe", lambda e, c=c, pk=pk: e.matmul(pk[:96, :T], lhsT=Wall[:, c, 640:736], rhs=hn[:, c, :],
                                                       start=(c == 0), stop=(c == 7)),
                     reads=[Wall.r(c), hn.r(c)], writes=[pk.r()])
            for c in range(8):
                P.op("pe", lambda e, c=c, pks=pks: e.matmul(pks[:96, :T], lhsT=Wall[:, c, 736:832], rhs=hn[:, c, :],
                                                         start=(c == 0), stop=(c == 7)),
                     reads=[Wall.r(c), hn.r(c)], writes=[pks.r()])
            P.op("dve", lambda e, pk=pk: e.tensor_tensor(out=t1[64:96, :], in0=pk[64:96, :T], in1=cs[64:96, 0, :], op=ALU.mult),
                 reads=[pk.r(), cs.r()], writes=[t1.r()])
            P.op("dve", lambda e, pks=pks: e.tensor_tensor(out=t2[64:96, :], in0=pks[64:96, :T], in1=cs[64:96, 1, :], op=ALU.mult),
                 reads=[pks.r(), cs.r()], writes=[t2.r()])
            for h in range(2):
                P.op("pool", lambda e, h=h, t0=t0: e.tensor_tensor(out=kT[h][64:96, t0:t0 + T], in0=t1[64:96, :], in1=t2[64:96, :], op=ALU.add),
                     reads=[t1.r(), t2.r()], writes=[kT[h].r(it)])
            for h in range(2):
                pq = next_ps(P)
                pqs = next_ps(P)
                for c in range(3):
                    P.op("pe", lambda e, c=c, h=h, pq=pq: e.matmul(
                        pq[:96, :T], lhsT=Wuq[:, c, h * 192:h * 192 + 96], rhs=cqn[:, c, :],
                        start=(c == 0), stop=(c == 2)), reads=[Wuq.r(c), cqn.r(c)], writes=[pq.r()])
                for c in range(3):
                    P.op("pe", lambda e, c=c, h=h, pqs=pqs: e.matmul(
                        pqs[:96, :T], lhsT=Wuq[:, c, h * 192 + 96:h * 192 + 192], rhs=cqn[:, c, :],
                        start=(c == 0), stop=(c == 2)), reads=[Wuq.r(c), cqn.r(c)], writes=[pqs.r()])
                q = qt[h]
                P.op("act", lambda e, pq=pq, q=q: e.copy(out=q[0:64, :], in_=pq[0:64, :T]), reads=[pq.r()], writes=[q.r()])
                P.op("dve", lambda e, pq=pq: e.tensor_tensor(out=t1[64:96, :], in0=pq[64:96, :T], in1=cs[64:96, 0, :], op=ALU.mult),
                     reads=[pq.r(), cs.r()], writes=[t1.r()])
                P.op("dve", lambda e, pqs=pqs: e.tensor_tensor(out=t2[64:96, :], in0=pqs[64:96, :T], in1=cs[64:96, 1, :], op=ALU.mult),
                     reads=[pqs.r(), cs.r()], writes=[t2.r()])
                P.op("pool", lambda e, q=q: e.tensor_tensor(out=q[64:96, :], in0=t1[64:96, :], in1=t2[64:96, :], op=ALU.add),
                     reads=[t1.r(), t2.r()], writes=[q.r()])
                P.dma(lambda e, h=h, q=q, t0=t0: e.dma_start(out=qTd[h, :, t0:t0 + T], in_=q[:, :]), reads=[q.r()])
                pkn = next_ps(P)
                for c in range(2):
                    P.op("pe", lambda e, c=c, h=h, pkn=pkn: e.matmul(
                        pkn[:64, :T], lhsT=Wukv[:, c, h * 64:(h + 1) * 64], rhs=cqn[:, 3 + c, :],
                        start=(c == 0), stop=(c == 1)), reads=[Wukv.r(c), cqn.r(3 + c)], writes=[pkn.r()])
                P.op("act", lambda e, h=h, pkn=pkn, t0=t0: e.copy(out=kT[h][0:64, t0:t0 + T], in_=pkn[0:64, :T]),
                     reads=[pkn.r()], writes=[kT[h].r(it)])
            for b4 in range(4):
                pv = next_ps(P)
                for c in range(2):
                    P.op("pe", lambda e, c=c, b4=b4, pv=pv: e.matmul(
                        pv[:, :128], lhsT=cqn[:, 3 + c, b4 * 128:(b4 + 1) * 128], rhs=Wukv[:, c, 128:256],
                        start=(c == 0), stop=(c == 1)), reads=[Wukv.r(c), cqn.r(3 + c)], writes=[pv.r()])
                kb = it * 4 + b4
                P.op("act", lambda e, pv=pv, kb=kb: e.copy(
                    out=Va[:, kb, :, 0:64], in_=pv[:, :128].rearrange("p (h d) -> p h d", h=2)),
                    reads=[pv.r()], writes=[Va.r(it)])
        pts = [P.sb(f"pt{i}", [128, T], BF16) for i in range(3)]
        qs = [P.sb(f"qs{i}", [96, T], BF16) for i in range(2)]
        osb = P.sb("osb", [65, T], F32)
        rden = P.sb("rden", [64, T], F32)
        on = [P.sb(f"on{i}", [64, T], F32) for i in range(2)]
        po = [P.ps(f"po{i}", [128, 512], F32) for i in range(2)]
        outs = []
        n = 0
        for h in range(2):
            for j in range(NTI):
                q = qs[n % 2]
                pO = po[n % 2]
                o_n = on[n % 2]
                n += 1
                P.dma(lambda e, h=h, q=q, j=j: e.dma_start(out=q[:, :], in_=qTd[h, :, j * T:(j + 1) * T]),
                      writes=[q.r()], q="act")
                nkb = 4 * j + 4
                for kb in range(nkb):
                    ps = next_ps(P)
                    pt = pts[kb % 3]
                    P.op("pe", lambda e, h=h, kb=kb, ps=ps, q=q: e.matmul(
                        ps[:, :T], lhsT=kT[h][:, kb * 128:(kb + 1) * 128], rhs=q[:, :], start=True, stop=True),
                        reads=[kT[h].r(kb // 4), q.r()], writes=[ps.r()])
                    P.op("act", lambda e, ps=ps, pt=pt: e.activation(out=pt[:, :], in_=ps[:, :T], func=AF.Exp, scale=SCALE),
                         reads=[ps.r()], writes=[pt.r()])
                    if kb >= 4 * j:
                        d = kb - 4 * j
                        eng = "dve" if d % 2 == 0 else "pool"
                        P.op(eng, lambda e, pt=pt, d=d: e.tensor_tensor(out=pt[:, :], in0=pt[:, :], in1=cm[:, d, :], op=ALU.mult),
                             reads=[pt.r(), cm.r()], writes=[pt.r()])
                    P.op("pe", lambda e, h=h, kb=kb, pt=pt, pO=pO, nkb=nkb: e.matmul(
                        pO[:65, :T], lhsT=Va[:, kb, h, :], rhs=pt[:, :], start=(kb == 0), stop=(kb == nkb - 1)),
                        reads=[Va.r(kb // 4), pt.r()], writes=[pO.r()])
                P.op("act", lambda e, pO=pO: e.copy(out=osb[:, :], in_=pO[:65, :T]), reads=[pO.r()], writes=[osb.r()])
                pd = next_ps(P)
                P.op("pe", lambda e, pd=pd: e.matmul(pd[:64, :T], lhsT=es[:, :], rhs=osb[:, :], start=True, stop=True),
                     reads=[es.r(), osb.r()], writes=[pd.r()])
                P.op("dve", lambda e, pd=pd: e.reciprocal(out=rden[:, :], in_=pd[:64, :T]), reads=[pd.r()], writes=[rden.r()])
                P.op("dve", lambda e, o_n=o_n: e.tensor_tensor(out=o_n[:, :], in0=osb[0:64, :], in1=rden[:, :], op=ALU.mult),
                     reads=[osb.r(), rden.r()], writes=[o_n.r()])
                outs.append(P.dma(lambda e, h=h, j=j, o_n=o_n: e.dma_start(
                    out=oT[h * 64:(h + 1) * 64, j * T:(j + 1) * T], in_=o_n[:, :]), reads=[o_n.r()]))
        P.emit(final_waits=outs)
    return nc


def build_gdn(L=16384):
    T = 512
    NTI = L // T
    nc = bass.Bass("TRN2", target_bir_lowering=False)
    hT = nc.dram_tensor("hT", [1024, L], F32, kind="ExternalInput").ap()
    g = nc.dram_tensor("g", [1024], F32, kind="ExternalInput").ap()
    wh = nc.dram_tensor("wh", [1024, 640], F32, kind="ExternalInput").ap()
    cw = nc.dram_tensor("cw", [384, 4], F32, kind="ExternalInput").ap()
    sc = nc.dram_tensor("sc", [128, 2], F32, kind="ExternalInput").ap()
    og = nc.dram_tensor("og", [128], F32, kind="ExternalInput").ap()
    cst = nc.dram_tensor("cst", [128, 5, 128], F32, kind="ExternalInput").ap()
    oT = nc.dram_tensor("oT", [128, L], F32, kind="ExternalOutput").ap()
    s_in = nc.dram_tensor("s_in", [128, 128], F32, kind="ExternalInput").ap()
    h_in = nc.dram_tensor("h_in", [128, 3, 3], F32, kind="ExternalInput").ap()
    s_out = nc.dram_tensor("s_out", [128, 128], F32, kind="ExternalOutput").ap()
    h_out = nc.dram_tensor("h_out", [128, 3, 3], F32, kind="ExternalOutput").ap()
    hv = hT.rearrange("(c p) t -> p c t", p=128)
    with ExitStack() as st:
        st.enter_context(nc.allow_low_precision("bf16 matmul operands for the input projection only"))
        P = Prog(nc, st)
        qbanks = [P.ps(f"qb{i}", [128, 512], F32) for i in range(5)]
        qp = [View(qbanks[i], 0, 128, f"qp{i}") for i in range(5)]
        hp = [View(qbanks[i], 0, 256, f"hp{i}") for i in range(5)]
        P.pss = [P.ps(f"fb{i}", [128, 512], F32) for i in range(3)]
        P._psi = 0
        cnt = {"q": 0, "h": 0}

        def nq():
            cnt["q"] += 1
            return qp[cnt["q"] % 5]

        def nh():
            cnt["q"] += 1
            return hp[cnt["q"] % 5]

        C = make_consts(P)
        ones_f = P.sb("ones_f", [128, 128], F32)
        P.op("dve", lambda e: e.memset(ones_f[:, :], 1.0), writes=[ones_f.r()])
        gt = load_vec_fm(P, g, 8, "g")
        ogt = load_vec_fm(P, og, 1, "og")
        cwt = load_vec_fm_2d = P.sb("cwt", [128, 3, 4], F32)
        P.dma(lambda e: e.dma_start(out=cwt[:, :, :], in_=cw.rearrange("(c p) j -> p c j", p=128)), writes=[cwt.r()])
        sct = load_const(P, sc, [128, 2], "sct")
        K = load_const(P, cst, [128, 5, 128], "K")
        ident = K[:, 0, :]
        maskS = K[:, 1, :]
        maskI = K[:, 2, :]
        blk1 = K[:, 3, :]
        cind = K[:, 4, 0:2]
        Wh = load_w_bf16(P, wh, 8, 640, "Wh")
        Whf = P.sb("Whf", [128, 8, 2], F32)
        P.dma(lambda e: e.dma_start(out=Whf[:, :, :], in_=wh.rearrange("(c p) f -> p c f", p=128)[:, :, 512:514]),
              writes=[Whf.r()])
        for c in range(8):
            P.op("dve", lambda e, c=c: e.tensor_scalar(out=Whf[:, c, :], in0=Whf[:, c, :], scalar1=gt[:, c:c + 1],
                                                       scalar2=None, op0=ALU.mult),
                 reads=[Whf.r(), gt.r()], writes=[Whf.r()])
        nA = P.sb("nA", [128, 1], F32)
        P.op("act", lambda e: e.activation(out=nA[:, :], in_=sct[:, 0:1], func=AF.Exp), reads=[sct.r()], writes=[nA.r()])
        P.op("dve", lambda e: e.tensor_scalar(out=nA[:, :], in0=nA[:, :], scalar1=-1.0, scalar2=None, op0=ALU.mult),
             reads=[nA.r()], writes=[nA.r()])
        onec = P.sb("onec", [128, 1], F32)
        P.op("dve", lambda e: e.memset(onec[:, :], 1.0), writes=[onec.r()])
        epsl2 = C.eps
        S = [P.sb(f"S{i}", [128, 128], F32) for i in range(2)]
        P.dma(lambda e: e.dma_start(out=S[0][:, :], in_=s_in), writes=[S[0].r()])
        sidx = [0]
        x = P.sb("x", [128, 8, T], F32)
        sq = P.sb("sq", [128, 8, T], BF16, nsub=8)
        hn = P.sb("hn", [128, 8, T], BF16, nsub=8)
        rstd = P.sb("rstd", [128, T], F32)
        raw = P.sb("raw", [128, 3, T + 3], F32, nsub=3)
        P.dma(lambda e: e.dma_start(out=raw[:, :, 0:3], in_=h_in), writes=raw.all())
        acc = P.sb("acc", [128, 3, T], F32, nsub=3)
        sil = P.sb("sil", [128, 3, T], F32, nsub=3)
        sq2 = P.sb("sq2", [128, 2, T], F32, nsub=2)
        rn = P.sb("rn", [128, 2, T], F32, nsub=2)
        tb = {}

        PERSIST = ("QpT", "O0T", "MT0", "MT1", "B0", "B1", "gateT", "oTt", "osq", "orr")

        def tbuf(par, name, shape=(128, 128)):
            if not name.rstrip("0123456789").endswith(PERSIST) and not any(name.startswith(p) for p in PERSIST):
                par = 0
            key = (par, name)
            if key not in tb:
                tb[key] = P.sb(f"t{par}_{name}", list(shape), F32)
            return tb[key]

        outs = []

        def gen_local(t):
            par = t % 2
            t0 = t * T
            qnT = tbuf(par, "qnT", (128, T))
            knT = tbuf(par, "knT", (128, T))
            vT = tbuf(par, "vT", (128, T))
            gateT = tbuf(par, "gateT", (128, T))
            P.dma(lambda e: e.dma_start(out=x[:, :, :], in_=hv[:, :, t0:t0 + T]), writes=[x.r()])
            norm_tile(P, C, x, gt, sq, hn, rstd, T)
            yield
            for s3 in range(3):
                ps = next_ps(P)
                for c in range(8):
                    P.op("pe", lambda e, c=c, s3=s3, ps=ps: e.matmul(
                        ps[:, :T], lhsT=Wh[:, c, s3 * 128:(s3 + 1) * 128], rhs=hn[:, c, :],
                        start=(c == 0), stop=(c == 7)), reads=[Wh.r(c), hn.r(c)], writes=[ps.r()])
                P.op("act", lambda e, s3=s3, ps=ps: e.copy(out=raw[:, s3, 3:T + 3], in_=ps[:, :T]),
                     reads=[ps.r()], writes=[raw.r(s3)])
            ps = next_ps(P)
            for c in range(8):
                P.op("pe", lambda e, c=c, ps=ps: e.matmul(
                    ps[:, :T], lhsT=Wh[:, c, 384:512], rhs=hn[:, c, :], start=(c == 0), stop=(c == 7)),
                    reads=[Wh.r(c), hn.r(c)], writes=[ps.r()])
            P.op("act", lambda e, ps=ps: e.activation(out=gateT[:, :], in_=ps[:, :T], func=AF.Silu),
                 reads=[ps.r()], writes=[gateT.r()])
            yield
            for s3 in range(3):
                eng = "dve" if s3 != 1 else "pool"
                P.op("dve", lambda e, s3=s3: e.tensor_scalar(
                    out=acc[:, s3, :], in0=raw[:, s3, 3:T + 3], scalar1=cwt[:, s3, 3:4], scalar2=None, op0=ALU.mult),
                    reads=[raw.r(s3), cwt.r()], writes=[acc.r(s3)])
                for j in range(3):
                    P.op("dve", lambda e, s3=s3, j=j: e.scalar_tensor_tensor(
                        out=acc[:, s3, :], in0=raw[:, s3, j:j + T], scalar=cwt[:, s3, j:j + 1], in1=acc[:, s3, :],
                        op0=ALU.mult, op1=ALU.add), reads=[raw.r(s3), cwt.r(), acc.r(s3)], writes=[acc.r(s3)])
                P.op("pool", lambda e, s3=s3: e.tensor_copy(out=raw[:, s3, 0:3], in_=raw[:, s3, T:T + 3]),
                     reads=[raw.r(s3)], writes=[raw.r(s3)])
                dst = vT if s3 == 2 else sil
                if s3 == 2:
                    P.op("act", lambda e: e.activation(out=vT[:, :], in_=acc[:, 2, :], func=AF.Silu),
                         reads=[acc.r(2)], writes=[vT.r()])
                else:
                    P.op("act", lambda e, s3=s3: e.activation(out=sil[:, s3, :], in_=acc[:, s3, :], func=AF.Silu),
                         reads=[acc.r(s3)], writes=[sil.r(s3)])
            yield
            for s2 in range(2):
                P.op("pool", lambda e, s2=s2: e.tensor_tensor(out=sq2[:, s2, :], in0=sil[:, s2, :], in1=sil[:, s2, :], op=ALU.mult),
                     reads=[sil.r(s2)], writes=[sq2.r(s2)])
                ps = next_ps(P)
                P.op("pe", lambda e, s2=s2, ps=ps: e.matmul(ps[:, :T], lhsT=ones_f[:, :], rhs=sq2[:, s2, :], start=True, stop=True),
                     reads=[ones_f.r(), sq2.r(s2)], writes=[ps.r()])
                P.op("act", lambda e, s2=s2, ps=ps: e.activation(out=rn[:, s2, :], in_=ps[:, :T], func=AF.Sqrt,
                                                                bias=epsl2[:, :], scale=1.0),
                     reads=[ps.r(), epsl2.r()], writes=[rn.r(s2)])
                P.op("dve", lambda e, s2=s2: e.reciprocal(out=rn[:, s2, :], in_=rn[:, s2, :]), reads=[rn.r(s2)], writes=[rn.r(s2)])
                dst = qnT if s2 == 0 else knT
                scl = (128.0 ** -0.5) if s2 == 0 else 1.0
                P.op("dve", lambda e, s2=s2, dst=dst, scl=scl: e.scalar_tensor_tensor(
                    out=dst[:, :], in0=sil[:, s2, :], scalar=scl, in1=rn[:, s2, :], op0=ALU.mult, op1=ALU.mult),
                    reads=[sil.r(s2), rn.r(s2)], writes=[dst.r()])
            yield
            B4 = range(4)
            tl = lambda s, name, shape=(128, 128): tbuf(par, f"{name}{s}", shape)
            pKK, pQK, pgc = {}, {}, {}
            for s in B4:
                ts = slice(s * 128, (s + 1) * 128)
                cols = tl(s, "cols", (128, 8))
                tcol = tl(s, "tcol", (128, 8))
                pc = nq()
                for c in range(8):
                    P.op("pe", lambda e, c=c, pc=pc, ts=ts: e.matmul(pc[:, 0:2], lhsT=x[:, c, ts], rhs=Whf[:, c, 0:2],
                                                                   start=(c == 0), stop=(c == 7)),
                         reads=[x.r(), Whf.r()], writes=[pc.r()])
                pss_ = nq()
                for c in range(8):
                    P.op("pe", lambda e, c=c, pss_=pss_, ts=ts: e.matmul(pss_[:, 0:2], lhsT=sq[:, c, ts], rhs=C.ones_bf[:, 0:2],
                                                                       start=(c == 0), stop=(c == 7)),
                         reads=[sq.r(c), C.ones_bf.r()], writes=[pss_.r()])
                P.op("act", lambda e, pss_=pss_, tcol=tcol: e.activation(out=tcol[:, 0:2], in_=pss_[:, 0:2], func=AF.Sqrt,
                                                                       bias=C.eps[:, :], scale=1.0 / 1024),
                     reads=[pss_.r(), C.eps.r()], writes=[tcol.r()])
                P.op("dve", lambda e, tcol=tcol: e.reciprocal(out=tcol[:, 0:2], in_=tcol[:, 0:2]), reads=[tcol.r()], writes=[tcol.r()])
                P.op("dve", lambda e, pc=pc, tcol=tcol: e.tensor_tensor(out=tcol[:, 2:4], in0=pc[:, 0:2], in1=tcol[:, 0:2], op=ALU.mult),
                     reads=[pc.r(), tcol.r()], writes=[tcol.r()])
                P.op("act", lambda e, tcol=tcol, cols=cols: e.activation(out=cols[:, 1:2], in_=tcol[:, 2:3], func=AF.Sigmoid),
                     reads=[tcol.r()], writes=[cols.r()])
                P.op("act", lambda e, tcol=tcol: e.activation(out=tcol[:, 4:5], in_=tcol[:, 3:4], func=AF.Exp, bias=sct[:, 1:2], scale=1.0),
                     reads=[tcol.r(), sct.r()], writes=[tcol.r()])
                P.op("act", lambda e, tcol=tcol: e.activation(out=tcol[:, 5:6], in_=tcol[:, 4:5], func=AF.Ln, bias=onec[:, 0:1], scale=1.0),
                     reads=[tcol.r(), onec.r()], writes=[tcol.r()])
                P.op("dve", lambda e, tcol=tcol, cols=cols: e.tensor_scalar(out=cols[:, 0:1], in0=tcol[:, 5:6], scalar1=nA[:, 0:1], scalar2=None, op0=ALU.mult),
                     reads=[tcol.r(), nA.r()], writes=[cols.r()])
                gB = tl(s, "gB"); bB = tl(s, "bB")
                P.op("dve", lambda e, gB=gB, cols=cols: e.tensor_scalar(out=gB[:, :], in0=ones_f[:, :], scalar1=cols[:, 0:1],
                                                                       scalar2=None, op0=ALU.mult),
                     reads=[ones_f.r(), cols.r()], writes=[gB.r()])
                P.op("pool", lambda e, bB=bB, cols=cols: e.tensor_scalar(out=bB[:, :], in0=ones_f[:, :], scalar1=cols[:, 1:2],
                                                                        scalar2=None, op0=ALU.mult),
                     reads=[ones_f.r(), cols.r()], writes=[bB.r()])
            yield
            for s in B4:
                ts = slice(s * 128, (s + 1) * 128)
                cols = tl(s, "cols", (128, 8)); gB = tl(s, "gB")
                pgc[s] = nq()
                P.op("pe", lambda e, p=pgc[s], gB=gB: e.matmul(p[:, :], lhsT=gB[:, :], rhs=maskI, start=True, stop=True),
                     reads=[gB.r(), K.r()], writes=[pgc[s].r()])
                pm = nq()
                P.op("pe", lambda e, pm=pm, cols=cols: e.matmul(pm[:, 0:2], lhsT=maskI, rhs=cols[:, 0:2], start=True, stop=True),
                     reads=[K.r(), cols.r()], writes=[pm.r()])
                P.op("pe", lambda e, pm=pm, cols=cols: e.matmul(pm[:, 2:4], lhsT=blk1, rhs=cols[:, 0:2], start=True, stop=True),
                     reads=[K.r(), cols.r()], writes=[pm.r()])
                P.op("pe", lambda e, pm=pm, gB=gB: e.matmul(pm[:, 4:6], lhsT=gB[:, :], rhs=cind, start=True, stop=True),
                     reads=[K.r(), gB.r()], writes=[pm.r()])
                P.op("act", lambda e, pm=pm, cols=cols: e.copy(out=cols[:, 2:3], in_=pm[:, 0:1]), reads=[pm.r()], writes=[cols.r()])
                P.op("act", lambda e, pm=pm, cols=cols: e.copy(out=cols[:, 3:4], in_=pm[:, 2:3]), reads=[pm.r()], writes=[cols.r()])
                glb = tl(s, "glb", (128, 2))
                P.op("act", lambda e, pm=pm, glb=glb: e.activation(out=glb[:, :], in_=pm[:, 4:6], func=AF.Exp),
                     reads=[pm.r()], writes=[glb.r()])
                E = tl(s, "E"); egc = tl(s, "egc")
                P.op("dve", lambda e, p=pgc[s], E=E, cols=cols: e.tensor_scalar(
                    out=E[:, :], in0=p[:, :], scalar1=cols[:, 2:3], scalar2=0.0, op0=ALU.subtract, op1=ALU.min),
                    reads=[pgc[s].r(), cols.r()], writes=[E.r()])
                P.op("act", lambda e, p=pgc[s], egc=egc: e.activation(out=egc[:, :], in_=p[:, :], func=AF.Exp),
                     reads=[pgc[s].r()], writes=[egc.r()])
                P.op("act", lambda e, cols=cols: e.activation(out=cols[:, 4:5], in_=cols[:, 2:3], func=AF.Exp),
                     reads=[cols.r()], writes=[cols.r()])
                P.op("dve", lambda e, cols=cols: e.tensor_tensor(out=cols[:, 5:6], in0=cols[:, 4:5], in1=cols[:, 1:2], op=ALU.mult),
                     reads=[cols.r()], writes=[cols.r()])
                P.op("dve", lambda e, cols=cols: e.tensor_tensor(out=cols[:, 6:7], in0=cols[:, 3:4], in1=cols[:, 2:3], op=ALU.subtract),
                     reads=[cols.r()], writes=[cols.r()])
                P.op("act", lambda e, cols=cols: e.activation(out=cols[:, 6:7], in_=cols[:, 6:7], func=AF.Exp),
                     reads=[cols.r()], writes=[cols.r()])
            yield
            for s in B4:
                ts = slice(s * 128, (s + 1) * 128)
                E = tl(s, "E"); EmS = tl(s, "EmS"); EmI = tl(s, "EmI")
                P.op("act", lambda e, E=E: e.activation(out=E[:, :], in_=E[:, :], func=AF.Exp), reads=[E.r()], writes=[E.r()])
                pbb = nq()
                bB = tl(s, "bB")
                P.op("pe", lambda e, pbb=pbb, bB=bB: e.matmul(pbb[:, :], lhsT=bB[:, :], rhs=ident, start=True, stop=True),
                     reads=[bB.r(), K.r()], writes=[pbb.r()])
                P.op("pool", lambda e, E=E, EmS=EmS: e.tensor_tensor(out=EmS[:, :], in0=E[:, :], in1=maskS, op=ALU.mult),
                     reads=[E.r(), K.r()], writes=[EmS.r()])
                P.op("pool", lambda e, E=E, EmI=EmI: e.tensor_tensor(out=EmI[:, :], in0=E[:, :], in1=maskI, op=ALU.mult),
                     reads=[E.r(), K.r()], writes=[EmI.r()])
                P.op("dve", lambda e, pbb=pbb, EmS=EmS: e.tensor_tensor(out=EmS[:, :], in0=pbb[:, :], in1=EmS[:, :], op=ALU.mult),
                     reads=[pbb.r(), EmS.r()], writes=[EmS.r()])
            yield
            for s in B4:
                ts = slice(s * 128, (s + 1) * 128)
                EmS = tl(s, "EmS"); EmI = tl(s, "EmI"); Q0 = tl(s, "Qa"); qkT = tl(s, "qkT")
                pKK[s] = nq()
                P.op("pe", lambda e, p=pKK[s], ts=ts: e.matmul(p[:, :], lhsT=knT[:, ts], rhs=knT[:, ts], start=True, stop=True),
                     reads=[knT.r()], writes=[pKK[s].r()])
                P.op("dve", lambda e, p=pKK[s], EmS=EmS, Q0=Q0: e.scalar_tensor_tensor(
                    out=Q0[:, :], in0=p[:, :], scalar=-1.0, in1=EmS[:, :], op0=ALU.mult, op1=ALU.mult),
                    reads=[pKK[s].r(), EmS.r()], writes=[Q0.r()])
                pQK[s] = nq()
                P.op("pe", lambda e, p=pQK[s], ts=ts: e.matmul(p[:, :], lhsT=knT[:, ts], rhs=qnT[:, ts], start=True, stop=True),
                     reads=[knT.r(), qnT.r()], writes=[pQK[s].r()])
                P.op("dve", lambda e, p=pQK[s], EmI=EmI, qkT=qkT: e.tensor_tensor(out=qkT[:, :], in0=p[:, :], in1=EmI[:, :], op=ALU.mult),
                     reads=[pQK[s].r(), EmI.r()], writes=[qkT.r()])
            yield
            for s in B4:
                Q0 = tl(s, "Qa"); N0 = tl(s, "Na"); R0 = tl(s, "Ra")
                pt = nq()
                P.op("pe", lambda e, pt=pt, Q0=Q0: e.transpose(pt[:, :], Q0[:, :], ident), reads=[Q0.r(), K.r()], writes=[pt.r()])
                P.op("act", lambda e, pt=pt, N0=N0: e.copy(out=N0[:, :], in_=pt[:, :]), reads=[pt.r()], writes=[N0.r()])
                P.op("pool", lambda e, Q0=Q0, R0=R0: e.tensor_tensor(out=R0[:, :], in0=Q0[:, :], in1=ident, op=ALU.add),
                     reads=[Q0.r(), K.r()], writes=[R0.r()])
            yield
            names = ["a", "b"]
            for i in range(1, 6):
                po, pn = names[(i - 1) % 2], names[i % 2]
                for s in B4:
                    Qo = tl(s, "Q" + po); No = tl(s, "N" + po); Ro = tl(s, "R" + po)
                    Qn = tl(s, "Q" + pn); Nn = tl(s, "N" + pn)
                    pN = nq()
                    P.op("pe", lambda e, pN=pN, Qo=Qo, No=No: e.matmul(pN[:, :], lhsT=Qo[:, :], rhs=No[:, :], start=True, stop=True),
                         reads=[Qo.r(), No.r()], writes=[pN.r()])
                    P.op("act", lambda e, pN=pN, Nn=Nn: e.copy(out=Nn[:, :], in_=pN[:, :]), reads=[pN.r()], writes=[Nn.r()])
                    if i < 5:
                        pQ = nq()
                        P.op("pe", lambda e, pQ=pQ, Qo=Qo, No=No: e.matmul(pQ[:, :], lhsT=No[:, :], rhs=Qo[:, :], start=True, stop=True),
                             reads=[Qo.r(), No.r()], writes=[pQ.r()])
                        P.op("dve", lambda e, pQ=pQ, Qn=Qn: e.tensor_copy(out=Qn[:, :], in_=pQ[:, :]), reads=[pQ.r()], writes=[Qn.r()])
                yield
                for s in B4:
                    Nn = tl(s, "N" + pn); Ro = tl(s, "R" + po); Rn = tl(s, "R" + pn)
                    pR = nq()
                    P.op("pe", lambda e, pR=pR, Nn=Nn, Ro=Ro: e.matmul(pR[:, :], lhsT=Nn[:, :], rhs=Ro[:, :], start=True, stop=True),
                         reads=[Nn.r(), Ro.r()], writes=[pR.r()])
                    P.op("dve", lambda e, pR=pR, Ro=Ro, Rn=Rn: e.tensor_tensor(out=Rn[:, :], in0=pR[:, :], in1=Ro[:, :], op=ALU.add),
                         reads=[pR.r(), Ro.r()], writes=[Rn.r()])
                yield
            TTn = names[5 % 2]
            for s in B4:
                ts = slice(s * 128, (s + 1) * 128)
                cols = tl(s, "cols", (128, 8))
                UWin = tl(s, "UWin", (128, 256)); kd = tl(s, "kd")
                pk = nq()
                P.op("pe", lambda e, pk=pk, ts=ts: e.transpose(pk[:, :], knT[:, ts], ident), reads=[knT.r(), K.r()], writes=[pk.r()])
                P.op("dve", lambda e, pk=pk, UWin=UWin, cols=cols: e.tensor_scalar(out=UWin[:, 128:256], in0=pk[:, :], scalar1=cols[:, 5:6], scalar2=None, op0=ALU.mult),
                     reads=[pk.r(), cols.r()], writes=[UWin.r()])
                P.op("dve", lambda e, pk=pk, kd=kd, cols=cols: e.tensor_scalar(out=kd[:, :], in0=pk[:, :], scalar1=cols[:, 6:7], scalar2=None, op0=ALU.mult),
                     reads=[pk.r(), cols.r()], writes=[kd.r()])
                pv = nq()
                P.op("pe", lambda e, pv=pv, ts=ts: e.transpose(pv[:, :], vT[:, ts], ident), reads=[vT.r(), K.r()], writes=[pv.r()])
                P.op("dve", lambda e, pv=pv, UWin=UWin, cols=cols: e.tensor_scalar(out=UWin[:, 0:128], in0=pv[:, :], scalar1=cols[:, 1:2], scalar2=None, op0=ALU.mult),
                     reads=[pv.r(), cols.r()], writes=[UWin.r()])
            yield
            for s in B4:
                ts = slice(s * 128, (s + 1) * 128)
                UWin = tl(s, "UWin", (128, 256)); uw = tl(s, "uw", (128, 256)); TT = tl(s, "R" + TTn)
                egc = tl(s, "egc"); qdT = tl(s, "qdT")
                pu = nh()
                P.op("pe", lambda e, pu=pu, TT=TT, UWin=UWin: e.matmul(pu[:, :], lhsT=TT[:, :], rhs=UWin[:, :], start=True, stop=True),
                     reads=[TT.r(), UWin.r()], writes=[pu.r()])
                P.op("act", lambda e, pu=pu, uw=uw: e.copy(out=uw[:, :], in_=pu[:, :]), reads=[pu.r()], writes=[uw.r()])
                P.op("pool", lambda e, qdT=qdT, egc=egc, ts=ts: e.tensor_tensor(out=qdT[:, :], in0=qnT[:, ts], in1=egc[:, :], op=ALU.mult),
                     reads=[qnT.r(), egc.r()], writes=[qdT.r()])
            yield
            for s in B4:
                uw = tl(s, "uw", (128, 256)); qkT = tl(s, "qkT"); qdT = tl(s, "qdT"); kd = tl(s, "kd")
                QpT = tl(s, "QpT"); O0T = tl(s, "O0T"); glb = tl(s, "glb", (128, 2))
                pw = nq()
                P.op("pe", lambda e, pw=pw, uw=uw, qkT=qkT: e.matmul(pw[:, :], lhsT=uw[:, 128:256], rhs=qkT[:, :], start=True, stop=True),
                     reads=[uw.r(), qkT.r()], writes=[pw.r()])
                P.op("dve", lambda e, pw=pw, qdT=qdT, QpT=QpT: e.tensor_tensor(out=QpT[:, :], in0=qdT[:, :], in1=pw[:, :], op=ALU.subtract),
                     reads=[pw.r(), qdT.r()], writes=[QpT.r()])
                po0 = nq()
                P.op("pe", lambda e, po0=po0, uw=uw, qkT=qkT: e.matmul(po0[:, :], lhsT=uw[:, 0:128], rhs=qkT[:, :], start=True, stop=True),
                     reads=[uw.r(), qkT.r()], writes=[po0.r()])
                P.op("act", lambda e, po0=po0, O0T=O0T: e.copy(out=O0T[:, :], in_=po0[:, :]), reads=[po0.r()], writes=[O0T.r()])
                for c2 in range(2):
                    r = slice(c2 * 64, (c2 + 1) * 64)
                    MT = tl(s, f"MT{c2}"); Bc = tl(s, f"B{c2}")
                    pM = nq()
                    P.op("pe", lambda e, pM=pM, uw=uw, kd=kd, r=r: e.matmul(pM[:, :], lhsT=uw[r, 128:256], rhs=kd[r, :], start=True, stop=True),
                         reads=[uw.r(), kd.r()], writes=[pM.r()])
                    P.op("dve", lambda e, pM=pM, MT=MT, glb=glb, c2=c2: e.scalar_tensor_tensor(
                        out=MT[:, :], in0=ident, scalar=glb[:, c2:c2 + 1], in1=pM[:, :], op0=ALU.mult, op1=ALU.subtract),
                        reads=[pM.r(), glb.r(), K.r()], writes=[MT.r()])
                    pB = nq()
                    P.op("pe", lambda e, pB=pB, uw=uw, kd=kd, r=r: e.matmul(pB[:, :], lhsT=kd[r, :], rhs=uw[r, 0:128], start=True, stop=True),
                         reads=[uw.r(), kd.r()], writes=[pB.r()])
                    P.op("act", lambda e, pB=pB, Bc=Bc: e.copy(out=Bc[:, :], in_=pB[:, :]), reads=[pB.r()], writes=[Bc.r()])
                yield

        def gen_rec(t):
            par = t % 2
            t0 = t * T
            tl = lambda s, name, shape=(128, 128): tbuf(par, f"{name}{s}", shape)
            oTt = tbuf(par, "oTt", (128, T))
            gateT = tbuf(par, "gateT", (128, T))
            for s in range(4):
                QpT = tl(s, "QpT"); O0T = tl(s, "O0T")
                for c2 in range(2):
                    r = slice(c2 * 64, (c2 + 1) * 64)
                    col = slice(s * 128 + c2 * 64, s * 128 + (c2 + 1) * 64)
                    MT = tl(s, f"MT{c2}"); Bc = tl(s, f"B{c2}")
                    So = S[sidx[0] % 2]
                    Sn = S[(sidx[0] + 1) % 2]
                    sidx[0] += 1
                    po = nq()
                    P.op("pe", lambda e, po=po, So=So, QpT=QpT, r=r: e.matmul(po[:, 0:64], lhsT=So[:, :], rhs=QpT[:, r], start=True, stop=True),
                         reads=[So.r(), QpT.r()], writes=[po.r()])
                    pS = nq()
                    P.op("pe", lambda e, pS=pS, So=So, MT=MT: e.matmul(pS[:, :], lhsT=MT[:, :], rhs=So[:, :], start=True, stop=True),
                         reads=[So.r(), MT.r()], writes=[pS.r()])
                    P.op("dve", lambda e, pS=pS, Bc=Bc, Sn=Sn: e.tensor_tensor(out=Sn[:, :], in0=pS[:, :], in1=Bc[:, :], op=ALU.add),
                         reads=[pS.r(), Bc.r()], writes=[Sn.r()])
                    P.op("dve", lambda e, po=po, O0T=O0T, r=r, col=col: e.tensor_tensor(out=oTt[:, col], in0=po[:, 0:64], in1=O0T[:, r], op=ALU.add),
                         reads=[po.r(), O0T.r()], writes=[oTt.r()])
                    yield
            osq = tbuf(par, "osq", (128, T))
            orr = tbuf(par, "orr", (128, T))
            P.op("pool", lambda e: e.tensor_tensor(out=osq[:, :], in0=oTt[:, :], in1=oTt[:, :], op=ALU.mult), reads=[oTt.r()], writes=[osq.r()])
            ps = next_ps(P)
            P.op("pe", lambda e, ps=ps: e.matmul(ps[:, :T], lhsT=ones_f[:, :], rhs=osq[:, :], start=True, stop=True),
                 reads=[ones_f.r(), osq.r()], writes=[ps.r()])
            P.op("act", lambda e, ps=ps: e.activation(out=orr[:, :], in_=ps[:, :T], func=AF.Sqrt, bias=C.eps[:, :], scale=1.0 / 128),
                 reads=[ps.r(), C.eps.r()], writes=[orr.r()])
            P.op("dve", lambda e: e.reciprocal(out=orr[:, :], in_=orr[:, :]), reads=[orr.r()], writes=[orr.r()])
            P.op("dve", lambda e: e.scalar_tensor_tensor(out=osq[:, :], in0=oTt[:, :], scalar=ogt[:, 0:1], in1=orr[:, :],
                                                        op0=ALU.mult, op1=ALU.mult),
                 reads=[oTt.r(), orr.r(), ogt.r()], writes=[osq.r()])
            P.op("pool", lambda e: e.tensor_tensor(out=osq[:, :], in0=osq[:, :], in1=gateT[:, :], op=ALU.mult),
                 reads=[osq.r(), gateT.r()], writes=[osq.r()])
            outs.append(P.dma(lambda e: e.dma_start(out=oT[:, t0:t0 + T], in_=osq[:, :]), reads=[osq.r()]))
            yield

        rec = None
        for t in range(NTI):
            loc = gen_local(t)
            while True:
                a = next(loc, "done")
                if rec is not None:
                    next(rec, None)
                if a == "done":
                    break
            if rec is not None:
                for _ in rec:
                    pass
            rec = gen_rec(t)
        for _ in rec:
            pass
        outs.append(P.dma(lambda e: e.dma_start(out=s_out, in_=S[sidx[0] % 2][:, :]), reads=[S[sidx[0] % 2].r()]))
        outs.append(P.dma(lambda e: e.dma_start(out=h_out, in_=raw[:, :, 0:3]), reads=raw.all()))
        P.emit(final_waits=outs)
    return nc


def build_dsa(L=16384, NIT=18, jset=None):
    T = 512
    NTI = L // T
    NQT = L // 256
    NJ_ALL = NQT // 8
    jset = list(range(NJ_ALL)) if jset is None else list(jset)
    NJ = len(jset)
    NQ = NJ * 256
    SCALE = 128.0 ** -0.5
    WSC = (8.0 ** -0.5) * (64.0 ** -0.5)
    nc = bass.Bass("TRN2", target_bir_lowering=False)
    xT = nc.dram_tensor("xT", [1024, L], F32, kind="ExternalInput").ap()
    xq = nc.dram_tensor("xq", [1024, NQ], F32, kind="ExternalInput").ap()
    g = nc.dram_tensor("g", [1024], F32, kind="ExternalInput").ap()
    win = nc.dram_tensor("win", [1024, 3656], F32, kind="ExternalInput").ap()
    lng = nc.dram_tensor("lng", [64], F32, kind="ExternalInput").ap()
    lnb = nc.dram_tensor("lnb", [64], F32, kind="ExternalInput").ap()
    qrel = nc.dram_tensor("qrel", [128, NJ * 2], F32, kind="ExternalInput").ap()
    kidx = nc.dram_tensor("kidx", [128, 2048], F32, kind="ExternalInput").ap()
    cst = nc.dram_tensor("cst", [128, 5, 128], F32, kind="ExternalInput").ap()
    oT = nc.dram_tensor("oT", [1024, NQ], F32, kind="ExternalOutput").ap()
    kTd = nc.dram_tensor("kTd", [1024, L], BF16, kind="Internal").ap()
    Vd = nc.dram_tensor("Vd", [L, 1024], BF16, kind="Internal").ap()
    qTd = nc.dram_tensor("qTd", [1024, NQ], BF16, kind="Internal").ap()
    qiTd = nc.dram_tensor("qiTd", [64, 8, NQ], F32, kind="Internal").ap()
    kiTd = nc.dram_tensor("kiTd", [64, L], F32, kind="Internal").ap()
    xv = xT.rearrange("(c p) t -> p c t", p=128)
    xqv = xq.rearrange("(c p) t -> p c t", p=128)
    kTv = kTd.rearrange("(h p) t -> p h t", p=128)
    Vv = Vd.rearrange("(n p) d -> p n d", p=128)
    qTv = qTd.rearrange("(h p) t -> p h t", p=128)
    oTv = oT.rearrange("(h p) t -> p h t", p=128)
    wv_ = win.rearrange("(c p) f -> p c f", p=128)
    with ExitStack() as st:
        st.enter_context(nc.allow_low_precision("bf16 matmul operands, fp32 accumulate"))
        P = Prog(nc, st)
        banks = [P.ps(f"b{i}", [128, 512], F32) for i in range(8)]
        P.pss = banks
        P._psi = 0
        C = make_consts(P)
        ones_f = P.sb("ones_f", [128, 128], F32)
        P.op("dve", lambda e: e.memset(ones_f[:, :], 1.0), writes=[ones_f.r()])
        gt = load_vec_fm(P, g, 8, "g")
        lngt = P.sb("lngt", [64, 1], F32)
        lnbt = P.sb("lnbt", [64, 1], F32)
        P.dma(lambda e: e.dma_start(out=lngt[:, :], in_=lng.rearrange("(p o) -> p o", o=1)), writes=[lngt.r()])
        P.dma(lambda e: e.dma_start(out=lnbt[:, :], in_=lnb.rearrange("(p o) -> p o", o=1)), writes=[lnbt.r()])
        qrt = load_const(P, qrel, [128, NJ * 2], "qrt")
        kit = load_const(P, kidx, [128, 2048], "kit")
        Kc = load_const(P, cst, [128, 5, 128], "Kc")
        idb = P.sb("idb", [128, 128], BF16)
        P.op("dve", lambda e: e.tensor_copy(out=idb[:, :], in_=Kc[:, 0, :]), reads=[Kc.r()], writes=[idb.r()])
        selb = P.sb("selb", [128, 2, 512], BF16)
        P.op("dve", lambda e: e.memset(selb[:, :, :], 0.0), writes=[selb.r()])
        for hf in range(2):
            for rep in range(2):
                c0 = rep * 256 + hf * 128
                P.op("dve", lambda e, hf=hf, c0=c0: e.tensor_copy(out=selb[:, hf, c0:c0 + 128], in_=Kc[:, 0, :]),
                     reads=[Kc.r(), selb.r()], writes=[selb.r()])
        wiT = P.sb("wiT", [128, NJ * 2, 8], F32)

        P.push_scope()
        Wq = P.sb("Wq", [128, 8, 1024], BF16, nsub=8)
        Wk = P.sb("Wk", [128, 8, 1024], BF16, nsub=8)
        Wv = P.sb("Wv", [128, 8, 1024], BF16, nsub=8)
        Wi = P.sb("Wi", [128, 8, 584], F32, nsub=8)
        hnf = P.sb("hnf", [128, 8, T], F32, nsub=8)
        for c in range(8):
            P.dma(lambda e, c=c: e.dma_start(out=Wk[:, c, :], in_=wv_[:, c, 1024:2048], max_dma_last_dim=8192), writes=[Wk.r(c)], q="pool")
            P.dma(lambda e, c=c: e.dma_start(out=Wv[:, c, :], in_=wv_[:, c, 2048:3072], max_dma_last_dim=8192), writes=[Wv.r(c)], q="pool")
            P.dma(lambda e, c=c: e.dma_start(out=Wi[:, c, :], in_=wv_[:, c, 3072:3656]), writes=[Wi.r(c)])
            P.dma(lambda e, c=c: e.dma_start(out=Wq[:, c, :], in_=wv_[:, c, 0:1024], max_dma_last_dim=8192), writes=[Wq.r(c)], q="pool")
        x = P.sb("x", [128, 8, T], F32)
        sq = P.sb("sq", [128, 8, T], BF16, nsub=8)
        hn = P.sb("hn", [128, 8, T], BF16, nsub=8)
        rstd = P.sb("rstd", [128, T], F32)
        ktb = [P.sb(f"ktb{i}", [128, 8, T], BF16, nsub=8) for i in range(2)]
        vtb = [P.sb(f"vtb{i}", [128, 4, 1024], BF16, nsub=8) for i in range(2)]
        kraw = P.sb("kraw", [64, T], F32)
        ksq = P.sb("ksq", [64, T], F32)
        kmean = P.sb("kmean", [64, T], F32)
        kvar = P.sb("kvar", [64, T], F32)
        kio = P.sb("kio", [64, T], F32)
        for it in range(NTI):
            t0 = it * T
            kt = ktb[it % 2]
            vt = vtb[it % 2]
            P.dma(lambda e, t0=t0: e.dma_start(out=x[:, :, :], in_=xv[:, :, t0:t0 + T]), writes=[x.r()])
            norm_tile(P, C, x, gt, sq, hn, rstd, T)
            for c in range(8):
                P.op("dve", lambda e, c=c: e.scalar_tensor_tensor(out=hnf[:, c, :], in0=x[:, c, :], scalar=gt[:, c:c + 1], in1=rstd[:, :],
                                                                 op0=ALU.mult, op1=ALU.mult), reads=[x.r(), rstd.r(), gt.r()], writes=[hnf.r(c)])
            for h in range(8):
                ps = next_ps(P)
                for c in range(8):
                    P.op("pe", lambda e, c=c, h=h, ps=ps: e.matmul(ps[:, :T], lhsT=Wk[:, c, h * 128:(h + 1) * 128], rhs=hn[:, c, :],
                                                                  start=(c == 0), stop=(c == 7)),
                         reads=[Wk.r(c), hn.r(c)], writes=[ps.r()])
                if h % 2 == 0:
                    P.op("act", lambda e, h=h, ps=ps, kt=kt: e.copy(out=kt[:, h, :], in_=ps[:, :T]), reads=[ps.r()], writes=[kt.r(h)])
                else:
                    P.op("dve", lambda e, h=h, ps=ps, kt=kt: e.tensor_copy(out=kt[:, h, :], in_=ps[:, :T]), reads=[ps.r()], writes=[kt.r(h)])
            P.dma(lambda e, kt=kt, t0=t0: e.dma_start(out=kTv[:, :, t0:t0 + T], in_=kt[:, :, :]), reads=kt.all())
            for b4 in range(4):
                for half in range(2):
                    ps = next_ps(P)
                    for c in range(8):
                        P.op("pe", lambda e, c=c, b4=b4, half=half, ps=ps: e.matmul(
                            ps[:, :512], lhsT=hn[:, c, b4 * 128:(b4 + 1) * 128], rhs=Wv[:, c, half * 512:(half + 1) * 512],
                            start=(c == 0), stop=(c == 7)), reads=[Wv.r(c), hn.r(c)], writes=[ps.r()])
                    if half == 0:
                        P.op("act", lambda e, b4=b4, half=half, ps=ps, vt=vt: e.copy(out=vt[:, b4, 0:512], in_=ps[:, :512]),
                             reads=[ps.r()], writes=[vt.r(b4 * 2)])
                    else:
                        P.op("dve", lambda e, b4=b4, half=half, ps=ps, vt=vt: e.tensor_copy(out=vt[:, b4, 512:1024], in_=ps[:, :512]),
                             reads=[ps.r()], writes=[vt.r(b4 * 2 + 1)])
            P.dma(lambda e, vt=vt, it=it: e.dma_start(out=Vv[:, it * 4:(it + 1) * 4, :], in_=vt[:, :, :]), reads=vt.all(), q="act")
            ps = next_ps(P)
            for c in range(8):
                P.op("pe", lambda e, c=c, ps=ps: e.matmul(ps[0:64, :T], lhsT=Wi[:, c, 512:576], rhs=hnf[:, c, :],
                                                       start=(c == 0), stop=(c == 7)),
                     reads=[Wi.r(c), hnf.r(c)], writes=[ps.r()])
            P.op("act", lambda e, ps=ps: e.copy(out=kraw[:, :], in_=ps[0:64, :T]), reads=[ps.r()], writes=[kraw.r()])
            P.op("pool", lambda e: e.tensor_tensor(out=ksq[:, :], in0=kraw[:, :], in1=kraw[:, :], op=ALU.mult), reads=[kraw.r()], writes=[ksq.r()])
            p1 = next_ps(P)
            P.op("pe", lambda e, p1=p1: e.matmul(p1[0:64, :T], lhsT=ones_f[0:64, 0:64], rhs=kraw[:, :], start=True, stop=True),
                 reads=[ones_f.r(), kraw.r()], writes=[p1.r()])
            p2 = next_ps(P)
            P.op("pe", lambda e, p2=p2: e.matmul(p2[0:64, :T], lhsT=ones_f[0:64, 0:64], rhs=ksq[:, :], start=True, stop=True),
                 reads=[ones_f.r(), ksq.r()], writes=[p2.r()])
            P.op("act", lambda e, p1=p1: e.activation(out=kmean[:, :], in_=p1[0:64, :T], func=AF.Copy, scale=1.0 / 64), reads=[p1.r()], writes=[kmean.r()])
            P.op("dve", lambda e: e.tensor_tensor(out=ksq[:, :], in0=kmean[:, :], in1=kmean[:, :], op=ALU.mult), reads=[kmean.r(), ksq.r()], writes=[ksq.r()])
            P.op("dve", lambda e, p2=p2: e.scalar_tensor_tensor(out=kvar[:, :], in0=p2[0:64, :T], scalar=1.0 / 64, in1=ksq[:, :],
                                                              op0=ALU.mult, op1=ALU.subtract), reads=[p2.r(), ksq.r()], writes=[kvar.r()])
            P.op("act", lambda e: e.activation(out=kvar[:, :], in_=kvar[:, :], func=AF.Sqrt, bias=C.eps[0:64, :], scale=1.0),
                 reads=[kvar.r(), C.eps.r()], writes=[kvar.r()])
            P.op("dve", lambda e: e.reciprocal(out=kvar[:, :], in_=kvar[:, :]), reads=[kvar.r()], writes=[kvar.r()])
            P.op("pool", lambda e: e.tensor_tensor(out=kraw[:, :], in0=kraw[:, :], in1=kmean[:, :], op=ALU.subtract), reads=[kraw.r(), kmean.r()], writes=[kraw.r()])
            P.op("dve", lambda e: e.scalar_tensor_tensor(out=kraw[:, :], in0=kraw[:, :], scalar=lngt[:, 0:1], in1=kvar[:, :],
                                                        op0=ALU.mult, op1=ALU.mult), reads=[kraw.r(), kvar.r(), lngt.r()], writes=[kraw.r()])
            P.op("act", lambda e: e.activation(out=kio[:, :], in_=kraw[:, :], func=AF.Identity, bias=lnbt[:, 0:1], scale=1.0),
                 reads=[kraw.r(), lnbt.r()], writes=[kio.r()])
            P.dma(lambda e, t0=t0: e.dma_start(out=kiTd[:, t0:t0 + T], in_=kio[:, :]), reads=[kio.r()])
        qtb = P.sb("qtb", [128, 8, 256], BF16, nsub=8)
        qitb = P.sb("qitb", [64, 8, 256], F32, nsub=8)
        for j in range(NJ):
            q0 = j * 256
            P.dma(lambda e, q0=q0: e.dma_start(out=x[:, :, 0:256], in_=xqv[:, :, q0:q0 + 256]), writes=[x.r()])
            norm_tile(P, C, x, gt, sq, hn, rstd, 256)
            for c in range(8):
                P.op("dve", lambda e, c=c: e.scalar_tensor_tensor(out=hnf[:, c, 0:256], in0=x[:, c, 0:256], scalar=gt[:, c:c + 1], in1=rstd[:, 0:256],
                                                                 op0=ALU.mult, op1=ALU.mult), reads=[x.r(), rstd.r(), gt.r()], writes=[hnf.r(c)])
            for h in range(8):
                ps = next_ps(P)
                for c in range(8):
                    P.op("pe", lambda e, c=c, h=h, ps=ps: e.matmul(ps[:, :256], lhsT=Wq[:, c, h * 128:(h + 1) * 128], rhs=hn[:, c, 0:256],
                                                                  start=(c == 0), stop=(c == 7)),
                         reads=[Wq.r(c), hn.r(c)], writes=[ps.r()])
                P.op("act", lambda e, h=h, ps=ps: e.copy(out=qtb[:, h, :], in_=ps[:, :256]), reads=[ps.r()], writes=[qtb.r(h)])
                ps2 = next_ps(P)
                for c in range(8):
                    P.op("pe", lambda e, c=c, h=h, ps2=ps2: e.matmul(ps2[0:64, :256], lhsT=Wi[:, c, h * 64:(h + 1) * 64], rhs=hnf[:, c, 0:256],
                                                                    start=(c == 0), stop=(c == 7)),
                         reads=[Wi.r(c), hnf.r(c)], writes=[ps2.r()])
                P.op("dve", lambda e, h=h, ps2=ps2: e.tensor_copy(out=qitb[:, h, :], in_=ps2[0:64, :256]), reads=[ps2.r()], writes=[qitb.r(h)])
            P.dma(lambda e, q0=q0: e.dma_start(out=qTv[:, :, q0:q0 + 256], in_=qtb[:, :, :]), reads=qtb.all())
            P.dma(lambda e, q0=q0: e.dma_start(out=qiTd[:, :, q0:q0 + 256], in_=qitb[:, :, :]), reads=qitb.all(), q="act")
            for hf in range(2):
                ps = next_ps(P)
                for c in range(8):
                    P.op("pe", lambda e, c=c, hf=hf, ps=ps: e.matmul(ps[:, 0:8], lhsT=hnf[:, c, hf * 128:(hf + 1) * 128], rhs=Wi[:, c, 576:584],
                                                                    start=(c == 0), stop=(c == 7)),
                         reads=[Wi.r(c), hnf.r(c)], writes=[ps.r()])
                P.op("act", lambda e, j=j, hf=hf, ps=ps: e.activation(out=wiT[:, j * 2 + hf, :], in_=ps[:, 0:8], func=AF.Copy, scale=WSC),
                     reads=[ps.r()], writes=[wiT.r()])
        P.pop_scope()

        I = P.sb("I", [128, L], F32)
        Mb = [P.sb(f"Mb{i}", [128, L], BF16) for i in range(2)]
        junk = P.sb("junk", [128, 2048], BF16)
        qih = P.sb("qih", [64, 8, 128], F32)
        dg = P.sb("dg", [128, 8, 128], F32)
        rl = [P.sb(f"rl{i}", [128, 512], F32) for i in range(3)]
        kics = [P.sb(f"kic{i}", [64, 512], F32) for i in range(3)]
        tmpb = P.sb("tmpb", [128, 512], F32)
        am = P.sb("am", [128, 32], F32)
        cn = P.sb("cn", [128, 8], F32)
        sm = P.sb("sm", [128, 8], F32)
        qt = P.sb("qt", [128, 4, 256], BF16)
        ktl = [P.sb(f"ktl{i}", [128, 4, 512], BF16) for i in range(2)]
        vtl = [P.sb(f"vtl{i}", [128, 4, 512], BF16) for i in range(2)]
        pts = [P.sb(f"pt{i}", [128, 512], BF16) for i in range(3)]
        rden = P.sb("rden", [128, 512], F32)
        ob = [P.sb(f"ob{i}", [128, 512], F32) for i in range(2)]
        bS = banks[0:3]
        bO = banks[3:5]
        bD = banks[5:7]
        bI = [banks[3], banks[4]]
        outs = []
        rot = {"s": 0, "p": 0, "l": 0, "r": 0, "i": 0, "k": 0}

        def nxt(lst, key):
            rot[key] += 1
            return lst[rot[key] % len(lst)]

        for j in range(NJ):
            Nmax = 256 * (8 * jset[j] + 8)
            ws = Nmax - 2048
            nck = Nmax // 512
            for hf in range(2):
                qi = j * 2 + hf
                M = Mb[hf]
                P.dma(lambda e, j=j, hf=hf: e.dma_start(out=qih[:, :, :], in_=qiTd[:, :, j * 256 + hf * 128:j * 256 + hf * 128 + 128]),
                      writes=[qih.r()])
                for h in range(8):
                    eng = "pool" if h % 2 == 0 else "dve"
                    P.op(eng, lambda e, h=h, qi=qi: e.tensor_scalar(out=dg[:, h, :], in0=Kc[:, 0, :], scalar1=wiT[:, qi, h:h + 1], scalar2=None, op0=ALU.mult),
                         reads=[Kc.r(), wiT.r()], writes=[dg.r()])
                for kc in range(nck):
                    pI = nxt(bI, "i")
                    kic = nxt(kics, "k")
                    P.dma(lambda e, kic=kic, kc=kc: e.dma_start(out=kic[:, :], in_=kiTd[:, kc * 512:(kc + 1) * 512]), writes=[kic.r()], q="act")
                    def score(h, kc=kc, kic=kic):
                        ps = nxt(bS, "s")
                        r = nxt(rl, "r")
                        P.op("pe", lambda e, h=h, ps=ps: e.matmul(ps[:, :512], lhsT=qih[:, h, :], rhs=kic[:, :], start=True, stop=True),
                             reads=[qih.r(), kic.r()], writes=[ps.r()])
                        P.op("act", lambda e, ps=ps, r=r: e.activation(out=r[:, :], in_=ps[:, :512], func=AF.Relu), reads=[ps.r()], writes=[r.r()])
                        return r
                    def accum(h, r, pI=pI):
                        P.op("pe", lambda e, h=h, r=r: e.matmul(pI[:, :512], lhsT=dg[:, h, :], rhs=r[:, :], start=(h == 0), stop=(h == 7)),
                             reads=[dg.r(), r.r()], writes=[pI.r()])
                    prev = score(0)
                    for h in range(1, 8):
                        cur = score(h)
                        accum(h - 1, prev)
                        prev = cur
                    accum(7, prev)
                    P.op("dve", lambda e, pI=pI, kc=kc: e.tensor_reduce(out=am[:, kc:kc + 1], in_=pI[:, :512], axis=AX.X, op=ALU.max, apply_absolute_value=True),
                         reads=[pI.r()], writes=[am.r()])
                    k0 = kc * 512
                    if k0 >= ws:
                        ro = k0 - ws
                        P.op("dve", lambda e, ro=ro, qi=qi: e.tensor_scalar(out=tmpb[:, :], in0=kit[:, ro:ro + 512], scalar1=qrt[:, qi:qi + 1], scalar2=-1e30,
                                                                          op0=ALU.is_gt, op1=ALU.mult), reads=[kit.r(), qrt.r()], writes=[tmpb.r()])
                        P.op("dve", lambda e, pI=pI, k0=k0: e.tensor_tensor(out=I[:, k0:k0 + 512], in0=pI[:, :512], in1=tmpb[:, :], op=ALU.add),
                             reads=[pI.r(), tmpb.r()], writes=[I.r()])
                    else:
                        P.op("dve", lambda e, pI=pI, k0=k0: e.tensor_copy(out=I[:, k0:k0 + 512], in_=pI[:, :512]), reads=[pI.r()], writes=[I.r()])
                P.op("dve", lambda e, nck=nck: e.tensor_reduce(out=sm[:, 0:1], in_=am[:, 0:nck], axis=AX.X, op=ALU.max), reads=[am.r()], writes=[sm.r()])
                P.op("dve", lambda e: e.tensor_scalar(out=sm[:, 1:2], in0=sm[:, 0:1], scalar1=2.0, scalar2=None, op0=ALU.mult), reads=[sm.r()], writes=[sm.r()])
                P.op("dve", lambda e: e.tensor_scalar(out=sm[:, 2:3], in0=sm[:, 0:1], scalar1=-1.0, scalar2=None, op0=ALU.mult), reads=[sm.r()], writes=[sm.r()])
                npc = (Nmax + 2047) // 2048
                for itn in range(NIT):
                    P.op("dve", lambda e, itn=itn: e.tensor_scalar(out=sm[:, 3:4], in0=sm[:, 1:2], scalar1=2.0 ** -(itn + 1), scalar2=None, op0=ALU.mult),
                         reads=[sm.r()], writes=[sm.r()])
                    P.op("dve", lambda e: e.tensor_tensor(out=sm[:, 4:5], in0=sm[:, 2:3], in1=sm[:, 3:4], op=ALU.add), reads=[sm.r()], writes=[sm.r()])
                    for pc in range(npc):
                        P.op("dve", lambda e, pc=pc: e.tensor_scalar(out=junk[:, :], in0=I[:, pc * 2048:(pc + 1) * 2048], scalar1=sm[:, 4:5], scalar2=None,
                                                                   op0=ALU.is_ge, op1=ALU.add, accum_out=cn[:, pc:pc + 1]),
                             reads=[I.r(), sm.r()], writes=[junk.r(), cn.r()])
                    P.op("dve", lambda e, npc=npc: e.tensor_reduce(out=sm[:, 5:6], in_=cn[:, 0:npc], axis=AX.X, op=ALU.add), reads=[cn.r()], writes=[sm.r()])
                    P.op("dve", lambda e: e.tensor_scalar(out=sm[:, 6:7], in0=sm[:, 5:6], scalar1=255.5, scalar2=sm[:, 3:4], op0=ALU.is_gt, op1=ALU.mult),
                         reads=[sm.r()], writes=[sm.r()])
                    P.op("dve", lambda e: e.tensor_tensor(out=sm[:, 2:3], in0=sm[:, 2:3], in1=sm[:, 6:7], op=ALU.add), reads=[sm.r()], writes=[sm.r()])
                for pc in range(npc):
                    P.op("dve", lambda e, pc=pc, M=M: e.tensor_scalar(out=M[:, pc * 2048:(pc + 1) * 2048], in0=I[:, pc * 2048:(pc + 1) * 2048],
                                                                     scalar1=sm[:, 2:3], scalar2=-30000.0, op0=ALU.is_lt, op1=ALU.mult),
                         reads=[I.r(), sm.r()], writes=[M.r()])
            for pz in range(2):
                P.dma(lambda e, j=j, pz=pz: e.dma_start(out=qt[:, :, :], in_=qTv[:, 4 * pz:4 * pz + 4, j * 256:(j + 1) * 256]), writes=[qt.r()])
                nkb = Nmax // 128
                for kb in range(nkb):
                    kbl = kb % 4
                    if kbl == 0:
                        kt = nxt(ktl, "l")
                        vt = vtl[rot["l"] % 2]
                        k4 = kb // 4
                        P.dma(lambda e, kt=kt, k4=k4, pz=pz: e.dma_start(out=kt[:, :, :], in_=kTv[:, 4 * pz:4 * pz + 4, k4 * 512:(k4 + 1) * 512]),
                              writes=[kt.r()])
                        P.dma(lambda e, vt=vt, k4=k4, pz=pz: e.dma_start(out=vt[:, :, :], in_=Vv[:, k4 * 4:(k4 + 1) * 4, pz * 512:(pz + 1) * 512]),
                              writes=[vt.r()], q="act")
                    for pair in range(2):
                        pS = nxt(bS, "s")
                        pt = nxt(pts, "p")
                        for hh in range(2):
                            hl = 2 * pair + hh
                            P.op("pe", lambda e, pS=pS, hh=hh, hl=hl, kt=kt, kbl=kbl: e.matmul(
                                pS[:, hh * 256:(hh + 1) * 256], lhsT=kt[:, hl, kbl * 128:(kbl + 1) * 128], rhs=qt[:, hl, :],
                                start=(hh == 0), stop=False, skip_group_check=True), reads=[kt.r(), qt.r()], writes=[pS.r()])
                        for hf in range(2):
                            P.op("pe", lambda e, pS=pS, hf=hf, kb=kb: e.matmul(
                                pS[:, 0:512], lhsT=Mb[hf][:, kb * 128:(kb + 1) * 128], rhs=selb[:, hf, :],
                                start=False, stop=(hf == 1), skip_group_check=True), reads=[Mb[hf].r(), selb.r()], writes=[pS.r()])
                        P.op("act", lambda e, pS=pS, pt=pt: e.activation(out=pt[:, :], in_=pS[:, 0:512], func=AF.Exp, scale=SCALE),
                             reads=[pS.r()], writes=[pt.r()])
                        for hh in range(2):
                            hl = 2 * pair + hh
                            P.op("pe", lambda e, pair=pair, hh=hh, hl=hl, vt=vt, kbl=kbl, pt=pt, kb=kb, nkb=nkb: e.matmul(
                                bO[pair][:, hh * 256:(hh + 1) * 256], lhsT=vt[:, kbl, hl * 128:(hl + 1) * 128], rhs=pt[:, hh * 256:(hh + 1) * 256],
                                start=(kb == 0 and hh == 0), stop=(kb == nkb - 1 and hh == 1), skip_group_check=True),
                                reads=[vt.r(), pt.r()], writes=[bO[pair].r()])
                        P.op("pe", lambda e, pair=pair, pt=pt, kb=kb, nkb=nkb: e.matmul(
                            bD[pair][:, 0:512], lhsT=C.ones_bf[:, :], rhs=pt[:, :], start=(kb == 0), stop=(kb == nkb - 1)),
                            reads=[C.ones_bf.r(), pt.r()], writes=[bD[pair].r()])
                for pair in range(2):
                    o = ob[pair]
                    P.op("dve", lambda e, pair=pair: e.reciprocal(out=rden[:, :], in_=bD[pair][:, 0:512]), reads=[bD[pair].r()], writes=[rden.r()])
                    P.op("dve", lambda e, pair=pair, o=o: e.tensor_tensor(out=o[:, :], in0=bO[pair][:, 0:512], in1=rden[:, :], op=ALU.mult),
                         reads=[bO[pair].r(), rden.r()], writes=[o.r()])
                    h0 = 4 * pz + 2 * pair
                    outs.append(P.dma(lambda e, o=o, h0=h0, j=j: e.dma_start(
                        out=oTv[:, h0:h0 + 2, j * 256:(j + 1) * 256], in_=o[:, :].rearrange("p (h q) -> p h q", h=2)), reads=[o.r()]))
        P.emit(final_waits=outs)
    return nc


def _rope_tables(L, dim, theta=10000.0):
    pos = np.arange(L, dtype=np.float32)
    inv = (theta ** (-np.arange(0, dim, 2, dtype=np.float32) / dim)).astype(np.float32)
    ang = pos[:, None] * inv[None, :]
    return np.cos(ang).astype(np.float32), np.sin(ang).astype(np.float32)


def _mla_inputs(hT, g, w_in, gq, w_uq, gkv, w_ukv, core):
    z64 = np.zeros((1024, 64), np.float32)
    kr = w_in[:, 640:672]
    krs = np.concatenate([kr[:, 16:], kr[:, :16]], 1)
    wall = np.concatenate([w_in[:, :640], z64, kr, z64, krs], 1)
    cols = []
    for h in (2 * core, 2 * core + 1):
        wq = w_uq[:, h * 96:(h + 1) * 96]
        wqs = np.concatenate([np.zeros((384, 64), np.float32), wq[:, 80:96], wq[:, 64:80]], 1)
        cols += [wq, wqs]
    wuq = np.concatenate(cols, 1)
    kn = [w_ukv[:, h * 128:h * 128 + 64] for h in (2 * core, 2 * core + 1)]
    vv = [w_ukv[:, h * 128 + 64:h * 128 + 128] for h in (2 * core, 2 * core + 1)]
    wukv = np.concatenate(kn + vv, 1)
    return dict(hT=hT, g=g, wall=np.ascontiguousarray(wall), gq=gq, gkv=gkv, wuq=np.ascontiguousarray(wuq),
                wukv=np.ascontiguousarray(wukv))


def _mla_consts(L):
    cos, sin = _rope_tables(L, 32)
    cos2 = np.zeros((96, L), np.float32)
    sin2 = np.zeros((96, L), np.float32)
    cos2[64:80] = cos.T
    cos2[80:96] = cos.T
    sin2[64:80] = -sin.T
    sin2[80:96] = sin.T
    k = np.arange(128)[:, None, None]
    d = np.arange(4)[None, :, None]
    q = np.arange(512)[None, None, :]
    cmask = ((d * 128 + k) <= q).astype(np.float32)
    esel = np.zeros((65, 64), np.float32)
    esel[64] = 1.0
    return dict(cos2=cos2, sin2=sin2, cmask=np.ascontiguousarray(cmask), esel=esel)


def _gdn_inputs(hT, g, w_in, conv_w, a_log, dt_bias, og, h):
    cols = [w_in[:, h * 128:(h + 1) * 128], w_in[:, 1024 + h * 128:1024 + (h + 1) * 128],
            w_in[:, 2048 + h * 128:2048 + (h + 1) * 128], w_in[:, 3072 + h * 128:3072 + (h + 1) * 128],
            w_in[:, 4096 + h:4097 + h], w_in[:, 4104 + h:4105 + h], np.zeros((1024, 126), np.float32)]
    wh = np.ascontiguousarray(np.concatenate(cols, 1))
    cw = np.concatenate([conv_w[:, h * 128:(h + 1) * 128], conv_w[:, 1024 + h * 128:1024 + (h + 1) * 128],
                         conv_w[:, 2048 + h * 128:2048 + (h + 1) * 128]], 1)
    sc = np.zeros((128, 2), np.float32)
    sc[:, 0] = a_log[h]
    sc[:, 1] = dt_bias[h]
    return dict(hT=hT, g=g, wh=wh, cw=np.ascontiguousarray(cw.T), sc=sc, og=og)


def _gdn_consts():
    j = np.arange(128)[:, None]
    i = np.arange(128)[None, :]
    same = (j // 64) == (i // 64)
    cst = np.zeros((128, 5, 128), np.float32)
    cst[:, 0] = np.eye(128)
    cst[:, 1] = (same & (i > j))
    cst[:, 2] = (same & (i >= j))
    cst[:, 3] = same
    cst[:, 4, 0] = (np.arange(128) < 64)
    cst[:, 4, 1] = (np.arange(128) >= 64)
    return dict(cst=cst)


def _dsa_inputs(xT, g, w_in, lg, lb, core, L, jset=None):
    jset = list(range(L // 256 // 8)) if jset is None else list(jset)
    NJ = len(jset)
    cols = []
    qrel = np.zeros((128, NJ * 2), np.float32)
    for jj, j in enumerate(jset):
        tq = 8 * j + core
        cols.append(xT[:, tq * 256:(tq + 1) * 256])
        ws = 256 * (8 * j + 8) - 2048
        for hf in range(2):
            qrel[:, jj * 2 + hf] = tq * 256 + hf * 128 + np.arange(128) - ws
    return dict(xT=xT, xq=np.ascontiguousarray(np.concatenate(cols, 1)), g=g, win=w_in, lng=lg, lnb=lb, qrel=qrel)


def _dsa_consts():
    kidx = np.tile(np.arange(2048, dtype=np.float32)[None, :], (128, 1))
    cst = np.zeros((128, 5, 128), np.float32)
    cst[:, 0] = np.eye(128)
    return dict(kidx=kidx, cst=cst)


def _dsa_gather(outs, L, jset=None, full=None):
    jset = list(range(L // 256 // 8)) if jset is None else list(jset)
    if full is None:
        full = np.zeros((1024, L), np.float32)
    for c, o in enumerate(outs):
        for jj, j in enumerate(jset):
            tq = 8 * j + c
            full[:, tq * 256:(tq + 1) * 256] = o[:, jj * 256:(jj + 1) * 256]
    return full


_PROGS = {}
DSA_SPLITS = ([0, 1, 2, 3, 4], [5, 6, 7])
GDN_SEG = 4096


def _prog(name, fn):
    if name not in _PROGS:
        _PROGS[name] = fn()
    return _PROGS[name]


def _run(nc, in_maps):
    res = run_bass_kernel_spmd(nc, in_maps, core_ids=list(range(8)))
    return [r["oT"] for r in res.results]


def _split(hT, TOK=2048):
    return [np.ascontiguousarray(hT[:, i * TOK:(i + 1) * TOK]) for i in range(8)]


def kernel(**inp):
    f32 = lambda a: np.ascontiguousarray(np.asarray(a, dtype=np.float32))
    L = 16384
    TOK = L // 8
    x = f32(inp["x"])[0]
    hT = np.ascontiguousarray(x.T)
    ident = np.eye(128, dtype=np.float32)

    def outproj(hT, mT, w):
        nc = _prog("outproj", lambda: build_outproj(TOK, 512))
        hs, ms = _split(hT), _split(mT)
        return np.concatenate(_run(nc, [dict(hT=hs[i], mT=ms[i], w=w) for i in range(8)]), axis=1)

    def mlp(hT, i, final=False):
        hs = _split(hT)
        g, w1, w2 = f32(inp["norm_mlp_g"][i]), f32(inp["mlp_w1"][i]), f32(inp["mlp_w2"][i])
        if final:
            nc = _prog("mlpf", lambda: build_mlp(TOK, 256, True))
            fg = f32(inp["final_g"])
            maps = [dict(hT=hs[c], g=g, w1=w1, w2=w2, fg=fg) for c in range(8)]
        else:
            nc = _prog("mlp", lambda: build_mlp(TOK, 256, False))
            maps = [dict(hT=hs[c], g=g, w1=w1, w2=w2) for c in range(8)]
        return np.concatenate(_run(nc, maps), axis=1)

    cst = _dsa_consts()
    g0, win0 = f32(inp["norm_mix_g"][0]), f32(inp["dsa_w_in"][0])
    lg, lb = f32(inp["dsa_idx_k_g"][0]), f32(inp["dsa_idx_k_b"][0])
    mT = None
    for js in DSA_SPLITS:
        nc = _prog("dsa" + str(js), lambda: build_dsa(L, 18, js))
        outs = _run(nc, [{**_dsa_inputs(hT, g0, win0, lg, lb, c, L, js), **cst} for c in range(8)])
        mT = _dsa_gather(outs, L, js, mT)
    hT = outproj(hT, mT, f32(inp["dsa_w_out"][0]))
    hT = mlp(hT, 0)
    nc = _prog("conv", lambda: build_conv(TOK, 256))
    hp = np.concatenate([np.zeros((1024, 30), np.float32), hT], axis=1)
    cw = dict(g=f32(inp["norm_mix_g"][1]), w1=f32(inp["conv_w_pw1"][0]), b1=f32(inp["conv_b_pw1"][0]),
              wdT=np.ascontiguousarray(f32(inp["conv_w_dw"][0]).T), bd=f32(inp["conv_b_dw"][0]),
              lg=f32(inp["conv_ln_g"][0]), lb=f32(inp["conv_ln_b"][0]), w2=f32(inp["conv_w_pw2"][0]),
              b2=f32(inp["conv_b_pw2"][0]), ident=ident)
    maps = [dict(hT=np.ascontiguousarray(hp[:, c * TOK:c * TOK + TOK + 30]),
                 hs=np.full((128, 1), 0.0 if c == 0 else 1.0, np.float32), **cw) for c in range(8)]
    hT = np.concatenate(_run(nc, maps), axis=1)
    hT = mlp(hT, 1)
    nc = _prog("mla", lambda: build_mla(L))
    cst = _mla_consts(L)
    maps = [{**_mla_inputs(hT, f32(inp["norm_mix_g"][2]), f32(inp["mla_w_in"][0]), f32(inp["mla_q_norm_g"][0]),
                           f32(inp["mla_w_uq"][0]), f32(inp["mla_kv_norm_g"][0]), f32(inp["mla_w_ukv"][0]), c), **cst}
            for c in range(8)]
    mT = np.concatenate(_run(nc, maps), axis=0)
    hT = outproj(hT, mT, f32(inp["mla_w_out"][0]))
    hT = mlp(hT, 2)
    LG = GDN_SEG
    nc = _prog("gdn", lambda: build_gdn(LG))
    cst = _gdn_consts()
    gi = [_gdn_inputs(None, f32(inp["norm_mix_g"][3]), f32(inp["gdn_w_in"][0]), f32(inp["gdn_conv_w"][0]),
                      f32(inp["gdn_a_log"][0]), f32(inp["gdn_dt_bias"][0]), f32(inp["gdn_o_norm_g"][0]), c) for c in range(8)]
    st = [np.zeros((128, 128), np.float32) for _ in range(8)]
    hl = [np.zeros((128, 3, 3), np.float32) for _ in range(8)]
    segs = []
    for s0 in range(0, L, LG):
        hseg = np.ascontiguousarray(hT[:, s0:s0 + LG])
        maps = [{**gi[c], **cst, "hT": hseg, "s_in": st[c], "h_in": hl[c]} for c in range(8)]
        res = run_bass_kernel_spmd(nc, maps, core_ids=list(range(8))).results
        segs.append(np.concatenate([r["oT"] for r in res], axis=0))
        st = [np.ascontiguousarray(r["s_out"]) for r in res]
        hl = [np.ascontiguousarray(r["h_out"]) for r in res]
    mT = np.concatenate(segs, axis=1)
    hT = outproj(hT, mT, f32(inp["gdn_w_out"][0]))
    hT = mlp(hT, 3, final=True)
    return np.ascontiguousarray(hT.T)[None].astype(np.float32)
```

```python
import numpy as np
import concourse.bass as bass
import concourse.mybir as mybir
from contextlib import ExitStack
from concourse.bass_utils import run_bass_kernel_spmd

F32 = mybir.dt.float32
BF16 = mybir.dt.bfloat16
U8 = mybir.dt.uint8
I32 = mybir.dt.int32
ALU = mybir.AluOpType
AF = mybir.ActivationFunctionType
AX = mybir.AxisListType

ENGS = ["pe", "act", "dve", "pool", "sp"]
NDMA = 8


class Res:
    __slots__ = ("name", "lastw", "readers")

    def __init__(self, name):
        self.name = name
        self.lastw = None
        self.readers = []


class Tile:
    def __init__(self, t, name, nsub=1):
        self.t = t
        self.name = name
        self.res = [Res(f"{name}.{i}") for i in range(nsub)]

    def __getitem__(self, idx):
        return self.t[idx]

    def r(self, i=0):
        return self.res[i]

    def all(self):
        return list(self.res)


class View:
    def __init__(self, base, off, width, name, share=True):
        self.base = base
        self.off = off
        self.width = width
        self.res = base.res if share else [Res(name)]

    def __getitem__(self, idx):
        rows, cols = idx
        start = cols.start or 0
        stop = self.width if cols.stop is None else cols.stop
        return self.base.t[rows, self.off + start:self.off + stop]

    def r(self, i=0):
        return self.res[0]

    def all(self):
        return list(self.res)


class Prog:
    def __init__(self, nc, stack):
        self.nc = nc
        self.stack = stack
        self.ops = {e: [] for e in ENGS}
        self.cnt = {e: 0 for e in ENGS}
        self.sem = {e: stack.enter_context(nc.semaphore(f"s_{e}")) for e in ENGS if e != "sp"}
        self.dsem = {q: [stack.enter_context(nc.semaphore(f"d_{q}{i}")) for i in range(NDMA)]
                     for q in ("sp", "pool", "act")}
        self.dcnt = {q: 0 for q in ("sp", "pool", "act")}
        self.dtok = {q: [] for q in ("sp", "pool", "act")}
        self.known = {e: {} for e in ENGS}
        self.nops = 0

    def sb(self, name, shape, dtype, nsub=1):
        t = self.stack.enter_context(self.nc.sbuf_tensor("sb_" + name, list(shape), dtype))
        return Tile(t, name, nsub)

    def ps(self, name, shape, dtype=F32, nsub=1):
        t = self.stack.enter_context(self.nc.psum_tensor("ps_" + name, list(shape), dtype))
        return Tile(t, name, nsub)

    def push_scope(self):
        self._saved_stack = self.stack
        self.stack = ExitStack()
        return self.stack

    def pop_scope(self):
        self.barrier()
        self.stack.close()
        self.stack = self._saved_stack

    def barrier(self):
        toks = []
        for e in ENGS:
            if e != "sp" and self.cnt[e] > 0:
                toks.append((("c", e), self.cnt[e], e))
        for q in self.dtok:
            toks += self.dtok[q][-NDMA:]
        self._pending = {e: list(toks) for e in ENGS}

    def _deps(self, eng, reads, writes):
        deps = {}
        for tok in getattr(self, "_pending", {}).get(eng, []):
            if not (tok[2] == eng and eng == "pe"):
                if deps.get(tok[0], (0,))[0] < tok[1]:
                    deps[tok[0]] = (tok[1], tok)
        if getattr(self, "_pending", None):
            self._pending[eng] = []
        def add(tok):
            if tok is None:
                return
            key, val, teng = tok
            if teng == eng and eng == "pe":
                return
            if deps.get(key, (0,))[0] < val:
                deps[key] = (val, tok)
        for r in reads:
            add(r.lastw)
        for w in writes:
            add(w.lastw)
            for t in w.readers:
                add(t)
        out = []
        kn = self.known[eng]
        for key, (val, tok) in deps.items():
            if kn.get(key, 0) >= val:
                continue
            kn[key] = val
            out.append(tok)
        return out

    def _commit(self, tok, reads, writes):
        for r in reads:
            r.readers.append(tok)
        for w in writes:
            w.lastw = tok
            w.readers = []

    def op(self, eng, fn, reads=(), writes=()):
        waits = self._deps(eng, reads, writes)
        self.cnt[eng] += 1
        tok = (("c", eng), self.cnt[eng], eng)
        self.ops[eng].append((waits, fn, tok))
        self._commit(tok, reads, writes)
        self.nops += 1
        return tok

    def dma(self, fn, reads=(), writes=(), q="sp"):
        eng = q
        waits = self._deps(eng, reads, writes)
        i = self.dcnt[q]
        self.dcnt[q] += 1
        slot = i % NDMA
        val = 16 * (i // NDMA + 1)
        if i >= NDMA:
            prev = self.dtok[q][i - NDMA]
            key, pval, _ = prev
            if self.known[eng].get(key, 0) < pval:
                self.known[eng][key] = pval
                waits.append(prev)
        tok = (("d", q, slot), val, "dma")
        self.dtok[q].append(tok)
        self.ops[eng].append((waits, fn, tok))
        self._commit(tok, reads, writes)
        self.nops += 1
        return tok

    def _semof(self, tok):
        key = tok[0]
        if key[0] == "c":
            return self.sem[key[1]]
        return self.dsem[key[1]][key[2]]

    def emit(self, final_waits=()):
        nc = self.nc
        with nc.Block() as block:
            def run(eng, e):
                for waits, fn, tok in self.ops[eng]:
                    for w in waits:
                        e.wait_ge(self._semof(w), w[1])
                    ins = fn(e)
                    if tok[2] == "dma":
                        ins.then_inc(self._semof(tok), 16)
                    else:
                        ins.then_inc(self._semof(tok), 1)
                if eng == "sp":
                    for w in final_waits:
                        e.wait_ge(self._semof(w), w[1])

            @block.tensor
            def _(e):
                run("pe", e)

            @block.scalar
            def _(e):
                run("act", e)

            @block.vector
            def _(e):
                run("dve", e)

            @block.gpsimd
            def _(e):
                run("pool", e)

            @block.sync
            def _(e):
                run("sp", e)


def load_w_bf16(P, w_ap, KC, Fo, name, q="pool"):
    t = P.sb(name, [128, KC, Fo], BF16, nsub=KC)
    wv = w_ap.rearrange("(c p) f -> p c f", p=128)
    for c in range(KC):
        P.dma(lambda e, c=c: e.dma_start(out=t[:, c, :], in_=wv[:, c, :], max_dma_last_dim=8192),
              writes=[t.r(c)], q=q)
    return t


def load_vec_fm(P, v_ap, KC, name):
    t = P.sb(name, [128, KC], F32)
    vv = v_ap.rearrange("(c p) -> p c", p=128)
    P.dma(lambda e: e.dma_start(out=t[:, :], in_=vv, allow_slow_non_contiguous=True), writes=[t.r()])
    return t


class Ctx:
    pass


def make_consts(P):
    C = Ctx()
    C.ones_bf = P.sb("ones_bf", [128, 128], BF16)
    P.op("dve", lambda e: e.memset(C.ones_bf[:, :], 1.0), writes=[C.ones_bf.r()])
    C.eps = P.sb("eps_t", [128, 1], F32)
    P.op("dve", lambda e: e.memset(C.eps[:, :], 1e-6), writes=[C.eps.r()])
    return C


def rms_rstd(P, C, x, KC, T, psum, sq, rstd, dim):
    for c in range(KC):
        P.op("act", lambda e, c=c: e.activation(out=sq[:, c, :], in_=x[:, c, :], func=AF.Square),
             reads=[x.r()], writes=[sq.r(c)])
    for c in range(KC):
        P.op("pe", lambda e, c=c: e.matmul(psum[:, :T], lhsT=C.ones_bf[:, :], rhs=sq[:, c, :],
                                          start=(c == 0), stop=(c == KC - 1)),
             reads=[sq.r(c), C.ones_bf.r()], writes=[psum.r()])
    P.op("act", lambda e: e.activation(out=rstd[:, :], in_=psum[:, :T], func=AF.Sqrt,
                                       bias=C.eps[:, :], scale=1.0 / dim),
         reads=[psum.r(), C.eps.r()], writes=[rstd.r()])
    P.op("dve", lambda e: e.reciprocal(out=rstd[:, :], in_=rstd[:, :]), reads=[rstd.r()], writes=[rstd.r()])


def build_mlp(TOK=2048, T=256, final=False):
    nc = bass.Bass("TRN2", target_bir_lowering=False)
    hT = nc.dram_tensor("hT", [1024, TOK], F32, kind="ExternalInput").ap()
    g = nc.dram_tensor("g", [1024], F32, kind="ExternalInput").ap()
    w1 = nc.dram_tensor("w1", [1024, 4096], F32, kind="ExternalInput").ap()
    w2 = nc.dram_tensor("w2", [4096, 1024], F32, kind="ExternalInput").ap()
    oT = nc.dram_tensor("oT", [1024, TOK], F32, kind="ExternalOutput").ap()
    with ExitStack() as st:
        st.enter_context(nc.allow_low_precision("bf16 matmul operands, fp32 accumulate"))
        P = Prog(nc, st)
        C = make_consts(P)
        gt = load_vec_fm(P, g, 8, "g")
        W1 = load_w_bf16(P, w1, 8, 4096, "W1")
        W2 = load_w_bf16(P, w2, 32, 1024, "W2")
        fgt = None
        if final:
            fg = nc.dram_tensor("fg", [1024], F32, kind="ExternalInput").ap()
            fgt = load_vec_fm(P, fg, 8, "fg")
        mlp_body(P, C, hT, oT, gt, W1, W2, TOK, T, fgt)
        P.emit(final_waits=P.out_toks)
    return nc


def mlp_body(P, C, hT, oT, gt, W1, W2, TOK, T, fgt=None):
    hv = hT.rearrange("(c p) t -> p c t", p=128)
    ov = oT.rearrange("(c p) t -> p c t", p=128)
    NB = 2
    xs = [P.sb(f"x{i}", [128, 8, T], F32) for i in range(NB)]
    sqs = [P.sb(f"sq{i}", [128, 8, T], BF16, nsub=8) for i in range(NB)]
    hns = [P.sb(f"hn{i}", [128, 8, T], BF16, nsub=8) for i in range(NB)]
    rstds = [P.sb(f"rstd{i}", [128, T], F32) for i in range(NB)]
    aT = P.sb("aT", [128, 32, T], BF16, nsub=32)
    rl = [P.sb(f"rl{i}", [128, T], BF16) for i in range(2)]
    ys = [P.sb(f"y{i}", [128, 8, T], F32, nsub=8) for i in range(NB)]
    pss = [P.ps(f"ps{i}", [128, 512], F32) for i in range(8)]
    P.out_toks = []
    pi = 0
    for it in range(TOK // T):
        b = it % NB
        x, sq, hn, rstd, y = xs[b], sqs[b], hns[b], rstds[b], ys[b]
        t0 = it * T
        P.dma(lambda e, x=x, t0=t0: e.dma_start(out=x[:, :, :], in_=hv[:, :, t0:t0 + T]), writes=[x.r()])
        ps = pss[pi % 8]; pi += 1
        rms_rstd(P, C, x, 8, T, ps, sq, rstd, 1024.0)
        for c in range(8):
            P.op("dve", lambda e, c=c, x=x, hn=hn, rstd=rstd: e.scalar_tensor_tensor(
                out=hn[:, c, :], in0=x[:, c, :], scalar=gt[:, c:c + 1], in1=rstd[:, :],
                op0=ALU.mult, op1=ALU.mult), reads=[x.r(), rstd.r(), gt.r()], writes=[hn.r(c)])
        for f in range(32):
            ps = pss[pi % 8]; pi += 1
            for c in range(8):
                P.op("pe", lambda e, c=c, f=f, ps=ps, hn=hn: e.matmul(
                    ps[:, :T], lhsT=W1[:, c, f * 128:(f + 1) * 128], rhs=hn[:, c, :],
                    start=(c == 0), stop=(c == 7)), reads=[W1.r(c), hn.r(c)], writes=[ps.r()])
            r = rl[f % 2]
            P.op("act", lambda e, ps=ps, r=r: e.activation(out=r[:, :], in_=ps[:, :T], func=AF.Relu),
                 reads=[ps.r()], writes=[r.r()])
            eng = "pool" if f % 2 == 0 else "dve"
            P.op(eng, lambda e, f=f, r=r: e.tensor_tensor(out=aT[:, f, :], in0=r[:, :], in1=r[:, :], op=ALU.mult),
                 reads=[r.r()], writes=[aT.r(f)])
        for o in range(8):
            ps = pss[pi % 8]; pi += 1
            for f in range(32):
                P.op("pe", lambda e, o=o, f=f, ps=ps: e.matmul(
                    ps[:, :T], lhsT=W2[:, f, o * 128:(o + 1) * 128], rhs=aT[:, f, :],
                    start=(f == 0), stop=(f == 31)), reads=[W2.r(f), aT.r(f)], writes=[ps.r()])
            P.op("dve", lambda e, o=o, ps=ps, x=x, y=y: e.tensor_tensor(
                out=y[:, o, :], in0=ps[:, :T], in1=x[:, o, :], op=ALU.add),
                reads=[ps.r(), x.r()], writes=[y.r(o)])
        if fgt is not None:
            ps = pss[pi % 8]; pi += 1
            for c in range(8):
                P.op("act", lambda e, c=c, y=y, sq=sq: e.activation(out=sq[:, c, :], in_=y[:, c, :], func=AF.Square),
                     reads=[y.r(c)], writes=[sq.r(c)])
            for c in range(8):
                P.op("pe", lambda e, c=c, ps=ps, sq=sq: e.matmul(ps[:, :T], lhsT=C.ones_bf[:, :], rhs=sq[:, c, :],
                                                              start=(c == 0), stop=(c == 7)),
                     reads=[sq.r(c), C.ones_bf.r()], writes=[ps.r()])
            P.op("act", lambda e, ps=ps, rstd=rstd: e.activation(out=rstd[:, :], in_=ps[:, :T], func=AF.Sqrt,
                                                               bias=C.eps[:, :], scale=1.0 / 1024),
                 reads=[ps.r(), C.eps.r()], writes=[rstd.r()])
            P.op("dve", lambda e, rstd=rstd: e.reciprocal(out=rstd[:, :], in_=rstd[:, :]), reads=[rstd.r()], writes=[rstd.r()])
            for c in range(8):
                P.op("dve", lambda e, c=c, y=y, rstd=rstd: e.scalar_tensor_tensor(
                    out=y[:, c, :], in0=y[:, c, :], scalar=fgt[:, c:c + 1], in1=rstd[:, :], op0=ALU.mult, op1=ALU.mult),
                    reads=[y.r(c), rstd.r(), fgt.r()], writes=[y.r(c)])
        tok = P.dma(lambda e, y=y, t0=t0: e.dma_start(out=ov[:, :, t0:t0 + T], in_=y[:, :, :]),
                    reads=y.all())
        P.out_toks.append(tok)


def next_ps(P):
    i = getattr(P, "_psi", 0)
    P._psi = i + 1
    return P.pss[i % len(P.pss)]


def alloc_ps(P, n=8):
    P.pss = [P.ps(f"ps{i}", [128, 512], F32) for i in range(n)]
    P._psi = 0


def load_const(P, ap, shape, name, dtype=F32, q="sp"):
    t = P.sb(name, shape, dtype)
    P.dma(lambda e: e.dma_start(out=t[tuple(slice(None) for _ in shape)], in_=ap), writes=[t.r()], q=q)
    return t


def build_outproj(TOK=2048, T=512):
    nc = bass.Bass("TRN2", target_bir_lowering=False)
    hT = nc.dram_tensor("hT", [1024, TOK], F32, kind="ExternalInput").ap()
    mT = nc.dram_tensor("mT", [1024, TOK], F32, kind="ExternalInput").ap()
    w = nc.dram_tensor("w", [1024, 1024], F32, kind="ExternalInput").ap()
    oT = nc.dram_tensor("oT", [1024, TOK], F32, kind="ExternalOutput").ap()
    hv = hT.rearrange("(c p) t -> p c t", p=128)
    mv = mT.rearrange("(c p) t -> p c t", p=128)
    ov = oT.rearrange("(c p) t -> p c t", p=128)
    with ExitStack() as st:
        st.enter_context(nc.allow_low_precision("bf16 matmul operands, fp32 accumulate"))
        P = Prog(nc, st)
        alloc_ps(P)
        W = load_w_bf16(P, w, 8, 1024, "W")
        NB = 2
        xs = [P.sb(f"x{i}", [128, 8, T], F32) for i in range(NB)]
        ms = [P.sb(f"m{i}", [128, 8, T], F32) for i in range(NB)]
        mb = [P.sb(f"mb{i}", [128, 8, T], BF16, nsub=8) for i in range(NB)]
        ys = [P.sb(f"y{i}", [128, 8, T], F32, nsub=8) for i in range(NB)]
        outs = []
        for it in range(TOK // T):
            b = it % NB
            x, m, mbb, y = xs[b], ms[b], mb[b], ys[b]
            t0 = it * T
            P.dma(lambda e, x=x, t0=t0: e.dma_start(out=x[:, :, :], in_=hv[:, :, t0:t0 + T]), writes=[x.r()])
            P.dma(lambda e, m=m, t0=t0: e.dma_start(out=m[:, :, :], in_=mv[:, :, t0:t0 + T]), writes=[m.r()], q="act")
            for c in range(8):
                eng = "act" if c % 2 == 0 else "pool"
                if eng == "act":
                    P.op("act", lambda e, c=c, m=m, mbb=mbb: e.copy(out=mbb[:, c, :], in_=m[:, c, :]),
                         reads=[m.r()], writes=[mbb.r(c)])
                else:
                    P.op("pool", lambda e, c=c, m=m, mbb=mbb: e.tensor_copy(out=mbb[:, c, :], in_=m[:, c, :]),
                         reads=[m.r()], writes=[mbb.r(c)])
            for o in range(8):
                ps = next_ps(P)
                for c in range(8):
                    P.op("pe", lambda e, o=o, c=c, ps=ps, mbb=mbb: e.matmul(
                        ps[:, :T], lhsT=W[:, c, o * 128:(o + 1) * 128], rhs=mbb[:, c, :],
                        start=(c == 0), stop=(c == 7)), reads=[W.r(c), mbb.r(c)], writes=[ps.r()])
                P.op("dve", lambda e, o=o, ps=ps, x=x, y=y: e.tensor_tensor(
                    out=y[:, o, :], in0=ps[:, :T], in1=x[:, o, :], op=ALU.add),
                    reads=[ps.r(), x.r()], writes=[y.r(o)])
            outs.append(P.dma(lambda e, y=y, t0=t0: e.dma_start(out=ov[:, :, t0:t0 + T], in_=y[:, :, :]),
                              reads=y.all()))
        P.emit(final_waits=outs)
    return nc


def build_conv(TOK=2048, T=256):
    HALO = 30
    NT = TOK + HALO
    nc = bass.Bass("TRN2", target_bir_lowering=False)
    hT = nc.dram_tensor("hT", [1024, NT], F32, kind="ExternalInput").ap()
    g = nc.dram_tensor("g", [1024], F32, kind="ExternalInput").ap()
    w1 = nc.dram_tensor("w1", [1024, 2048], F32, kind="ExternalInput").ap()
    b1 = nc.dram_tensor("b1", [2048], F32, kind="ExternalInput").ap()
    wdT = nc.dram_tensor("wdT", [1024, 31], F32, kind="ExternalInput").ap()
    bd = nc.dram_tensor("bd", [1024], F32, kind="ExternalInput").ap()
    lg = nc.dram_tensor("lg", [1024], F32, kind="ExternalInput").ap()
    lb = nc.dram_tensor("lb", [1024], F32, kind="ExternalInput").ap()
    w2 = nc.dram_tensor("w2", [1024, 1024], F32, kind="ExternalInput").ap()
    b2 = nc.dram_tensor("b2", [1024], F32, kind="ExternalInput").ap()
    hs = nc.dram_tensor("hs", [128, 1], F32, kind="ExternalInput").ap()
    ident = nc.dram_tensor("ident", [128, 128], F32, kind="ExternalInput").ap()
    oT = nc.dram_tensor("oT", [1024, TOK], F32, kind="ExternalOutput").ap()
    hv = hT.rearrange("(c p) t -> p c t", p=128)
    ov = oT.rearrange("(c p) t -> p c t", p=128)
    with ExitStack() as st:
        st.enter_context(nc.allow_low_precision("bf16 matmul operands, fp32 accumulate"))
        P = Prog(nc, st)
        alloc_ps(P)
        C = make_consts(P)
        ones_f = P.sb("ones_f", [128, 128], F32)
        P.op("dve", lambda e: e.memset(ones_f[:, :], 1.0), writes=[ones_f.r()])
        gt = load_vec_fm(P, g, 8, "g")
        b1t = load_vec_fm(P, b1, 16, "b1")
        bdt = load_vec_fm(P, bd, 8, "bd")
        lgt = load_vec_fm(P, lg, 8, "lg")
        lbt = load_vec_fm(P, lb, 8, "lb")
        b2t = load_vec_fm(P, b2, 8, "b2")
        hst = load_const(P, hs, [128, 1], "hs")
        idf = load_const(P, ident, [128, 128], "idf")
        wd = P.sb("wd", [128, 8, 31], F32)
        P.dma(lambda e: e.dma_start(out=wd[:, :, :], in_=wdT.rearrange("(c p) j -> p c j", p=128)), writes=[wd.r()])
        W1 = load_w_bf16(P, w1, 8, 2048, "W1")
        W2 = load_w_bf16(P, w2, 8, 1024, "W2")
        diag = P.sb("diag", [128, 8 * 31, 128], BF16, nsub=8)
        for c in range(8):
            for j in range(31):
                eng = "pool" if (j % 2 == 0) else "dve"
                P.op(eng, lambda e, c=c, j=j: e.tensor_scalar(
                    out=diag[:, c * 31 + j, :], in0=idf[:, :], scalar1=wd[:, c, j:j + 1], scalar2=None,
                    op0=ALU.mult), reads=[idf.r(), wd.r()], writes=[diag.r(c)])
        uT = P.sb("uT", [128, 8, NT], BF16, nsub=8)
        NB = 2
        xs = [P.sb(f"x{i}", [128, 8, T], F32) for i in range(NB)]
        sqs = [P.sb(f"sq{i}", [128, 8, T], BF16, nsub=8) for i in range(1)] * 2
        hns = [P.sb(f"hn{i}", [128, 8, T], BF16, nsub=8) for i in range(1)] * 2
        rstds = [P.sb(f"rstd{i}", [128, T], F32) for i in range(1)] * 2
        sig = [P.sb(f"sig{i}", [128, T], F32) for i in range(2)]
        segs = [(0, HALO)] + [(HALO + k * T, T) for k in range(TOK // T)]
        for it, (s0, L) in enumerate(segs):
            b = it % NB
            x, sq, hn, rstd = xs[b], sqs[b], hns[b], rstds[b]
            P.dma(lambda e, x=x, s0=s0, L=L: e.dma_start(out=x[:, :, :L], in_=hv[:, :, s0:s0 + L]), writes=[x.r()])
            ps = next_ps(P)
            for c in range(8):
                P.op("act", lambda e, c=c, x=x, sq=sq, L=L: e.activation(out=sq[:, c, :L], in_=x[:, c, :L], func=AF.Square),
                     reads=[x.r()], writes=[sq.r(c)])
            for c in range(8):
                P.op("pe", lambda e, c=c, ps=ps, sq=sq, L=L: e.matmul(ps[:, :L], lhsT=C.ones_bf[:, :], rhs=sq[:, c, :L],
                                                                  start=(c == 0), stop=(c == 7)),
                     reads=[sq.r(c), C.ones_bf.r()], writes=[ps.r()])
            P.op("act", lambda e, ps=ps, rstd=rstd, L=L: e.activation(out=rstd[:, :L], in_=ps[:, :L], func=AF.Sqrt,
                                                               bias=C.eps[:, :], scale=1.0 / 1024),
                 reads=[ps.r(), C.eps.r()], writes=[rstd.r()])
            P.op("dve", lambda e, rstd=rstd, L=L: e.reciprocal(out=rstd[:, :L], in_=rstd[:, :L]),
                 reads=[rstd.r()], writes=[rstd.r()])
            for c in range(8):
                P.op("dve", lambda e, c=c, x=x, hn=hn, rstd=rstd, L=L: e.scalar_tensor_tensor(
                    out=hn[:, c, :L], in0=x[:, c, :L], scalar=gt[:, c:c + 1], in1=rstd[:, :L],
                    op0=ALU.mult, op1=ALU.mult), reads=[x.r(), rstd.r(), gt.r()], writes=[hn.r(c)])
            for j in range(8):
                psa = next_ps(P)
                psg = next_ps(P)
                for c in range(8):
                    P.op("pe", lambda e, c=c, j=j, psa=psa, hn=hn, L=L: e.matmul(
                        psa[:, :L], lhsT=W1[:, c, j * 128:(j + 1) * 128], rhs=hn[:, c, :L],
                        start=(c == 0), stop=(c == 7)), reads=[W1.r(c), hn.r(c)], writes=[psa.r()])
                for c in range(8):
                    P.op("pe", lambda e, c=c, j=j, psg=psg, hn=hn, L=L: e.matmul(
                        psg[:, :L], lhsT=W1[:, c, 1024 + j * 128:1024 + (j + 1) * 128], rhs=hn[:, c, :L],
                        start=(c == 0), stop=(c == 7)), reads=[W1.r(c), hn.r(c)], writes=[psg.r()])
                sg = sig[j % 2]
                P.op("act", lambda e, j=j, psg=psg, sg=sg, L=L: e.activation(
                    out=sg[:, :L], in_=psg[:, :L], func=AF.Sigmoid, bias=b1t[:, 8 + j:9 + j], scale=1.0),
                    reads=[psg.r(), b1t.r()], writes=[sg.r()])
                P.op("dve", lambda e, j=j, psa=psa, sg=sg, s0=s0, L=L: e.scalar_tensor_tensor(
                    out=uT[:, j, s0:s0 + L], in0=psa[:, :L], scalar=b1t[:, j:j + 1], in1=sg[:, :L],
                    op0=ALU.add, op1=ALU.mult), reads=[psa.r(), sg.r(), b1t.r()], writes=[uT.r(j)])
            if it == 0:
                for j in range(8):
                    P.op("dve", lambda e, j=j: e.tensor_scalar(
                        out=uT[:, j, 0:HALO], in0=uT[:, j, 0:HALO], scalar1=hst[:, 0:1], scalar2=None, op0=ALU.mult),
                        reads=[uT.r(j), hst.r()], writes=[uT.r(j)])
        vs = [P.sb(f"v{i}", [128, 8, T], F32, nsub=8) for i in range(1)] * 2
        zs = [P.sb(f"z{i}", [128, 8, T], BF16, nsub=8) for i in range(1)] * 2
        ys = [P.sb(f"y{i}", [128, 8, T], F32, nsub=8) for i in range(1)] * 2
        v2 = P.sb("v2", [128, 8, T], F32, nsub=8)
        mean = P.sb("mean", [128, T], F32)
        msq = P.sb("msq", [128, T], F32)
        lrstd = P.sb("lrstd", [128, T], F32)
        dd = [P.sb(f"dd{i}", [128, T], F32) for i in range(2)]
        outs = []
        for it in range(TOK // T):
            b = it % NB
            x, v, z, y = xs[b], vs[b], zs[b], ys[b]
            tl = it * T
            P.dma(lambda e, x=x, tl=tl: e.dma_start(out=x[:, :, :], in_=hv[:, :, HALO + tl:HALO + tl + T]), writes=[x.r()])
            for c in range(8):
                ps = next_ps(P)
                for j in range(31):
                    P.op("pe", lambda e, c=c, j=j, ps=ps, tl=tl: e.matmul(
                        ps[:, :T], lhsT=diag[:, c * 31 + j, :], rhs=uT[:, c, tl + j:tl + j + T],
                        start=(j == 0), stop=(j == 30)), reads=[diag.r(c), uT.r(c)], writes=[ps.r()])
                P.op("act", lambda e, c=c, ps=ps, v=v: e.activation(
                    out=v[:, c, :], in_=ps[:, :T], func=AF.Identity, bias=bdt[:, c:c + 1], scale=1.0),
                    reads=[ps.r(), bdt.r()], writes=[v.r(c)])
                P.op("pool", lambda e, c=c, v=v: e.tensor_tensor(out=v2[:, c, :], in0=v[:, c, :], in1=v[:, c, :], op=ALU.mult),
                     reads=[v.r(c)], writes=[v2.r(c)])
            ps1 = next_ps(P)
            ps2 = next_ps(P)
            for c in range(8):
                P.op("pe", lambda e, c=c, ps1=ps1, v=v: e.matmul(ps1[:, :T], lhsT=ones_f[:, :], rhs=v[:, c, :],
                                                              start=(c == 0), stop=(c == 7)),
                     reads=[ones_f.r(), v.r(c)], writes=[ps1.r()])
            for c in range(8):
                P.op("pe", lambda e, c=c, ps2=ps2: e.matmul(ps2[:, :T], lhsT=ones_f[:, :], rhs=v2[:, c, :],
                                                         start=(c == 0), stop=(c == 7)),
                     reads=[ones_f.r(), v2.r(c)], writes=[ps2.r()])
            P.op("act", lambda e, ps1=ps1: e.activation(out=mean[:, :], in_=ps1[:, :T], func=AF.Copy, scale=1.0 / 1024),
                 reads=[ps1.r()], writes=[mean.r()])
            P.op("dve", lambda e: e.tensor_tensor(out=msq[:, :], in0=mean[:, :], in1=mean[:, :], op=ALU.mult),
                 reads=[mean.r()], writes=[msq.r()])
            P.op("dve", lambda e, ps2=ps2: e.scalar_tensor_tensor(
                out=lrstd[:, :], in0=ps2[:, :T], scalar=1.0 / 1024, in1=msq[:, :], op0=ALU.mult, op1=ALU.subtract),
                reads=[ps2.r(), msq.r()], writes=[lrstd.r()])
            P.op("act", lambda e: e.activation(out=lrstd[:, :], in_=lrstd[:, :], func=AF.Sqrt, bias=C.eps[:, :], scale=1.0),
                 reads=[lrstd.r(), C.eps.r()], writes=[lrstd.r()])
            P.op("dve", lambda e: e.reciprocal(out=lrstd[:, :], in_=lrstd[:, :]), reads=[lrstd.r()], writes=[lrstd.r()])
            for c in range(8):
                d = dd[c % 2]
                P.op("pool", lambda e, c=c, d=d, v=v: e.tensor_tensor(out=d[:, :], in0=v[:, c, :], in1=mean[:, :], op=ALU.subtract),
                     reads=[v.r(c), mean.r()], writes=[d.r()])
                P.op("dve", lambda e, c=c, d=d: e.scalar_tensor_tensor(
                    out=d[:, :], in0=d[:, :], scalar=lgt[:, c:c + 1], in1=lrstd[:, :], op0=ALU.mult, op1=ALU.mult),
                    reads=[d.r(), lrstd.r(), lgt.r()], writes=[d.r()])
                P.op("act", lambda e, c=c, d=d, z=z: e.activation(
                    out=z[:, c, :], in_=d[:, :], func=AF.Silu, bias=lbt[:, c:c + 1], scale=1.0),
                    reads=[d.r(), lbt.r()], writes=[z.r(c)])
            for o in range(8):
                ps = next_ps(P)
                for c in range(8):
                    P.op("pe", lambda e, o=o, c=c, ps=ps, z=z: e.matmul(
                        ps[:, :T], lhsT=W2[:, c, o * 128:(o + 1) * 128], rhs=z[:, c, :],
                        start=(c == 0), stop=(c == 7)), reads=[W2.r(c), z.r(c)], writes=[ps.r()])
                P.op("dve", lambda e, o=o, ps=ps, x=x, y=y: e.scalar_tensor_tensor(
                    out=y[:, o, :], in0=ps[:, :T], scalar=b2t[:, o:o + 1], in1=x[:, o, :], op0=ALU.add, op1=ALU.add),
                    reads=[ps.r(), x.r(), b2t.r()], writes=[y.r(o)])
            outs.append(P.dma(lambda e, y=y, tl=tl: e.dma_start(out=ov[:, :, tl:tl + T], in_=y[:, :, :]),
                              reads=y.all()))
        P.emit(final_waits=outs)
    return nc


def norm_tile(P, C, x, gt, sq, hn, rstd, L, dim=1024.0, KC=8):
    ps = next_ps(P)
    for c in range(KC):
        P.op("act", lambda e, c=c: e.activation(out=sq[:, c, :L], in_=x[:, c, :L], func=AF.Square),
             reads=[x.r()], writes=[sq.r(c)])
    for c in range(KC):
        P.op("pe", lambda e, c=c: e.matmul(ps[:, :L], lhsT=C.ones_bf[:, :], rhs=sq[:, c, :L],
                                          start=(c == 0), stop=(c == KC - 1)),
             reads=[sq.r(c), C.ones_bf.r()], writes=[ps.r()])
    P.op("act", lambda e: e.activation(out=rstd[:, :L], in_=ps[:, :L], func=AF.Sqrt,
                                       bias=C.eps[:, :], scale=1.0 / dim),
         reads=[ps.r(), C.eps.r()], writes=[rstd.r()])
    P.op("dve", lambda e: e.reciprocal(out=rstd[:, :L], in_=rstd[:, :L]), reads=[rstd.r()], writes=[rstd.r()])
    for c in range(KC):
        P.op("dve", lambda e, c=c: e.scalar_tensor_tensor(
            out=hn[:, c, :L], in0=x[:, c, :L], scalar=gt[:, c:c + 1], in1=rstd[:, :L],
            op0=ALU.mult, op1=ALU.mult), reads=[x.r(), rstd.r(), gt.r()], writes=[hn.r(c)])


def build_mla(L=16384):
    T = 512
    NTI = L // T
    NKB = L // 128
    SCALE = 96.0 ** -0.5
    nc = bass.Bass("TRN2", target_bir_lowering=False)
    hT = nc.dram_tensor("hT", [1024, L], F32, kind="ExternalInput").ap()
    g = nc.dram_tensor("g", [1024], F32, kind="ExternalInput").ap()
    wall = nc.dram_tensor("wall", [1024, 832], F32, kind="ExternalInput").ap()
    gq = nc.dram_tensor("gq", [384], F32, kind="ExternalInput").ap()
    gkv = nc.dram_tensor("gkv", [256], F32, kind="ExternalInput").ap()
    wuq = nc.dram_tensor("wuq", [384, 384], F32, kind="ExternalInput").ap()
    wukv = nc.dram_tensor("wukv", [256, 256], F32, kind="ExternalInput").ap()
    cos2 = nc.dram_tensor("cos2", [96, L], F32, kind="ExternalInput").ap()
    sin2 = nc.dram_tensor("sin2", [96, L], F32, kind="ExternalInput").ap()
    cmask = nc.dram_tensor("cmask", [128, 4, 512], F32, kind="ExternalInput").ap()
    esel = nc.dram_tensor("esel", [65, 64], F32, kind="ExternalInput").ap()
    oT = nc.dram_tensor("oT", [128, L], F32, kind="ExternalOutput").ap()
    qTd = nc.dram_tensor("qTd", [2, 96, L], BF16, kind="Internal").ap()
    hv = hT.rearrange("(c p) t -> p c t", p=128)
    with ExitStack() as st:
        st.enter_context(nc.allow_low_precision("bf16 matmul operands, fp32 accumulate"))
        P = Prog(nc, st)
        alloc_ps(P, 6)
        C = make_consts(P)
        gt = load_vec_fm(P, g, 8, "g")
        gqt = load_vec_fm(P, gq, 3, "gq")
        gkvt = load_vec_fm(P, gkv, 2, "gkv")
        Wall = load_w_bf16(P, wall, 8, 832, "Wall")
        Wuq = load_w_bf16(P, wuq, 3, 384, "Wuq")
        Wukv = load_w_bf16(P, wukv, 2, 256, "Wukv")
        cm_f = load_const(P, cmask, [128, 4, 512], "cm_f")
        cm = P.sb("cm", [128, 4, 512], BF16)
        P.op("dve", lambda e: e.tensor_copy(out=cm[:, :, :], in_=cm_f[:, :, :]), reads=[cm_f.r()], writes=[cm.r()])
        es = P.sb("es", [128, 64], F32)
        P.op("dve", lambda e: e.memset(es[:, :], 0.0), writes=[es.r()])
        P.dma(lambda e: e.dma_start(out=es[0:65, :], in_=esel), reads=[es.r()], writes=[es.r()])
        kT = [P.sb(f"kT{h}", [96, L], BF16, nsub=NTI) for h in range(2)]
        qres = [[Res(f"qTd{h}_{i}") for i in range(NTI)] for h in range(2)]
        Va = P.sb("Va", [128, NKB, 2, 65], BF16, nsub=NTI)
        P.op("pool", lambda e: e.memset(Va[:, :, :, :], 1.0), writes=Va.all())
        x = P.sb("x", [128, 8, T], F32)
        sq = P.sb("sq", [128, 8, T], BF16, nsub=8)
        hn = P.sb("hn", [128, 8, T], BF16, nsub=8)
        rstd = P.sb("rstd", [128, T], F32)
        cq = P.sb("cq", [128, 5, T], F32, nsub=5)
        cqs = P.sb("cqs", [128, 5, T], BF16, nsub=5)
        cqn = P.sb("cqn", [128, 5, T], BF16, nsub=5)
        rq = P.sb("rq", [128, T], F32)
        rkv = P.sb("rkv", [128, T], F32)
        cs = P.sb("cs", [96, 2, T], F32)
        t1 = P.sb("t1", [96, T], F32)
        t2 = P.sb("t2", [96, T], F32)
        qt = [P.sb(f"qt{h}", [96, T], BF16) for h in range(2)]
        for it in range(NTI):
            t0 = it * T
            P.dma(lambda e, t0=t0: e.dma_start(out=x[:, :, :], in_=hv[:, :, t0:t0 + T]), writes=[x.r()])
            P.dma(lambda e, t0=t0: e.dma_start(out=cs[:, 0, :], in_=cos2[:, t0:t0 + T]), writes=[cs.r()], q="act")
            P.dma(lambda e, t0=t0: e.dma_start(out=cs[:, 1, :], in_=sin2[:, t0:t0 + T]), writes=[cs.r()], q="act")
            norm_tile(P, C, x, gt, sq, hn, rstd, T)
            for j in range(5):
                ps = next_ps(P)
                for c in range(8):
                    P.op("pe", lambda e, c=c, j=j, ps=ps: e.matmul(
                        ps[:, :T], lhsT=Wall[:, c, j * 128:(j + 1) * 128], rhs=hn[:, c, :],
                        start=(c == 0), stop=(c == 7)), reads=[Wall.r(c), hn.r(c)], writes=[ps.r()])
                P.op("act", lambda e, j=j, ps=ps: e.copy(out=cq[:, j, :], in_=ps[:, :T]), reads=[ps.r()], writes=[cq.r(j)])
                P.op("pool", lambda e, j=j: e.tensor_tensor(out=cqs[:, j, :], in0=cq[:, j, :], in1=cq[:, j, :], op=ALU.mult),
                     reads=[cq.r(j)], writes=[cqs.r(j)])
            for (lo, hi, rr, dim, gg) in ((0, 3, rq, 384.0, gqt), (3, 5, rkv, 256.0, gkvt)):
                ps = next_ps(P)
                for j in range(lo, hi):
                    P.op("pe", lambda e, j=j, ps=ps, lo=lo, hi=hi: e.matmul(
                        ps[:, :T], lhsT=C.ones_bf[:, :], rhs=cqs[:, j, :], start=(j == lo), stop=(j == hi - 1)),
                        reads=[cqs.r(j), C.ones_bf.r()], writes=[ps.r()])
                P.op("act", lambda e, ps=ps, rr=rr, dim=dim: e.activation(
                    out=rr[:, :], in_=ps[:, :T], func=AF.Sqrt, bias=C.eps[:, :], scale=1.0 / dim),
                    reads=[ps.r(), C.eps.r()], writes=[rr.r()])
                P.op("dve", lambda e, rr=rr: e.reciprocal(out=rr[:, :], in_=rr[:, :]), reads=[rr.r()], writes=[rr.r()])
                for j in range(lo, hi):
                    P.op("dve", lambda e, j=j, rr=rr, gg=gg, lo=lo: e.scalar_tensor_tensor(
                        out=cqn[:, j, :], in0=cq[:, j, :], scalar=gg[:, j - lo:j - lo + 1], in1=rr[:, :],
                        op0=ALU.mult, op1=ALU.mult), reads=[cq.r(j), rr.r(), gg.r()], writes=[cqn.r(j)])
            pk = next_ps(P)
            pks = next_ps(P)
            for c in range(8):
                P.op("pe", lambda e, c=c, pk=pk: e.matmul(pk[:96, :T], lhsT=Wall[:, c, 640:736], rhs=hn[:, c, :],
                                                       start=(c == 0), stop=(c == 7)),
                     reads=[Wall.r(c), hn.r(c)], writes=[pk.r()])
            for c in range(8):
                P.op("pe", lambda e, c=c, pks=pks: e.matmul(pks[:96, :T], lhsT=Wall[:, c, 736:832], rhs=hn[:, c, :],
                                                         start=(c == 0), stop=(c == 7)),
                     reads=[Wall.r(c), hn.r(c)], writes=[pks.r()])
            P.op("dve", lambda e, pk=pk: e.tensor_tensor(out=t1[64:96, :], in0=pk[64:96, :T], in1=cs[64:96, 0, :], op=ALU.mult),
                 reads=[pk.r(), cs.r()], writes=[t1.r()])
            P.op("dve", lambda e, pks=pks: e.tensor_tensor(out=t2[64:96, :], in0=pks[64:96, :T], in1=cs[64:96, 1, :], op=ALU.mult),
                 reads=[pks.r(), cs.r()], writes=[t2.r()])
            for h in range(2):
                P.op("pool", lambda e, h=h, t0=t0: e.tensor_tensor(out=kT[h][64:96, t0:t0 + T], in0=t1[64:96, :], in1=t2[64:96, :], op=ALU.add),
                     reads=[t1.r(), t2.r()], writes=[kT[h].r(it)])
            for h in range(2):
                pq = next_ps(P)
                pqs = next_ps(P)
                for c in range(3):
                    P.op("pe", lambda e, c=c, h=h, pq=pq: e.matmul(
                        pq[:96, :T], lhsT=Wuq[:, c, h * 192:h * 192 + 96], rhs=cqn[:, c, :],
                        start=(c == 0), stop=(c == 2)), reads=[Wuq.r(c), cqn.r(c)], writes=[pq.r()])
                for c in range(3):
                    P.op("pe", lambda e, c=c, h=h, pqs=pqs: e.matmul(
                        pqs[:96, :T], lhsT=Wuq[:, c, h * 192 + 96:h * 192 + 192], rhs=cqn[:, c, :],
                        start=(c == 0), stop=(c == 2)), reads=[Wuq.r(c), cqn.r(c)], writes=[pqs.r()])
                q = qt[h]
                P.op("act", lambda e, pq=pq, q=q: e.copy(out=q[0:64, :], in_=pq[0:64, :T]), reads=[pq.r()], writes=[q.r()])
                P.op("dve", lambda e, pq=pq: e.tensor_tensor(out=t1[64:96, :], in0=pq[64:96, :T], in1=cs[64:96, 0, :], op=ALU.mult),
                     reads=[pq.r(), cs.r()], writes=[t1.r()])
                P.op("dve", lambda e, pqs=pqs: e.tensor_tensor(out=t2[64:96, :], in0=pqs[64:96, :T], in1=cs[64:96, 1, :], op=ALU.mult),
                     reads=[pqs.r(), cs.r()], writes=[t2.r()])
                P.op("pool", lambda e, q=q: e.tensor_tensor(out=q[64:96, :], in0=t1[64:96, :], in1=t2[64:96, :], op=ALU.add),
                     reads=[t1.r(), t2.r()], writes=[q.r()])
                P.dma(lambda e, h=h, q=q, t0=t0: e.dma_start(out=qTd[h, :, t0:t0 + T], in_=q[:, :]), reads=[q.r()], writes=[qres[h][it]])
                pkn = next_ps(P)
                for c in range(2):
                    P.op("pe", lambda e, c=c, h=h, pkn=pkn: e.matmul(
                        pkn[:64, :T], lhsT=Wukv[:, c, h * 64:(h + 1) * 64], rhs=cqn[:, 3 + c, :],
                        start=(c == 0), stop=(c == 1)), reads=[Wukv.r(c), cqn.r(3 + c)], writes=[pkn.r()])
                P.op("act", lambda e, h=h, pkn=pkn, t0=t0: e.copy(out=kT[h][0:64, t0:t0 + T], in_=pkn[0:64, :T]),
                     reads=[pkn.r()], writes=[kT[h].r(it)])
            for b4 in range(4):
                pv = next_ps(P)
                for c in range(2):
                    P.op("pe", lambda e, c=c, b4=b4, pv=pv: e.matmul(
                        pv[:, :128], lhsT=cqn[:, 3 + c, b4 * 128:(b4 + 1) * 128], rhs=Wukv[:, c, 128:256],
                        start=(c == 0), stop=(c == 1)), reads=[Wukv.r(c), cqn.r(3 + c)], writes=[pv.r()])
                kb = it * 4 + b4
                P.op("act", lambda e, pv=pv, kb=kb: e.copy(
                    out=Va[:, kb, :, 0:64], in_=pv[:, :128].rearrange("p (h d) -> p h d", h=2)),
                    reads=[pv.r()], writes=[Va.r(it)])
        pts = [P.sb(f"pt{i}", [128, T], BF16) for i in range(4)]
        qs = [P.sb(f"qs{i}", [96, T], BF16) for i in range(2)]
        osb = P.sb("osb", [128, T], F32)
        P.op("dve", lambda e: e.memset(osb[:, :], 0.0), writes=[osb.r()])
        rden = P.sb("rden", [64, T], F32)
        on = [P.sb(f"on{i}", [64, T], F32) for i in range(2)]
        po = [P.ps(f"po{i}", [128, 512], F32) for i in range(2)]
        outs = []
        n = 0
        for h in range(2):
            for j in range(NTI):
                q = qs[n % 2]
                pO = po[n % 2]
                o_n = on[n % 2]
                n += 1
                P.dma(lambda e, h=h, q=q, j=j: e.dma_start(out=q[:, :], in_=qTd[h, :, j * T:(j + 1) * T]),
                      reads=[qres[h][j]], writes=[q.r()], q="act")
                nkb = 4 * j + 4
                LA = 2
                pend = []

                def pv(kb, pt, h=h, pO=pO, nkb=nkb):
                    P.op("pe", lambda e, h=h, kb=kb, pt=pt, pO=pO, nkb=nkb: e.matmul(
                        pO[:65, :T], lhsT=Va[:, kb, h, :], rhs=pt[:, :], start=(kb == 0), stop=(kb == nkb - 1)),
                        reads=[Va.r(kb // 4), pt.r()], writes=[pO.r()])

                for kb in range(nkb):
                    ps = next_ps(P)
                    pt = pts[kb % len(pts)]
                    P.op("pe", lambda e, h=h, kb=kb, ps=ps, q=q: e.matmul(
                        ps[:, :T], lhsT=kT[h][:, kb * 128:(kb + 1) * 128], rhs=q[:, :], start=True, stop=True),
                        reads=[kT[h].r(kb // 4), q.r()], writes=[ps.r()])
                    P.op("act", lambda e, ps=ps, pt=pt: e.activation(out=pt[:, :], in_=ps[:, :T], func=AF.Exp, scale=SCALE),
                         reads=[ps.r()], writes=[pt.r()])
                    if kb >= 4 * j:
                        d = kb - 4 * j
                        eng = "dve" if d % 2 == 0 else "pool"
                        P.op(eng, lambda e, pt=pt, d=d: e.tensor_tensor(out=pt[:, :], in0=pt[:, :], in1=cm[:, d, :], op=ALU.mult),
                             reads=[pt.r(), cm.r()], writes=[pt.r()])
                    pend.append((kb, pt))
                    if len(pend) > LA:
                        pv(*pend.pop(0))
                while pend:
                    pv(*pend.pop(0))
                P.op("act", lambda e, pO=pO: e.copy(out=osb[0:65, :], in_=pO[:65, :T]), reads=[pO.r()], writes=[osb.r()])
                pd = next_ps(P)
                P.op("pe", lambda e, pd=pd: e.matmul(pd[:64, :T], lhsT=es[:, :], rhs=osb[:, :], start=True, stop=True),
                     reads=[es.r(), osb.r()], writes=[pd.r()])
                P.op("dve", lambda e, pd=pd: e.reciprocal(out=rden[:, :], in_=pd[:64, :T]), reads=[pd.r()], writes=[rden.r()])
                P.op("dve", lambda e, o_n=o_n: e.tensor_tensor(out=o_n[:, :], in0=osb[0:64, :], in1=rden[:, :], op=ALU.mult),
                     reads=[osb.r(), rden.r()], writes=[o_n.r()])
                outs.append(P.dma(lambda e, h=h, j=j, o_n=o_n: e.dma_start(
                    out=oT[h * 64:(h + 1) * 64, j * T:(j + 1) * T], in_=o_n[:, :]), reads=[o_n.r()]))
        P.emit(final_waits=outs)
    return nc


def build_gdn(L=16384):
    T = 512
    NTI = L // T
    nc = bass.Bass("TRN2", target_bir_lowering=False)
    hT = nc.dram_tensor("hT", [1024, L], F32, kind="ExternalInput").ap()
    g = nc.dram_tensor("g", [1024], F32, kind="ExternalInput").ap()
    wh = nc.dram_tensor("wh", [1024, 640], F32, kind="ExternalInput").ap()
    cw = nc.dram_tensor("cw", [384, 4], F32, kind="ExternalInput").ap()
    sc = nc.dram_tensor("sc", [128, 2], F32, kind="ExternalInput").ap()
    og = nc.dram_tensor("og", [128], F32, kind="ExternalInput").ap()
    cst = nc.dram_tensor("cst", [128, 5, 128], F32, kind="ExternalInput").ap()
    oT = nc.dram_tensor("oT", [128, L], F32, kind="ExternalOutput").ap()
    s_in = nc.dram_tensor("s_in", [128, 128], F32, kind="ExternalInput").ap()
    h_in = nc.dram_tensor("h_in", [128, 3, 3], F32, kind="ExternalInput").ap()
    s_out = nc.dram_tensor("s_out", [128, 128], F32, kind="ExternalOutput").ap()
    h_out = nc.dram_tensor("h_out", [128, 3, 3], F32, kind="ExternalOutput").ap()
    hv = hT.rearrange("(c p) t -> p c t", p=128)
    with ExitStack() as st:
        st.enter_context(nc.allow_low_precision("bf16 matmul operands for the input projection only"))
        P = Prog(nc, st)
        qbanks = [P.ps(f"qb{i}", [128, 512], F32) for i in range(5)]
        qp = [View(qbanks[i], 0, 128, f"qp{i}") for i in range(5)]
        hp = [View(qbanks[i], 0, 256, f"hp{i}") for i in range(5)]
        P.pss = [P.ps(f"fb{i}", [128, 512], F32) for i in range(3)]
        P._psi = 0
        cnt = {"q": 0, "h": 0}

        def nq():
            cnt["q"] += 1
            return qp[cnt["q"] % 5]

        def nh():
            cnt["q"] += 1
            return hp[cnt["q"] % 5]

        C = make_consts(P)
        ones_f = P.sb("ones_f", [128, 128], F32)
        P.op("dve", lambda e: e.memset(ones_f[:, :], 1.0), writes=[ones_f.r()])
        gt = load_vec_fm(P, g, 8, "g")
        ogt = load_vec_fm(P, og, 1, "og")
        cwt = load_vec_fm_2d = P.sb("cwt", [128, 3, 4], F32)
        P.dma(lambda e: e.dma_start(out=cwt[:, :, :], in_=cw.rearrange("(c p) j -> p c j", p=128)), writes=[cwt.r()])
        sct = load_const(P, sc, [128, 2], "sct")
        K = load_const(P, cst, [128, 5, 128], "K")
        ident = K[:, 0, :]
        maskS = K[:, 1, :]
        maskI = K[:, 2, :]
        blk1 = K[:, 3, :]
        cind = K[:, 4, 0:2]
        Wh = load_w_bf16(P, wh, 8, 640, "Wh")
        Whf = P.sb("Whf", [128, 8, 2], F32)
        P.dma(lambda e: e.dma_start(out=Whf[:, :, :], in_=wh.rearrange("(c p) f -> p c f", p=128)[:, :, 512:514]),
              writes=[Whf.r()])
        for c in range(8):
            P.op("dve", lambda e, c=c: e.tensor_scalar(out=Whf[:, c, :], in0=Whf[:, c, :], scalar1=gt[:, c:c + 1],
                                                       scalar2=None, op0=ALU.mult),
                 reads=[Whf.r(), gt.r()], writes=[Whf.r()])
        nA = P.sb("nA", [128, 1], F32)
        P.op("act", lambda e: e.activation(out=nA[:, :], in_=sct[:, 0:1], func=AF.Exp), reads=[sct.r()], writes=[nA.r()])
        P.op("dve", lambda e: e.tensor_scalar(out=nA[:, :], in0=nA[:, :], scalar1=-1.0, scalar2=None, op0=ALU.mult),
             reads=[nA.r()], writes=[nA.r()])
        onec = P.sb("onec", [128, 1], F32)
        P.op("dve", lambda e: e.memset(onec[:, :], 1.0), writes=[onec.r()])
        epsl2 = C.eps
        S = [P.sb(f"S{i}", [128, 128], F32) for i in range(2)]
        P.dma(lambda e: e.dma_start(out=S[0][:, :], in_=s_in), writes=[S[0].r()])
        sidx = [0]
        x = P.sb("x", [128, 8, T], F32)
        sq = P.sb("sq", [128, 8, T], BF16, nsub=8)
        hn = P.sb("hn", [128, 8, T], BF16, nsub=8)
        rstd = P.sb("rstd", [128, T], F32)
        raw = P.sb("raw", [128, 3, T + 3], F32, nsub=3)
        P.dma(lambda e: e.dma_start(out=raw[:, :, 0:3], in_=h_in), writes=raw.all())
        acc = P.sb("acc", [128, 3, T], F32, nsub=3)
        sil = P.sb("sil", [128, 3, T], F32, nsub=3)
        sq2 = P.sb("sq2", [128, 2, T], F32, nsub=2)
        rn = P.sb("rn", [128, 2, T], F32, nsub=2)
        tb = {}

        PERSIST = ("QpT", "O0T", "MT0", "MT1", "B0", "B1", "gateT", "oTt", "osq", "orr")

        def tbuf(par, name, shape=(128, 128)):
            if not name.rstrip("0123456789").endswith(PERSIST) and not any(name.startswith(p) for p in PERSIST):
                par = 0
            key = (par, name)
            if key not in tb:
                tb[key] = P.sb(f"t{par}_{name}", list(shape), F32)
            return tb[key]

        outs = []

        def gen_local(t):
            par = t % 2
            t0 = t * T
            qnT = tbuf(par, "qnT", (128, T))
            knT = tbuf(par, "knT", (128, T))
            vT = tbuf(par, "vT", (128, T))
            gateT = tbuf(par, "gateT", (128, T))
            P.dma(lambda e: e.dma_start(out=x[:, :, :], in_=hv[:, :, t0:t0 + T]), writes=[x.r()])
            norm_tile(P, C, x, gt, sq, hn, rstd, T)
            yield
            for s3 in range(3):
                ps = next_ps(P)
                for c in range(8):
                    P.op("pe", lambda e, c=c, s3=s3, ps=ps: e.matmul(
                        ps[:, :T], lhsT=Wh[:, c, s3 * 128:(s3 + 1) * 128], rhs=hn[:, c, :],
                        start=(c == 0), stop=(c == 7)), reads=[Wh.r(c), hn.r(c)], writes=[ps.r()])
                P.op("act", lambda e, s3=s3, ps=ps: e.copy(out=raw[:, s3, 3:T + 3], in_=ps[:, :T]),
                     reads=[ps.r()], writes=[raw.r(s3)])
            ps = next_ps(P)
            for c in range(8):
                P.op("pe", lambda e, c=c, ps=ps: e.matmul(
                    ps[:, :T], lhsT=Wh[:, c, 384:512], rhs=hn[:, c, :], start=(c == 0), stop=(c == 7)),
                    reads=[Wh.r(c), hn.r(c)], writes=[ps.r()])
            P.op("act", lambda e, ps=ps: e.activation(out=gateT[:, :], in_=ps[:, :T], func=AF.Silu),
                 reads=[ps.r()], writes=[gateT.r()])
            yield
            for s3 in range(3):
                eng = "dve" if s3 != 1 else "pool"
                P.op("dve", lambda e, s3=s3: e.tensor_scalar(
                    out=acc[:, s3, :], in0=raw[:, s3, 3:T + 3], scalar1=cwt[:, s3, 3:4], scalar2=None, op0=ALU.mult),
                    reads=[raw.r(s3), cwt.r()], writes=[acc.r(s3)])
                for j in range(3):
                    P.op("dve", lambda e, s3=s3, j=j: e.scalar_tensor_tensor(
                        out=acc[:, s3, :], in0=raw[:, s3, j:j + T], scalar=cwt[:, s3, j:j + 1], in1=acc[:, s3, :],
                        op0=ALU.mult, op1=ALU.add), reads=[raw.r(s3), cwt.r(), acc.r(s3)], writes=[acc.r(s3)])
                P.op("pool", lambda e, s3=s3: e.tensor_copy(out=raw[:, s3, 0:3], in_=raw[:, s3, T:T + 3]),
                     reads=[raw.r(s3)], writes=[raw.r(s3)])
                dst = vT if s3 == 2 else sil
                if s3 == 2:
                    P.op("act", lambda e: e.activation(out=vT[:, :], in_=acc[:, 2, :], func=AF.Silu),
                         reads=[acc.r(2)], writes=[vT.r()])
                else:
                    P.op("act", lambda e, s3=s3: e.activation(out=sil[:, s3, :], in_=acc[:, s3, :], func=AF.Silu),
                         reads=[acc.r(s3)], writes=[sil.r(s3)])
            yield
            for s2 in range(2):
                P.op("pool", lambda e, s2=s2: e.tensor_tensor(out=sq2[:, s2, :], in0=sil[:, s2, :], in1=sil[:, s2, :], op=ALU.mult),
                     reads=[sil.r(s2)], writes=[sq2.r(s2)])
                ps = next_ps(P)
                P.op("pe", lambda e, s2=s2, ps=ps: e.matmul(ps[:, :T], lhsT=ones_f[:, :], rhs=sq2[:, s2, :], start=True, stop=True),
                     reads=[ones_f.r(), sq2.r(s2)], writes=[ps.r()])
                P.op("act", lambda e, s2=s2, ps=ps: e.activation(out=rn[:, s2, :], in_=ps[:, :T], func=AF.Sqrt,
                                                                bias=epsl2[:, :], scale=1.0),
                     reads=[ps.r(), epsl2.r()], writes=[rn.r(s2)])
                P.op("dve", lambda e, s2=s2: e.reciprocal(out=rn[:, s2, :], in_=rn[:, s2, :]), reads=[rn.r(s2)], writes=[rn.r(s2)])
                dst = qnT if s2 == 0 else knT
                scl = (128.0 ** -0.5) if s2 == 0 else 1.0
                P.op("dve", lambda e, s2=s2, dst=dst, scl=scl: e.scalar_tensor_tensor(
                    out=dst[:, :], in0=sil[:, s2, :], scalar=scl, in1=rn[:, s2, :], op0=ALU.mult, op1=ALU.mult),
                    reads=[sil.r(s2), rn.r(s2)], writes=[dst.r()])
            yield
            B4 = range(4)
            tl = lambda s, name, shape=(128, 128): tbuf(par, f"{name}{s}", shape)
            pKK, pQK, pgc = {}, {}, {}
            for s in B4:
                ts = slice(s * 128, (s + 1) * 128)
                cols = tl(s, "cols", (128, 8))
                tcol = tl(s, "tcol", (128, 8))
                pc = nq()
                for c in range(8):
                    P.op("pe", lambda e, c=c, pc=pc, ts=ts: e.matmul(pc[:, 0:2], lhsT=x[:, c, ts], rhs=Whf[:, c, 0:2],
                                                                   start=(c == 0), stop=(c == 7)),
                         reads=[x.r(), Whf.r()], writes=[pc.r()])
                pss_ = nq()
                for c in range(8):
                    P.op("pe", lambda e, c=c, pss_=pss_, ts=ts: e.matmul(pss_[:, 0:2], lhsT=sq[:, c, ts], rhs=C.ones_bf[:, 0:2],
                                                                       start=(c == 0), stop=(c == 7)),
                         reads=[sq.r(c), C.ones_bf.r()], writes=[pss_.r()])
                P.op("act", lambda e, pss_=pss_, tcol=tcol: e.activation(out=tcol[:, 0:2], in_=pss_[:, 0:2], func=AF.Sqrt,
                                                                       bias=C.eps[:, :], scale=1.0 / 1024),
                     reads=[pss_.r(), C.eps.r()], writes=[tcol.r()])
                P.op("dve", lambda e, tcol=tcol: e.reciprocal(out=tcol[:, 0:2], in_=tcol[:, 0:2]), reads=[tcol.r()], writes=[tcol.r()])
                P.op("dve", lambda e, pc=pc, tcol=tcol: e.tensor_tensor(out=tcol[:, 2:4], in0=pc[:, 0:2], in1=tcol[:, 0:2], op=ALU.mult),
                     reads=[pc.r(), tcol.r()], writes=[tcol.r()])
                P.op("act", lambda e, tcol=tcol, cols=cols: e.activation(out=cols[:, 1:2], in_=tcol[:, 2:3], func=AF.Sigmoid),
                     reads=[tcol.r()], writes=[cols.r()])
                P.op("act", lambda e, tcol=tcol: e.activation(out=tcol[:, 4:5], in_=tcol[:, 3:4], func=AF.Exp, bias=sct[:, 1:2], scale=1.0),
                     reads=[tcol.r(), sct.r()], writes=[tcol.r()])
                P.op("act", lambda e, tcol=tcol: e.activation(out=tcol[:, 5:6], in_=tcol[:, 4:5], func=AF.Ln, bias=onec[:, 0:1], scale=1.0),
                     reads=[tcol.r(), onec.r()], writes=[tcol.r()])
                P.op("dve", lambda e, tcol=tcol, cols=cols: e.tensor_scalar(out=cols[:, 0:1], in0=tcol[:, 5:6], scalar1=nA[:, 0:1], scalar2=None, op0=ALU.mult),
                     reads=[tcol.r(), nA.r()], writes=[cols.r()])
                gB = tl(s, "gB"); bB = tl(s, "bB")
                P.op("dve", lambda e, gB=gB, cols=cols: e.tensor_scalar(out=gB[:, :], in0=ones_f[:, :], scalar1=cols[:, 0:1],
                                                                       scalar2=None, op0=ALU.mult),
                     reads=[ones_f.r(), cols.r()], writes=[gB.r()])
                P.op("pool", lambda e, bB=bB, cols=cols: e.tensor_scalar(out=bB[:, :], in0=ones_f[:, :], scalar1=cols[:, 1:2],
                                                                        scalar2=None, op0=ALU.mult),
                     reads=[ones_f.r(), cols.r()], writes=[bB.r()])
            yield
            for s in B4:
                ts = slice(s * 128, (s + 1) * 128)
                cols = tl(s, "cols", (128, 8)); gB = tl(s, "gB")
                pgc[s] = nq()
                P.op("pe", lambda e, p=pgc[s], gB=gB: e.matmul(p[:, :], lhsT=gB[:, :], rhs=maskI, start=True, stop=True),
                     reads=[gB.r(), K.r()], writes=[pgc[s].r()])
                pm = nq()
                P.op("pe", lambda e, pm=pm, cols=cols: e.matmul(pm[:, 0:2], lhsT=maskI, rhs=cols[:, 0:2], start=True, stop=True),
                     reads=[K.r(), cols.r()], writes=[pm.r()])
                P.op("pe", lambda e, pm=pm, cols=cols: e.matmul(pm[:, 2:4], lhsT=blk1, rhs=cols[:, 0:2], start=True, stop=True),
                     reads=[K.r(), cols.r()], writes=[pm.r()])
                P.op("pe", lambda e, pm=pm, gB=gB: e.matmul(pm[:, 4:6], lhsT=gB[:, :], rhs=cind, start=True, stop=True),
                     reads=[K.r(), gB.r()], writes=[pm.r()])
                P.op("act", lambda e, pm=pm, cols=cols: e.copy(out=cols[:, 2:3], in_=pm[:, 0:1]), reads=[pm.r()], writes=[cols.r()])
                P.op("act", lambda e, pm=pm, cols=cols: e.copy(out=cols[:, 3:4], in_=pm[:, 2:3]), reads=[pm.r()], writes=[cols.r()])
                glb = tl(s, "glb", (128, 2))
                P.op("act", lambda e, pm=pm, glb=glb: e.activation(out=glb[:, :], in_=pm[:, 4:6], func=AF.Exp),
                     reads=[pm.r()], writes=[glb.r()])
                E = tl(s, "E"); egc = tl(s, "egc")
                P.op("dve", lambda e, p=pgc[s], E=E, cols=cols: e.tensor_scalar(
                    out=E[:, :], in0=p[:, :], scalar1=cols[:, 2:3], scalar2=0.0, op0=ALU.subtract, op1=ALU.min),
                    reads=[pgc[s].r(), cols.r()], writes=[E.r()])
                P.op("act", lambda e, p=pgc[s], egc=egc: e.activation(out=egc[:, :], in_=p[:, :], func=AF.Exp),
                     reads=[pgc[s].r()], writes=[egc.r()])
                P.op("act", lambda e, cols=cols: e.activation(out=cols[:, 4:5], in_=cols[:, 2:3], func=AF.Exp),
                     reads=[cols.r()], writes=[cols.r()])
                P.op("dve", lambda e, cols=cols: e.tensor_tensor(out=cols[:, 5:6], in0=cols[:, 4:5], in1=cols[:, 1:2], op=ALU.mult),
                     reads=[cols.r()], writes=[cols.r()])
                P.op("dve", lambda e, cols=cols: e.tensor_tensor(out=cols[:, 6:7], in0=cols[:, 3:4], in1=cols[:, 2:3], op=ALU.subtract),
                     reads=[cols.r()], writes=[cols.r()])
                P.op("act", lambda e, cols=cols: e.activation(out=cols[:, 6:7], in_=cols[:, 6:7], func=AF.Exp),
                     reads=[cols.r()], writes=[cols.r()])
            yield
            for s in B4:
                ts = slice(s * 128, (s + 1) * 128)
                E = tl(s, "E"); EmS = tl(s, "EmS"); EmI = tl(s, "EmI")
                P.op("act", lambda e, E=E: e.activation(out=E[:, :], in_=E[:, :], func=AF.Exp), reads=[E.r()], writes=[E.r()])
                pbb = nq()
                bB = tl(s, "bB")
                P.op("pe", lambda e, pbb=pbb, bB=bB: e.matmul(pbb[:, :], lhsT=bB[:, :], rhs=ident, start=True, stop=True),
                     reads=[bB.r(), K.r()], writes=[pbb.r()])
                P.op("pool", lambda e, E=E, EmS=EmS: e.tensor_tensor(out=EmS[:, :], in0=E[:, :], in1=maskS, op=ALU.mult),
                     reads=[E.r(), K.r()], writes=[EmS.r()])
                P.op("pool", lambda e, E=E, EmI=EmI: e.tensor_tensor(out=EmI[:, :], in0=E[:, :], in1=maskI, op=ALU.mult),
                     reads=[E.r(), K.r()], writes=[EmI.r()])
                P.op("dve", lambda e, pbb=pbb, EmS=EmS: e.tensor_tensor(out=EmS[:, :], in0=pbb[:, :], in1=EmS[:, :], op=ALU.mult),
                     reads=[pbb.r(), EmS.r()], writes=[EmS.r()])
            yield
            for s in B4:
                ts = slice(s * 128, (s + 1) * 128)
                EmS = tl(s, "EmS"); EmI = tl(s, "EmI"); Q0 = tl(s, "Qa"); qkT = tl(s, "qkT")
                pKK[s] = nq()
                P.op("pe", lambda e, p=pKK[s], ts=ts: e.matmul(p[:, :], lhsT=knT[:, ts], rhs=knT[:, ts], start=True, stop=True),
                     reads=[knT.r()], writes=[pKK[s].r()])
                P.op("dve", lambda e, p=pKK[s], EmS=EmS, Q0=Q0: e.scalar_tensor_tensor(
                    out=Q0[:, :], in0=p[:, :], scalar=-1.0, in1=EmS[:, :], op0=ALU.mult, op1=ALU.mult),
                    reads=[pKK[s].r(), EmS.r()], writes=[Q0.r()])
                pQK[s] = nq()
                P.op("pe", lambda e, p=pQK[s], ts=ts: e.matmul(p[:, :], lhsT=knT[:, ts], rhs=qnT[:, ts], start=True, stop=True),
                     reads=[knT.r(), qnT.r()], writes=[pQK[s].r()])
                P.op("dve", lambda e, p=pQK[s], EmI=EmI, qkT=qkT: e.tensor_tensor(out=qkT[:, :], in0=p[:, :], in1=EmI[:, :], op=ALU.mult),
                     reads=[pQK[s].r(), EmI.r()], writes=[qkT.r()])
            yield
            for s in B4:
                Q0 = tl(s, "Qa"); N0 = tl(s, "Na"); R0 = tl(s, "Ra")
                pt = nq()
                P.op("pe", lambda e, pt=pt, Q0=Q0: e.transpose(pt[:, :], Q0[:, :], ident), reads=[Q0.r(), K.r()], writes=[pt.r()])
                P.op("act", lambda e, pt=pt, N0=N0: e.copy(out=N0[:, :], in_=pt[:, :]), reads=[pt.r()], writes=[N0.r()])
                P.op("pool", lambda e, Q0=Q0, R0=R0: e.tensor_tensor(out=R0[:, :], in0=Q0[:, :], in1=ident, op=ALU.add),
                     reads=[Q0.r(), K.r()], writes=[R0.r()])
            yield
            names = ["a", "b"]
            for i in range(1, 6):
                po, pn = names[(i - 1) % 2], names[i % 2]
                for s in B4:
                    Qo = tl(s, "Q" + po); No = tl(s, "N" + po); Ro = tl(s, "R" + po)
                    Qn = tl(s, "Q" + pn); Nn = tl(s, "N" + pn)
                    pN = nq()
                    P.op("pe", lambda e, pN=pN, Qo=Qo, No=No: e.matmul(pN[:, :], lhsT=Qo[:, :], rhs=No[:, :], start=True, stop=True),
                         reads=[Qo.r(), No.r()], writes=[pN.r()])
                    P.op("act", lambda e, pN=pN, Nn=Nn: e.copy(out=Nn[:, :], in_=pN[:, :]), reads=[pN.r()], writes=[Nn.r()])
                    if i < 5:
                        pQ = nq()
                        P.op("pe", lambda e, pQ=pQ, Qo=Qo, No=No: e.matmul(pQ[:, :], lhsT=No[:, :], rhs=Qo[:, :], start=True, stop=True),
                             reads=[Qo.r(), No.r()], writes=[pQ.r()])
                        P.op("dve", lambda e, pQ=pQ, Qn=Qn: e.tensor_copy(out=Qn[:, :], in_=pQ[:, :]), reads=[pQ.r()], writes=[Qn.r()])
                yield
                for s in B4:
                    Nn = tl(s, "N" + pn); Ro = tl(s, "R" + po); Rn = tl(s, "R" + pn)
                    pR = nq()
                    P.op("pe", lambda e, pR=pR, Nn=Nn, Ro=Ro: e.matmul(pR[:, :], lhsT=Nn[:, :], rhs=Ro[:, :], start=True, stop=True),
                         reads=[Nn.r(), Ro.r()], writes=[pR.r()])
                    P.op("dve", lambda e, pR=pR, Ro=Ro, Rn=Rn: e.tensor_tensor(out=Rn[:, :], in0=pR[:, :], in1=Ro[:, :], op=ALU.add),
                         reads=[pR.r(), Ro.r()], writes=[Rn.r()])
                yield
            TTn = names[5 % 2]
            for s in B4:
                ts = slice(s * 128, (s + 1) * 128)
                cols = tl(s, "cols", (128, 8))
                UWin = tl(s, "UWin", (128, 256)); kd = tl(s, "kd")
                pk = nq()
                P.op("pe", lambda e, pk=pk, ts=ts: e.transpose(pk[:, :], knT[:, ts], ident), reads=[knT.r(), K.r()], writes=[pk.r()])
                P.op("dve", lambda e, pk=pk, UWin=UWin, cols=cols: e.tensor_scalar(out=UWin[:, 128:256], in0=pk[:, :], scalar1=cols[:, 5:6], scalar2=None, op0=ALU.mult),
                     reads=[pk.r(), cols.r()], writes=[UWin.r()])
                P.op("dve", lambda e, pk=pk, kd=kd, cols=cols: e.tensor_scalar(out=kd[:, :], in0=pk[:, :], scalar1=cols[:, 6:7], scalar2=None, op0=ALU.mult),
                     reads=[pk.r(), cols.r()], writes=[kd.r()])
                pv = nq()
                P.op("pe", lambda e, pv=pv, ts=ts: e.transpose(pv[:, :], vT[:, ts], ident), reads=[vT.r(), K.r()], writes=[pv.r()])
                P.op("dve", lambda e, pv=pv, UWin=UWin, cols=cols: e.tensor_scalar(out=UWin[:, 0:128], in0=pv[:, :], scalar1=cols[:, 1:2], scalar2=None, op0=ALU.mult),
                     reads=[pv.r(), cols.r()], writes=[UWin.r()])
            yield
            for s in B4:
                ts = slice(s * 128, (s + 1) * 128)
                UWin = tl(s, "UWin", (128, 256)); uw = tl(s, "uw", (128, 256)); TT = tl(s, "R" + TTn)
                egc = tl(s, "egc"); qdT = tl(s, "qdT")
                pu = nh()
                P.op("pe", lambda e, pu=pu, TT=TT, UWin=UWin: e.matmul(pu[:, :], lhsT=TT[:, :], rhs=UWin[:, :], start=True, stop=True),
                     reads=[TT.r(), UWin.r()], writes=[pu.r()])
                P.op("act", lambda e, pu=pu, uw=uw: e.copy(out=uw[:, :], in_=pu[:, :]), reads=[pu.r()], writes=[uw.r()])
                P.op("pool", lambda e, qdT=qdT, egc=egc, ts=ts: e.tensor_tensor(out=qdT[:, :], in0=qnT[:, ts], in1=egc[:, :], op=ALU.mult),
                     reads=[qnT.r(), egc.r()], writes=[qdT.r()])
            yield
            for s in B4:
                uw = tl(s, "uw", (128, 256)); qkT = tl(s, "qkT"); qdT = tl(s, "qdT"); kd = tl(s, "kd")
                QpT = tl(s, "QpT"); O0T = tl(s, "O0T"); glb = tl(s, "glb", (128, 2))
                pw = nq()
                P.op("pe", lambda e, pw=pw, uw=uw, qkT=qkT: e.matmul(pw[:, :], lhsT=uw[:, 128:256], rhs=qkT[:, :], start=True, stop=True),
                     reads=[uw.r(), qkT.r()], writes=[pw.r()])
                P.op("dve", lambda e, pw=pw, qdT=qdT, QpT=QpT: e.tensor_tensor(out=QpT[:, :], in0=qdT[:, :], in1=pw[:, :], op=ALU.subtract),
                     reads=[pw.r(), qdT.r()], writes=[QpT.r()])
                po0 = nq()
                P.op("pe", lambda e, po0=po0, uw=uw, qkT=qkT: e.matmul(po0[:, :], lhsT=uw[:, 0:128], rhs=qkT[:, :], start=True, stop=True),
                     reads=[uw.r(), qkT.r()], writes=[po0.r()])
                P.op("act", lambda e, po0=po0, O0T=O0T: e.copy(out=O0T[:, :], in_=po0[:, :]), reads=[po0.r()], writes=[O0T.r()])
                for c2 in range(2):
                    r = slice(c2 * 64, (c2 + 1) * 64)
                    MT = tl(s, f"MT{c2}"); Bc = tl(s, f"B{c2}")
                    pM = nq()
                    P.op("pe", lambda e, pM=pM, uw=uw, kd=kd, r=r: e.matmul(pM[:, :], lhsT=uw[r, 128:256], rhs=kd[r, :], start=True, stop=True),
                         reads=[uw.r(), kd.r()], writes=[pM.r()])
                    P.op("dve", lambda e, pM=pM, MT=MT, glb=glb, c2=c2: e.scalar_tensor_tensor(
                        out=MT[:, :], in0=ident, scalar=glb[:, c2:c2 + 1], in1=pM[:, :], op0=ALU.mult, op1=ALU.subtract),
                        reads=[pM.r(), glb.r(), K.r()], writes=[MT.r()])
                    pB = nq()
                    P.op("pe", lambda e, pB=pB, uw=uw, kd=kd, r=r: e.matmul(pB[:, :], lhsT=kd[r, :], rhs=uw[r, 0:128], start=True, stop=True),
                         reads=[uw.r(), kd.r()], writes=[pB.r()])
                    P.op("act", lambda e, pB=pB, Bc=Bc: e.copy(out=Bc[:, :], in_=pB[:, :]), reads=[pB.r()], writes=[Bc.r()])
                yield

        def gen_rec(t):
            par = t % 2
            t0 = t * T
            tl = lambda s, name, shape=(128, 128): tbuf(par, f"{name}{s}", shape)
            oTt = tbuf(par, "oTt", (128, T))
            gateT = tbuf(par, "gateT", (128, T))
            for s in range(4):
                QpT = tl(s, "QpT"); O0T = tl(s, "O0T")
                for c2 in range(2):
                    r = slice(c2 * 64, (c2 + 1) * 64)
                    col = slice(s * 128 + c2 * 64, s * 128 + (c2 + 1) * 64)
                    MT = tl(s, f"MT{c2}"); Bc = tl(s, f"B{c2}")
                    So = S[sidx[0] % 2]
                    Sn = S[(sidx[0] + 1) % 2]
                    sidx[0] += 1
                    po = nq()
                    P.op("pe", lambda e, po=po, So=So, QpT=QpT, r=r: e.matmul(po[:, 0:64], lhsT=So[:, :], rhs=QpT[:, r], start=True, stop=True),
                         reads=[So.r(), QpT.r()], writes=[po.r()])
                    pS = nq()
                    P.op("pe", lambda e, pS=pS, So=So, MT=MT: e.matmul(pS[:, :], lhsT=MT[:, :], rhs=So[:, :], start=True, stop=True),
                         reads=[So.r(), MT.r()], writes=[pS.r()])
                    P.op("dve", lambda e, pS=pS, Bc=Bc, Sn=Sn: e.tensor_tensor(out=Sn[:, :], in0=pS[:, :], in1=Bc[:, :], op=ALU.add),
                         reads=[pS.r(), Bc.r()], writes=[Sn.r()])
                    P.op("dve", lambda e, po=po, O0T=O0T, r=r, col=col: e.tensor_tensor(out=oTt[:, col], in0=po[:, 0:64], in1=O0T[:, r], op=ALU.add),
                         reads=[po.r(), O0T.r()], writes=[oTt.r()])
                    yield
            osq = tbuf(par, "osq", (128, T))
            orr = tbuf(par, "orr", (128, T))
            P.op("pool", lambda e: e.tensor_tensor(out=osq[:, :], in0=oTt[:, :], in1=oTt[:, :], op=ALU.mult), reads=[oTt.r()], writes=[osq.r()])
            ps = next_ps(P)
            P.op("pe", lambda e, ps=ps: e.matmul(ps[:, :T], lhsT=ones_f[:, :], rhs=osq[:, :], start=True, stop=True),
                 reads=[ones_f.r(), osq.r()], writes=[ps.r()])
            P.op("act", lambda e, ps=ps: e.activation(out=orr[:, :], in_=ps[:, :T], func=AF.Sqrt, bias=C.eps[:, :], scale=1.0 / 128),
                 reads=[ps.r(), C.eps.r()], writes=[orr.r()])
            P.op("dve", lambda e: e.reciprocal(out=orr[:, :], in_=orr[:, :]), reads=[orr.r()], writes=[orr.r()])
            P.op("dve", lambda e: e.scalar_tensor_tensor(out=osq[:, :], in0=oTt[:, :], scalar=ogt[:, 0:1], in1=orr[:, :],
                                                        op0=ALU.mult, op1=ALU.mult),
                 reads=[oTt.r(), orr.r(), ogt.r()], writes=[osq.r()])
            P.op("pool", lambda e: e.tensor_tensor(out=osq[:, :], in0=osq[:, :], in1=gateT[:, :], op=ALU.mult),
                 reads=[osq.r(), gateT.r()], writes=[osq.r()])
            outs.append(P.dma(lambda e: e.dma_start(out=oT[:, t0:t0 + T], in_=osq[:, :]), reads=[osq.r()]))
            yield

        rec = None
        for t in range(NTI):
            loc = gen_local(t)
            while True:
                a = next(loc, "done")
                if rec is not None:
                    next(rec, None)
                if a == "done":
                    break
            if rec is not None:
                for _ in rec:
                    pass
            rec = gen_rec(t)
        for _ in rec:
            pass
        outs.append(P.dma(lambda e: e.dma_start(out=s_out, in_=S[sidx[0] % 2][:, :]), reads=[S[sidx[0] % 2].r()]))
        outs.append(P.dma(lambda e: e.dma_start(out=h_out, in_=raw[:, :, 0:3]), reads=raw.all()))
        P.emit(final_waits=outs)
    return nc


def build_dsa(L=16384, NIT=18, jset=None):
    T = 512
    NTI = L // T
    NQT = L // 256
    NJ_ALL = NQT // 8
    jset = list(range(NJ_ALL)) if jset is None else list(jset)
    NJ = len(jset)
    NQ = NJ * 256
    SCALE = 128.0 ** -0.5
    WSC = (8.0 ** -0.5) * (64.0 ** -0.5)
    nc = bass.Bass("TRN2", target_bir_lowering=False)
    xT = nc.dram_tensor("xT", [1024, L], F32, kind="ExternalInput").ap()
    xq = nc.dram_tensor("xq", [1024, NQ], F32, kind="ExternalInput").ap()
    g = nc.dram_tensor("g", [1024], F32, kind="ExternalInput").ap()
    win = nc.dram_tensor("win", [1024, 3656], F32, kind="ExternalInput").ap()
    lng = nc.dram_tensor("lng", [64], F32, kind="ExternalInput").ap()
    lnb = nc.dram_tensor("lnb", [64], F32, kind="ExternalInput").ap()
    qrel = nc.dram_tensor("qrel", [128, NJ * 2], F32, kind="ExternalInput").ap()
    kidx = nc.dram_tensor("kidx", [128, 2048], F32, kind="ExternalInput").ap()
    cst = nc.dram_tensor("cst", [128, 5, 128], F32, kind="ExternalInput").ap()
    oT = nc.dram_tensor("oT", [1024, NQ], F32, kind="ExternalOutput").ap()
    kTd = nc.dram_tensor("kTd", [1024, L], BF16, kind="Internal").ap()
    Vd = nc.dram_tensor("Vd", [L, 1024], BF16, kind="Internal").ap()
    qTd = nc.dram_tensor("qTd", [1024, NQ], BF16, kind="Internal").ap()
    qiTd = nc.dram_tensor("qiTd", [64, 8, NQ], F32, kind="Internal").ap()
    kiTd = nc.dram_tensor("kiTd", [64, L], F32, kind="Internal").ap()
    xv = xT.rearrange("(c p) t -> p c t", p=128)
    xqv = xq.rearrange("(c p) t -> p c t", p=128)
    kTv = kTd.rearrange("(h p) t -> p h t", p=128)
    Vv = Vd.rearrange("(n p) d -> p n d", p=128)
    qTv = qTd.rearrange("(h p) t -> p h t", p=128)
    oTv = oT.rearrange("(h p) t -> p h t", p=128)
    wv_ = win.rearrange("(c p) f -> p c f", p=128)
    with ExitStack() as st:
        st.enter_context(nc.allow_low_precision("bf16 matmul operands, fp32 accumulate"))
        P = Prog(nc, st)
        banks = [P.ps(f"b{i}", [128, 512], F32) for i in range(8)]
        P.pss = banks
        P._psi = 0
        C = make_consts(P)
        ones_f = P.sb("ones_f", [128, 128], F32)
        P.op("dve", lambda e: e.memset(ones_f[:, :], 1.0), writes=[ones_f.r()])
        gt = load_vec_fm(P, g, 8, "g")
        lngt = P.sb("lngt", [64, 1], F32)
        lnbt = P.sb("lnbt", [64, 1], F32)
        P.dma(lambda e: e.dma_start(out=lngt[:, :], in_=lng.rearrange("(p o) -> p o", o=1)), writes=[lngt.r()])
        P.dma(lambda e: e.dma_start(out=lnbt[:, :], in_=lnb.rearrange("(p o) -> p o", o=1)), writes=[lnbt.r()])
        qrt = load_const(P, qrel, [128, NJ * 2], "qrt")
        kit = load_const(P, kidx, [128, 2048], "kit")
        Kc = load_const(P, cst, [128, 5, 128], "Kc")
        idb = P.sb("idb", [128, 128], BF16)
        P.op("dve", lambda e: e.tensor_copy(out=idb[:, :], in_=Kc[:, 0, :]), reads=[Kc.r()], writes=[idb.r()])
        selb = P.sb("selb", [128, 2, 512], BF16)
        P.op("dve", lambda e: e.memset(selb[:, :, :], 0.0), writes=[selb.r()])
        for hf in range(2):
            for rep in range(2):
                c0 = rep * 256 + hf * 128
                P.op("dve", lambda e, hf=hf, c0=c0: e.tensor_copy(out=selb[:, hf, c0:c0 + 128], in_=Kc[:, 0, :]),
                     reads=[Kc.r(), selb.r()], writes=[selb.r()])
        wiT = P.sb("wiT", [128, NJ * 2, 8], F32)

        P.push_scope()
        Wq = P.sb("Wq", [128, 8, 1024], BF16, nsub=8)
        Wk = P.sb("Wk", [128, 8, 1024], BF16, nsub=8)
        Wv = P.sb("Wv", [128, 8, 1024], BF16, nsub=8)
        Wi = P.sb("Wi", [128, 8, 584], F32, nsub=8)
        hnf = P.sb("hnf", [128, 8, T], F32, nsub=8)
        for c in range(8):
            P.dma(lambda e, c=c: e.dma_start(out=Wk[:, c, :], in_=wv_[:, c, 1024:2048], max_dma_last_dim=8192), writes=[Wk.r(c)], q="pool")
            P.dma(lambda e, c=c: e.dma_start(out=Wv[:, c, :], in_=wv_[:, c, 2048:3072], max_dma_last_dim=8192), writes=[Wv.r(c)], q="pool")
            P.dma(lambda e, c=c: e.dma_start(out=Wi[:, c, :], in_=wv_[:, c, 3072:3656]), writes=[Wi.r(c)])
            P.dma(lambda e, c=c: e.dma_start(out=Wq[:, c, :], in_=wv_[:, c, 0:1024], max_dma_last_dim=8192), writes=[Wq.r(c)], q="pool")
        x = P.sb("x", [128, 8, T], F32)
        sq = P.sb("sq", [128, 8, T], BF16, nsub=8)
        hn = P.sb("hn", [128, 8, T], BF16, nsub=8)
        rstd = P.sb("rstd", [128, T], F32)
        ktb = [P.sb(f"ktb{i}", [128, 8, T], BF16, nsub=8) for i in range(2)]
        vtb = [P.sb(f"vtb{i}", [128, 4, 1024], BF16, nsub=8) for i in range(2)]
        kraw = P.sb("kraw", [64, T], F32)
        ksq = P.sb("ksq", [64, T], F32)
        kmean = P.sb("kmean", [64, T], F32)
        kvar = P.sb("kvar", [64, T], F32)
        kio = P.sb("kio", [64, T], F32)
        for it in range(NTI):
            t0 = it * T
            kt = ktb[it % 2]
            vt = vtb[it % 2]
            P.dma(lambda e, t0=t0: e.dma_start(out=x[:, :, :], in_=xv[:, :, t0:t0 + T]), writes=[x.r()])
            norm_tile(P, C, x, gt, sq, hn, rstd, T)
            for c in range(8):
                P.op("dve", lambda e, c=c: e.scalar_tensor_tensor(out=hnf[:, c, :], in0=x[:, c, :], scalar=gt[:, c:c + 1], in1=rstd[:, :],
                                                                 op0=ALU.mult, op1=ALU.mult), reads=[x.r(), rstd.r(), gt.r()], writes=[hnf.r(c)])
            for h in range(8):
                ps = next_ps(P)
                for c in range(8):
                    P.op("pe", lambda e, c=c, h=h, ps=ps: e.matmul(ps[:, :T], lhsT=Wk[:, c, h * 128:(h + 1) * 128], rhs=hn[:, c, :],
                                                                  start=(c == 0), stop=(c == 7)),
                         reads=[Wk.r(c), hn.r(c)], writes=[ps.r()])
                if h % 2 == 0:
                    P.op("act", lambda e, h=h, ps=ps, kt=kt: e.copy(out=kt[:, h, :], in_=ps[:, :T]), reads=[ps.r()], writes=[kt.r(h)])
                else:
                    P.op("dve", lambda e, h=h, ps=ps, kt=kt: e.tensor_copy(out=kt[:, h, :], in_=ps[:, :T]), reads=[ps.r()], writes=[kt.r(h)])
            P.dma(lambda e, kt=kt, t0=t0: e.dma_start(out=kTv[:, :, t0:t0 + T], in_=kt[:, :, :]), reads=kt.all())
            for b4 in range(4):
                for half in range(2):
                    ps = next_ps(P)
                    for c in range(8):
                        P.op("pe", lambda e, c=c, b4=b4, half=half, ps=ps: e.matmul(
                            ps[:, :512], lhsT=hn[:, c, b4 * 128:(b4 + 1) * 128], rhs=Wv[:, c, half * 512:(half + 1) * 512],
                            start=(c == 0), stop=(c == 7)), reads=[Wv.r(c), hn.r(c)], writes=[ps.r()])
                    if half == 0:
                        P.op("act", lambda e, b4=b4, half=half, ps=ps, vt=vt: e.copy(out=vt[:, b4, 0:512], in_=ps[:, :512]),
                             reads=[ps.r()], writes=[vt.r(b4 * 2)])
                    else:
                        P.op("dve", lambda e, b4=b4, half=half, ps=ps, vt=vt: e.tensor_copy(out=vt[:, b4, 512:1024], in_=ps[:, :512]),
                             reads=[ps.r()], writes=[vt.r(b4 * 2 + 1)])
            P.dma(lambda e, vt=vt, it=it: e.dma_start(out=Vv[:, it * 4:(it + 1) * 4, :], in_=vt[:, :, :]), reads=vt.all(), q="act")
            ps = next_ps(P)
            for c in range(8):
                P.op("pe", lambda e, c=c, ps=ps: e.matmul(ps[0:64, :T], lhsT=Wi[:, c, 512:576], rhs=hnf[:, c, :],
                                                       start=(c == 0), stop=(c == 7)),
                     reads=[Wi.r(c), hnf.r(c)], writes=[ps.r()])
            P.op("act", lambda e, ps=ps: e.copy(out=kraw[:, :], in_=ps[0:64, :T]), reads=[ps.r()], writes=[kraw.r()])
            P.op("pool", lambda e: e.tensor_tensor(out=ksq[:, :], in0=kraw[:, :], in1=kraw[:, :], op=ALU.mult), reads=[kraw.r()], writes=[ksq.r()])
            p1 = next_ps(P)
            P.op("pe", lambda e, p1=p1: e.matmul(p1[0:64, :T], lhsT=ones_f[0:64, 0:64], rhs=kraw[:, :], start=True, stop=True),
                 reads=[ones_f.r(), kraw.r()], writes=[p1.r()])
            p2 = next_ps(P)
            P.op("pe", lambda e, p2=p2: e.matmul(p2[0:64, :T], lhsT=ones_f[0:64, 0:64], rhs=ksq[:, :], start=True, stop=True),
                 reads=[ones_f.r(), ksq.r()], writes=[p2.r()])
            P.op("act", lambda e, p1=p1: e.activation(out=kmean[:, :], in_=p1[0:64, :T], func=AF.Copy, scale=1.0 / 64), reads=[p1.r()], writes=[kmean.r()])
            P.op("dve", lambda e: e.tensor_tensor(out=ksq[:, :], in0=kmean[:, :], in1=kmean[:, :], op=ALU.mult), reads=[kmean.r(), ksq.r()], writes=[ksq.r()])
            P.op("dve", lambda e, p2=p2: e.scalar_tensor_tensor(out=kvar[:, :], in0=p2[0:64, :T], scalar=1.0 / 64, in1=ksq[:, :],
                                                              op0=ALU.mult, op1=ALU.subtract), reads=[p2.r(), ksq.r()], writes=[kvar.r()])
            P.op("act", lambda e: e.activation(out=kvar[:, :], in_=kvar[:, :], func=AF.Sqrt, bias=C.eps[0:64, :], scale=1.0),
                 reads=[kvar.r(), C.eps.r()], writes=[kvar.r()])
            P.op("dve", lambda e: e.reciprocal(out=kvar[:, :], in_=kvar[:, :]), reads=[kvar.r()], writes=[kvar.r()])
            P.op("pool", lambda e: e.tensor_tensor(out=kraw[:, :], in0=kraw[:, :], in1=kmean[:, :], op=ALU.subtract), reads=[kraw.r(), kmean.r()], writes=[kraw.r()])
            P.op("dve", lambda e: e.scalar_tensor_tensor(out=kraw[:, :], in0=kraw[:, :], scalar=lngt[:, 0:1], in1=kvar[:, :],
                                                        op0=ALU.mult, op1=ALU.mult), reads=[kraw.r(), kvar.r(), lngt.r()], writes=[kraw.r()])
            P.op("act", lambda e: e.activation(out=kio[:, :], in_=kraw[:, :], func=AF.Identity, bias=lnbt[:, 0:1], scale=1.0),
                 reads=[kraw.r(), lnbt.r()], writes=[kio.r()])
            P.dma(lambda e, t0=t0: e.dma_start(out=kiTd[:, t0:t0 + T], in_=kio[:, :]), reads=[kio.r()])
        qtb = P.sb("qtb", [128, 8, 256], BF16, nsub=8)
        qitb = P.sb("qitb", [64, 8, 256], F32, nsub=8)
        for j in range(NJ):
            q0 = j * 256
            P.dma(lambda e, q0=q0: e.dma_start(out=x[:, :, 0:256], in_=xqv[:, :, q0:q0 + 256]), writes=[x.r()])
            norm_tile(P, C, x, gt, sq, hn, rstd, 256)
            for c in range(8):
                P.op("dve", lambda e, c=c: e.scalar_tensor_tensor(out=hnf[:, c, 0:256], in0=x[:, c, 0:256], scalar=gt[:, c:c + 1], in1=rstd[:, 0:256],
                                                                 op0=ALU.mult, op1=ALU.mult), reads=[x.r(), rstd.r(), gt.r()], writes=[hnf.r(c)])
            for h in range(8):
                ps = next_ps(P)
                for c in range(8):
                    P.op("pe", lambda e, c=c, h=h, ps=ps: e.matmul(ps[:, :256], lhsT=Wq[:, c, h * 128:(h + 1) * 128], rhs=hn[:, c, 0:256],
                                                                  start=(c == 0), stop=(c == 7)),
                         reads=[Wq.r(c), hn.r(c)], writes=[ps.r()])
                P.op("act", lambda e, h=h, ps=ps: e.copy(out=qtb[:, h, :], in_=ps[:, :256]), reads=[ps.r()], writes=[qtb.r(h)])
                ps2 = next_ps(P)
                for c in range(8):
                    P.op("pe", lambda e, c=c, h=h, ps2=ps2: e.matmul(ps2[0:64, :256], lhsT=Wi[:, c, h * 64:(h + 1) * 64], rhs=hnf[:, c, 0:256],
                                                                    start=(c == 0), stop=(c == 7)),
                         reads=[Wi.r(c), hnf.r(c)], writes=[ps2.r()])
                P.op("dve", lambda e, h=h, ps2=ps2: e.tensor_copy(out=qitb[:, h, :], in_=ps2[0:64, :256]), reads=[ps2.r()], writes=[qitb.r(h)])
            P.dma(lambda e, q0=q0: e.dma_start(out=qTv[:, :, q0:q0 + 256], in_=qtb[:, :, :]), reads=qtb.all())
            P.dma(lambda e, q0=q0: e.dma_start(out=qiTd[:, :, q0:q0 + 256], in_=qitb[:, :, :]), reads=qitb.all(), q="act")
            for hf in range(2):
                ps = next_ps(P)
                for c in range(8):
                    P.op("pe", lambda e, c=c, hf=hf, ps=ps: e.matmul(ps[:, 0:8], lhsT=hnf[:, c, hf * 128:(hf + 1) * 128], rhs=Wi[:, c, 576:584],
                                                                    start=(c == 0), stop=(c == 7)),
                         reads=[Wi.r(c), hnf.r(c)], writes=[ps.r()])
                P.op("act", lambda e, j=j, hf=hf, ps=ps: e.activation(out=wiT[:, j * 2 + hf, :], in_=ps[:, 0:8], func=AF.Copy, scale=WSC),
                     reads=[ps.r()], writes=[wiT.r()])
        P.pop_scope()

        I = P.sb("I", [128, L], F32)
        Mb = [P.sb(f"Mb{i}", [128, L], BF16) for i in range(2)]
        junk = P.sb("junk", [128, 2048], BF16)
        qih = P.sb("qih", [64, 8, 128], F32)
        dg = P.sb("dg", [128, 8, 128], F32)
        rl = [P.sb(f"rl{i}", [128, 512], F32) for i in range(3)]
        kics = [P.sb(f"kic{i}", [64, 512], F32) for i in range(3)]
        tmpb = P.sb("tmpb", [128, 512], F32)
        am = P.sb("am", [128, 32], F32)
        cn = P.sb("cn", [128, 8], F32)
        sm = P.sb("sm", [128, 8], F32)
        qt = P.sb("qt", [128, 4, 256], BF16)
        ktl = [P.sb(f"ktl{i}", [128, 4, 512], BF16) for i in range(3)]
        vtl = [P.sb(f"vtl{i}", [128, 4, 512], BF16) for i in range(3)]
        pts = [P.sb(f"pt{i}", [128, 512], BF16) for i in range(5)]
        rden = P.sb("rden", [128, 512], F32)
        ob = [P.sb(f"ob{i}", [128, 512], F32) for i in range(2)]
        bS = banks[0:3]
        bO = banks[3:5]
        bD = banks[5:7]
        bI = [banks[3], banks[4]]
        outs = []
        rot = {"s": 0, "p": 0, "l": 0, "r": 0, "i": 0, "k": 0}

        def nxt(lst, key):
            rot[key] += 1
            return lst[rot[key] % len(lst)]

        for j in range(NJ):
            Nmax = 256 * (8 * jset[j] + 8)
            ws = Nmax - 2048
            nck = Nmax // 512
            for hf in range(2):
                qi = j * 2 + hf
                M = Mb[hf]
                P.dma(lambda e, j=j, hf=hf: e.dma_start(out=qih[:, :, :], in_=qiTd[:, :, j * 256 + hf * 128:j * 256 + hf * 128 + 128]),
                      writes=[qih.r()])
                for h in range(8):
                    eng = "pool" if h % 2 == 0 else "dve"
                    P.op(eng, lambda e, h=h, qi=qi: e.tensor_scalar(out=dg[:, h, :], in0=Kc[:, 0, :], scalar1=wiT[:, qi, h:h + 1], scalar2=None, op0=ALU.mult),
                         reads=[Kc.r(), wiT.r()], writes=[dg.r()])
                for kc in range(nck):
                    pI = nxt(bI, "i")
                    kic = nxt(kics, "k")
                    P.dma(lambda e, kic=kic, kc=kc: e.dma_start(out=kic[:, :], in_=kiTd[:, kc * 512:(kc + 1) * 512]), writes=[kic.r()], q="act")
                    def score(h, kc=kc, kic=kic):
                        ps = nxt(bS, "s")
                        r = nxt(rl, "r")
                        P.op("pe", lambda e, h=h, ps=ps: e.matmul(ps[:, :512], lhsT=qih[:, h, :], rhs=kic[:, :], start=True, stop=True),
                             reads=[qih.r(), kic.r()], writes=[ps.r()])
                        P.op("act", lambda e, ps=ps, r=r: e.activation(out=r[:, :], in_=ps[:, :512], func=AF.Relu), reads=[ps.r()], writes=[r.r()])
                        return r
                    def accum(h, r, pI=pI):
                        P.op("pe", lambda e, h=h, r=r: e.matmul(pI[:, :512], lhsT=dg[:, h, :], rhs=r[:, :], start=(h == 0), stop=(h == 7)),
                             reads=[dg.r(), r.r()], writes=[pI.r()])
                    prev = score(0)
                    for h in range(1, 8):
                        cur = score(h)
                        accum(h - 1, prev)
                        prev = cur
                    accum(7, prev)
                    P.op("dve", lambda e, pI=pI, kc=kc: e.tensor_reduce(out=am[:, kc:kc + 1], in_=pI[:, :512], axis=AX.X, op=ALU.max, apply_absolute_value=True),
                         reads=[pI.r()], writes=[am.r()])
                    k0 = kc * 512
                    if k0 >= ws:
                        ro = k0 - ws
                        P.op("dve", lambda e, ro=ro, qi=qi: e.tensor_scalar(out=tmpb[:, :], in0=kit[:, ro:ro + 512], scalar1=qrt[:, qi:qi + 1], scalar2=-1e30,
                                                                          op0=ALU.is_gt, op1=ALU.mult), reads=[kit.r(), qrt.r()], writes=[tmpb.r()])
                        P.op("dve", lambda e, pI=pI, k0=k0: e.tensor_tensor(out=I[:, k0:k0 + 512], in0=pI[:, :512], in1=tmpb[:, :], op=ALU.add),
                             reads=[pI.r(), tmpb.r()], writes=[I.r()])
                    else:
                        P.op("dve", lambda e, pI=pI, k0=k0: e.tensor_copy(out=I[:, k0:k0 + 512], in_=pI[:, :512]), reads=[pI.r()], writes=[I.r()])
                P.op("dve", lambda e, nck=nck: e.tensor_reduce(out=sm[:, 0:1], in_=am[:, 0:nck], axis=AX.X, op=ALU.max), reads=[am.r()], writes=[sm.r()])
                P.op("dve", lambda e: e.tensor_scalar(out=sm[:, 1:2], in0=sm[:, 0:1], scalar1=2.0, scalar2=None, op0=ALU.mult), reads=[sm.r()], writes=[sm.r()])
                P.op("dve", lambda e: e.tensor_scalar(out=sm[:, 2:3], in0=sm[:, 0:1], scalar1=-1.0, scalar2=None, op0=ALU.mult), reads=[sm.r()], writes=[sm.r()])
                npc = (Nmax + 2047) // 2048
                for itn in range(NIT):
                    P.op("dve", lambda e, itn=itn: e.tensor_scalar(out=sm[:, 3:4], in0=sm[:, 1:2], scalar1=2.0 ** -(itn + 1), scalar2=None, op0=ALU.mult),
                         reads=[sm.r()], writes=[sm.r()])
                    P.op("dve", lambda e: e.tensor_tensor(out=sm[:, 4:5], in0=sm[:, 2:3], in1=sm[:, 3:4], op=ALU.add), reads=[sm.r()], writes=[sm.r()])
                    for pc in range(npc):
                        P.op("dve", lambda e, pc=pc: e.tensor_scalar(out=junk[:, :], in0=I[:, pc * 2048:(pc + 1) * 2048], scalar1=sm[:, 4:5], scalar2=None,
                                                                   op0=ALU.is_ge, op1=ALU.add, accum_out=cn[:, pc:pc + 1]),
                             reads=[I.r(), sm.r()], writes=[junk.r(), cn.r()])
                    P.op("dve", lambda e, npc=npc: e.tensor_reduce(out=sm[:, 5:6], in_=cn[:, 0:npc], axis=AX.X, op=ALU.add), reads=[cn.r()], writes=[sm.r()])
                    P.op("dve", lambda e: e.tensor_scalar(out=sm[:, 6:7], in0=sm[:, 5:6], scalar1=255.5, scalar2=sm[:, 3:4], op0=ALU.is_gt, op1=ALU.mult),
                         reads=[sm.r()], writes=[sm.r()])
                    P.op("dve", lambda e: e.tensor_tensor(out=sm[:, 2:3], in0=sm[:, 2:3], in1=sm[:, 6:7], op=ALU.add), reads=[sm.r()], writes=[sm.r()])
                for pc in range(npc):
                    P.op("dve", lambda e, pc=pc, M=M: e.tensor_scalar(out=M[:, pc * 2048:(pc + 1) * 2048], in0=I[:, pc * 2048:(pc + 1) * 2048],
                                                                     scalar1=sm[:, 2:3], scalar2=-30000.0, op0=ALU.is_lt, op1=ALU.mult),
                         reads=[I.r(), sm.r()], writes=[M.r()])
            for pz in range(2):
                P.dma(lambda e, j=j, pz=pz: e.dma_start(out=qt[:, :, :], in_=qTv[:, 4 * pz:4 * pz + 4, j * 256:(j + 1) * 256]), writes=[qt.r()])
                nkb = Nmax // 128
                LA = 2
                pend = []

                def back(kb, pair, pt, vt, kbl, nkb=nkb):
                    for hh in range(2):
                        hl = 2 * pair + hh
                        P.op("pe", lambda e, pair=pair, hh=hh, hl=hl, vt=vt, kbl=kbl, pt=pt, kb=kb, nkb=nkb: e.matmul(
                            bO[pair][:, hh * 256:(hh + 1) * 256], lhsT=vt[:, kbl, hl * 128:(hl + 1) * 128], rhs=pt[:, hh * 256:(hh + 1) * 256],
                            start=(kb == 0 and hh == 0), stop=(kb == nkb - 1 and hh == 1), skip_group_check=True),
                            reads=[vt.r(), pt.r()], writes=[bO[pair].r()])
                    P.op("pe", lambda e, pair=pair, pt=pt, kb=kb, nkb=nkb: e.matmul(
                        bD[pair][:, 0:512], lhsT=C.ones_bf[:, :], rhs=pt[:, :], start=(kb == 0), stop=(kb == nkb - 1)),
                        reads=[C.ones_bf.r(), pt.r()], writes=[bD[pair].r()])

                for kb in range(nkb):
                    kbl = kb % 4
                    if kbl == 0:
                        kt = nxt(ktl, "l")
                        vt = vtl[rot["l"] % len(vtl)]
                        k4 = kb // 4
                        P.dma(lambda e, kt=kt, k4=k4, pz=pz: e.dma_start(out=kt[:, :, :], in_=kTv[:, 4 * pz:4 * pz + 4, k4 * 512:(k4 + 1) * 512]),
                              writes=[kt.r()])
                        P.dma(lambda e, vt=vt, k4=k4, pz=pz: e.dma_start(out=vt[:, :, :], in_=Vv[:, k4 * 4:(k4 + 1) * 4, pz * 512:(pz + 1) * 512]),
                              writes=[vt.r()], q="act")
                    for pair in range(2):
                        pS = nxt(bS, "s")
                        pt = nxt(pts, "p")
                        for hh in range(2):
                            hl = 2 * pair + hh
                            P.op("pe", lambda e, pS=pS, hh=hh, hl=hl, kt=kt, kbl=kbl: e.matmul(
                                pS[:, hh * 256:(hh + 1) * 256], lhsT=kt[:, hl, kbl * 128:(kbl + 1) * 128], rhs=qt[:, hl, :],
                                start=(hh == 0), stop=False, skip_group_check=True), reads=[kt.r(), qt.r()], writes=[pS.r()])
                        for hf in range(2):
                            P.op("pe", lambda e, pS=pS, hf=hf, kb=kb: e.matmul(
                                pS[:, 0:512], lhsT=Mb[hf][:, kb * 128:(kb + 1) * 128], rhs=selb[:, hf, :],
                                start=False, stop=(hf == 1), skip_group_check=True), reads=[Mb[hf].r(), selb.r()], writes=[pS.r()])
                        P.op("act", lambda e, pS=pS, pt=pt: e.activation(out=pt[:, :], in_=pS[:, 0:512], func=AF.Exp, scale=SCALE),
                             reads=[pS.r()], writes=[pt.r()])
                        pend.append((kb, pair, pt, vt, kbl))
                        if len(pend) > LA:
                            back(*pend.pop(0))
                while pend:
                    back(*pend.pop(0))
                for pair in range(2):
                    o = ob[pair]
                    P.op("dve", lambda e, pair=pair: e.reciprocal(out=rden[:, :], in_=bD[pair][:, 0:512]), reads=[bD[pair].r()], writes=[rden.r()])
                    P.op("dve", lambda e, pair=pair, o=o: e.tensor_tensor(out=o[:, :], in0=bO[pair][:, 0:512], in1=rden[:, :], op=ALU.mult),
                         reads=[bO[pair].r(), rden.r()], writes=[o.r()])
                    h0 = 4 * pz + 2 * pair
                    outs.append(P.dma(lambda e, o=o, h0=h0, j=j: e.dma_start(
                        out=oTv[:, h0:h0 + 2, j * 256:(j + 1) * 256], in_=o[:, :].rearrange("p (h q) -> p h q", h=2)), reads=[o.r()]))
        P.emit(final_waits=outs)
    return nc


def _rope_tables(L, dim, theta=10000.0):
    pos = np.arange(L, dtype=np.float32)
    inv = (theta ** (-np.arange(0, dim, 2, dtype=np.float32) / dim)).astype(np.float32)
    ang = pos[:, None] * inv[None, :]
    return np.cos(ang).astype(np.float32), np.sin(ang).astype(np.float32)


def _mla_inputs(hT, g, w_in, gq, w_uq, gkv, w_ukv, core):
    z64 = np.zeros((1024, 64), np.float32)
    kr = w_in[:, 640:672]
    krs = np.concatenate([kr[:, 16:], kr[:, :16]], 1)
    wall = np.concatenate([w_in[:, :640], z64, kr, z64, krs], 1)
    cols = []
    for h in (2 * core, 2 * core + 1):
        wq = w_uq[:, h * 96:(h + 1) * 96]
        wqs = np.concatenate([np.zeros((384, 64), np.float32), wq[:, 80:96], wq[:, 64:80]], 1)
        cols += [wq, wqs]
    wuq = np.concatenate(cols, 1)
    kn = [w_ukv[:, h * 128:h * 128 + 64] for h in (2 * core, 2 * core + 1)]
    vv = [w_ukv[:, h * 128 + 64:h * 128 + 128] for h in (2 * core, 2 * core + 1)]
    wukv = np.concatenate(kn + vv, 1)
    return dict(hT=hT, g=g, wall=np.ascontiguousarray(wall), gq=gq, gkv=gkv, wuq=np.ascontiguousarray(wuq),
                wukv=np.ascontiguousarray(wukv))


def _mla_consts(L):
    cos, sin = _rope_tables(L, 32)
    cos2 = np.zeros((96, L), np.float32)
    sin2 = np.zeros((96, L), np.float32)
    cos2[64:80] = cos.T
    cos2[80:96] = cos.T
    sin2[64:80] = -sin.T
    sin2[80:96] = sin.T
    k = np.arange(128)[:, None, None]
    d = np.arange(4)[None, :, None]
    q = np.arange(512)[None, None, :]
    cmask = ((d * 128 + k) <= q).astype(np.float32)
    esel = np.zeros((65, 64), np.float32)
    esel[64] = 1.0
    return dict(cos2=cos2, sin2=sin2, cmask=np.ascontiguousarray(cmask), esel=esel)


def _gdn_inputs(hT, g, w_in, conv_w, a_log, dt_bias, og, h):
    cols = [w_in[:, h * 128:(h + 1) * 128], w_in[:, 1024 + h * 128:1024 + (h + 1) * 128],
            w_in[:, 2048 + h * 128:2048 + (h + 1) * 128], w_in[:, 3072 + h * 128:3072 + (h + 1) * 128],
            w_in[:, 4096 + h:4097 + h], w_in[:, 4104 + h:4105 + h], np.zeros((1024, 126), np.float32)]
    wh = np.ascontiguousarray(np.concatenate(cols, 1))
    cw = np.concatenate([conv_w[:, h * 128:(h + 1) * 128], conv_w[:, 1024 + h * 128:1024 + (h + 1) * 128],
                         conv_w[:, 2048 + h * 128:2048 + (h + 1) * 128]], 1)
    sc = np.zeros((128, 2), np.float32)
    sc[:, 0] = a_log[h]
    sc[:, 1] = dt_bias[h]
    return dict(hT=hT, g=g, wh=wh, cw=np.ascontiguousarray(cw.T), sc=sc, og=og)


def _gdn_consts():
    j = np.arange(128)[:, None]
    i = np.arange(128)[None, :]
    same = (j // 64) == (i // 64)
    cst = np.zeros((128, 5, 128), np.float32)
    cst[:, 0] = np.eye(128)
    cst[:, 1] = (same & (i > j))
    cst[:, 2] = (same & (i >= j))
    cst[:, 3] = same
    cst[:, 4, 0] = (np.arange(128) < 64)
    cst[:, 4, 1] = (np.arange(128) >= 64)
    return dict(cst=cst)


def _dsa_inputs(xT, g, w_in, lg, lb, core, L, jset=None):
    jset = list(range(L // 256 // 8)) if jset is None else list(jset)
    NJ = len(jset)
    cols = []
    qrel = np.zeros((128, NJ * 2), np.float32)
    for jj, j in enumerate(jset):
        tq = 8 * j + core
        cols.append(xT[:, tq * 256:(tq + 1) * 256])
        ws = 256 * (8 * j + 8) - 2048
        for hf in range(2):
            qrel[:, jj * 2 + hf] = tq * 256 + hf * 128 + np.arange(128) - ws
    return dict(xT=xT, xq=np.ascontiguousarray(np.concatenate(cols, 1)), g=g, win=w_in, lng=lg, lnb=lb, qrel=qrel)


def _dsa_consts():
    kidx = np.tile(np.arange(2048, dtype=np.float32)[None, :], (128, 1))
    cst = np.zeros((128, 5, 128), np.float32)
    cst[:, 0] = np.eye(128)
    return dict(kidx=kidx, cst=cst)


def _dsa_gather(outs, L, jset=None, full=None):
    jset = list(range(L // 256 // 8)) if jset is None else list(jset)
    if full is None:
        full = np.zeros((1024, L), np.float32)
    for c, o in enumerate(outs):
        for jj, j in enumerate(jset):
            tq = 8 * j + c
            full[:, tq * 256:(tq + 1) * 256] = o[:, jj * 256:(jj + 1) * 256]
    return full


_PROGS = {}
DSA_SPLITS = ([0, 1, 2, 3, 4], [5, 6, 7])
GDN_SEG = 4096


def _prog(name, fn):
    if name not in _PROGS:
        _PROGS[name] = fn()
    return _PROGS[name]


def _run(nc, in_maps):
    res = run_bass_kernel_spmd(nc, in_maps, core_ids=list(range(8)))
    return [r["oT"] for r in res.results]


def _split(hT, TOK=2048):
    return [np.ascontiguousarray(hT[:, i * TOK:(i + 1) * TOK]) for i in range(8)]


def kernel(**inp):
    f32 = lambda a: np.ascontiguousarray(np.asarray(a, dtype=np.float32))
    L = 16384
    TOK = L // 8
    x = f32(inp["x"])[0]
    hT = np.ascontiguousarray(x.T)
    ident = np.eye(128, dtype=np.float32)

    def outproj(hT, mT, w):
        nc = _prog("outproj", lambda: build_outproj(TOK, 512))
        hs, ms = _split(hT), _split(mT)
        return np.concatenate(_run(nc, [dict(hT=hs[i], mT=ms[i], w=w) for i in range(8)]), axis=1)

    def mlp(hT, i, final=False):
        hs = _split(hT)
        g, w1, w2 = f32(inp["norm_mlp_g"][i]), f32(inp["mlp_w1"][i]), f32(inp["mlp_w2"][i])
        if final:
            nc = _prog("mlpf", lambda: build_mlp(TOK, 256, True))
            fg = f32(inp["final_g"])
            maps = [dict(hT=hs[c], g=g, w1=w1, w2=w2, fg=fg) for c in range(8)]
        else:
            nc = _prog("mlp", lambda: build_mlp(TOK, 256, False))
            maps = [dict(hT=hs[c], g=g, w1=w1, w2=w2) for c in range(8)]
        return np.concatenate(_run(nc, maps), axis=1)

    cst = _dsa_consts()
    g0, win0 = f32(inp["norm_mix_g"][0]), f32(inp["dsa_w_in"][0])
    lg, lb = f32(inp["dsa_idx_k_g"][0]), f32(inp["dsa_idx_k_b"][0])
    mT = None
    for js in DSA_SPLITS:
        nc = _prog("dsa" + str(js), lambda: build_dsa(L, 18, js))
        outs = _run(nc, [{**_dsa_inputs(hT, g0, win0, lg, lb, c, L, js), **cst} for c in range(8)])
        mT = _dsa_gather(outs, L, js, mT)
    hT = outproj(hT, mT, f32(inp["dsa_w_out"][0]))
    hT = mlp(hT, 0)
    nc = _prog("conv", lambda: build_conv(TOK, 256))
    hp = np.concatenate([np.zeros((1024, 30), np.float32), hT], axis=1)
    cw = dict(g=f32(inp["norm_mix_g"][1]), w1=f32(inp["conv_w_pw1"][0]), b1=f32(inp["conv_b_pw1"][0]),
              wdT=np.ascontiguousarray(f32(inp["conv_w_dw"][0]).T), bd=f32(inp["conv_b_dw"][0]),
              lg=f32(inp["conv_ln_g"][0]), lb=f32(inp["conv_ln_b"][0]), w2=f32(inp["conv_w_pw2"][0]),
              b2=f32(inp["conv_b_pw2"][0]), ident=ident)
    maps = [dict(hT=np.ascontiguousarray(hp[:, c * TOK:c * TOK + TOK + 30]),
                 hs=np.full((128, 1), 0.0 if c == 0 else 1.0, np.float32), **cw) for c in range(8)]
    hT = np.concatenate(_run(nc, maps), axis=1)
    hT = mlp(hT, 1)
    nc = _prog("mla", lambda: build_mla(L))
    cst = _mla_consts(L)
    maps = [{**_mla_inputs(hT, f32(inp["norm_mix_g"][2]), f32(inp["mla_w_in"][0]), f32(inp["mla_q_norm_g"][0]),
                           f32(inp["mla_w_uq"][0]), f32(inp["mla_kv_norm_g"][0]), f32(inp["mla_w_ukv"][0]), c), **cst}
            for c in range(8)]
    mT = np.concatenate(_run(nc, maps), axis=0)
    hT = outproj(hT, mT, f32(inp["mla_w_out"][0]))
    hT = mlp(hT, 2)
    LG = GDN_SEG
    nc = _prog("gdn", lambda: build_gdn(LG))
    cst = _gdn_consts()
    gi = [_gdn_inputs(None, f32(inp["norm_mix_g"][3]), f32(inp["gdn_w_in"][0]), f32(inp["gdn_conv_w"][0]),
                      f32(inp["gdn_a_log"][0]), f32(inp["gdn_dt_bias"][0]), f32(inp["gdn_o_norm_g"][0]), c) for c in range(8)]
    st = [np.zeros((128, 128), np.float32) for _ in range(8)]
    hl = [np.zeros((128, 3, 3), np.float32) for _ in range(8)]
    segs = []
    for s0 in range(0, L, LG):
        hseg = np.ascontiguousarray(hT[:, s0:s0 + LG])
        maps = [{**gi[c], **cst, "hT": hseg, "s_in": st[c], "h_in": hl[c]} for c in range(8)]
        res = run_bass_kernel_spmd(nc, maps, core_ids=list(range(8))).results
        segs.append(np.concatenate([r["oT"] for r in res], axis=0))
        st = [np.ascontiguousarray(r["s_out"]) for r in res]
        hl = [np.ascontiguousarray(r["h_out"]) for r in res]
    mT = np.concatenate(segs, axis=1)
    hT = outproj(hT, mT, f32(inp["gdn_w_out"][0]))
    hT = mlp(hT, 3, final=True)
    return np.ascontiguousarray(hT.T)[None].astype(np.float32)
```

```python
import numpy as np
import concourse.bass as bass
import concourse.mybir as mybir
from contextlib import ExitStack
from concourse.bass_utils import run_bass_kernel_spmd

F32 = mybir.dt.float32
BF16 = mybir.dt.bfloat16
U8 = mybir.dt.uint8
I32 = mybir.dt.int32
ALU = mybir.AluOpType
AF = mybir.ActivationFunctionType
AX = mybir.AxisListType

ENGS = ["pe", "act", "dve", "pool", "sp"]
NDMA = 8


class Res:
    __slots__ = ("name", "lastw", "readers")

    def __init__(self, name):
        self.name = name
        self.lastw = None
        self.readers = []


class Tile:
    def __init__(self, t, name, nsub=1):
        self.t = t
        self.name = name
        self.res = [Res(f"{name}.{i}") for i in range(nsub)]

    def __getitem__(self, idx):
        return self.t[idx]

    def r(self, i=0):
        return self.res[i]

    def all(self):
        return list(self.res)


class View:
    def __init__(self, base, off, width, name, share=True):
        self.base = base
        self.off = off
        self.width = width
        self.res = base.res if share else [Res(name)]

    def __getitem__(self, idx):
        rows, cols = idx
        start = cols.start or 0
        stop = self.width if cols.stop is None else cols.stop
        return self.base.t[rows, self.off + start:self.off + stop]

    def r(self, i=0):
        return self.res[0]

    def all(self):
        return list(self.res)


class Prog:
    def __init__(self, nc, stack):
        self.nc = nc
        self.stack = stack
        self.ops = {e: [] for e in ENGS}
        self.cnt = {e: 0 for e in ENGS}
        self.sem = {e: stack.enter_context(nc.semaphore(f"s_{e}")) for e in ENGS if e != "sp"}
        self.dsem = {q: [stack.enter_context(nc.semaphore(f"d_{q}{i}")) for i in range(NDMA)]
                     for q in ("sp", "pool", "act")}
        self.dcnt = {q: 0 for q in ("sp", "pool", "act")}
        self.dtok = {q: [] for q in ("sp", "pool", "act")}
        self.known = {e: {} for e in ENGS}
        self.nops = 0

    def sb(self, name, shape, dtype, nsub=1):
        t = self.stack.enter_context(self.nc.sbuf_tensor("sb_" + name, list(shape), dtype))
        return Tile(t, name, nsub)

    def ps(self, name, shape, dtype=F32, nsub=1):
        t = self.stack.enter_context(self.nc.psum_tensor("ps_" + name, list(shape), dtype))
        return Tile(t, name, nsub)

    def push_scope(self):
        self._saved_stack = self.stack
        self.stack = ExitStack()
        return self.stack

    def pop_scope(self):
        self.barrier()
        self.stack.close()
        self.stack = self._saved_stack

    def barrier(self):
        toks = []
        for e in ENGS:
            if e != "sp" and self.cnt[e] > 0:
                toks.append((("c", e), self.cnt[e], e))
        for q in self.dtok:
            toks += self.dtok[q][-NDMA:]
        self._pending = {e: list(toks) for e in ENGS}

    def _deps(self, eng, reads, writes):
        deps = {}
        for tok in getattr(self, "_pending", {}).get(eng, []):
            if not (tok[2] == eng and eng == "pe"):
                if deps.get(tok[0], (0,))[0] < tok[1]:
                    deps[tok[0]] = (tok[1], tok)
        if getattr(self, "_pending", None):
            self._pending[eng] = []
        def add(tok):
            if tok is None:
                return
            key, val, teng = tok
            if teng == eng and eng == "pe":
                return
            if deps.get(key, (0,))[0] < val:
                deps[key] = (val, tok)
        for r in reads:
            add(r.lastw)
        for w in writes:
            add(w.lastw)
            for t in w.readers:
                add(t)
        out = []
        kn = self.known[eng]
        for key, (val, tok) in deps.items():
            if kn.get(key, 0) >= val:
                continue
            kn[key] = val
            out.append(tok)
        return out

    def _commit(self, tok, reads, writes):
        for r in reads:
            r.readers.append(tok)
        for w in writes:
            w.lastw = tok
            w.readers = []

    def op(self, eng, fn, reads=(), writes=()):
        waits = self._deps(eng, reads, writes)
        self.cnt[eng] += 1
        tok = (("c", eng), self.cnt[eng], eng)
        self.ops[eng].append((waits, fn, tok))
        self._commit(tok, reads, writes)
        self.nops += 1
        return tok

    def dma(self, fn, reads=(), writes=(), q="sp"):
        eng = q
        waits = self._deps(eng, reads, writes)
        i = self.dcnt[q]
        self.dcnt[q] += 1
        slot = i % NDMA
        val = 16 * (i // NDMA + 1)
        if i >= NDMA:
            prev = self.dtok[q][i - NDMA]
            key, pval, _ = prev
            if self.known[eng].get(key, 0) < pval:
                self.known[eng][key] = pval
                waits.append(prev)
        tok = (("d", q, slot), val, "dma")
        self.dtok[q].append(tok)
        self.ops[eng].append((waits, fn, tok))
        self._commit(tok, reads, writes)
        self.nops += 1
        return tok

    def _semof(self, tok):
        key = tok[0]
        if key[0] == "c":
            return self.sem[key[1]]
        return self.dsem[key[1]][key[2]]

    def emit(self, final_waits=()):
        nc = self.nc
        with nc.Block() as block:
            def run(eng, e):
                for waits, fn, tok in self.ops[eng]:
                    for w in waits:
                        e.wait_ge(self._semof(w), w[1])
                    ins = fn(e)
                    if tok[2] == "dma":
                        ins.then_inc(self._semof(tok), 16)
                    else:
                        ins.then_inc(self._semof(tok), 1)
                if eng == "sp":
                    for w in final_waits:
                        e.wait_ge(self._semof(w), w[1])

            @block.tensor
            def _(e):
                run("pe", e)

            @block.scalar
            def _(e):
                run("act", e)

            @block.vector
            def _(e):
                run("dve", e)

            @block.gpsimd
            def _(e):
                run("pool", e)

            @block.sync
            def _(e):
                run("sp", e)


def load_w_bf16(P, w_ap, KC, Fo, name, q="pool"):
    t = P.sb(name, [128, KC, Fo], BF16, nsub=KC)
    wv = w_ap.rearrange("(c p) f -> p c f", p=128)
    for c in range(KC):
        P.dma(lambda e, c=c: e.dma_start(out=t[:, c, :], in_=wv[:, c, :], max_dma_last_dim=8192),
              writes=[t.r(c)], q=q)
    return t


def load_vec_fm(P, v_ap, KC, name):
    t = P.sb(name, [128, KC], F32)
    vv = v_ap.rearrange("(c p) -> p c", p=128)
    P.dma(lambda e: e.dma_start(out=t[:, :], in_=vv, allow_slow_non_contiguous=True), writes=[t.r()])
    return t


class Ctx:
    pass


def make_consts(P):
    C = Ctx()
    C.ones_bf = P.sb("ones_bf", [128, 128], BF16)
    P.op("dve", lambda e: e.memset(C.ones_bf[:, :], 1.0), writes=[C.ones_bf.r()])
    C.eps = P.sb("eps_t", [128, 1], F32)
    P.op("dve", lambda e: e.memset(C.eps[:, :], 1e-6), writes=[C.eps.r()])
    return C


def rms_rstd(P, C, x, KC, T, psum, sq, rstd, dim):
    for c in range(KC):
        P.op("act", lambda e, c=c: e.activation(out=sq[:, c, :], in_=x[:, c, :], func=AF.Square),
             reads=[x.r()], writes=[sq.r(c)])
    for c in range(KC):
        P.op("pe", lambda e, c=c: e.matmul(psum[:, :T], lhsT=C.ones_bf[:, :], rhs=sq[:, c, :],
                                          start=(c == 0), stop=(c == KC - 1)),
             reads=[sq.r(c), C.ones_bf.r()], writes=[psum.r()])
    P.op("act", lambda e: e.activation(out=rstd[:, :], in_=psum[:, :T], func=AF.Sqrt,
                                       bias=C.eps[:, :], scale=1.0 / dim),
         reads=[psum.r(), C.eps.r()], writes=[rstd.r()])
    P.op("dve", lambda e: e.reciprocal(out=rstd[:, :], in_=rstd[:, :]), reads=[rstd.r()], writes=[rstd.r()])


def build_mlp(TOK=2048, T=256, final=False):
    nc = bass.Bass("TRN2", target_bir_lowering=False)
    hT = nc.dram_tensor("hT", [1024, TOK], F32, kind="ExternalInput").ap()
    g = nc.dram_tensor("g", [1024], F32, kind="ExternalInput").ap()
    w1 = nc.dram_tensor("w1", [1024, 4096], F32, kind="ExternalInput").ap()
    w2 = nc.dram_tensor("w2", [4096, 1024], F32, kind="ExternalInput").ap()
    oT = nc.dram_tensor("oT", [1024, TOK], F32, kind="ExternalOutput").ap()
    with ExitStack() as st:
        st.enter_context(nc.allow_low_precision("bf16 matmul operands, fp32 accumulate"))
        P = Prog(nc, st)
        C = make_consts(P)
        gt = load_vec_fm(P, g, 8, "g")
        W1 = load_w_bf16(P, w1, 8, 4096, "W1")
        W2 = load_w_bf16(P, w2, 32, 1024, "W2")
        fgt = None
        if final:
            fg = nc.dram_tensor("fg", [1024], F32, kind="ExternalInput").ap()
            fgt = load_vec_fm(P, fg, 8, "fg")
        mlp_body(P, C, hT, oT, gt, W1, W2, TOK, T, fgt)
        P.emit(final_waits=P.out_toks)
    return nc


def mlp_body(P, C, hT, oT, gt, W1, W2, TOK, T, fgt=None):
    hv = hT.rearrange("(c p) t -> p c t", p=128)
    ov = oT.rearrange("(c p) t -> p c t", p=128)
    NB = 2
    xs = [P.sb(f"x{i}", [128, 8, T], F32) for i in range(NB)]
    sqs = [P.sb(f"sq{i}", [128, 8, T], BF16, nsub=8) for i in range(NB)]
    hns = [P.sb(f"hn{i}", [128, 8, T], BF16, nsub=8) for i in range(NB)]
    rstds = [P.sb(f"rstd{i}", [128, T], F32) for i in range(NB)]
    aT = P.sb("aT", [128, 32, T], BF16, nsub=32)
    rl = [P.sb(f"rl{i}", [128, T], BF16) for i in range(2)]
    ys = [P.sb(f"y{i}", [128, 8, T], F32, nsub=8) for i in range(NB)]
    pss = [P.ps(f"ps{i}", [128, 512], F32) for i in range(8)]
    P.out_toks = []
    pi = 0
    for it in range(TOK // T):
        b = it % NB
        x, sq, hn, rstd, y = xs[b], sqs[b], hns[b], rstds[b], ys[b]
        t0 = it * T
        P.dma(lambda e, x=x, t0=t0: e.dma_start(out=x[:, :, :], in_=hv[:, :, t0:t0 + T]), writes=[x.r()])
        ps = pss[pi % 8]; pi += 1
        rms_rstd(P, C, x, 8, T, ps, sq, rstd, 1024.0)
        for c in range(8):
            P.op("dve", lambda e, c=c, x=x, hn=hn, rstd=rstd: e.scalar_tensor_tensor(
                out=hn[:, c, :], in0=x[:, c, :], scalar=gt[:, c:c + 1], in1=rstd[:, :],
                op0=ALU.mult, op1=ALU.mult), reads=[x.r(), rstd.r(), gt.r()], writes=[hn.r(c)])
        for f in range(32):
            ps = pss[pi % 8]; pi += 1
            for c in range(8):
                P.op("pe", lambda e, c=c, f=f, ps=ps, hn=hn: e.matmul(
                    ps[:, :T], lhsT=W1[:, c, f * 128:(f + 1) * 128], rhs=hn[:, c, :],
                    start=(c == 0), stop=(c == 7)), reads=[W1.r(c), hn.r(c)], writes=[ps.r()])
            r = rl[f % 2]
            P.op("act", lambda e, ps=ps, r=r: e.activation(out=r[:, :], in_=ps[:, :T], func=AF.Relu),
                 reads=[ps.r()], writes=[r.r()])
            eng = "pool" if f % 2 == 0 else "dve"
            P.op(eng, lambda e, f=f, r=r: e.tensor_tensor(out=aT[:, f, :], in0=r[:, :], in1=r[:, :], op=ALU.mult),
                 reads=[r.r()], writes=[aT.r(f)])
        for o in range(8):
            ps = pss[pi % 8]; pi += 1
            for f in range(32):
                P.op("pe", lambda e, o=o, f=f, ps=ps: e.matmul(
                    ps[:, :T], lhsT=W2[:, f, o * 128:(o + 1) * 128], rhs=aT[:, f, :],
                    start=(f == 0), stop=(f == 31)), reads=[W2.r(f), aT.r(f)], writes=[ps.r()])
            P.op("dve", lambda e, o=o, ps=ps, x=x, y=y: e.tensor_tensor(
                out=y[:, o, :], in0=ps[:, :T], in1=x[:, o, :], op=ALU.add),
                reads=[ps.r(), x.r()], writes=[y.r(o)])
        if fgt is not None:
            ps = pss[pi % 8]; pi += 1
            for c in range(8):
                P.op("act", lambda e, c=c, y=y, sq=sq: e.activation(out=sq[:, c, :], in_=y[:, c, :], func=AF.Square),
                     reads=[y.r(c)], writes=[sq.r(c)])
            for c in range(8):
                P.op("pe", lambda e, c=c, ps=ps, sq=sq: e.matmul(ps[:, :T], lhsT=C.ones_bf[:, :], rhs=sq[:, c, :],
                                                              start=(c == 0), stop=(c == 7)),
                     reads=[sq.r(c), C.ones_bf.r()], writes=[ps.r()])
            P.op("act", lambda e, ps=ps, rstd=rstd: e.activation(out=rstd[:, :], in_=ps[:, :T], func=AF.Sqrt,
                                                               bias=C.eps[:, :], scale=1.0 / 1024),
                 reads=[ps.r(), C.eps.r()], writes=[rstd.r()])
            P.op("dve", lambda e, rstd=rstd: e.reciprocal(out=rstd[:, :], in_=rstd[:, :]), reads=[rstd.r()], writes=[rstd.r()])
            for c in range(8):
                P.op("dve", lambda e, c=c, y=y, rstd=rstd: e.scalar_tensor_tensor(
                    out=y[:, c, :], in0=y[:, c, :], scalar=fgt[:, c:c + 1], in1=rstd[:, :], op0=ALU.mult, op1=ALU.mult),
                    reads=[y.r(c), rstd.r(), fgt.r()], writes=[y.r(c)])
        tok = P.dma(lambda e, y=y, t0=t0: e.dma_start(out=ov[:, :, t0:t0 + T], in_=y[:, :, :]),
                    reads=y.all())
        P.out_toks.append(tok)


def next_ps(P):
    i = getattr(P, "_psi", 0)
    P._psi = i + 1
    return P.pss[i % len(P.pss)]


def alloc_ps(P, n=8):
    P.pss = [P.ps(f"ps{i}", [128, 512], F32) for i in range(n)]
    P._psi = 0


def load_const(P, ap, shape, name, dtype=F32, q="sp"):
    t = P.sb(name, shape, dtype)
    P.dma(lambda e: e.dma_start(out=t[tuple(slice(None) for _ in shape)], in_=ap), writes=[t.r()], q=q)
    return t


def build_outproj(TOK=2048, T=512):
    nc = bass.Bass("TRN2", target_bir_lowering=False)
    hT = nc.dram_tensor("hT", [1024, TOK], F32, kind="ExternalInput").ap()
    mT = nc.dram_tensor("mT", [1024, TOK], F32, kind="ExternalInput").ap()
    w = nc.dram_tensor("w", [1024, 1024], F32, kind="ExternalInput").ap()
    oT = nc.dram_tensor("oT", [1024, TOK], F32, kind="ExternalOutput").ap()
    hv = hT.rearrange("(c p) t -> p c t", p=128)
    mv = mT.rearrange("(c p) t -> p c t", p=128)
    ov = oT.rearrange("(c p) t -> p c t", p=128)
    with ExitStack() as st:
        st.enter_context(nc.allow_low_precision("bf16 matmul operands, fp32 accumulate"))
        P = Prog(nc, st)
        alloc_ps(P)
        W = load_w_bf16(P, w, 8, 1024, "W")
        NB = 2
        xs = [P.sb(f"x{i}", [128, 8, T], F32) for i in range(NB)]
        ms = [P.sb(f"m{i}", [128, 8, T], F32) for i in range(NB)]
        mb = [P.sb(f"mb{i}", [128, 8, T], BF16, nsub=8) for i in range(NB)]
        ys = [P.sb(f"y{i}", [128, 8, T], F32, nsub=8) for i in range(NB)]
        outs = []
        for it in range(TOK // T):
            b = it % NB
            x, m, mbb, y = xs[b], ms[b], mb[b], ys[b]
            t0 = it * T
            P.dma(lambda e, x=x, t0=t0: e.dma_start(out=x[:, :, :], in_=hv[:, :, t0:t0 + T]), writes=[x.r()])
            P.dma(lambda e, m=m, t0=t0: e.dma_start(out=m[:, :, :], in_=mv[:, :, t0:t0 + T]), writes=[m.r()], q="act")
            for c in range(8):
                eng = "act" if c % 2 == 0 else "pool"
                if eng == "act":
                    P.op("act", lambda e, c=c, m=m, mbb=mbb: e.copy(out=mbb[:, c, :], in_=m[:, c, :]),
                         reads=[m.r()], writes=[mbb.r(c)])
                else:
                    P.op("pool", lambda e, c=c, m=m, mbb=mbb: e.tensor_copy(out=mbb[:, c, :], in_=m[:, c, :]),
                         reads=[m.r()], writes=[mbb.r(c)])
            for o in range(8):
                ps = next_ps(P)
                for c in range(8):
                    P.op("pe", lambda e, o=o, c=c, ps=ps, mbb=mbb: e.matmul(
                        ps[:, :T], lhsT=W[:, c, o * 128:(o + 1) * 128], rhs=mbb[:, c, :],
                        start=(c == 0), stop=(c == 7)), reads=[W.r(c), mbb.r(c)], writes=[ps.r()])
                P.op("dve", lambda e, o=o, ps=ps, x=x, y=y: e.tensor_tensor(
                    out=y[:, o, :], in0=ps[:, :T], in1=x[:, o, :], op=ALU.add),
                    reads=[ps.r(), x.r()], writes=[y.r(o)])
            outs.append(P.dma(lambda e, y=y, t0=t0: e.dma_start(out=ov[:, :, t0:t0 + T], in_=y[:, :, :]),
                              reads=y.all()))
        P.emit(final_waits=outs)
    return nc


def build_conv(TOK=2048, T=256):
    HALO = 30
    NT = TOK + HALO
    nc = bass.Bass("TRN2", target_bir_lowering=False)
    hT = nc.dram_tensor("hT", [1024, NT], F32, kind="ExternalInput").ap()
    g = nc.dram_tensor("g", [1024], F32, kind="ExternalInput").ap()
    w1 = nc.dram_tensor("w1", [1024, 2048], F32, kind="ExternalInput").ap()
    b1 = nc.dram_tensor("b1", [2048], F32, kind="ExternalInput").ap()
    wdT = nc.dram_tensor("wdT", [1024, 31], F32, kind="ExternalInput").ap()
    bd = nc.dram_tensor("bd", [1024], F32, kind="ExternalInput").ap()
    lg = nc.dram_tensor("lg", [1024], F32, kind="ExternalInput").ap()
    lb = nc.dram_tensor("lb", [1024], F32, kind="ExternalInput").ap()
    w2 = nc.dram_tensor("w2", [1024, 1024], F32, kind="ExternalInput").ap()
    b2 = nc.dram_tensor("b2", [1024], F32, kind="ExternalInput").ap()
    hs = nc.dram_tensor("hs", [128, 1], F32, kind="ExternalInput").ap()
    ident = nc.dram_tensor("ident", [128, 128], F32, kind="ExternalInput").ap()
    oT = nc.dram_tensor("oT", [1024, TOK], F32, kind="ExternalOutput").ap()
    hv = hT.rearrange("(c p) t -> p c t", p=128)
    ov = oT.rearrange("(c p) t -> p c t", p=128)
    with ExitStack() as st:
        st.enter_context(nc.allow_low_precision("bf16 matmul operands, fp32 accumulate"))
        P = Prog(nc, st)
        alloc_ps(P)
        C = make_consts(P)
        ones_f = P.sb("ones_f", [128, 128], F32)
        P.op("dve", lambda e: e.memset(ones_f[:, :], 1.0), writes=[ones_f.r()])
        gt = load_vec_fm(P, g, 8, "g")
        b1t = load_vec_fm(P, b1, 16, "b1")
        bdt = load_vec_fm(P, bd, 8, "bd")
        lgt = load_vec_fm(P, lg, 8, "lg")
        lbt = load_vec_fm(P, lb, 8, "lb")
        b2t = load_vec_fm(P, b2, 8, "b2")
        hst = load_const(P, hs, [128, 1], "hs")
        idf = load_const(P, ident, [128, 128], "idf")
        wd = P.sb("wd", [128, 8, 31], F32)
        P.dma(lambda e: e.dma_start(out=wd[:, :, :], in_=wdT.rearrange("(c p) j -> p c j", p=128)), writes=[wd.r()])
        W1 = load_w_bf16(P, w1, 8, 2048, "W1")
        W2 = load_w_bf16(P, w2, 8, 1024, "W2")
        diag = P.sb("diag", [128, 8 * 31, 128], BF16, nsub=8)
        for c in range(8):
            for j in range(31):
                eng = "pool" if (j % 2 == 0) else "dve"
                P.op(eng, lambda e, c=c, j=j: e.tensor_scalar(
                    out=diag[:, c * 31 + j, :], in0=idf[:, :], scalar1=wd[:, c, j:j + 1], scalar2=None,
                    op0=ALU.mult), reads=[idf.r(), wd.r()], writes=[diag.r(c)])
        uT = P.sb("uT", [128, 8, NT], BF16, nsub=8)
        NB = 2
        xs = [P.sb(f"x{i}", [128, 8, T], F32) for i in range(NB)]
        sqs = [P.sb(f"sq{i}", [128, 8, T], BF16, nsub=8) for i in range(1)] * 2
        hns = [P.sb(f"hn{i}", [128, 8, T], BF16, nsub=8) for i in range(1)] * 2
        rstds = [P.sb(f"rstd{i}", [128, T], F32) for i in range(1)] * 2
        sig = [P.sb(f"sig{i}", [128, T], F32) for i in range(2)]
        segs = [(0, HALO)] + [(HALO + k * T, T) for k in range(TOK // T)]
        for it, (s0, L) in enumerate(segs):
            b = it % NB
            x, sq, hn, rstd = xs[b], sqs[b], hns[b], rstds[b]
            P.dma(lambda e, x=x, s0=s0, L=L: e.dma_start(out=x[:, :, :L], in_=hv[:, :, s0:s0 + L]), writes=[x.r()])
            ps = next_ps(P)
            for c in range(8):
                P.op("act", lambda e, c=c, x=x, sq=sq, L=L: e.activation(out=sq[:, c, :L], in_=x[:, c, :L], func=AF.Square),
                     reads=[x.r()], writes=[sq.r(c)])
            for c in range(8):
                P.op("pe", lambda e, c=c, ps=ps, sq=sq, L=L: e.matmul(ps[:, :L], lhsT=C.ones_bf[:, :], rhs=sq[:, c, :L],
                                                                  start=(c == 0), stop=(c == 7)),
                     reads=[sq.r(c), C.ones_bf.r()], writes=[ps.r()])
            P.op("act", lambda e, ps=ps, rstd=rstd, L=L: e.activation(out=rstd[:, :L], in_=ps[:, :L], func=AF.Sqrt,
                                                               bias=C.eps[:, :], scale=1.0 / 1024),
                 reads=[ps.r(), C.eps.r()], writes=[rstd.r()])
            P.op("dve", lambda e, rstd=rstd, L=L: e.reciprocal(out=rstd[:, :L], in_=rstd[:, :L]),
                 reads=[rstd.r()], writes=[rstd.r()])
            for c in range(8):
                P.op("dve", lambda e, c=c, x=x, hn=hn, rstd=rstd, L=L: e.scalar_tensor_tensor(
                    out=hn[:, c, :L], in0=x[:, c, :L], scalar=gt[:, c:c + 1], in1=rstd[:, :L],
                    op0=ALU.mult, op1=ALU.mult), reads=[x.r(), rstd.r(), gt.r()], writes=[hn.r(c)])
            for j in range(8):
                psa = next_ps(P)
                psg = next_ps(P)
                for c in range(8):
                    P.op("pe", lambda e, c=c, j=j, psa=psa, hn=hn, L=L: e.matmul(
                        psa[:, :L], lhsT=W1[:, c, j * 128:(j + 1) * 128], rhs=hn[:, c, :L],
                        start=(c == 0), stop=(c == 7)), reads=[W1.r(c), hn.r(c)], writes=[psa.r()])
                for c in range(8):
                    P.op("pe", lambda e, c=c, j=j, psg=psg, hn=hn, L=L: e.matmul(
                        psg[:, :L], lhsT=W1[:, c, 1024 + j * 128:1024 + (j + 1) * 128], rhs=hn[:, c, :L],
                        start=(c == 0), stop=(c == 7)), reads=[W1.r(c), hn.r(c)], writes=[psg.r()])
                sg = sig[j % 2]
                P.op("act", lambda e, j=j, psg=psg, sg=sg, L=L: e.activation(
                    out=sg[:, :L], in_=psg[:, :L], func=AF.Sigmoid, bias=b1t[:, 8 + j:9 + j], scale=1.0),
                    reads=[psg.r(), b1t.r()], writes=[sg.r()])
                P.op("dve", lambda e, j=j, psa=psa, sg=sg, s0=s0, L=L: e.scalar_tensor_tensor(
                    out=uT[:, j, s0:s0 + L], in0=psa[:, :L], scalar=b1t[:, j:j + 1], in1=sg[:, :L],
                    op0=ALU.add, op1=ALU.mult), reads=[psa.r(), sg.r(), b1t.r()], writes=[uT.r(j)])
            if it == 0:
                for j in range(8):
                    P.op("dve", lambda e, j=j: e.tensor_scalar(
                        out=uT[:, j, 0:HALO], in0=uT[:, j, 0:HALO], scalar1=hst[:, 0:1], scalar2=None, op0=ALU.mult),
                        reads=[uT.r(j), hst.r()], writes=[uT.r(j)])
        vs = [P.sb(f"v{i}", [128, 8, T], F32, nsub=8) for i in range(1)] * 2
        zs = [P.sb(f"z{i}", [128, 8, T], BF16, nsub=8) for i in range(1)] * 2
        ys = [P.sb(f"y{i}", [128, 8, T], F32, nsub=8) for i in range(1)] * 2
        v2 = P.sb("v2", [128, 8, T], F32, nsub=8)
        mean = P.sb("mean", [128, T], F32)
        msq = P.sb("msq", [128, T], F32)
        lrstd = P.sb("lrstd", [128, T], F32)
        dd = [P.sb(f"dd{i}", [128, T], F32) for i in range(2)]
        outs = []
        for it in range(TOK // T):
            b = it % NB
            x, v, z, y = xs[b], vs[b], zs[b], ys[b]
            tl = it * T
            P.dma(lambda e, x=x, tl=tl: e.dma_start(out=x[:, :, :], in_=hv[:, :, HALO + tl:HALO + tl + T]), writes=[x.r()])
            for c in range(8):
                ps = next_ps(P)
                for j in range(31):
                    P.op("pe", lambda e, c=c, j=j, ps=ps, tl=tl: e.matmul(
                        ps[:, :T], lhsT=diag[:, c * 31 + j, :], rhs=uT[:, c, tl + j:tl + j + T],
                        start=(j == 0), stop=(j == 30)), reads=[diag.r(c), uT.r(c)], writes=[ps.r()])
                P.op("act", lambda e, c=c, ps=ps, v=v: e.activation(
                    out=v[:, c, :], in_=ps[:, :T], func=AF.Identity, bias=bdt[:, c:c + 1], scale=1.0),
                    reads=[ps.r(), bdt.r()], writes=[v.r(c)])
                P.op("pool", lambda e, c=c, v=v: e.tensor_tensor(out=v2[:, c, :], in0=v[:, c, :], in1=v[:, c, :], op=ALU.mult),
                     reads=[v.r(c)], writes=[v2.r(c)])
            ps1 = next_ps(P)
            ps2 = next_ps(P)
            for c in range(8):
                P.op("pe", lambda e, c=c, ps1=ps1, v=v: e.matmul(ps1[:, :T], lhsT=ones_f[:, :], rhs=v[:, c, :],
                                                              start=(c == 0), stop=(c == 7)),
                     reads=[ones_f.r(), v.r(c)], writes=[ps1.r()])
            for c in range(8):
                P.op("pe", lambda e, c=c, ps2=ps2: e.matmul(ps2[:, :T], lhsT=ones_f[:, :], rhs=v2[:, c, :],
                                                         start=(c == 0), stop=(c == 7)),
                     reads=[ones_f.r(), v2.r(c)], writes=[ps2.r()])
            P.op("act", lambda e, ps1=ps1: e.activation(out=mean[:, :], in_=ps1[:, :T], func=AF.Copy, scale=1.0 / 1024),
                 reads=[ps1.r()], writes=[mean.r()])
            P.op("dve", lambda e: e.tensor_tensor(out=msq[:, :], in0=mean[:, :], in1=mean[:, :], op=ALU.mult),
                 reads=[mean.r()], writes=[msq.r()])
            P.op("dve", lambda e, ps2=ps2: e.scalar_tensor_tensor(
                out=lrstd[:, :], in0=ps2[:, :T], scalar=1.0 / 1024, in1=msq[:, :], op0=ALU.mult, op1=ALU.subtract),
                reads=[ps2.r(), msq.r()], writes=[lrstd.r()])
            P.op("act", lambda e: e.activation(out=lrstd[:, :], in_=lrstd[:, :], func=AF.Sqrt, bias=C.eps[:, :], scale=1.0),
                 reads=[lrstd.r(), C.eps.r()], writes=[lrstd.r()])
            P.op("dve", lambda e: e.reciprocal(out=lrstd[:, :], in_=lrstd[:, :]), reads=[lrstd.r()], writes=[lrstd.r()])
            for c in range(8):
                d = dd[c % 2]
                P.op("pool", lambda e, c=c, d=d, v=v: e.tensor_tensor(out=d[:, :], in0=v[:, c, :], in1=mean[:, :], op=ALU.subtract),
                     reads=[v.r(c), mean.r()], writes=[d.r()])
                P.op("dve", lambda e, c=c, d=d: e.scalar_tensor_tensor(
                    out=d[:, :], in0=d[:, :], scalar=lgt[:, c:c + 1], in1=lrstd[:, :], op0=ALU.mult, op1=ALU.mult),
                    reads=[d.r(), lrstd.r(), lgt.r()], writes=[d.r()])
                P.op("act", lambda e, c=c, d=d, z=z: e.activation(
                    out=z[:, c, :], in_=d[:, :], func=AF.Silu, bias=lbt[:, c:c + 1], scale=1.0),
                    reads=[d.r(), lbt.r()], writes=[z.r(c)])
            for o in range(8):
                ps = next_ps(P)
                for c in range(8):
                    P.op("pe", lambda e, o=o, c=c, ps=ps, z=z: e.matmul(
                        ps[:, :T], lhsT=W2[:, c, o * 128:(o + 1) * 128], rhs=z[:, c, :],
                        start=(c == 0), stop=(c == 7)), reads=[W2.r(c), z.r(c)], writes=[ps.r()])
                P.op("dve", lambda e, o=o, ps=ps, x=x, y=y: e.scalar_tensor_tensor(
                    out=y[:, o, :], in0=ps[:, :T], scalar=b2t[:, o:o + 1], in1=x[:, o, :], op0=ALU.add, op1=ALU.add),
                    reads=[ps.r(), x.r(), b2t.r()], writes=[y.r(o)])
            outs.append(P.dma(lambda e, y=y, tl=tl: e.dma_start(out=ov[:, :, tl:tl + T], in_=y[:, :, :]),
                              reads=y.all()))
        P.emit(final_waits=outs)
    return nc


def norm_tile(P, C, x, gt, sq, hn, rstd, L, dim=1024.0, KC=8):
    ps = next_ps(P)
    for c in range(KC):
        P.op("act", lambda e, c=c: e.activation(out=sq[:, c, :L], in_=x[:, c, :L], func=AF.Square),
             reads=[x.r()], writes=[sq.r(c)])
    for c in range(KC):
        P.op("pe", lambda e, c=c: e.matmul(ps[:, :L], lhsT=C.ones_bf[:, :], rhs=sq[:, c, :L],
                                          start=(c == 0), stop=(c == KC - 1)),
             reads=[sq.r(c), C.ones_bf.r()], writes=[ps.r()])
    P.op("act", lambda e: e.activation(out=rstd[:, :L], in_=ps[:, :L], func=AF.Sqrt,
                                       bias=C.eps[:, :], scale=1.0 / dim),
         reads=[ps.r(), C.eps.r()], writes=[rstd.r()])
    P.op("dve", lambda e: e.reciprocal(out=rstd[:, :L], in_=rstd[:, :L]), reads=[rstd.r()], writes=[rstd.r()])
    for c in range(KC):
        P.op("dve", lambda e, c=c: e.scalar_tensor_tensor(
            out=hn[:, c, :L], in0=x[:, c, :L], scalar=gt[:, c:c + 1], in1=rstd[:, :L],
            op0=ALU.mult, op1=ALU.mult), reads=[x.r(), rstd.r(), gt.r()], writes=[hn.r(c)])


def build_mla(L=16384):
    T = 512
    NTI = L // T
    NKB = L // 128
    SCALE = 96.0 ** -0.5
    nc = bass.Bass("TRN2", target_bir_lowering=False)
    hT = nc.dram_tensor("hT", [1024, L], F32, kind="ExternalInput").ap()
    g = nc.dram_tensor("g", [1024], F32, kind="ExternalInput").ap()
    wall = nc.dram_tensor("wall", [1024, 832], F32, kind="ExternalInput").ap()
    gq = nc.dram_tensor("gq", [384], F32, kind="ExternalInput").ap()
    gkv = nc.dram_tensor("gkv", [256], F32, kind="ExternalInput").ap()
    wuq = nc.dram_tensor("wuq", [384, 384], F32, kind="ExternalInput").ap()
    wukv = nc.dram_tensor("wukv", [256, 256], F32, kind="ExternalInput").ap()
    cos2 = nc.dram_tensor("cos2", [96, L], F32, kind="ExternalInput").ap()
    sin2 = nc.dram_tensor("sin2", [96, L], F32, kind="ExternalInput").ap()
    cmask = nc.dram_tensor("cmask", [128, 4, 512], F32, kind="ExternalInput").ap()
    esel = nc.dram_tensor("esel", [65, 64], F32, kind="ExternalInput").ap()
    oT = nc.dram_tensor("oT", [128, L], F32, kind="ExternalOutput").ap()
    qTd = nc.dram_tensor("qTd", [2, 96, L], BF16, kind="Internal").ap()
    hv = hT.rearrange("(c p) t -> p c t", p=128)
    with ExitStack() as st:
        st.enter_context(nc.allow_low_precision("bf16 matmul operands, fp32 accumulate"))
        P = Prog(nc, st)
        alloc_ps(P, 6)
        C = make_consts(P)
        gt = load_vec_fm(P, g, 8, "g")
        gqt = load_vec_fm(P, gq, 3, "gq")
        gkvt = load_vec_fm(P, gkv, 2, "gkv")
        Wall = load_w_bf16(P, wall, 8, 832, "Wall")
        Wuq = load_w_bf16(P, wuq, 3, 384, "Wuq")
        Wukv = load_w_bf16(P, wukv, 2, 256, "Wukv")
        cm_f = load_const(P, cmask, [128, 4, 512], "cm_f")
        cm = P.sb("cm", [128, 4, 512], BF16)
        P.op("dve", lambda e: e.tensor_copy(out=cm[:, :, :], in_=cm_f[:, :, :]), reads=[cm_f.r()], writes=[cm.r()])
        es = P.sb("es", [128, 64], F32)
        P.op("dve", lambda e: e.memset(es[:, :], 0.0), writes=[es.r()])
        P.dma(lambda e: e.dma_start(out=es[0:65, :], in_=esel), reads=[es.r()], writes=[es.r()])
        kT = [P.sb(f"kT{h}", [96, L], BF16, nsub=NTI) for h in range(2)]
        qres = [[Res(f"qTd{h}_{i}") for i in range(NTI)] for h in range(2)]
        Va = P.sb("Va", [128, NKB, 2, 65], BF16, nsub=NTI)
        P.op("pool", lambda e: e.memset(Va[:, :, :, :], 1.0), writes=Va.all())
        x = P.sb("x", [128, 8, T], F32)
        sq = P.sb("sq", [128, 8, T], BF16, nsub=8)
        hn = P.sb("hn", [128, 8, T], BF16, nsub=8)
        rstd = P.sb("rstd", [128, T], F32)
        cq = P.sb("cq", [128, 5, T], F32, nsub=5)
        cqs = P.sb("cqs", [128, 5, T], BF16, nsub=5)
        cqn = P.sb("cqn", [128, 5, T], BF16, nsub=5)
        rq = P.sb("rq", [128, T], F32)
        rkv = P.sb("rkv", [128, T], F32)
        cs = P.sb("cs", [96, 2, T], F32)
        t1 = P.sb("t1", [96, T], F32)
        t2 = P.sb("t2", [96, T], F32)
        qt = [P.sb(f"qt{h}", [96, T], BF16) for h in range(2)]
        for it in range(NTI):
            t0 = it * T
            P.dma(lambda e, t0=t0: e.dma_start(out=x[:, :, :], in_=hv[:, :, t0:t0 + T]), writes=[x.r()])
            P.dma(lambda e, t0=t0: e.dma_start(out=cs[:, 0, :], in_=cos2[:, t0:t0 + T]), writes=[cs.r()], q="act")
            P.dma(lambda e, t0=t0: e.dma_start(out=cs[:, 1, :], in_=sin2[:, t0:t0 + T]), writes=[cs.r()], q="act")
            norm_tile(P, C, x, gt, sq, hn, rstd, T)
            for j in range(5):
                ps = next_ps(P)
                for c in range(8):
                    P.op("pe", lambda e, c=c, j=j, ps=ps: e.matmul(
                        ps[:, :T], lhsT=Wall[:, c, j * 128:(j + 1) * 128], rhs=hn[:, c, :],
                        start=(c == 0), stop=(c == 7)), reads=[Wall.r(c), hn.r(c)], writes=[ps.r()])
                P.op("act", lambda e, j=j, ps=ps: e.copy(out=cq[:, j, :], in_=ps[:, :T]), reads=[ps.r()], writes=[cq.r(j)])
                P.op("pool", lambda e, j=j: e.tensor_tensor(out=cqs[:, j, :], in0=cq[:, j, :], in1=cq[:, j, :], op=ALU.mult),
                     reads=[cq.r(j)], writes=[cqs.r(j)])
            for (lo, hi, rr, dim, gg) in ((0, 3, rq, 384.0, gqt), (3, 5, rkv, 256.0, gkvt)):
                ps = next_ps(P)
                for j in range(lo, hi):
                    P.op("pe", lambda e, j=j, ps=ps, lo=lo, hi=hi: e.matmul(
                        ps[:, :T], lhsT=C.ones_bf[:, :], rhs=cqs[:, j, :], start=(j == lo), stop=(j == hi - 1)),
                        reads=[cqs.r(j), C.ones_bf.r()], writes=[ps.r()])
                P.op("act", lambda e, ps=ps, rr=rr, dim=dim: e.activation(
                    out=rr[:, :], in_=ps[:, :T], func=AF.Sqrt, bias=C.eps[:, :], scale=1.0 / dim),
                    reads=[ps.r(), C.eps.r()], writes=[rr.r()])
                P.op("dve", lambda e, rr=rr: e.reciprocal(out=rr[:, :], in_=rr[:, :]), reads=[rr.r()], writes=[rr.r()])
                for j in range(lo, hi):
                    P.op("dve", lambda e, j=j, rr=rr, gg=gg, lo=lo: e.scalar_tensor_tensor(
                        out=cqn[:, j, :], in0=cq[:, j, :], scalar=gg[:, j - lo:j - lo + 1], in1=rr[:, :],
                        op0=ALU.mult, op1=ALU.mult), reads=[cq.r(j), rr.r(), gg.r()], writes=[cqn.r(j)])
            pk = next_ps(P)
            pks = next_ps(P)
            for c in range(8):
                P.op("pe", lambda e, c=c, pk=pk: e.matmul(pk[:96, :T], lhsT=Wall[:, c, 640:736], rhs=hn[:, c, :],
                                                       start=(c == 0), stop=(c == 7)),
                     reads=[Wall.r(c), hn.r(c)], writes=[pk.r()])
            for c in range(8):
                P.op("pe", lambda e, c=c, pks=pks: e.matmul(pks[:96, :T], lhsT=Wall[:, c, 736:832], rhs=hn[:, c, :],
                                                         start=(c == 0), stop=(c == 7)),
                     reads=[Wall.r(c), hn.r(c)], writes=[pks.r()])
            P.op("dve", lambda e, pk=pk: e.tensor_tensor(out=t1[64:96, :], in0=pk[64:96, :T], in1=cs[64:96, 0, :], op=ALU.mult),
                 reads=[pk.r(), cs.r()], writes=[t1.r()])
            P.op("dve", lambda e, pks=pks: e.tensor_tensor(out=t2[64:96, :], in0=pks[64:96, :T], in1=cs[64:96, 1, :], op=ALU.mult),
                 reads=[pks.r(), cs.r()], writes=[t2.r()])
            for h in range(2):
                P.op("pool", lambda e, h=h, t0=t0: e.tensor_tensor(out=kT[h][64:96, t0:t0 + T], in0=t1[64:96, :], in1=t2[64:96, :], op=ALU.add),
                     reads=[t1.r(), t2.r()], writes=[kT[h].r(it)])
            for h in range(2):
                pq = next_ps(P)
                pqs = next_ps(P)
                for c in range(3):
                    P.op("pe", lambda e, c=c, h=h, pq=pq: e.matmul(
                        pq[:96, :T], lhsT=Wuq[:, c, h * 192:h * 192 + 96], rhs=cqn[:, c, :],
                        start=(c == 0), stop=(c == 2)), reads=[Wuq.r(c), cqn.r(c)], writes=[pq.r()])
                for c in range(3):
                    P.op("pe", lambda e, c=c, h=h, pqs=pqs: e.matmul(
                        pqs[:96, :T], lhsT=Wuq[:, c, h * 192 + 96:h * 192 + 192], rhs=cqn[:, c, :],
                        start=(c == 0), stop=(c == 2)), reads=[Wuq.r(c), cqn.r(c)], writes=[pqs.r()])
                q = qt[h]
                P.op("act", lambda e, pq=pq, q=q: e.copy(out=q[0:64, :], in_=pq[0:64, :T]), reads=[pq.r()], writes=[q.r()])
                P.op("dve", lambda e, pq=pq: e.tensor_tensor(out=t1[64:96, :], in0=pq[64:96, :T], in1=cs[64:96, 0, :], op=ALU.mult),
                     reads=[pq.r(), cs.r()], writes=[t1.r()])
                P.op("dve", lambda e, pqs=pqs: e.tensor_tensor(out=t2[64:96, :], in0=pqs[64:96, :T], in1=cs[64:96, 1, :], op=ALU.mult),
                     reads=[pqs.r(), cs.r()], writes=[t2.r()])
                P.op("pool", lambda e, q=q: e.tensor_tensor(out=q[64:96, :], in0=t1[64:96, :], in1=t2[64:96, :], op=ALU.add),
                     reads=[t1.r(), t2.r()], writes=[q.r()])
                P.dma(lambda e, h=h, q=q, t0=t0: e.dma_start(out=qTd[h, :, t0:t0 + T], in_=q[:, :]), reads=[q.r()], writes=[qres[h][it]])
                pkn = next_ps(P)
                for c in range(2):
                    P.op("pe", lambda e, c=c, h=h, pkn=pkn: e.matmul(
                        pkn[:64, :T], lhsT=Wukv[:, c, h * 64:(h + 1) * 64], rhs=cqn[:, 3 + c, :],
                        start=(c == 0), stop=(c == 1)), reads=[Wukv.r(c), cqn.r(3 + c)], writes=[pkn.r()])
                P.op("act", lambda e, h=h, pkn=pkn, t0=t0: e.copy(out=kT[h][0:64, t0:t0 + T], in_=pkn[0:64, :T]),
                     reads=[pkn.r()], writes=[kT[h].r(it)])
            for b4 in range(4):
                pv = next_ps(P)
                for c in range(2):
                    P.op("pe", lambda e, c=c, b4=b4, pv=pv: e.matmul(
                        pv[:, :128], lhsT=cqn[:, 3 + c, b4 * 128:(b4 + 1) * 128], rhs=Wukv[:, c, 128:256],
                        start=(c == 0), stop=(c == 1)), reads=[Wukv.r(c), cqn.r(3 + c)], writes=[pv.r()])
                kb = it * 4 + b4
                P.op("act", lambda e, pv=pv, kb=kb: e.copy(
                    out=Va[:, kb, :, 0:64], in_=pv[:, :128].rearrange("p (h d) -> p h d", h=2)),
                    reads=[pv.r()], writes=[Va.r(it)])
        pts = [P.sb(f"pt{i}", [128, T], BF16) for i in range(4)]
        qs = [P.sb(f"qs{i}", [96, T], BF16) for i in range(2)]
        osb = P.sb("osb", [128, T], F32)
        P.op("dve", lambda e: e.memset(osb[:, :], 0.0), writes=[osb.r()])
        rden = P.sb("rden", [64, T], F32)
        on = [P.sb(f"on{i}", [64, T], F32) for i in range(2)]
        po = [P.ps(f"po{i}", [128, 512], F32) for i in range(2)]
        outs = []
        n = 0
        for h in range(2):
            for j in range(NTI):
                q = qs[n % 2]
                pO = po[n % 2]
                o_n = on[n % 2]
                n += 1
                P.dma(lambda e, h=h, q=q, j=j: e.dma_start(out=q[:, :], in_=qTd[h, :, j * T:(j + 1) * T]),
                      reads=[qres[h][j]], writes=[q.r()], q="act")
                nkb = 4 * j + 4
                LA = 2
                pend = []

                def pv(kb, pt, h=h, pO=pO, nkb=nkb):
                    P.op("pe", lambda e, h=h, kb=kb, pt=pt, pO=pO, nkb=nkb: e.matmul(
                        pO[:65, :T], lhsT=Va[:, kb, h, :], rhs=pt[:, :], start=(kb == 0), stop=(kb == nkb - 1)),
                        reads=[Va.r(kb // 4), pt.r()], writes=[pO.r()])

                for kb in range(nkb):
                    ps = next_ps(P)
                    pt = pts[kb % len(pts)]
                    P.op("pe", lambda e, h=h, kb=kb, ps=ps, q=q: e.matmul(
                        ps[:, :T], lhsT=kT[h][:, kb * 128:(kb + 1) * 128], rhs=q[:, :], start=True, stop=True),
                        reads=[kT[h].r(kb // 4), q.r()], writes=[ps.r()])
                    P.op("act", lambda e, ps=ps, pt=pt: e.activation(out=pt[:, :], in_=ps[:, :T], func=AF.Exp, scale=SCALE),
                         reads=[ps.r()], writes=[pt.r()])
                    if kb >= 4 * j:
                        d = kb - 4 * j
                        eng = "dve" if d % 2 == 0 else "pool"
                        P.op(eng, lambda e, pt=pt, d=d: e.tensor_tensor(out=pt[:, :], in0=pt[:, :], in1=cm[:, d, :], op=ALU.mult),
                             reads=[pt.r(), cm.r()], writes=[pt.r()])
                    pend.append((kb, pt))
                    if len(pend) > LA:
                        pv(*pend.pop(0))
                while pend:
                    pv(*pend.pop(0))
                P.op("act", lambda e, pO=pO: e.copy(out=osb[0:65, :], in_=pO[:65, :T]), reads=[pO.r()], writes=[osb.r()])
                pd = next_ps(P)
                P.op("pe", lambda e, pd=pd: e.matmul(pd[:64, :T], lhsT=es[:, :], rhs=osb[:, :], start=True, stop=True),
                     reads=[es.r(), osb.r()], writes=[pd.r()])
                P.op("dve", lambda e, pd=pd: e.reciprocal(out=rden[:, :], in_=pd[:64, :T]), reads=[pd.r()], writes=[rden.r()])
                P.op("dve", lambda e, o_n=o_n: e.tensor_tensor(out=o_n[:, :], in0=osb[0:64, :], in1=rden[:, :], op=ALU.mult),
                     reads=[osb.r(), rden.r()], writes=[o_n.r()])
                outs.append(P.dma(lambda e, h=h, j=j, o_n=o_n: e.dma_start(
                    out=oT[h * 64:(h + 1) * 64, j * T:(j + 1) * T], in_=o_n[:, :]), reads=[o_n.r()]))
        P.emit(final_waits=outs)
    return nc


def build_gdn(L=16384):
    T = 512
    NTI = L // T
    nc = bass.Bass("TRN2", target_bir_lowering=False)
    hT = nc.dram_tensor("hT", [1024, L], F32, kind="ExternalInput").ap()
    g = nc.dram_tensor("g", [1024], F32, kind="ExternalInput").ap()
    wh = nc.dram_tensor("wh", [1024, 640], F32, kind="ExternalInput").ap()
    cw = nc.dram_tensor("cw", [384, 4], F32, kind="ExternalInput").ap()
    sc = nc.dram_tensor("sc", [128, 2], F32, kind="ExternalInput").ap()
    og = nc.dram_tensor("og", [128], F32, kind="ExternalInput").ap()
    cst = nc.dram_tensor("cst", [128, 5, 128], F32, kind="ExternalInput").ap()
    oT = nc.dram_tensor("oT", [128, L], F32, kind="ExternalOutput").ap()
    s_in = nc.dram_tensor("s_in", [128, 128], F32, kind="ExternalInput").ap()
    h_in = nc.dram_tensor("h_in", [128, 3, 3], F32, kind="ExternalInput").ap()
    s_out = nc.dram_tensor("s_out", [128, 128], F32, kind="ExternalOutput").ap()
    h_out = nc.dram_tensor("h_out", [128, 3, 3], F32, kind="ExternalOutput").ap()
    hv = hT.rearrange("(c p) t -> p c t", p=128)
    with ExitStack() as st:
        st.enter_context(nc.allow_low_precision("bf16 matmul operands for the input projection only"))
        P = Prog(nc, st)
        qbanks = [P.ps(f"qb{i}", [128, 512], F32) for i in range(5)]
        qp = [View(qbanks[i], 0, 128, f"qp{i}") for i in range(5)]
        hp = [View(qbanks[i], 0, 256, f"hp{i}") for i in range(5)]
        P.pss = [P.ps(f"fb{i}", [128, 512], F32) for i in range(3)]
        P._psi = 0
        cnt = {"q": 0, "h": 0}

        def nq():
            cnt["q"] += 1
            return qp[cnt["q"] % 5]

        def nh():
            cnt["q"] += 1
            return hp[cnt["q"] % 5]

        C = make_consts(P)
        ones_f = P.sb("ones_f", [128, 128], F32)
        P.op("dve", lambda e: e.memset(ones_f[:, :], 1.0), writes=[ones_f.r()])
        gt = load_vec_fm(P, g, 8, "g")
        ogt = load_vec_fm(P, og, 1, "og")
        cwt = load_vec_fm_2d = P.sb("cwt", [128, 3, 4], F32)
        P.dma(lambda e: e.dma_start(out=cwt[:, :, :], in_=cw.rearrange("(c p) j -> p c j", p=128)), writes=[cwt.r()])
        sct = load_const(P, sc, [128, 2], "sct")
        K = load_const(P, cst, [128, 5, 128], "K")
        ident = K[:, 0, :]
        maskS = K[:, 1, :]
        maskI = K[:, 2, :]
        blk1 = K[:, 3, :]
        cind = K[:, 4, 0:2]
        Wh = load_w_bf16(P, wh, 8, 640, "Wh")
        Whf = P.sb("Whf", [128, 8, 2], F32)
        P.dma(lambda e: e.dma_start(out=Whf[:, :, :], in_=wh.rearrange("(c p) f -> p c f", p=128)[:, :, 512:514]),
              writes=[Whf.r()])
        for c in range(8):
            P.op("dve", lambda e, c=c: e.tensor_scalar(out=Whf[:, c, :], in0=Whf[:, c, :], scalar1=gt[:, c:c + 1],
                                                       scalar2=None, op0=ALU.mult),
                 reads=[Whf.r(), gt.r()], writes=[Whf.r()])
        nA = P.sb("nA", [128, 1], F32)
        P.op("act", lambda e: e.activation(out=nA[:, :], in_=sct[:, 0:1], func=AF.Exp), reads=[sct.r()], writes=[nA.r()])
        P.op("dve", lambda e: e.tensor_scalar(out=nA[:, :], in0=nA[:, :], scalar1=-1.0, scalar2=None, op0=ALU.mult),
             reads=[nA.r()], writes=[nA.r()])
        onec = P.sb("onec", [128, 1], F32)
        P.op("dve", lambda e: e.memset(onec[:, :], 1.0), writes=[onec.r()])
        epsl2 = C.eps
        S = [P.sb(f"S{i}", [128, 128], F32) for i in range(2)]
        P.dma(lambda e: e.dma_start(out=S[0][:, :], in_=s_in), writes=[S[0].r()])
        sidx = [0]
        x = P.sb("x", [128, 8, T], F32)
        sq = P.sb("sq", [128, 8, T], BF16, nsub=8)
        hn = P.sb("hn", [128, 8, T], BF16, nsub=8)
        rstd = P.sb("rstd", [128, T], F32)
        raw = P.sb("raw", [128, 3, T + 3], F32, nsub=3)
        P.dma(lambda e: e.dma_start(out=raw[:, :, 0:3], in_=h_in), writes=raw.all())
        acc = P.sb("acc", [128, 3, T], F32, nsub=3)
        sil = P.sb("sil", [128, 3, T], F32, nsub=3)
        sq2 = P.sb("sq2", [128, 2, T], F32, nsub=2)
        rn = P.sb("rn", [128, 2, T], F32, nsub=2)
        tb = {}

        PERSIST = ("QpT", "O0T", "MT0", "MT1", "B0", "B1", "gateT", "oTt", "osq", "orr")

        def tbuf(par, name, shape=(128, 128)):
            if not name.rstrip("0123456789").endswith(PERSIST) and not any(name.startswith(p) for p in PERSIST):
                par = 0
            key = (par, name)
            if key not in tb:
                tb[key] = P.sb(f"t{par}_{name}", list(shape), F32)
            return tb[key]

        outs = []

        def gen_local(t):
            par = t % 2
            t0 = t * T
            qnT = tbuf(par, "qnT", (128, T))
            knT = tbuf(par, "knT", (128, T))
            vT = tbuf(par, "vT", (128, T))
            gateT = tbuf(par, "gateT", (128, T))
            P.dma(lambda e: e.dma_start(out=x[:, :, :], in_=hv[:, :, t0:t0 + T]), writes=[x.r()])
            norm_tile(P, C, x, gt, sq, hn, rstd, T)
            yield
            for s3 in range(3):
                ps = next_ps(P)
                for c in range(8):
                    P.op("pe", lambda e, c=c, s3=s3, ps=ps: e.matmul(
                        ps[:, :T], lhsT=Wh[:, c, s3 * 128:(s3 + 1) * 128], rhs=hn[:, c, :],
                        start=(c == 0), stop=(c == 7)), reads=[Wh.r(c), hn.r(c)], writes=[ps.r()])
                P.op("act", lambda e, s3=s3, ps=ps: e.copy(out=raw[:, s3, 3:T + 3], in_=ps[:, :T]),
                     reads=[ps.r()], writes=[raw.r(s3)])
            ps = next_ps(P)
            for c in range(8):
                P.op("pe", lambda e, c=c, ps=ps: e.matmul(
                    ps[:, :T], lhsT=Wh[:, c, 384:512], rhs=hn[:, c, :], start=(c == 0), stop=(c == 7)),
                    reads=[Wh.r(c), hn.r(c)], writes=[ps.r()])
            P.op("act", lambda e, ps=ps: e.activation(out=gateT[:, :], in_=ps[:, :T], func=AF.Silu),
                 reads=[ps.r()], writes=[gateT.r()])
            yield
            for s3 in range(3):
                eng = "dve" if s3 != 1 else "pool"
                P.op("dve", lambda e, s3=s3: e.tensor_scalar(
                    out=acc[:, s3, :], in0=raw[:, s3, 3:T + 3], scalar1=cwt[:, s3, 3:4], scalar2=None, op0=ALU.mult),
                    reads=[raw.r(s3), cwt.r()], writes=[acc.r(s3)])
                for j in range(3):
                    P.op("dve", lambda e, s3=s3, j=j: e.scalar_tensor_tensor(
                        out=acc[:, s3, :], in0=raw[:, s3, j:j + T], scalar=cwt[:, s3, j:j + 1], in1=acc[:, s3, :],
                        op0=ALU.mult, op1=ALU.add), reads=[raw.r(s3), cwt.r(), acc.r(s3)], writes=[acc.r(s3)])
                P.op("pool", lambda e, s3=s3: e.tensor_copy(out=raw[:, s3, 0:3], in_=raw[:, s3, T:T + 3]),
                     reads=[raw.r(s3)], writes=[raw.r(s3)])
                dst = vT if s3 == 2 else sil
                if s3 == 2:
                    P.op("act", lambda e: e.activation(out=vT[:, :], in_=acc[:, 2, :], func=AF.Silu),
                         reads=[acc.r(2)], writes=[vT.r()])
                else:
                    P.op("act", lambda e, s3=s3: e.activation(out=sil[:, s3, :], in_=acc[:, s3, :], func=AF.Silu),
                         reads=[acc.r(s3)], writes=[sil.r(s3)])
            yield
            for s2 in range(2):
                P.op("pool", lambda e, s2=s2: e.tensor_tensor(out=sq2[:, s2, :], in0=sil[:, s2, :], in1=sil[:, s2, :], op=ALU.mult),
                     reads=[sil.r(s2)], writes=[sq2.r(s2)])
                ps = next_ps(P)
                P.op("pe", lambda e, s2=s2, ps=ps: e.matmul(ps[:, :T], lhsT=ones_f[:, :], rhs=sq2[:, s2, :], start=True, stop=True),
                     reads=[ones_f.r(), sq2.r(s2)], writes=[ps.r()])
                P.op("act", lambda e, s2=s2, ps=ps: e.activation(out=rn[:, s2, :], in_=ps[:, :T], func=AF.Sqrt,
                                                                bias=epsl2[:, :], scale=1.0),
                     reads=[ps.r(), epsl2.r()], writes=[rn.r(s2)])
                P.op("dve", lambda e, s2=s2: e.reciprocal(out=rn[:, s2, :], in_=rn[:, s2, :]), reads=[rn.r(s2)], writes=[rn.r(s2)])
                dst = qnT if s2 == 0 else knT
                scl = (128.0 ** -0.5) if s2 == 0 else 1.0
                P.op("dve", lambda e, s2=s2, dst=dst, scl=scl: e.scalar_tensor_tensor(
                    out=dst[:, :], in0=sil[:, s2, :], scalar=scl, in1=rn[:, s2, :], op0=ALU.mult, op1=ALU.mult),
                    reads=[sil.r(s2), rn.r(s2)], writes=[dst.r()])
            yield
            B4 = range(4)
            tl = lambda s, name, shape=(128, 128): tbuf(par, f"{name}{s}", shape)
            pKK, pQK, pgc = {}, {}, {}
            for s in B4:
                ts = slice(s * 128, (s + 1) * 128)
                cols = tl(s, "cols", (128, 8))
                tcol = tl(s, "tcol", (128, 8))
                pc = nq()
                for c in range(8):
                    P.op("pe", lambda e, c=c, pc=pc, ts=ts: e.matmul(pc[:, 0:2], lhsT=x[:, c, ts], rhs=Whf[:, c, 0:2],
                                                                   start=(c == 0), stop=(c == 7)),
                         reads=[x.r(), Whf.r()], writes=[pc.r()])
                pss_ = nq()
                for c in range(8):
                    P.op("pe", lambda e, c=c, pss_=pss_, ts=ts: e.matmul(pss_[:, 0:2], lhsT=sq[:, c, ts], rhs=C.ones_bf[:, 0:2],
                                                                       start=(c == 0), stop=(c == 7)),
                         reads=[sq.r(c), C.ones_bf.r()], writes=[pss_.r()])
                P.op("act", lambda e, pss_=pss_, tcol=tcol: e.activation(out=tcol[:, 0:2], in_=pss_[:, 0:2], func=AF.Sqrt,
                                                                       bias=C.eps[:, :], scale=1.0 / 1024),
                     reads=[pss_.r(), C.eps.r()], writes=[tcol.r()])
                P.op("dve", lambda e, tcol=tcol: e.reciprocal(out=tcol[:, 0:2], in_=tcol[:, 0:2]), reads=[tcol.r()], writes=[tcol.r()])
                P.op("dve", lambda e, pc=pc, tcol=tcol: e.tensor_tensor(out=tcol[:, 2:4], in0=pc[:, 0:2], in1=tcol[:, 0:2], op=ALU.mult),
                     reads=[pc.r(), tcol.r()], writes=[tcol.r()])
                P.op("act", lambda e, tcol=tcol, cols=cols: e.activation(out=cols[:, 1:2], in_=tcol[:, 2:3], func=AF.Sigmoid),
                     reads=[tcol.r()], writes=[cols.r()])
                P.op("act", lambda e, tcol=tcol: e.activation(out=tcol[:, 4:5], in_=tcol[:, 3:4], func=AF.Exp, bias=sct[:, 1:2], scale=1.0),
                     reads=[tcol.r(), sct.r()], writes=[tcol.r()])
                P.op("act", lambda e, tcol=tcol: e.activation(out=tcol[:, 5:6], in_=tcol[:, 4:5], func=AF.Ln, bias=onec[:, 0:1], scale=1.0),
                     reads=[tcol.r(), onec.r()], writes=[tcol.r()])
                P.op("dve", lambda e, tcol=tcol, cols=cols: e.tensor_scalar(out=cols[:, 0:1], in0=tcol[:, 5:6], scalar1=nA[:, 0:1], scalar2=None, op0=ALU.mult),
                     reads=[tcol.r(), nA.r()], writes=[cols.r()])
                gB = tl(s, "gB"); bB = tl(s, "bB")
                P.op("dve", lambda e, gB=gB, cols=cols: e.tensor_scalar(out=gB[:, :], in0=ones_f[:, :], scalar1=cols[:, 0:1],
                                                                       scalar2=None, op0=ALU.mult),
                     reads=[ones_f.r(), cols.r()], writes=[gB.r()])
                P.op("pool", lambda e, bB=bB, cols=cols: e.tensor_scalar(out=bB[:, :], in0=ones_f[:, :], scalar1=cols[:, 1:2],
                                                                        scalar2=None, op0=ALU.mult),
                     reads=[ones_f.r(), cols.r()], writes=[bB.r()])
            yield
            for s in B4:
                ts = slice(s * 128, (s + 1) * 128)
                cols = tl(s, "cols", (128, 8)); gB = tl(s, "gB")
                pgc[s] = nq()
                P.op("pe", lambda e, p=pgc[s], gB=gB: e.matmul(p[:, :], lhsT=gB[:, :], rhs=maskI, start=True, stop=True),
                     reads=[gB.r(), K.r()], writes=[pgc[s].r()])
                pm = nq()
                P.op("pe", lambda e, pm=pm, cols=cols: e.matmul(pm[:, 0:2], lhsT=maskI, rhs=cols[:, 0:2], start=True, stop=True),
                     reads=[K.r(), cols.r()], writes=[pm.r()])
                P.op("pe", lambda e, pm=pm, cols=cols: e.matmul(pm[:, 2:4], lhsT=blk1, rhs=cols[:, 0:2], start=True, stop=True),
                     reads=[K.r(), cols.r()], writes=[pm.r()])
                P.op("pe", lambda e, pm=pm, gB=gB: e.matmul(pm[:, 4:6], lhsT=gB[:, :], rhs=cind, start=True, stop=True),
                     reads=[K.r(), gB.r()], writes=[pm.r()])
                P.op("act", lambda e, pm=pm, cols=cols: e.copy(out=cols[:, 2:3], in_=pm[:, 0:1]), reads=[pm.r()], writes=[cols.r()])
                P.op("act", lambda e, pm=pm, cols=cols: e.copy(out=cols[:, 3:4], in_=pm[:, 2:3]), reads=[pm.r()], writes=[cols.r()])
                glb = tl(s, "glb", (128, 2))
                P.op("act", lambda e, pm=pm, glb=glb: e.activation(out=glb[:, :], in_=pm[:, 4:6], func=AF.Exp),
                     reads=[pm.r()], writes=[glb.r()])
                E = tl(s, "E"); egc = tl(s, "egc")
                P.op("dve", lambda e, p=pgc[s], E=E, cols=cols: e.tensor_scalar(
                    out=E[:, :], in0=p[:, :], scalar1=cols[:, 2:3], scalar2=0.0, op0=ALU.subtract, op1=ALU.min),
                    reads=[pgc[s].r(), cols.r()], writes=[E.r()])
                P.op("act", lambda e, p=pgc[s], egc=egc: e.activation(out=egc[:, :], in_=p[:, :], func=AF.Exp),
                     reads=[pgc[s].r()], writes=[egc.r()])
                P.op("act", lambda e, cols=cols: e.activation(out=cols[:, 4:5], in_=cols[:, 2:3], func=AF.Exp),
                     reads=[cols.r()], writes=[cols.r()])
                P.op("dve", lambda e, cols=cols: e.tensor_tensor(out=cols[:, 5:6], in0=cols[:, 4:5], in1=cols[:, 1:2], op=ALU.mult),
                     reads=[cols.r()], writes=[cols.r()])
                P.op("dve", lambda e, cols=cols: e.tensor_tensor(out=cols[:, 6:7], in0=cols[:, 3:4], in1=cols[:, 2:3], op=ALU.subtract),
                     reads=[cols.r()], writes=[cols.r()])
                P.op("act", lambda e, cols=cols: e.activation(out=cols[:, 6:7], in_=cols[:, 6:7], func=AF.Exp),
                     reads=[cols.r()], writes=[cols.r()])
            yield
            for s in B4:
                ts = slice(s * 128, (s + 1) * 128)
                E = tl(s, "E"); EmS = tl(s, "EmS"); EmI = tl(s, "EmI")
                P.op("act", lambda e, E=E: e.activation(out=E[:, :], in_=E[:, :], func=AF.Exp), reads=[E.r()], writes=[E.r()])
                pbb = nq()
                bB = tl(s, "bB")
                P.op("pe", lambda e, pbb=pbb, bB=bB: e.matmul(pbb[:, :], lhsT=bB[:, :], rhs=ident, start=True, stop=True),
                     reads=[bB.r(), K.r()], writes=[pbb.r()])
                P.op("pool", lambda e, E=E, EmS=EmS: e.tensor_tensor(out=EmS[:, :], in0=E[:, :], in1=maskS, op=ALU.mult),
                     reads=[E.r(), K.r()], writes=[EmS.r()])
                P.op("pool", lambda e, E=E, EmI=EmI: e.tensor_tensor(out=EmI[:, :], in0=E[:, :], in1=maskI, op=ALU.mult),
                     reads=[E.r(), K.r()], writes=[EmI.r()])
                P.op("dve", lambda e, pbb=pbb, EmS=EmS: e.tensor_tensor(out=EmS[:, :], in0=pbb[:, :], in1=EmS[:, :], op=ALU.mult),
                     reads=[pbb.r(), EmS.r()], writes=[EmS.r()])
            yield
            for s in B4:
                ts = slice(s * 128, (s + 1) * 128)
                EmS = tl(s, "EmS"); EmI = tl(s, "EmI"); Q0 = tl(s, "Qa"); qkT = tl(s, "qkT")
                pKK[s] = nq()
                P.op("pe", lambda e, p=pKK[s], ts=ts: e.matmul(p[:, :], lhsT=knT[:, ts], rhs=knT[:, ts], start=True, stop=True),
                     reads=[knT.r()], writes=[pKK[s].r()])
                P.op("dve", lambda e, p=pKK[s], EmS=EmS, Q0=Q0: e.scalar_tensor_tensor(
                    out=Q0[:, :], in0=p[:, :], scalar=-1.0, in1=EmS[:, :], op0=ALU.mult, op1=ALU.mult),
                    reads=[pKK[s].r(), EmS.r()], writes=[Q0.r()])
                pQK[s] = nq()
                P.op("pe", lambda e, p=pQK[s], ts=ts: e.matmul(p[:, :], lhsT=knT[:, ts], rhs=qnT[:, ts], start=True, stop=True),
                     reads=[knT.r(), qnT.r()], writes=[pQK[s].r()])
                P.op("dve", lambda e, p=pQK[s], EmI=EmI, qkT=qkT: e.tensor_tensor(out=qkT[:, :], in0=p[:, :], in1=EmI[:, :], op=ALU.mult),
                     reads=[pQK[s].r(), EmI.r()], writes=[qkT.r()])
            yield
            for s in B4:
                Q0 = tl(s, "Qa"); N0 = tl(s, "Na"); R0 = tl(s, "Ra")
                pt = nq()
                P.op("pe", lambda e, pt=pt, Q0=Q0: e.transpose(pt[:, :], Q0[:, :], ident), reads=[Q0.r(), K.r()], writes=[pt.r()])
                P.op("act", lambda e, pt=pt, N0=N0: e.copy(out=N0[:, :], in_=pt[:, :]), reads=[pt.r()], writes=[N0.r()])
                P.op("pool", lambda e, Q0=Q0, R0=R0: e.tensor_tensor(out=R0[:, :], in0=Q0[:, :], in1=ident, op=ALU.add),
                     reads=[Q0.r(), K.r()], writes=[R0.r()])
            yield
            names = ["a", "b"]
            for i in range(1, 6):
                po, pn = names[(i - 1) % 2], names[i % 2]
                for s in B4:
                    Qo = tl(s, "Q" + po); No = tl(s, "N" + po); Ro = tl(s, "R" + po)
                    Qn = tl(s, "Q" + pn); Nn = tl(s, "N" + pn)
                    pN = nq()
                    P.op("pe", lambda e, pN=pN, Qo=Qo, No=No: e.matmul(pN[:, :], lhsT=Qo[:, :], rhs=No[:, :], start=True, stop=True),
                         reads=[Qo.r(), No.r()], writes=[pN.r()])
                    P.op("act", lambda e, pN=pN, Nn=Nn: e.copy(out=Nn[:, :], in_=pN[:, :]), reads=[pN.r()], writes=[Nn.r()])
                    if i < 5:
                        pQ = nq()
                        P.op("pe", lambda e, pQ=pQ, Qo=Qo, No=No: e.matmul(pQ[:, :], lhsT=No[:, :], rhs=Qo[:, :], start=True, stop=True),
                             reads=[Qo.r(), No.r()], writes=[pQ.r()])
                        P.op("dve", lambda e, pQ=pQ, Qn=Qn: e.tensor_copy(out=Qn[:, :], in_=pQ[:, :]), reads=[pQ.r()], writes=[Qn.r()])
                yield
                for s in B4:
                    Nn = tl(s, "N" + pn); Ro = tl(s, "R" + po); Rn = tl(s, "R" + pn)
                    pR = nq()
                    P.op("pe", lambda e, pR=pR, Nn=Nn, Ro=Ro: e.matmul(pR[:, :], lhsT=Nn[:, :], rhs=Ro[:, :], start=True, stop=True),
                         reads=[Nn.r(), Ro.r()], writes=[pR.r()])
                    P.op("dve", lambda e, pR=pR, Ro=Ro, Rn=Rn: e.tensor_tensor(out=Rn[:, :], in0=pR[:, :], in1=Ro[:, :], op=ALU.add),
                         reads=[pR.r(), Ro.r()], writes=[Rn.r()])
                yield
            TTn = names[5 % 2]
            for s in B4:
                ts = slice(s * 128, (s + 1) * 128)
                cols = tl(s, "cols", (128, 8))
                UWin = tl(s, "UWin", (128, 256)); kd = tl(s, "kd")
                pk = nq()
                P.op("pe", lambda e, pk=pk, ts=ts: e.transpose(pk[:, :], knT[:, ts], ident), reads=[knT.r(), K.r()], writes=[pk.r()])
                P.op("dve", lambda e, pk=pk, UWin=UWin, cols=cols: e.tensor_scalar(out=UWin[:, 128:256], in0=pk[:, :], scalar1=cols[:, 5:6], scalar2=None, op0=ALU.mult),
                     reads=[pk.r(), cols.r()], writes=[UWin.r()])
                P.op("dve", lambda e, pk=pk, kd=kd, cols=cols: e.tensor_scalar(out=kd[:, :], in0=pk[:, :], scalar1=cols[:, 6:7], scalar2=None, op0=ALU.mult),
                     reads=[pk.r(), cols.r()], writes=[kd.r()])
                pv = nq()
                P.op("pe", lambda e, pv=pv, ts=ts: e.transpose(pv[:, :], vT[:, ts], ident), reads=[vT.r(), K.r()], writes=[pv.r()])
                P.op("dve", lambda e, pv=pv, UWin=UWin, cols=cols: e.tensor_scalar(out=UWin[:, 0:128], in0=pv[:, :], scalar1=cols[:, 1:2], scalar2=None, op0=ALU.mult),
                     reads=[pv.r(), cols.r()], writes=[UWin.r()])
            yield
            for s in B4:
                ts = slice(s * 128, (s + 1) * 128)
                UWin = tl(s, "UWin", (128, 256)); uw = tl(s, "uw", (128, 256)); TT = tl(s, "R" + TTn)
                egc = tl(s, "egc"); qdT = tl(s, "qdT")
                pu = nh()
                P.op("pe", lambda e, pu=pu, TT=TT, UWin=UWin: e.matmul(pu[:, :], lhsT=TT[:, :], rhs=UWin[:, :], start=True, stop=True),
                     reads=[TT.r(), UWin.r()], writes=[pu.r()])
                P.op("act", lambda e, pu=pu, uw=uw: e.copy(out=uw[:, :], in_=pu[:, :]), reads=[pu.r()], writes=[uw.r()])
                P.op("pool", lambda e, qdT=qdT, egc=egc, ts=ts: e.tensor_tensor(out=qdT[:, :], in0=qnT[:, ts], in1=egc[:, :], op=ALU.mult),
                     reads=[qnT.r(), egc.r()], writes=[qdT.r()])
            yield
            for s in B4:
                uw = tl(s, "uw", (128, 256)); qkT = tl(s, "qkT"); qdT = tl(s, "qdT"); kd = tl(s, "kd")
                QpT = tl(s, "QpT"); O0T = tl(s, "O0T"); glb = tl(s, "glb", (128, 2))
                pw = nq()
                P.op("pe", lambda e, pw=pw, uw=uw, qkT=qkT: e.matmul(pw[:, :], lhsT=uw[:, 128:256], rhs=qkT[:, :], start=True, stop=True),
                     reads=[uw.r(), qkT.r()], writes=[pw.r()])
                P.op("dve", lambda e, pw=pw, qdT=qdT, QpT=QpT: e.tensor_tensor(out=QpT[:, :], in0=qdT[:, :], in1=pw[:, :], op=ALU.subtract),
                     reads=[pw.r(), qdT.r()], writes=[QpT.r()])
                po0 = nq()
                P.op("pe", lambda e, po0=po0, uw=uw, qkT=qkT: e.matmul(po0[:, :], lhsT=uw[:, 0:128], rhs=qkT[:, :], start=True, stop=True),
                     reads=[uw.r(), qkT.r()], writes=[po0.r()])
                P.op("act", lambda e, po0=po0, O0T=O0T: e.copy(out=O0T[:, :], in_=po0[:, :]), reads=[po0.r()], writes=[O0T.r()])
                for c2 in range(2):
                    r = slice(c2 * 64, (c2 + 1) * 64)
                    MT = tl(s, f"MT{c2}"); Bc = tl(s, f"B{c2}")
                    pM = nq()
                    P.op("pe", lambda e, pM=pM, uw=uw, kd=kd, r=r: e.matmul(pM[:, :], lhsT=uw[r, 128:256], rhs=kd[r, :], start=True, stop=True),
                         reads=[uw.r(), kd.r()], writes=[pM.r()])
                    P.op("dve", lambda e, pM=pM, MT=MT, glb=glb, c2=c2: e.scalar_tensor_tensor(
                        out=MT[:, :], in0=ident, scalar=glb[:, c2:c2 + 1], in1=pM[:, :], op0=ALU.mult, op1=ALU.subtract),
                        reads=[pM.r(), glb.r(), K.r()], writes=[MT.r()])
                    pB = nq()
                    P.op("pe", lambda e, pB=pB, uw=uw, kd=kd, r=r: e.matmul(pB[:, :], lhsT=kd[r, :], rhs=uw[r, 0:128], start=True, stop=True),
                         reads=[uw.r(), kd.r()], writes=[pB.r()])
                    P.op("act", lambda e, pB=pB, Bc=Bc: e.copy(out=Bc[:, :], in_=pB[:, :]), reads=[pB.r()], writes=[Bc.r()])
                yield

        def gen_rec(t):
            par = t % 2
            t0 = t * T
            tl = lambda s, name, shape=(128, 128): tbuf(par, f"{name}{s}", shape)
            oTt = tbuf(par, "oTt", (128, T))
            gateT = tbuf(par, "gateT", (128, T))
            for s in range(4):
                QpT = tl(s, "QpT"); O0T = tl(s, "O0T")
                for c2 in range(2):
                    r = slice(c2 * 64, (c2 + 1) * 64)
                    col = slice(s * 128 + c2 * 64, s * 128 + (c2 + 1) * 64)
                    MT = tl(s, f"MT{c2}"); Bc = tl(s, f"B{c2}")
                    So = S[sidx[0] % 2]
                    Sn = S[(sidx[0] + 1) % 2]
                    sidx[0] += 1
                    po = nq()
                    P.op("pe", lambda e, po=po, So=So, QpT=QpT, r=r: e.matmul(po[:, 0:64], lhsT=So[:, :], rhs=QpT[:, r], start=True, stop=True),
                         reads=[So.r(), QpT.r()], writes=[po.r()])
                    pS = nq()
                    P.op("pe", lambda e, pS=pS, So=So, MT=MT: e.matmul(pS[:, :], lhsT=MT[:, :], rhs=So[:, :], start=True, stop=True),
                         reads=[So.r(), MT.r()], writes=[pS.r()])
                    P.op("dve", lambda e, pS=pS, Bc=Bc, Sn=Sn: e.tensor_tensor(out=Sn[:, :], in0=pS[:, :], in1=Bc[:, :], op=ALU.add),
                         reads=[pS.r(), Bc.r()], writes=[Sn.r()])
                    P.op("dve", lambda e, po=po, O0T=O0T, r=r, col=col: e.tensor_tensor(out=oTt[:, col], in0=po[:, 0:64], in1=O0T[:, r], op=ALU.add),
                         reads=[po.r(), O0T.r()], writes=[oTt.r()])
                    yield
            osq = tbuf(par, "osq", (128, T))
            orr = tbuf(par, "orr", (128, T))
            P.op("pool", lambda e: e.tensor_tensor(out=osq[:, :], in0=oTt[:, :], in1=oTt[:, :], op=ALU.mult), reads=[oTt.r()], writes=[osq.r()])
            ps = next_ps(P)
            P.op("pe", lambda e, ps=ps: e.matmul(ps[:, :T], lhsT=ones_f[:, :], rhs=osq[:, :], start=True, stop=True),
                 reads=[ones_f.r(), osq.r()], writes=[ps.r()])
            P.op("act", lambda e, ps=ps: e.activation(out=orr[:, :], in_=ps[:, :T], func=AF.Sqrt, bias=C.eps[:, :], scale=1.0 / 128),
                 reads=[ps.r(), C.eps.r()], writes=[orr.r()])
            P.op("dve", lambda e: e.reciprocal(out=orr[:, :], in_=orr[:, :]), reads=[orr.r()], writes=[orr.r()])
            P.op("dve", lambda e: e.scalar_tensor_tensor(out=osq[:, :], in0=oTt[:, :], scalar=ogt[:, 0:1], in1=orr[:, :],
                                                        op0=ALU.mult, op1=ALU.mult),
                 reads=[oTt.r(), orr.r(), ogt.r()], writes=[osq.r()])
            P.op("pool", lambda e: e.tensor_tensor(out=osq[:, :], in0=osq[:, :], in1=gateT[:, :], op=ALU.mult),
                 reads=[osq.r(), gateT.r()], writes=[osq.r()])
            outs.append(P.dma(lambda e: e.dma_start(out=oT[:, t0:t0 + T], in_=osq[:, :]), reads=[osq.r()]))
            yield

        rec = None
        for t in range(NTI):
            loc = gen_local(t)
            while True:
                a = next(loc, "done")
                if rec is not None:
                    next(rec, None)
                if a == "done":
                    break
            if rec is not None:
                for _ in rec:
                    pass
            rec = gen_rec(t)
        for _ in rec:
            pass
        outs.append(P.dma(lambda e: e.dma_start(out=s_out, in_=S[sidx[0] % 2][:, :]), reads=[S[sidx[0] % 2].r()]))
        outs.append(P.dma(lambda e: e.dma_start(out=h_out, in_=raw[:, :, 0:3]), reads=raw.all()))
        P.emit(final_waits=outs)
    return nc


def build_dsa(L=16384, NIT=18, jset=None):
    T = 512
    NTI = L // T
    NQT = L // 256
    NJ_ALL = NQT // 8
    jset = list(range(NJ_ALL)) if jset is None else list(jset)
    NJ = len(jset)
    NQ = NJ * 256
    SCALE = 128.0 ** -0.5
    WSC = (8.0 ** -0.5) * (64.0 ** -0.5)
    nc = bass.Bass("TRN2", target_bir_lowering=False)
    xT = nc.dram_tensor("xT", [1024, L], F32, kind="ExternalInput").ap()
    xq = nc.dram_tensor("xq", [1024, NQ], F32, kind="ExternalInput").ap()
    g = nc.dram_tensor("g", [1024], F32, kind="ExternalInput").ap()
    win = nc.dram_tensor("win", [1024, 3656], F32, kind="ExternalInput").ap()
    lng = nc.dram_tensor("lng", [64], F32, kind="ExternalInput").ap()
    lnb = nc.dram_tensor("lnb", [64], F32, kind="ExternalInput").ap()
    qrel = nc.dram_tensor("qrel", [128, NJ * 2], F32, kind="ExternalInput").ap()
    kidx = nc.dram_tensor("kidx", [128, 2048], F32, kind="ExternalInput").ap()
    cst = nc.dram_tensor("cst", [128, 5, 128], F32, kind="ExternalInput").ap()
    oT = nc.dram_tensor("oT", [1024, NQ], F32, kind="ExternalOutput").ap()
    kTd = nc.dram_tensor("kTd", [1024, L], BF16, kind="Internal").ap()
    Vd = nc.dram_tensor("Vd", [L, 1024], BF16, kind="Internal").ap()
    qTd = nc.dram_tensor("qTd", [1024, NQ], BF16, kind="Internal").ap()
    qiTd = nc.dram_tensor("qiTd", [64, 8, NQ], F32, kind="Internal").ap()
    kiTd = nc.dram_tensor("kiTd", [64, L], F32, kind="Internal").ap()
    xv = xT.rearrange("(c p) t -> p c t", p=128)
    xqv = xq.rearrange("(c p) t -> p c t", p=128)
    kTv = kTd.rearrange("(h p) t -> p h t", p=128)
    Vv = Vd.rearrange("(n p) d -> p n d", p=128)
    qTv = qTd.rearrange("(h p) t -> p h t", p=128)
    oTv = oT.rearrange("(h p) t -> p h t", p=128)
    wv_ = win.rearrange("(c p) f -> p c f", p=128)
    with ExitStack() as st:
        st.enter_context(nc.allow_low_precision("bf16 matmul operands, fp32 accumulate"))
        P = Prog(nc, st)
        banks = [P.ps(f"b{i}", [128, 512], F32) for i in range(8)]
        P.pss = banks
        P._psi = 0
        C = make_consts(P)
        ones_f = P.sb("ones_f", [128, 128], F32)
        P.op("dve", lambda e: e.memset(ones_f[:, :], 1.0), writes=[ones_f.r()])
        gt = load_vec_fm(P, g, 8, "g")
        lngt = P.sb("lngt", [64, 1], F32)
        lnbt = P.sb("lnbt", [64, 1], F32)
        P.dma(lambda e: e.dma_start(out=lngt[:, :], in_=lng.rearrange("(p o) -> p o", o=1)), writes=[lngt.r()])
        P.dma(lambda e: e.dma_start(out=lnbt[:, :], in_=lnb.rearrange("(p o) -> p o", o=1)), writes=[lnbt.r()])
        qrt = load_const(P, qrel, [128, NJ * 2], "qrt")
        kit = load_const(P, kidx, [128, 2048], "kit")
        Kc = load_const(P, cst, [128, 5, 128], "Kc")
        idb = P.sb("idb", [128, 128], BF16)
        P.op("dve", lambda e: e.tensor_copy(out=idb[:, :], in_=Kc[:, 0, :]), reads=[Kc.r()], writes=[idb.r()])
        selb = P.sb("selb", [128, 2, 512], BF16)
        P.op("dve", lambda e: e.memset(selb[:, :, :], 0.0), writes=[selb.r()])
        for hf in range(2):
            for rep in range(2):
                c0 = rep * 256 + hf * 128
                P.op("dve", lambda e, hf=hf, c0=c0: e.tensor_copy(out=selb[:, hf, c0:c0 + 128], in_=Kc[:, 0, :]),
                     reads=[Kc.r(), selb.r()], writes=[selb.r()])
        wiT = P.sb("wiT", [128, NJ * 2, 8], F32)

        P.push_scope()
        Wq = P.sb("Wq", [128, 8, 1024], BF16, nsub=8)
        Wk = P.sb("Wk", [128, 8, 1024], BF16, nsub=8)
        Wv = P.sb("Wv", [128, 8, 1024], BF16, nsub=8)
        Wi = P.sb("Wi", [128, 8, 584], F32, nsub=8)
        hnf = P.sb("hnf", [128, 8, T], F32, nsub=8)
        for c in range(8):
            P.dma(lambda e, c=c: e.dma_start(out=Wk[:, c, :], in_=wv_[:, c, 1024:2048], max_dma_last_dim=8192), writes=[Wk.r(c)], q="pool")
            P.dma(lambda e, c=c: e.dma_start(out=Wv[:, c, :], in_=wv_[:, c, 2048:3072], max_dma_last_dim=8192), writes=[Wv.r(c)], q="pool")
            P.dma(lambda e, c=c: e.dma_start(out=Wi[:, c, :], in_=wv_[:, c, 3072:3656]), writes=[Wi.r(c)])
            P.dma(lambda e, c=c: e.dma_start(out=Wq[:, c, :], in_=wv_[:, c, 0:1024], max_dma_last_dim=8192), writes=[Wq.r(c)], q="pool")
        x = P.sb("x", [128, 8, T], F32)
        sq = P.sb("sq", [128, 8, T], BF16, nsub=8)
        hn = P.sb("hn", [128, 8, T], BF16, nsub=8)
        rstd = P.sb("rstd", [128, T], F32)
        ktb = [P.sb(f"ktb{i}", [128, 8, T], BF16, nsub=8) for i in range(2)]
        vtb = [P.sb(f"vtb{i}", [128, 4, 1024], BF16, nsub=8) for i in range(2)]
        kraw = P.sb("kraw", [64, T], F32)
        ksq = P.sb("ksq", [64, T], F32)
        kmean = P.sb("kmean", [64, T], F32)
        kvar = P.sb("kvar", [64, T], F32)
        kio = P.sb("kio", [64, T], F32)
        for it in range(NTI):
            t0 = it * T
            kt = ktb[it % 2]
            vt = vtb[it % 2]
            P.dma(lambda e, t0=t0: e.dma_start(out=x[:, :, :], in_=xv[:, :, t0:t0 + T]), writes=[x.r()])
            norm_tile(P, C, x, gt, sq, hn, rstd, T)
            for c in range(8):
                P.op("dve", lambda e, c=c: e.scalar_tensor_tensor(out=hnf[:, c, :], in0=x[:, c, :], scalar=gt[:, c:c + 1], in1=rstd[:, :],
                                                                 op0=ALU.mult, op1=ALU.mult), reads=[x.r(), rstd.r(), gt.r()], writes=[hnf.r(c)])
            for h in range(8):
                ps = next_ps(P)
                for c in range(8):
                    P.op("pe", lambda e, c=c, h=h, ps=ps: e.matmul(ps[:, :T], lhsT=Wk[:, c, h * 128:(h + 1) * 128], rhs=hn[:, c, :],
                                                                  start=(c == 0), stop=(c == 7)),
                         reads=[Wk.r(c), hn.r(c)], writes=[ps.r()])
                if h % 2 == 0:
                    P.op("act", lambda e, h=h, ps=ps, kt=kt: e.copy(out=kt[:, h, :], in_=ps[:, :T]), reads=[ps.r()], writes=[kt.r(h)])
                else:
                    P.op("dve", lambda e, h=h, ps=ps, kt=kt: e.tensor_copy(out=kt[:, h, :], in_=ps[:, :T]), reads=[ps.r()], writes=[kt.r(h)])
            P.dma(lambda e, kt=kt, t0=t0: e.dma_start(out=kTv[:, :, t0:t0 + T], in_=kt[:, :, :]), reads=kt.all())
            for b4 in range(4):
                for half in range(2):
                    ps = next_ps(P)
                    for c in range(8):
                        P.op("pe", lambda e, c=c, b4=b4, half=half, ps=ps: e.matmul(
                            ps[:, :512], lhsT=hn[:, c, b4 * 128:(b4 + 1) * 128], rhs=Wv[:, c, half * 512:(half + 1) * 512],
                            start=(c == 0), stop=(c == 7)), reads=[Wv.r(c), hn.r(c)], writes=[ps.r()])
                    if half == 0:
                        P.op("act", lambda e, b4=b4, half=half, ps=ps, vt=vt: e.copy(out=vt[:, b4, 0:512], in_=ps[:, :512]),
                             reads=[ps.r()], writes=[vt.r(b4 * 2)])
                    else:
                        P.op("dve", lambda e, b4=b4, half=half, ps=ps, vt=vt: e.tensor_copy(out=vt[:, b4, 512:1024], in_=ps[:, :512]),
                             reads=[ps.r()], writes=[vt.r(b4 * 2 + 1)])
            P.dma(lambda e, vt=vt, it=it: e.dma_start(out=Vv[:, it * 4:(it + 1) * 4, :], in_=vt[:, :, :]), reads=vt.all(), q="act")
            ps = next_ps(P)
            for c in range(8):
                P.op("pe", lambda e, c=c, ps=ps: e.matmul(ps[0:64, :T], lhsT=Wi[:, c, 512:576], rhs=hnf[:, c, :],
                                                       start=(c == 0), stop=(c == 7)),
                     reads=[Wi.r(c), hnf.r(c)], writes=[ps.r()])
            P.op("act", lambda e, ps=ps: e.copy(out=kraw[:, :], in_=ps[0:64, :T]), reads=[ps.r()], writes=[kraw.r()])
            P.op("pool", lambda e: e.tensor_tensor(out=ksq[:, :], in0=kraw[:, :], in1=kraw[:, :], op=ALU.mult), reads=[kraw.r()], writes=[ksq.r()])
            p1 = next_ps(P)
            P.op("pe", lambda e, p1=p1: e.matmul(p1[0:64, :T], lhsT=ones_f[0:64, 0:64], rhs=kraw[:, :], start=True, stop=True),
                 reads=[ones_f.r(), kraw.r()], writes=[p1.r()])
            p2 = next_ps(P)
            P.op("pe", lambda e, p2=p2: e.matmul(p2[0:64, :T], lhsT=ones_f[0:64, 0:64], rhs=ksq[:, :], start=True, stop=True),
                 reads=[ones_f.r(), ksq.r()], writes=[p2.r()])
            P.op("act", lambda e, p1=p1: e.activation(out=kmean[:, :], in_=p1[0:64, :T], func=AF.Copy, scale=1.0 / 64), reads=[p1.r()], writes=[kmean.r()])
            P.op("dve", lambda e: e.tensor_tensor(out=ksq[:, :], in0=kmean[:, :], in1=kmean[:, :], op=ALU.mult), reads=[kmean.r(), ksq.r()], writes=[ksq.r()])
            P.op("dve", lambda e, p2=p2: e.scalar_tensor_tensor(out=kvar[:, :], in0=p2[0:64, :T], scalar=1.0 / 64, in1=ksq[:, :],
                                                              op0=ALU.mult, op1=ALU.subtract), reads=[p2.r(), ksq.r()], writes=[kvar.r()])
            P.op("act", lambda e: e.activation(out=kvar[:, :], in_=kvar[:, :], func=AF.Sqrt, bias=C.eps[0:64, :], scale=1.0),
                 reads=[kvar.r(), C.eps.r()], writes=[kvar.r()])
            P.op("dve", lambda e: e.reciprocal(out=kvar[:, :], in_=kvar[:, :]), reads=[kvar.r()], writes=[kvar.r()])
            P.op("pool", lambda e: e.tensor_tensor(out=kraw[:, :], in0=kraw[:, :], in1=kmean[:, :], op=ALU.subtract), reads=[kraw.r(), kmean.r()], writes=[kraw.r()])
            P.op("dve", lambda e: e.scalar_tensor_tensor(out=kraw[:, :], in0=kraw[:, :], scalar=lngt[:, 0:1], in1=kvar[:, :],
                                                        op0=ALU.mult, op1=ALU.mult), reads=[kraw.r(), kvar.r(), lngt.r()], writes=[kraw.r()])
            P.op("act", lambda e: e.activation(out=kio[:, :], in_=kraw[:, :], func=AF.Identity, bias=lnbt[:, 0:1], scale=1.0),
                 reads=[kraw.r(), lnbt.r()], writes=[kio.r()])
            P.dma(lambda e, t0=t0: e.dma_start(out=kiTd[:, t0:t0 + T], in_=kio[:, :]), reads=[kio.r()])
        qtb = P.sb("qtb", [128, 8, 256], BF16, nsub=8)
        qitb = P.sb("qitb", [64, 8, 256], F32, nsub=8)
        for j in range(NJ):
            q0 = j * 256
            P.dma(lambda e, q0=q0: e.dma_start(out=x[:, :, 0:256], in_=xqv[:, :, q0:q0 + 256]), writes=[x.r()])
            norm_tile(P, C, x, gt, sq, hn, rstd, 256)
            for c in range(8):
                P.op("dve", lambda e, c=c: e.scalar_tensor_tensor(out=hnf[:, c, 0:256], in0=x[:, c, 0:256], scalar=gt[:, c:c + 1], in1=rstd[:, 0:256],
                                                                 op0=ALU.mult, op1=ALU.mult), reads=[x.r(), rstd.r(), gt.r()], writes=[hnf.r(c)])
            for h in range(8):
                ps = next_ps(P)
                for c in range(8):
                    P.op("pe", lambda e, c=c, h=h, ps=ps: e.matmul(ps[:, :256], lhsT=Wq[:, c, h * 128:(h + 1) * 128], rhs=hn[:, c, 0:256],
                                                                  start=(c == 0), stop=(c == 7)),
                         reads=[Wq.r(c), hn.r(c)], writes=[ps.r()])
                P.op("act", lambda e, h=h, ps=ps: e.copy(out=qtb[:, h, :], in_=ps[:, :256]), reads=[ps.r()], writes=[qtb.r(h)])
                ps2 = next_ps(P)
                for c in range(8):
                    P.op("pe", lambda e, c=c, h=h, ps2=ps2: e.matmul(ps2[0:64, :256], lhsT=Wi[:, c, h * 64:(h + 1) * 64], rhs=hnf[:, c, 0:256],
                                                                    start=(c == 0), stop=(c == 7)),
                         reads=[Wi.r(c), hnf.r(c)], writes=[ps2.r()])
                P.op("dve", lambda e, h=h, ps2=ps2: e.tensor_copy(out=qitb[:, h, :], in_=ps2[0:64, :256]), reads=[ps2.r()], writes=[qitb.r(h)])
            P.dma(lambda e, q0=q0: e.dma_start(out=qTv[:, :, q0:q0 + 256], in_=qtb[:, :, :]), reads=qtb.all())
            P.dma(lambda e, q0=q0: e.dma_start(out=qiTd[:, :, q0:q0 + 256], in_=qitb[:, :, :]), reads=qitb.all(), q="act")
            for hf in range(2):
                ps = next_ps(P)
                for c in range(8):
                    P.op("pe", lambda e, c=c, hf=hf, ps=ps: e.matmul(ps[:, 0:8], lhsT=hnf[:, c, hf * 128:(hf + 1) * 128], rhs=Wi[:, c, 576:584],
                                                                    start=(c == 0), stop=(c == 7)),
                         reads=[Wi.r(c), hnf.r(c)], writes=[ps.r()])
                P.op("act", lambda e, j=j, hf=hf, ps=ps: e.activation(out=wiT[:, j * 2 + hf, :], in_=ps[:, 0:8], func=AF.Copy, scale=WSC),
                     reads=[ps.r()], writes=[wiT.r()])
        P.pop_scope()

        I = P.sb("I", [128, L], F32)
        Mb = [P.sb(f"Mb{i}", [128, L], BF16) for i in range(2)]
        junk = P.sb("junk", [128, 2048], BF16)
        junkA = P.sb("junkA", [128, 2048], BF16)
        cnA = P.sb("cnA", [128, 8], F32)
        qih = P.sb("qih", [64, 8, 128], F32)
        dg = P.sb("dg", [128, 8, 128], F32)
        rl = [P.sb(f"rl{i}", [128, 512], F32) for i in range(3)]
        kics = [P.sb(f"kic{i}", [64, 512], F32) for i in range(3)]
        tmpb = P.sb("tmpb", [128, 512], F32)
        am = P.sb("am", [128, 32], F32)
        cn = P.sb("cn", [128, 8], F32)
        sm = P.sb("sm", [128, 8], F32)
        qt = P.sb("qt", [128, 4, 256], BF16)
        ktl = [P.sb(f"ktl{i}", [128, 4, 512], BF16) for i in range(2)]
        vtl = [P.sb(f"vtl{i}", [128, 4, 512], BF16) for i in range(2)]
        pts = [P.sb(f"pt{i}", [128, 512], BF16) for i in range(5)]
        rden = P.sb("rden", [128, 512], F32)
        ob = [P.sb(f"ob{i}", [128, 512], F32) for i in range(2)]
        bS = banks[0:3]
        bO = banks[3:5]
        bD = banks[5:7]
        bI = [banks[3], banks[4]]
        outs = []
        rot = {"s": 0, "p": 0, "l": 0, "r": 0, "i": 0, "k": 0}

        def nxt(lst, key):
            rot[key] += 1
            return lst[rot[key] % len(lst)]

        for j in range(NJ):
            Nmax = 256 * (8 * jset[j] + 8)
            ws = Nmax - 2048
            nck = Nmax // 512
            for hf in range(2):
                qi = j * 2 + hf
                M = Mb[hf]
                P.dma(lambda e, j=j, hf=hf: e.dma_start(out=qih[:, :, :], in_=qiTd[:, :, j * 256 + hf * 128:j * 256 + hf * 128 + 128]),
                      writes=[qih.r()])
                for h in range(8):
                    eng = "pool" if h % 2 == 0 else "dve"
                    P.op(eng, lambda e, h=h, qi=qi: e.tensor_scalar(out=dg[:, h, :], in0=Kc[:, 0, :], scalar1=wiT[:, qi, h:h + 1], scalar2=None, op0=ALU.mult),
                         reads=[Kc.r(), wiT.r()], writes=[dg.r()])
                for kc in range(nck):
                    pI = nxt(bI, "i")
                    kic = nxt(kics, "k")
                    P.dma(lambda e, kic=kic, kc=kc: e.dma_start(out=kic[:, :], in_=kiTd[:, kc * 512:(kc + 1) * 512]), writes=[kic.r()], q="act")
                    def score(h, kc=kc, kic=kic):
                        ps = nxt(bS, "s")
                        r = nxt(rl, "r")
                        P.op("pe", lambda e, h=h, ps=ps: e.matmul(ps[:, :512], lhsT=qih[:, h, :], rhs=kic[:, :], start=True, stop=True),
                             reads=[qih.r(), kic.r()], writes=[ps.r()])
                        P.op("act", lambda e, ps=ps, r=r: e.activation(out=r[:, :], in_=ps[:, :512], func=AF.Relu), reads=[ps.r()], writes=[r.r()])
                        return r
                    def accum(h, r, pI=pI):
                        P.op("pe", lambda e, h=h, r=r: e.matmul(pI[:, :512], lhsT=dg[:, h, :], rhs=r[:, :], start=(h == 0), stop=(h == 7)),
                             reads=[dg.r(), r.r()], writes=[pI.r()])
                    prev = score(0)
                    for h in range(1, 8):
                        cur = score(h)
                        accum(h - 1, prev)
                        prev = cur
                    accum(7, prev)
                    P.op("dve", lambda e, pI=pI, kc=kc: e.tensor_reduce(out=am[:, kc:kc + 1], in_=pI[:, :512], axis=AX.X, op=ALU.max, apply_absolute_value=True),
                         reads=[pI.r()], writes=[am.r()])
                    k0 = kc * 512
                    if k0 >= ws:
                        ro = k0 - ws
                        P.op("dve", lambda e, ro=ro, qi=qi: e.tensor_scalar(out=tmpb[:, :], in0=kit[:, ro:ro + 512], scalar1=qrt[:, qi:qi + 1], scalar2=-1e30,
                                                                          op0=ALU.is_gt, op1=ALU.mult), reads=[kit.r(), qrt.r()], writes=[tmpb.r()])
                        P.op("dve", lambda e, pI=pI, k0=k0: e.tensor_tensor(out=I[:, k0:k0 + 512], in0=pI[:, :512], in1=tmpb[:, :], op=ALU.add),
                             reads=[pI.r(), tmpb.r()], writes=[I.r()])
                    else:
                        P.op("dve", lambda e, pI=pI, k0=k0: e.tensor_copy(out=I[:, k0:k0 + 512], in_=pI[:, :512]), reads=[pI.r()], writes=[I.r()])
                P.op("dve", lambda e, nck=nck: e.tensor_reduce(out=sm[:, 0:1], in_=am[:, 0:nck], axis=AX.X, op=ALU.max), reads=[am.r()], writes=[sm.r()])
                P.op("dve", lambda e: e.tensor_scalar(out=sm[:, 1:2], in0=sm[:, 0:1], scalar1=2.0, scalar2=None, op0=ALU.mult), reads=[sm.r()], writes=[sm.r()])
                P.op("dve", lambda e: e.tensor_scalar(out=sm[:, 2:3], in0=sm[:, 0:1], scalar1=-1.0, scalar2=None, op0=ALU.mult), reads=[sm.r()], writes=[sm.r()])
                npc = (Nmax + 2047) // 2048
                for itn in range(NIT):
                    P.op("dve", lambda e, itn=itn: e.tensor_scalar(out=sm[:, 3:4], in0=sm[:, 1:2], scalar1=2.0 ** -(itn + 1), scalar2=None, op0=ALU.mult),
                         reads=[sm.r()], writes=[sm.r()])
                    P.op("dve", lambda e: e.tensor_tensor(out=sm[:, 4:5], in0=sm[:, 2:3], in1=sm[:, 3:4], op=ALU.add), reads=[sm.r()], writes=[sm.r()])
                    dpc = [pc for pc in range(npc) if pc % 2 == 0]
                    apc = [pc for pc in range(npc) if pc % 2 == 1]
                    if apc:
                        P.op("dve", lambda e: e.tensor_scalar(out=sm[:, 7:8], in0=sm[:, 4:5], scalar1=-1.0, scalar2=None, op0=ALU.mult),
                             reads=[sm.r()], writes=[sm.r()])
                    for k, pc in enumerate(dpc):
                        P.op("dve", lambda e, pc=pc, k=k: e.tensor_scalar(out=junk[:, :], in0=I[:, pc * 2048:(pc + 1) * 2048], scalar1=sm[:, 4:5], scalar2=None,
                                                                   op0=ALU.is_ge, op1=ALU.add, accum_out=cn[:, k:k + 1]),
                             reads=[I.r(), sm.r()], writes=[junk.r(), cn.r()])
                    for k, pc in enumerate(apc):
                        P.op("act", lambda e, pc=pc, k=k: e.activation(out=junkA[:, :], in_=I[:, pc * 2048:(pc + 1) * 2048], func=AF.Sign,
                                                                      bias=sm[:, 7:8], scale=1.0, accum_out=cnA[:, k:k + 1]),
                             reads=[I.r(), sm.r()], writes=[junkA.r(), cnA.r()])
                    P.op("dve", lambda e, nd=len(dpc): e.tensor_reduce(out=sm[:, 5:6], in_=cn[:, 0:nd], axis=AX.X, op=ALU.add), reads=[cn.r()], writes=[sm.r()])
                    if apc:
                        na = len(apc)
                        P.op("dve", lambda e, na=na: e.tensor_reduce(out=sm[:, 6:7], in_=cnA[:, 0:na], axis=AX.X, op=ALU.add), reads=[cnA.r()], writes=[sm.r()])
                        P.op("dve", lambda e, na=na: e.tensor_scalar(out=sm[:, 6:7], in0=sm[:, 6:7], scalar1=0.5, scalar2=1024.0 * na, op0=ALU.mult, op1=ALU.add),
                             reads=[sm.r()], writes=[sm.r()])
                        P.op("dve", lambda e: e.tensor_tensor(out=sm[:, 5:6], in0=sm[:, 5:6], in1=sm[:, 6:7], op=ALU.add), reads=[sm.r()], writes=[sm.r()])
                    P.op("dve", lambda e: e.tensor_scalar(out=sm[:, 6:7], in0=sm[:, 5:6], scalar1=255.5, scalar2=sm[:, 3:4], op0=ALU.is_gt, op1=ALU.mult),
                         reads=[sm.r()], writes=[sm.r()])
                    P.op("dve", lambda e: e.tensor_tensor(out=sm[:, 2:3], in0=sm[:, 2:3], in1=sm[:, 6:7], op=ALU.add), reads=[sm.r()], writes=[sm.r()])
                for pc in range(npc):
                    P.op("dve", lambda e, pc=pc, M=M: e.tensor_scalar(out=M[:, pc * 2048:(pc + 1) * 2048], in0=I[:, pc * 2048:(pc + 1) * 2048],
                                                                     scalar1=sm[:, 2:3], scalar2=-30000.0, op0=ALU.is_lt, op1=ALU.mult),
                         reads=[I.r(), sm.r()], writes=[M.r()])
            for pz in range(2):
                P.dma(lambda e, j=j, pz=pz: e.dma_start(out=qt[:, :, :], in_=qTv[:, 4 * pz:4 * pz + 4, j * 256:(j + 1) * 256]), writes=[qt.r()])
                nkb = Nmax // 128
                LA = 2
                pend = []

                def back(kb, pair, pt, vt, kbl, nkb=nkb):
                    for hh in range(2):
                        hl = 2 * pair + hh
                        P.op("pe", lambda e, pair=pair, hh=hh, hl=hl, vt=vt, kbl=kbl, pt=pt, kb=kb, nkb=nkb: e.matmul(
                            bO[pair][:, hh * 256:(hh + 1) * 256], lhsT=vt[:, kbl, hl * 128:(hl + 1) * 128], rhs=pt[:, hh * 256:(hh + 1) * 256],
                            start=(kb == 0 and hh == 0), stop=(kb == nkb - 1 and hh == 1), skip_group_check=True),
                            reads=[vt.r(), pt.r()], writes=[bO[pair].r()])
                    P.op("pe", lambda e, pair=pair, pt=pt, kb=kb, nkb=nkb: e.matmul(
                        bD[pair][:, 0:512], lhsT=C.ones_bf[:, :], rhs=pt[:, :], start=(kb == 0), stop=(kb == nkb - 1)),
                        reads=[C.ones_bf.r(), pt.r()], writes=[bD[pair].r()])

                for kb in range(nkb):
                    kbl = kb % 4
                    if kbl == 0:
                        kt = nxt(ktl, "l")
                        vt = vtl[rot["l"] % len(vtl)]
                        k4 = kb // 4
                        P.dma(lambda e, kt=kt, k4=k4, pz=pz: e.dma_start(out=kt[:, :, :], in_=kTv[:, 4 * pz:4 * pz + 4, k4 * 512:(k4 + 1) * 512]),
                              writes=[kt.r()])
                        P.dma(lambda e, vt=vt, k4=k4, pz=pz: e.dma_start(out=vt[:, :, :], in_=Vv[:, k4 * 4:(k4 + 1) * 4, pz * 512:(pz + 1) * 512]),
                              writes=[vt.r()], q="act")
                    for pair in range(2):
                        pS = nxt(bS, "s")
                        pt = nxt(pts, "p")
                        for hh in range(2):
                            hl = 2 * pair + hh
                            P.op("pe", lambda e, pS=pS, hh=hh, hl=hl, kt=kt, kbl=kbl: e.matmul(
                                pS[:, hh * 256:(hh + 1) * 256], lhsT=kt[:, hl, kbl * 128:(kbl + 1) * 128], rhs=qt[:, hl, :],
                                start=(hh == 0), stop=False, skip_group_check=True), reads=[kt.r(), qt.r()], writes=[pS.r()])
                        for hf in range(2):
                            P.op("pe", lambda e, pS=pS, hf=hf, kb=kb: e.matmul(
                                pS[:, 0:512], lhsT=Mb[hf][:, kb * 128:(kb + 1) * 128], rhs=selb[:, hf, :],
                                start=False, stop=(hf == 1), skip_group_check=True), reads=[Mb[hf].r(), selb.r()], writes=[pS.r()])
                        P.op("act", lambda e, pS=pS, pt=pt: e.activation(out=pt[:, :], in_=pS[:, 0:512], func=AF.Exp, scale=SCALE),
                             reads=[pS.r()], writes=[pt.r()])
                        pend.append((kb, pair, pt, vt, kbl))
                        if len(pend) > LA:
                            back(*pend.pop(0))
                while pend:
                    back(*pend.pop(0))
                for pair in range(2):
                    o = ob[pair]
                    P.op("dve", lambda e, pair=pair: e.reciprocal(out=rden[:, :], in_=bD[pair][:, 0:512]), reads=[bD[pair].r()], writes=[rden.r()])
                    P.op("dve", lambda e, pair=pair, o=o: e.tensor_tensor(out=o[:, :], in0=bO[pair][:, 0:512], in1=rden[:, :], op=ALU.mult),
                         reads=[bO[pair].r(), rden.r()], writes=[o.r()])
                    h0 = 4 * pz + 2 * pair
                    outs.append(P.dma(lambda e, o=o, h0=h0, j=j: e.dma_start(
                        out=oTv[:, h0:h0 + 2, j * 256:(j + 1) * 256], in_=o[:, :].rearrange("p (h q) -> p h q", h=2)), reads=[o.r()]))
        P.emit(final_waits=outs)
    return nc


def _rope_tables(L, dim, theta=10000.0):
    pos = np.arange(L, dtype=np.float32)
    inv = (theta ** (-np.arange(0, dim, 2, dtype=np.float32) / dim)).astype(np.float32)
    ang = pos[:, None] * inv[None, :]
    return np.cos(ang).astype(np.float32), np.sin(ang).astype(np.float32)


def _mla_inputs(hT, g, w_in, gq, w_uq, gkv, w_ukv, core):
    z64 = np.zeros((1024, 64), np.float32)
    kr = w_in[:, 640:672]
    krs = np.concatenate([kr[:, 16:], kr[:, :16]], 1)
    wall = np.concatenate([w_in[:, :640], z64, kr, z64, krs], 1)
    cols = []
    for h in (2 * core, 2 * core + 1):
        wq = w_uq[:, h * 96:(h + 1) * 96]
        wqs = np.concatenate([np.zeros((384, 64), np.float32), wq[:, 80:96], wq[:, 64:80]], 1)
        cols += [wq, wqs]
    wuq = np.concatenate(cols, 1)
    kn = [w_ukv[:, h * 128:h * 128 + 64] for h in (2 * core, 2 * core + 1)]
    vv = [w_ukv[:, h * 128 + 64:h * 128 + 128] for h in (2 * core, 2 * core + 1)]
    wukv = np.concatenate(kn + vv, 1)
    return dict(hT=hT, g=g, wall=np.ascontiguousarray(wall), gq=gq, gkv=gkv, wuq=np.ascontiguousarray(wuq),
                wukv=np.ascontiguousarray(wukv))


def _mla_consts(L):
    cos, sin = _rope_tables(L, 32)
    cos2 = np.zeros((96, L), np.float32)
    sin2 = np.zeros((96, L), np.float32)
    cos2[64:80] = cos.T
    cos2[80:96] = cos.T
    sin2[64:80] = -sin.T
    sin2[80:96] = sin.T
    k = np.arange(128)[:, None, None]
    d = np.arange(4)[None, :, None]
    q = np.arange(512)[None, None, :]
    cmask = ((d * 128 + k) <= q).astype(np.float32)
    esel = np.zeros((65, 64), np.float32)
    esel[64] = 1.0
    return dict(cos2=cos2, sin2=sin2, cmask=np.ascontiguousarray(cmask), esel=esel)


def _gdn_inputs(hT, g, w_in, conv_w, a_log, dt_bias, og, h):
    cols = [w_in[:, h * 128:(h + 1) * 128], w_in[:, 1024 + h * 128:1024 + (h + 1) * 128],
            w_in[:, 2048 + h * 128:2048 + (h + 1) * 128], w_in[:, 3072 + h * 128:3072 + (h + 1) * 128],
            w_in[:, 4096 + h:4097 + h], w_in[:, 4104 + h:4105 + h], np.zeros((1024, 126), np.float32)]
    wh = np.ascontiguousarray(np.concatenate(cols, 1))
    cw = np.concatenate([conv_w[:, h * 128:(h + 1) * 128], conv_w[:, 1024 + h * 128:1024 + (h + 1) * 128],
                         conv_w[:, 2048 + h * 128:2048 + (h + 1) * 128]], 1)
    sc = np.zeros((128, 2), np.float32)
    sc[:, 0] = a_log[h]
    sc[:, 1] = dt_bias[h]
    return dict(hT=hT, g=g, wh=wh, cw=np.ascontiguousarray(cw.T), sc=sc, og=og)


def _gdn_consts():
    j = np.arange(128)[:, None]
    i = np.arange(128)[None, :]
    same = (j // 64) == (i // 64)
    cst = np.zeros((128, 5, 128), np.float32)
    cst[:, 0] = np.eye(128)
    cst[:, 1] = (same & (i > j))
    cst[:, 2] = (same & (i >= j))
    cst[:, 3] = same
    cst[:, 4, 0] = (np.arange(128) < 64)
    cst[:, 4, 1] = (np.arange(128) >= 64)
    return dict(cst=cst)


def _dsa_inputs(xT, g, w_in, lg, lb, core, L, jset=None):
    jset = list(range(L // 256 // 8)) if jset is None else list(jset)
    NJ = len(jset)
    cols = []
    qrel = np.zeros((128, NJ * 2), np.float32)
    for jj, j in enumerate(jset):
        tq = 8 * j + core
        cols.append(xT[:, tq * 256:(tq + 1) * 256])
        ws = 256 * (8 * j + 8) - 2048
        for hf in range(2):
            qrel[:, jj * 2 + hf] = tq * 256 + hf * 128 + np.arange(128) - ws
    return dict(xT=xT, xq=np.ascontiguousarray(np.concatenate(cols, 1)), g=g, win=w_in, lng=lg, lnb=lb, qrel=qrel)


def _dsa_consts():
    kidx = np.tile(np.arange(2048, dtype=np.float32)[None, :], (128, 1))
    cst = np.zeros((128, 5, 128), np.float32)
    cst[:, 0] = np.eye(128)
    return dict(kidx=kidx, cst=cst)


def _dsa_gather(outs, L, jset=None, full=None):
    jset = list(range(L // 256 // 8)) if jset is None else list(jset)
    if full is None:
        full = np.zeros((1024, L), np.float32)
    for c, o in enumerate(outs):
        for jj, j in enumerate(jset):
            tq = 8 * j + c
            full[:, tq * 256:(tq + 1) * 256] = o[:, jj * 256:(jj + 1) * 256]
    return full


_PROGS = {}
DSA_SPLITS = ([0, 1, 2, 3, 4], [5, 6, 7])
GDN_SEG = 4096


def _prog(name, fn):
    if name not in _PROGS:
        _PROGS[name] = fn()
    return _PROGS[name]


def _run(nc, in_maps):
    res = run_bass_kernel_spmd(nc, in_maps, core_ids=list(range(8)))
    return [r["oT"] for r in res.results]


def _split(hT, TOK=2048):
    return [np.ascontiguousarray(hT[:, i * TOK:(i + 1) * TOK]) for i in range(8)]


def kernel(**inp):
    f32 = lambda a: np.ascontiguousarray(np.asarray(a, dtype=np.float32))
    L = 16384
    TOK = L // 8
    x = f32(inp["x"])[0]
    hT = np.ascontiguousarray(x.T)
    ident = np.eye(128, dtype=np.float32)

    def outproj(hT, mT, w):
        nc = _prog("outproj", lambda: build_outproj(TOK, 512))
        hs, ms = _split(hT), _split(mT)
        return np.concatenate(_run(nc, [dict(hT=hs[i], mT=ms[i], w=w) for i in range(8)]), axis=1)

    def mlp(hT, i, final=False):
        hs = _split(hT)
        g, w1, w2 = f32(inp["norm_mlp_g"][i]), f32(inp["mlp_w1"][i]), f32(inp["mlp_w2"][i])
        if final:
            nc = _prog("mlpf", lambda: build_mlp(TOK, 256, True))
            fg = f32(inp["final_g"])
            maps = [dict(hT=hs[c], g=g, w1=w1, w2=w2, fg=fg) for c in range(8)]
        else:
            nc = _prog("mlp", lambda: build_mlp(TOK, 256, False))
            maps = [dict(hT=hs[c], g=g, w1=w1, w2=w2) for c in range(8)]
        return np.concatenate(_run(nc, maps), axis=1)

    cst = _dsa_consts()
    g0, win0 = f32(inp["norm_mix_g"][0]), f32(inp["dsa_w_in"][0])
    lg, lb = f32(inp["dsa_idx_k_g"][0]), f32(inp["dsa_idx_k_b"][0])
    mT = None
    for js in DSA_SPLITS:
        nc = _prog("dsa" + str(js), lambda: build_dsa(L, 18, js))
        outs = _run(nc, [{**_dsa_inputs(hT, g0, win0, lg, lb, c, L, js), **cst} for c in range(8)])
        mT = _dsa_gather(outs, L, js, mT)
    hT = outproj(hT, mT, f32(inp["dsa_w_out"][0]))
    hT = mlp(hT, 0)
    nc = _prog("conv", lambda: build_conv(TOK, 256))
    hp = np.concatenate([np.zeros((1024, 30), np.float32), hT], axis=1)
    cw = dict(g=f32(inp["norm_mix_g"][1]), w1=f32(inp["conv_w_pw1"][0]), b1=f32(inp["conv_b_pw1"][0]),
              wdT=np.ascontiguousarray(f32(inp["conv_w_dw"][0]).T), bd=f32(inp["conv_b_dw"][0]),
              lg=f32(inp["conv_ln_g"][0]), lb=f32(inp["conv_ln_b"][0]), w2=f32(inp["conv_w_pw2"][0]),
              b2=f32(inp["conv_b_pw2"][0]), ident=ident)
    maps = [dict(hT=np.ascontiguousarray(hp[:, c * TOK:c * TOK + TOK + 30]),
                 hs=np.full((128, 1), 0.0 if c == 0 else 1.0, np.float32), **cw) for c in range(8)]
    hT = np.concatenate(_run(nc, maps), axis=1)
    hT = mlp(hT, 1)
    nc = _prog("mla", lambda: build_mla(L))
    cst = _mla_consts(L)
    maps = [{**_mla_inputs(hT, f32(inp["norm_mix_g"][2]), f32(inp["mla_w_in"][0]), f32(inp["mla_q_norm_g"][0]),
                           f32(inp["mla_w_uq"][0]), f32(inp["mla_kv_norm_g"][0]), f32(inp["mla_w_ukv"][0]), c), **cst}
            for c in range(8)]
    mT = np.concatenate(_run(nc, maps), axis=0)
    hT = outproj(hT, mT, f32(inp["mla_w_out"][0]))
    hT = mlp(hT, 2)
    LG = GDN_SEG
    nc = _prog("gdn", lambda: build_gdn(LG))
    cst = _gdn_consts()
    gi = [_gdn_inputs(None, f32(inp["norm_mix_g"][3]), f32(inp["gdn_w_in"][0]), f32(inp["gdn_conv_w"][0]),
                      f32(inp["gdn_a_log"][0]), f32(inp["gdn_dt_bias"][0]), f32(inp["gdn_o_norm_g"][0]), c) for c in range(8)]
    st = [np.zeros((128, 128), np.float32) for _ in range(8)]
    hl = [np.zeros((128, 3, 3), np.float32) for _ in range(8)]
    segs = []
    for s0 in range(0, L, LG):
        hseg = np.ascontiguousarray(hT[:, s0:s0 + LG])
        maps = [{**gi[c], **cst, "hT": hseg, "s_in": st[c], "h_in": hl[c]} for c in range(8)]
        res = run_bass_kernel_spmd(nc, maps, core_ids=list(range(8))).results
        segs.append(np.concatenate([r["oT"] for r in res], axis=0))
        st = [np.ascontiguousarray(r["s_out"]) for r in res]
        hl = [np.ascontiguousarray(r["h_out"]) for r in res]
    mT = np.concatenate(segs, axis=1)
    hT = outproj(hT, mT, f32(inp["gdn_w_out"][0]))
    hT = mlp(hT, 3, final=True)
    return np.ascontiguousarray(hT.T)[None].astype(np.float32)
```

```python
import numpy as np
import concourse.bass as bass
import concourse.mybir as mybir
from contextlib import ExitStack
from concourse.bass_utils import run_bass_kernel_spmd

F32 = mybir.dt.float32
BF16 = mybir.dt.bfloat16
U8 = mybir.dt.uint8
I32 = mybir.dt.int32
ALU = mybir.AluOpType
AF = mybir.ActivationFunctionType
AX = mybir.AxisListType

ENGS = ["pe", "act", "dve", "pool", "sp"]
NDMA = 8


class Res:
    __slots__ = ("name", "lastw", "readers")

    def __init__(self, name):
        self.name = name
        self.lastw = None
        self.readers = []


class Tile:
    def __init__(self, t, name, nsub=1):
        self.t = t
        self.name = name
        self.res = [Res(f"{name}.{i}") for i in range(nsub)]

    def __getitem__(self, idx):
        return self.t[idx]

    def r(self, i=0):
        return self.res[i]

    def all(self):
        return list(self.res)


class View:
    def __init__(self, base, off, width, name, share=True):
        self.base = base
        self.off = off
        self.width = width
        self.res = base.res if share else [Res(name)]

    def __getitem__(self, idx):
        rows, cols = idx
        start = cols.start or 0
        stop = self.width if cols.stop is None else cols.stop
        return self.base.t[rows, self.off + start:self.off + stop]

    def r(self, i=0):
        return self.res[0]

    def all(self):
        return list(self.res)


class Prog:
    def __init__(self, nc, stack):
        self.nc = nc
        self.stack = stack
        self.ops = {e: [] for e in ENGS}
        self.cnt = {e: 0 for e in ENGS}
        self.sem = {e: stack.enter_context(nc.semaphore(f"s_{e}")) for e in ENGS if e != "sp"}
        self.dsem = {q: [stack.enter_context(nc.semaphore(f"d_{q}{i}")) for i in range(NDMA)]
                     for q in ("sp", "pool", "act")}
        self.dcnt = {q: 0 for q in ("sp", "pool", "act")}
        self.dtok = {q: [] for q in ("sp", "pool", "act")}
        self.known = {e: {} for e in ENGS}
        self.nops = 0

    def sb(self, name, shape, dtype, nsub=1):
        t = self.stack.enter_context(self.nc.sbuf_tensor("sb_" + name, list(shape), dtype))
        return Tile(t, name, nsub)

    def ps(self, name, shape, dtype=F32, nsub=1):
        t = self.stack.enter_context(self.nc.psum_tensor("ps_" + name, list(shape), dtype))
        return Tile(t, name, nsub)

    def push_scope(self):
        self._saved_stack = self.stack
        self.stack = ExitStack()
        return self.stack

    def pop_scope(self):
        self.barrier()
        self.stack.close()
        self.stack = self._saved_stack

    def barrier(self):
        toks = []
        for e in ENGS:
            if e != "sp" and self.cnt[e] > 0:
                toks.append((("c", e), self.cnt[e], e))
        for q in self.dtok:
            toks += self.dtok[q][-NDMA:]
        self._pending = {e: list(toks) for e in ENGS}

    def _deps(self, eng, reads, writes):
        deps = {}
        for tok in getattr(self, "_pending", {}).get(eng, []):
            if not (tok[2] == eng and eng == "pe"):
                if deps.get(tok[0], (0,))[0] < tok[1]:
                    deps[tok[0]] = (tok[1], tok)
        if getattr(self, "_pending", None):
            self._pending[eng] = []
        def add(tok):
            if tok is None:
                return
            key, val, teng = tok
            if teng == eng and eng == "pe":
                return
            if deps.get(key, (0,))[0] < val:
                deps[key] = (val, tok)
        for r in reads:
            add(r.lastw)
        for w in writes:
            add(w.lastw)
            for t in w.readers:
                add(t)
        out = []
        kn = self.known[eng]
        for key, (val, tok) in deps.items():
            if kn.get(key, 0) >= val:
                continue
            kn[key] = val
            out.append(tok)
        return out

    def _commit(self, tok, reads, writes):
        for r in reads:
            r.readers.append(tok)
        for w in writes:
            w.lastw = tok
            w.readers = []

    def op(self, eng, fn, reads=(), writes=()):
        waits = self._deps(eng, reads, writes)
        self.cnt[eng] += 1
        tok = (("c", eng), self.cnt[eng], eng)
        self.ops[eng].append((waits, fn, tok))
        self._commit(tok, reads, writes)
        self.nops += 1
        return tok

    def dma(self, fn, reads=(), writes=(), q="sp"):
        eng = q
        waits = self._deps(eng, reads, writes)
        i = self.dcnt[q]
        self.dcnt[q] += 1
        slot = i % NDMA
        val = 16 * (i // NDMA + 1)
        if i >= NDMA:
            prev = self.dtok[q][i - NDMA]
            key, pval, _ = prev
            if self.known[eng].get(key, 0) < pval:
                self.known[eng][key] = pval
                waits.append(prev)
        tok = (("d", q, slot), val, "dma")
        self.dtok[q].append(tok)
        self.ops[eng].append((waits, fn, tok))
        self._commit(tok, reads, writes)
        self.nops += 1
        return tok

    def _semof(self, tok):
        key = tok[0]
        if key[0] == "c":
            return self.sem[key[1]]
        return self.dsem[key[1]][key[2]]

    def emit(self, final_waits=()):
        nc = self.nc
        with nc.Block() as block:
            def run(eng, e):
                for waits, fn, tok in self.ops[eng]:
                    for w in waits:
                        e.wait_ge(self._semof(w), w[1])
                    ins = fn(e)
                    if tok[2] == "dma":
                        ins.then_inc(self._semof(tok), 16)
                    else:
                        ins.then_inc(self._semof(tok), 1)
                if eng == "sp":
                    for w in final_waits:
                        e.wait_ge(self._semof(w), w[1])

            @block.tensor
            def _(e):
                run("pe", e)

            @block.scalar
            def _(e):
                run("act", e)

            @block.vector
            def _(e):
                run("dve", e)

            @block.gpsimd
            def _(e):
                run("pool", e)

            @block.sync
            def _(e):
                run("sp", e)


def load_w_bf16(P, w_ap, KC, Fo, name, q="pool"):
    t = P.sb(name, [128, KC, Fo], BF16, nsub=KC)
    wv = w_ap.rearrange("(c p) f -> p c f", p=128)
    for c in range(KC):
        P.dma(lambda e, c=c: e.dma_start(out=t[:, c, :], in_=wv[:, c, :], max_dma_last_dim=8192),
              writes=[t.r(c)], q=q)
    return t


def load_vec_fm(P, v_ap, KC, name):
    t = P.sb(name, [128, KC], F32)
    vv = v_ap.rearrange("(c p) -> p c", p=128)
    P.dma(lambda e: e.dma_start(out=t[:, :], in_=vv, allow_slow_non_contiguous=True), writes=[t.r()])
    return t


class Ctx:
    pass


def make_consts(P):
    C = Ctx()
    C.ones_bf = P.sb("ones_bf", [128, 128], BF16)
    P.op("dve", lambda e: e.memset(C.ones_bf[:, :], 1.0), writes=[C.ones_bf.r()])
    C.eps = P.sb("eps_t", [128, 1], F32)
    P.op("dve", lambda e: e.memset(C.eps[:, :], 1e-6), writes=[C.eps.r()])
    return C


def rms_rstd(P, C, x, KC, T, psum, sq, rstd, dim):
    for c in range(KC):
        P.op("act", lambda e, c=c: e.activation(out=sq[:, c, :], in_=x[:, c, :], func=AF.Square),
             reads=[x.r()], writes=[sq.r(c)])
    for c in range(KC):
        P.op("pe", lambda e, c=c: e.matmul(psum[:, :T], lhsT=C.ones_bf[:, :], rhs=sq[:, c, :],
                                          start=(c == 0), stop=(c == KC - 1)),
             reads=[sq.r(c), C.ones_bf.r()], writes=[psum.r()])
    P.op("act", lambda e: e.activation(out=rstd[:, :], in_=psum[:, :T], func=AF.Sqrt,
                                       bias=C.eps[:, :], scale=1.0 / dim),
         reads=[psum.r(), C.eps.r()], writes=[rstd.r()])
    P.op("dve", lambda e: e.reciprocal(out=rstd[:, :], in_=rstd[:, :]), reads=[rstd.r()], writes=[rstd.r()])


def build_mlp(TOK=2048, T=256, final=False):
    nc = bass.Bass("TRN2", target_bir_lowering=False)
    hT = nc.dram_tensor("hT", [1024, TOK], F32, kind="ExternalInput").ap()
    g = nc.dram_tensor("g", [1024], F32, kind="ExternalInput").ap()
    w1 = nc.dram_tensor("w1", [1024, 4096], F32, kind="ExternalInput").ap()
    w2 = nc.dram_tensor("w2", [4096, 1024], F32, kind="ExternalInput").ap()
    oT = nc.dram_tensor("oT", [1024, TOK], F32, kind="ExternalOutput").ap()
    with ExitStack() as st:
        st.enter_context(nc.allow_low_precision("bf16 matmul operands, fp32 accumulate"))
        P = Prog(nc, st)
        C = make_consts(P)
        gt = load_vec_fm(P, g, 8, "g")
        W1 = load_w_bf16(P, w1, 8, 4096, "W1")
        W2 = load_w_bf16(P, w2, 32, 1024, "W2")
        fgt = None
        if final:
            fg = nc.dram_tensor("fg", [1024], F32, kind="ExternalInput").ap()
            fgt = load_vec_fm(P, fg, 8, "fg")
        mlp_body(P, C, hT, oT, gt, W1, W2, TOK, T, fgt)
        P.emit(final_waits=P.out_toks)
    return nc


def mlp_body(P, C, hT, oT, gt, W1, W2, TOK, T, fgt=None):
    hv = hT.rearrange("(c p) t -> p c t", p=128)
    ov = oT.rearrange("(c p) t -> p c t", p=128)
    NB = 2
    xs = [P.sb(f"x{i}", [128, 8, T], F32) for i in range(NB)]
    sqs = [P.sb(f"sq{i}", [128, 8, T], BF16, nsub=8) for i in range(NB)]
    hns = [P.sb(f"hn{i}", [128, 8, T], BF16, nsub=8) for i in range(NB)]
    rstds = [P.sb(f"rstd{i}", [128, T], F32) for i in range(NB)]
    aT = P.sb("aT", [128, 32, T], BF16, nsub=32)
    rl = [P.sb(f"rl{i}", [128, T], BF16) for i in range(2)]
    ys = [P.sb(f"y{i}", [128, 8, T], F32, nsub=8) for i in range(NB)]
    pss = [P.ps(f"ps{i}", [128, 512], F32) for i in range(8)]
    P.out_toks = []
    pi = 0
    for it in range(TOK // T):
        b = it % NB
        x, sq, hn, rstd, y = xs[b], sqs[b], hns[b], rstds[b], ys[b]
        t0 = it * T
        P.dma(lambda e, x=x, t0=t0: e.dma_start(out=x[:, :, :], in_=hv[:, :, t0:t0 + T]), writes=[x.r()])
        ps = pss[pi % 8]; pi += 1
        rms_rstd(P, C, x, 8, T, ps, sq, rstd, 1024.0)
        for c in range(8):
            P.op("dve", lambda e, c=c, x=x, hn=hn, rstd=rstd: e.scalar_tensor_tensor(
                out=hn[:, c, :], in0=x[:, c, :], scalar=gt[:, c:c + 1], in1=rstd[:, :],
                op0=ALU.mult, op1=ALU.mult), reads=[x.r(), rstd.r(), gt.r()], writes=[hn.r(c)])
        for f in range(32):
            ps = pss[pi % 8]; pi += 1
            for c in range(8):
                P.op("pe", lambda e, c=c, f=f, ps=ps, hn=hn: e.matmul(
                    ps[:, :T], lhsT=W1[:, c, f * 128:(f + 1) * 128], rhs=hn[:, c, :],
                    start=(c == 0), stop=(c == 7)), reads=[W1.r(c), hn.r(c)], writes=[ps.r()])
            r = rl[f % 2]
            P.op("act", lambda e, ps=ps, r=r: e.activation(out=r[:, :], in_=ps[:, :T], func=AF.Relu),
                 reads=[ps.r()], writes=[r.r()])
            eng = "pool" if f % 2 == 0 else "dve"
            P.op(eng, lambda e, f=f, r=r: e.tensor_tensor(out=aT[:, f, :], in0=r[:, :], in1=r[:, :], op=ALU.mult),
                 reads=[r.r()], writes=[aT.r(f)])
        for o in range(8):
            ps = pss[pi % 8]; pi += 1
            for f in range(32):
                P.op("pe", lambda e, o=o, f=f, ps=ps: e.matmul(
                    ps[:, :T], lhsT=W2[:, f, o * 128:(o + 1) * 128], rhs=aT[:, f, :],
                    start=(f == 0), stop=(f == 31)), reads=[W2.r(f), aT.r(f)], writes=[ps.r()])
            P.op("dve", lambda e, o=o, ps=ps, x=x, y=y: e.tensor_tensor(
                out=y[:, o, :], in0=ps[:, :T], in1=x[:, o, :], op=ALU.add),
                reads=[ps.r(), x.r()], writes=[y.r(o)])
        if fgt is not None:
            ps = pss[pi % 8]; pi += 1
            for c in range(8):
                P.op("act", lambda e, c=c, y=y, sq=sq: e.activation(out=sq[:, c, :], in_=y[:, c, :], func=AF.Square),
                     reads=[y.r(c)], writes=[sq.r(c)])
            for c in range(8):
                P.op("pe", lambda e, c=c, ps=ps, sq=sq: e.matmul(ps[:, :T], lhsT=C.ones_bf[:, :], rhs=sq[:, c, :],
                                                              start=(c == 0), stop=(c == 7)),
                     reads=[sq.r(c), C.ones_bf.r()], writes=[ps.r()])
            P.op("act", lambda e, ps=ps, rstd=rstd: e.activation(out=rstd[:, :], in_=ps[:, :T], func=AF.Sqrt,
                                                               bias=C.eps[:, :], scale=1.0 / 1024),
                 reads=[ps.r(), C.eps.r()], writes=[rstd.r()])
            P.op("dve", lambda e, rstd=rstd: e.reciprocal(out=rstd[:, :], in_=rstd[:, :]), reads=[rstd.r()], writes=[rstd.r()])
            for c in range(8):
                P.op("dve", lambda e, c=c, y=y, rstd=rstd: e.scalar_tensor_tensor(
                    out=y[:, c, :], in0=y[:, c, :], scalar=fgt[:, c:c + 1], in1=rstd[:, :], op0=ALU.mult, op1=ALU.mult),
                    reads=[y.r(c), rstd.r(), fgt.r()], writes=[y.r(c)])
        tok = P.dma(lambda e, y=y, t0=t0: e.dma_start(out=ov[:, :, t0:t0 + T], in_=y[:, :, :]),
                    reads=y.all())
        P.out_toks.append(tok)


def next_ps(P):
    i = getattr(P, "_psi", 0)
    P._psi = i + 1
    return P.pss[i % len(P.pss)]


def alloc_ps(P, n=8):
    P.pss = [P.ps(f"ps{i}", [128, 512], F32) for i in range(n)]
    P._psi = 0


def load_const(P, ap, shape, name, dtype=F32, q="sp"):
    t = P.sb(name, shape, dtype)
    P.dma(lambda e: e.dma_start(out=t[tuple(slice(None) for _ in shape)], in_=ap), writes=[t.r()], q=q)
    return t


def build_outproj(TOK=2048, T=512):
    nc = bass.Bass("TRN2", target_bir_lowering=False)
    hT = nc.dram_tensor("hT", [1024, TOK], F32, kind="ExternalInput").ap()
    mT = nc.dram_tensor("mT", [1024, TOK], F32, kind="ExternalInput").ap()
    w = nc.dram_tensor("w", [1024, 1024], F32, kind="ExternalInput").ap()
    oT = nc.dram_tensor("oT", [1024, TOK], F32, kind="ExternalOutput").ap()
    hv = hT.rearrange("(c p) t -> p c t", p=128)
    mv = mT.rearrange("(c p) t -> p c t", p=128)
    ov = oT.rearrange("(c p) t -> p c t", p=128)
    with ExitStack() as st:
        st.enter_context(nc.allow_low_precision("bf16 matmul operands, fp32 accumulate"))
        P = Prog(nc, st)
        alloc_ps(P)
        W = load_w_bf16(P, w, 8, 1024, "W")
        NB = 2
        xs = [P.sb(f"x{i}", [128, 8, T], F32) for i in range(NB)]
        ms = [P.sb(f"m{i}", [128, 8, T], F32) for i in range(NB)]
        mb = [P.sb(f"mb{i}", [128, 8, T], BF16, nsub=8) for i in range(NB)]
        ys = [P.sb(f"y{i}", [128, 8, T], F32, nsub=8) for i in range(NB)]
        outs = []
        for it in range(TOK // T):
            b = it % NB
            x, m, mbb, y = xs[b], ms[b], mb[b], ys[b]
            t0 = it * T
            P.dma(lambda e, x=x, t0=t0: e.dma_start(out=x[:, :, :], in_=hv[:, :, t0:t0 + T]), writes=[x.r()])
            P.dma(lambda e, m=m, t0=t0: e.dma_start(out=m[:, :, :], in_=mv[:, :, t0:t0 + T]), writes=[m.r()], q="act")
            for c in range(8):
                eng = "act" if c % 2 == 0 else "pool"
                if eng == "act":
                    P.op("act", lambda e, c=c, m=m, mbb=mbb: e.copy(out=mbb[:, c, :], in_=m[:, c, :]),
                         reads=[m.r()], writes=[mbb.r(c)])
                else:
                    P.op("pool", lambda e, c=c, m=m, mbb=mbb: e.tensor_copy(out=mbb[:, c, :], in_=m[:, c, :]),
                         reads=[m.r()], writes=[mbb.r(c)])
            for o in range(8):
                ps = next_ps(P)
                for c in range(8):
                    P.op("pe", lambda e, o=o, c=c, ps=ps, mbb=mbb: e.matmul(
                        ps[:, :T], lhsT=W[:, c, o * 128:(o + 1) * 128], rhs=mbb[:, c, :],
                        start=(c == 0), stop=(c == 7)), reads=[W.r(c), mbb.r(c)], writes=[ps.r()])
                P.op("dve", lambda e, o=o, ps=ps, x=x, y=y: e.tensor_tensor(
                    out=y[:, o, :], in0=ps[:, :T], in1=x[:, o, :], op=ALU.add),
                    reads=[ps.r(), x.r()], writes=[y.r(o)])
            outs.append(P.dma(lambda e, y=y, t0=t0: e.dma_start(out=ov[:, :, t0:t0 + T], in_=y[:, :, :]),
                              reads=y.all()))
        P.emit(final_waits=outs)
    return nc


def build_conv(TOK=2048, T=256):
    HALO = 30
    NT = TOK + HALO
    nc = bass.Bass("TRN2", target_bir_lowering=False)
    hT = nc.dram_tensor("hT", [1024, NT], F32, kind="ExternalInput").ap()
    g = nc.dram_tensor("g", [1024], F32, kind="ExternalInput").ap()
    w1 = nc.dram_tensor("w1", [1024, 2048], F32, kind="ExternalInput").ap()
    b1 = nc.dram_tensor("b1", [2048], F32, kind="ExternalInput").ap()
    wdT = nc.dram_tensor("wdT", [1024, 31], F32, kind="ExternalInput").ap()
    bd = nc.dram_tensor("bd", [1024], F32, kind="ExternalInput").ap()
    lg = nc.dram_tensor("lg", [1024], F32, kind="ExternalInput").ap()
    lb = nc.dram_tensor("lb", [1024], F32, kind="ExternalInput").ap()
    w2 = nc.dram_tensor("w2", [1024, 1024], F32, kind="ExternalInput").ap()
    b2 = nc.dram_tensor("b2", [1024], F32, kind="ExternalInput").ap()
    hs = nc.dram_tensor("hs", [128, 1], F32, kind="ExternalInput").ap()
    ident = nc.dram_tensor("ident", [128, 128], F32, kind="ExternalInput").ap()
    oT = nc.dram_tensor("oT", [1024, TOK], F32, kind="ExternalOutput").ap()
    hv = hT.rearrange("(c p) t -> p c t", p=128)
    ov = oT.rearrange("(c p) t -> p c t", p=128)
    with ExitStack() as st:
        st.enter_context(nc.allow_low_precision("bf16 matmul operands, fp32 accumulate"))
        P = Prog(nc, st)
        alloc_ps(P)
        C = make_consts(P)
        ones_f = P.sb("ones_f", [128, 128], F32)
        P.op("dve", lambda e: e.memset(ones_f[:, :], 1.0), writes=[ones_f.r()])
        gt = load_vec_fm(P, g, 8, "g")
        b1t = load_vec_fm(P, b1, 16, "b1")
        bdt = load_vec_fm(P, bd, 8, "bd")
        lgt = load_vec_fm(P, lg, 8, "lg")
        lbt = load_vec_fm(P, lb, 8, "lb")
        b2t = load_vec_fm(P, b2, 8, "b2")
        hst = load_const(P, hs, [128, 1], "hs")
        idf = load_const(P, ident, [128, 128], "idf")
        wd = P.sb("wd", [128, 8, 31], F32)
        P.dma(lambda e: e.dma_start(out=wd[:, :, :], in_=wdT.rearrange("(c p) j -> p c j", p=128)), writes=[wd.r()])
        W1 = load_w_bf16(P, w1, 8, 2048, "W1")
        W2 = load_w_bf16(P, w2, 8, 1024, "W2")
        diag = P.sb("diag", [128, 8 * 31, 128], BF16, nsub=8)
        for c in range(8):
            for j in range(31):
                eng = "pool" if (j % 2 == 0) else "dve"
                P.op(eng, lambda e, c=c, j=j: e.tensor_scalar(
                    out=diag[:, c * 31 + j, :], in0=idf[:, :], scalar1=wd[:, c, j:j + 1], scalar2=None,
                    op0=ALU.mult), reads=[idf.r(), wd.r()], writes=[diag.r(c)])
        uT = P.sb("uT", [128, 8, NT], BF16, nsub=8)
        NB = 2
        xs = [P.sb(f"x{i}", [128, 8, T], F32) for i in range(NB)]
        sqs = [P.sb(f"sq{i}", [128, 8, T], BF16, nsub=8) for i in range(1)] * 2
        hns = [P.sb(f"hn{i}", [128, 8, T], BF16, nsub=8) for i in range(1)] * 2
        rstds = [P.sb(f"rstd{i}", [128, T], F32) for i in range(1)] * 2
        sig = [P.sb(f"sig{i}", [128, T], F32) for i in range(2)]
        segs = [(0, HALO)] + [(HALO + k * T, T) for k in range(TOK // T)]
        for it, (s0, L) in enumerate(segs):
            b = it % NB
            x, sq, hn, rstd = xs[b], sqs[b], hns[b], rstds[b]
            P.dma(lambda e, x=x, s0=s0, L=L: e.dma_start(out=x[:, :, :L], in_=hv[:, :, s0:s0 + L]), writes=[x.r()])
            ps = next_ps(P)
            for c in range(8):
                P.op("act", lambda e, c=c, x=x, sq=sq, L=L: e.activation(out=sq[:, c, :L], in_=x[:, c, :L], func=AF.Square),
                     reads=[x.r()], writes=[sq.r(c)])
            for c in range(8):
                P.op("pe", lambda e, c=c, ps=ps, sq=sq, L=L: e.matmul(ps[:, :L], lhsT=C.ones_bf[:, :], rhs=sq[:, c, :L],
                                                                  start=(c == 0), stop=(c == 7)),
                     reads=[sq.r(c), C.ones_bf.r()], writes=[ps.r()])
            P.op("act", lambda e, ps=ps, rstd=rstd, L=L: e.activation(out=rstd[:, :L], in_=ps[:, :L], func=AF.Sqrt,
                                                               bias=C.eps[:, :], scale=1.0 / 1024),
                 reads=[ps.r(), C.eps.r()], writes=[rstd.r()])
            P.op("dve", lambda e, rstd=rstd, L=L: e.reciprocal(out=rstd[:, :L], in_=rstd[:, :L]),
                 reads=[rstd.r()], writes=[rstd.r()])
            for c in range(8):
                P.op("dve", lambda e, c=c, x=x, hn=hn, rstd=rstd, L=L: e.scalar_tensor_tensor(
                    out=hn[:, c, :L], in0=x[:, c, :L], scalar=gt[:, c:c + 1], in1=rstd[:, :L],
                    op0=ALU.mult, op1=ALU.mult), reads=[x.r(), rstd.r(), gt.r()], writes=[hn.r(c)])
            for j in range(8):
                psa = next_ps(P)
                psg = next_ps(P)
                for c in range(8):
                    P.op("pe", lambda e, c=c, j=j, psa=psa, hn=hn, L=L: e.matmul(
                        psa[:, :L], lhsT=W1[:, c, j * 128:(j + 1) * 128], rhs=hn[:, c, :L],
                        start=(c == 0), stop=(c == 7)), reads=[W1.r(c), hn.r(c)], writes=[psa.r()])
                for c in range(8):
                    P.op("pe", lambda e, c=c, j=j, psg=psg, hn=hn, L=L: e.matmul(
                        psg[:, :L], lhsT=W1[:, c, 1024 + j * 128:1024 + (j + 1) * 128], rhs=hn[:, c, :L],
                        start=(c == 0), stop=(c == 7)), reads=[W1.r(c), hn.r(c)], writes=[psg.r()])
                sg = sig[j % 2]
                P.op("act", lambda e, j=j, psg=psg, sg=sg, L=L: e.activation(
                    out=sg[:, :L], in_=psg[:, :L], func=AF.Sigmoid, bias=b1t[:, 8 + j:9 + j], scale=1.0),
                    reads=[psg.r(), b1t.r()], writes=[sg.r()])
                P.op("dve", lambda e, j=j, psa=psa, sg=sg, s0=s0, L=L: e.scalar_tensor_tensor(
                    out=uT[:, j, s0:s0 + L], in0=psa[:, :L], scalar=b1t[:, j:j + 1], in1=sg[:, :L],
                    op0=ALU.add, op1=ALU.mult), reads=[psa.r(), sg.r(), b1t.r()], writes=[uT.r(j)])
            if it == 0:
                for j in range(8):
                    P.op("dve", lambda e, j=j: e.tensor_scalar(
                        out=uT[:, j, 0:HALO], in0=uT[:, j, 0:HALO], scalar1=hst[:, 0:1], scalar2=None, op0=ALU.mult),
                        reads=[uT.r(j), hst.r()], writes=[uT.r(j)])
        vs = [P.sb(f"v{i}", [128, 8, T], F32, nsub=8) for i in range(1)] * 2
        zs = [P.sb(f"z{i}", [128, 8, T], BF16, nsub=8) for i in range(1)] * 2
        ys = [P.sb(f"y{i}", [128, 8, T], F32, nsub=8) for i in range(1)] * 2
        v2 = P.sb("v2", [128, 8, T], F32, nsub=8)
        mean = P.sb("mean", [128, T], F32)
        msq = P.sb("msq", [128, T], F32)
        lrstd = P.sb("lrstd", [128, T], F32)
        dd = [P.sb(f"dd{i}", [128, T], F32) for i in range(2)]
        outs = []
        for it in range(TOK // T):
            b = it % NB
            x, v, z, y = xs[b], vs[b], zs[b], ys[b]
            tl = it * T
            P.dma(lambda e, x=x, tl=tl: e.dma_start(out=x[:, :, :], in_=hv[:, :, HALO + tl:HALO + tl + T]), writes=[x.r()])
            for c in range(8):
                ps = next_ps(P)
                for j in range(31):
                    P.op("pe", lambda e, c=c, j=j, ps=ps, tl=tl: e.matmul(
                        ps[:, :T], lhsT=diag[:, c * 31 + j, :], rhs=uT[:, c, tl + j:tl + j + T],
                        start=(j == 0), stop=(j == 30)), reads=[diag.r(c), uT.r(c)], writes=[ps.r()])
                P.op("act", lambda e, c=c, ps=ps, v=v: e.activation(
                    out=v[:, c, :], in_=ps[:, :T], func=AF.Identity, bias=bdt[:, c:c + 1], scale=1.0),
                    reads=[ps.r(), bdt.r()], writes=[v.r(c)])
                P.op("pool", lambda e, c=c, v=v: e.tensor_tensor(out=v2[:, c, :], in0=v[:, c, :], in1=v[:, c, :], op=ALU.mult),
                     reads=[v.r(c)], writes=[v2.r(c)])
            ps1 = next_ps(P)
            ps2 = next_ps(P)
            for c in range(8):
                P.op("pe", lambda e, c=c, ps1=ps1, v=v: e.matmul(ps1[:, :T], lhsT=ones_f[:, :], rhs=v[:, c, :],
                                                              start=(c == 0), stop=(c == 7)),
                     reads=[ones_f.r(), v.r(c)], writes=[ps1.r()])
            for c in range(8):
                P.op("pe", lambda e, c=c, ps2=ps2: e.matmul(ps2[:, :T], lhsT=ones_f[:, :], rhs=v2[:, c, :],
                                                         start=(c == 0), stop=(c == 7)),
                     reads=[ones_f.r(), v2.r(c)], writes=[ps2.r()])
            P.op("act", lambda e, ps1=ps1: e.activation(out=mean[:, :], in_=ps1[:, :T], func=AF.Copy, scale=1.0 / 1024),
                 reads=[ps1.r()], writes=[mean.r()])
            P.op("dve", lambda e: e.tensor_tensor(out=msq[:, :], in0=mean[:, :], in1=mean[:, :], op=ALU.mult),
                 reads=[mean.r()], writes=[msq.r()])
            P.op("dve", lambda e, ps2=ps2: e.scalar_tensor_tensor(
                out=lrstd[:, :], in0=ps2[:, :T], scalar=1.0 / 1024, in1=msq[:, :], op0=ALU.mult, op1=ALU.subtract),
                reads=[ps2.r(), msq.r()], writes=[lrstd.r()])
            P.op("act", lambda e: e.activation(out=lrstd[:, :], in_=lrstd[:, :], func=AF.Sqrt, bias=C.eps[:, :], scale=1.0),
                 reads=[lrstd.r(), C.eps.r()], writes=[lrstd.r()])
            P.op("dve", lambda e: e.reciprocal(out=lrstd[:, :], in_=lrstd[:, :]), reads=[lrstd.r()], writes=[lrstd.r()])
            for c in range(8):
                d = dd[c % 2]
                P.op("pool", lambda e, c=c, d=d, v=v: e.tensor_tensor(out=d[:, :], in0=v[:, c, :], in1=mean[:, :], op=ALU.subtract),
                     reads=[v.r(c), mean.r()], writes=[d.r()])
                P.op("dve", lambda e, c=c, d=d: e.scalar_tensor_tensor(
                    out=d[:, :], in0=d[:, :], scalar=lgt[:, c:c + 1], in1=lrstd[:, :], op0=ALU.mult, op1=ALU.mult),
                    reads=[d.r(), lrstd.r(), lgt.r()], writes=[d.r()])
                P.op("act", lambda e, c=c, d=d, z=z: e.activation(
                    out=z[:, c, :], in_=d[:, :], func=AF.Silu, bias=lbt[:, c:c + 1], scale=1.0),
                    reads=[d.r(), lbt.r()], writes=[z.r(c)])
            for o in range(8):
                ps = next_ps(P)
                for c in range(8):
                    P.op("pe", lambda e, o=o, c=c, ps=ps, z=z: e.matmul(
                        ps[:, :T], lhsT=W2[:, c, o * 128:(o + 1) * 128], rhs=z[:, c, :],
                        start=(c == 0), stop=(c == 7)), reads=[W2.r(c), z.r(c)], writes=[ps.r()])
                P.op("dve", lambda e, o=o, ps=ps, x=x, y=y: e.scalar_tensor_tensor(
                    out=y[:, o, :], in0=ps[:, :T], scalar=b2t[:, o:o + 1], in1=x[:, o, :], op0=ALU.add, op1=ALU.add),
                    reads=[ps.r(), x.r(), b2t.r()], writes=[y.r(o)])
            outs.append(P.dma(lambda e, y=y, tl=tl: e.dma_start(out=ov[:, :, tl:tl + T], in_=y[:, :, :]),
                              reads=y.all()))
        P.emit(final_waits=outs)
    return nc


def norm_tile(P, C, x, gt, sq, hn, rstd, L, dim=1024.0, KC=8):
    ps = next_ps(P)
    for c in range(KC):
        P.op("act", lambda e, c=c: e.activation(out=sq[:, c, :L], in_=x[:, c, :L], func=AF.Square),
             reads=[x.r()], writes=[sq.r(c)])
    for c in range(KC):
        P.op("pe", lambda e, c=c: e.matmul(ps[:, :L], lhsT=C.ones_bf[:, :], rhs=sq[:, c, :L],
                                          start=(c == 0), stop=(c == KC - 1)),
             reads=[sq.r(c), C.ones_bf.r()], writes=[ps.r()])
    P.op("act", lambda e: e.activation(out=rstd[:, :L], in_=ps[:, :L], func=AF.Sqrt,
                                       bias=C.eps[:, :], scale=1.0 / dim),
         reads=[ps.r(), C.eps.r()], writes=[rstd.r()])
    P.op("dve", lambda e: e.reciprocal(out=rstd[:, :L], in_=rstd[:, :L]), reads=[rstd.r()], writes=[rstd.r()])
    for c in range(KC):
        P.op("dve", lambda e, c=c: e.scalar_tensor_tensor(
            out=hn[:, c, :L], in0=x[:, c, :L], scalar=gt[:, c:c + 1], in1=rstd[:, :L],
            op0=ALU.mult, op1=ALU.mult), reads=[x.r(), rstd.r(), gt.r()], writes=[hn.r(c)])


def build_mla(L=16384):
    T = 512
    NTI = L // T
    NKB = L // 128
    SCALE = 96.0 ** -0.5
    nc = bass.Bass("TRN2", target_bir_lowering=False)
    hT = nc.dram_tensor("hT", [1024, L], F32, kind="ExternalInput").ap()
    g = nc.dram_tensor("g", [1024], F32, kind="ExternalInput").ap()
    wall = nc.dram_tensor("wall", [1024, 832], F32, kind="ExternalInput").ap()
    gq = nc.dram_tensor("gq", [384], F32, kind="ExternalInput").ap()
    gkv = nc.dram_tensor("gkv", [256], F32, kind="ExternalInput").ap()
    wuq = nc.dram_tensor("wuq", [384, 384], F32, kind="ExternalInput").ap()
    wukv = nc.dram_tensor("wukv", [256, 256], F32, kind="ExternalInput").ap()
    cos2 = nc.dram_tensor("cos2", [96, L], F32, kind="ExternalInput").ap()
    sin2 = nc.dram_tensor("sin2", [96, L], F32, kind="ExternalInput").ap()
    cmask = nc.dram_tensor("cmask", [128, 4, 512], F32, kind="ExternalInput").ap()
    esel = nc.dram_tensor("esel", [65, 64], F32, kind="ExternalInput").ap()
    oT = nc.dram_tensor("oT", [128, L], F32, kind="ExternalOutput").ap()
    qTd = nc.dram_tensor("qTd", [2, 96, L], BF16, kind="Internal").ap()
    hv = hT.rearrange("(c p) t -> p c t", p=128)
    with ExitStack() as st:
        st.enter_context(nc.allow_low_precision("bf16 matmul operands, fp32 accumulate"))
        P = Prog(nc, st)
        alloc_ps(P, 6)
        C = make_consts(P)
        gt = load_vec_fm(P, g, 8, "g")
        gqt = load_vec_fm(P, gq, 3, "gq")
        gkvt = load_vec_fm(P, gkv, 2, "gkv")
        Wall = load_w_bf16(P, wall, 8, 832, "Wall")
        Wuq = load_w_bf16(P, wuq, 3, 384, "Wuq")
        Wukv = load_w_bf16(P, wukv, 2, 256, "Wukv")
        cm_f = load_const(P, cmask, [128, 4, 512], "cm_f")
        cm = P.sb("cm", [128, 4, 512], BF16)
        P.op("dve", lambda e: e.tensor_copy(out=cm[:, :, :], in_=cm_f[:, :, :]), reads=[cm_f.r()], writes=[cm.r()])
        es = P.sb("es", [128, 64], F32)
        P.op("dve", lambda e: e.memset(es[:, :], 0.0), writes=[es.r()])
        P.dma(lambda e: e.dma_start(out=es[0:65, :], in_=esel), reads=[es.r()], writes=[es.r()])
        kT = [P.sb(f"kT{h}", [96, L], BF16, nsub=NTI) for h in range(2)]
        qres = [[Res(f"qTd{h}_{i}") for i in range(NTI)] for h in range(2)]
        Va = P.sb("Va", [128, NKB, 2, 65], BF16, nsub=NTI)
        P.op("pool", lambda e: e.memset(Va[:, :, :, :], 1.0), writes=Va.all())
        x = P.sb("x", [128, 8, T], F32)
        sq = P.sb("sq", [128, 8, T], BF16, nsub=8)
        hn = P.sb("hn", [128, 8, T], BF16, nsub=8)
        rstd = P.sb("rstd", [128, T], F32)
        cq = P.sb("cq", [128, 5, T], F32, nsub=5)
        cqs = P.sb("cqs", [128, 5, T], BF16, nsub=5)
        cqn = P.sb("cqn", [128, 5, T], BF16, nsub=5)
        rq = P.sb("rq", [128, T], F32)
        rkv = P.sb("rkv", [128, T], F32)
        cs = P.sb("cs", [96, 2, T], F32)
        t1 = P.sb("t1", [96, T], F32)
        t2 = P.sb("t2", [96, T], F32)
        qt = [P.sb(f"qt{h}", [96, T], BF16) for h in range(2)]
        for it in range(NTI):
            t0 = it * T
            P.dma(lambda e, t0=t0: e.dma_start(out=x[:, :, :], in_=hv[:, :, t0:t0 + T]), writes=[x.r()])
            P.dma(lambda e, t0=t0: e.dma_start(out=cs[:, 0, :], in_=cos2[:, t0:t0 + T]), writes=[cs.r()], q="act")
            P.dma(lambda e, t0=t0: e.dma_start(out=cs[:, 1, :], in_=sin2[:, t0:t0 + T]), writes=[cs.r()], q="act")
            norm_tile(P, C, x, gt, sq, hn, rstd, T)
            for j in range(5):
                ps = next_ps(P)
                for c in range(8):
                    P.op("pe", lambda e, c=c, j=j, ps=ps: e.matmul(
                        ps[:, :T], lhsT=Wall[:, c, j * 128:(j + 1) * 128], rhs=hn[:, c, :],
                        start=(c == 0), stop=(c == 7)), reads=[Wall.r(c), hn.r(c)], writes=[ps.r()])
                P.op("act", lambda e, j=j, ps=ps: e.copy(out=cq[:, j, :], in_=ps[:, :T]), reads=[ps.r()], writes=[cq.r(j)])
                P.op("pool", lambda e, j=j: e.tensor_tensor(out=cqs[:, j, :], in0=cq[:, j, :], in1=cq[:, j, :], op=ALU.mult),
                     reads=[cq.r(j)], writes=[cqs.r(j)])
            for (lo, hi, rr, dim, gg) in ((0, 3, rq, 384.0, gqt), (3, 5, rkv, 256.0, gkvt)):
                ps = next_ps(P)
                for j in range(lo, hi):
                    P.op("pe", lambda e, j=j, ps=ps, lo=lo, hi=hi: e.matmul(
                        ps[:, :T], lhsT=C.ones_bf[:, :], rhs=cqs[:, j, :], start=(j == lo), stop=(j == hi - 1)),
                        reads=[cqs.r(j), C.ones_bf.r()], writes=[ps.r()])
                P.op("act", lambda e, ps=ps, rr=rr, dim=dim: e.activation(
                    out=rr[:, :], in_=ps[:, :T], func=AF.Sqrt, bias=C.eps[:, :], scale=1.0 / dim),
                    reads=[ps.r(), C.eps.r()], writes=[rr.r()])
                P.op("dve", lambda e, rr=rr: e.reciprocal(out=rr[:, :], in_=rr[:, :]), reads=[rr.r()], writes=[rr.r()])
                for j in range(lo, hi):
                    P.op("dve", lambda e, j=j, rr=rr, gg=gg, lo=lo: e.scalar_tensor_tensor(
                        out=cqn[:, j, :], in0=cq[:, j, :], scalar=gg[:, j - lo:j - lo + 1], in1=rr[:, :],
                        op0=ALU.mult, op1=ALU.mult), reads=[cq.r(j), rr.r(), gg.r()], writes=[cqn.r(j)])
            pk = next_ps(P)
            pks = next_ps(P)
            for c in range(8):
                P.op("pe", lambda e, c=c, pk=pk: e.matmul(pk[:96, :T], lhsT=Wall[:, c, 640:736], rhs=hn[:, c, :],
                                                       start=(c == 0), stop=(c == 7)),
                     reads=[Wall.r(c), hn.r(c)], writes=[pk.r()])
            for c in range(8):
                P.op("pe", lambda e, c=c, pks=pks: e.matmul(pks[:96, :T], lhsT=Wall[:, c, 736:832], rhs=hn[:, c, :],
                                                         start=(c == 0), stop=(c == 7)),
                     reads=[Wall.r(c), hn.r(c)], writes=[pks.r()])
            P.op("dve", lambda e, pk=pk: e.tensor_tensor(out=t1[64:96, :], in0=pk[64:96, :T], in1=cs[64:96, 0, :], op=ALU.mult),
                 reads=[pk.r(), cs.r()], writes=[t1.r()])
            P.op("dve", lambda e, pks=pks: e.tensor_tensor(out=t2[64:96, :], in0=pks[64:96, :T], in1=cs[64:96, 1, :], op=ALU.mult),
                 reads=[pks.r(), cs.r()], writes=[t2.r()])
            for h in range(2):
                P.op("pool", lambda e, h=h, t0=t0: e.tensor_tensor(out=kT[h][64:96, t0:t0 + T], in0=t1[64:96, :], in1=t2[64:96, :], op=ALU.add),
                     reads=[t1.r(), t2.r()], writes=[kT[h].r(it)])
            for h in range(2):
                pq = next_ps(P)
                pqs = next_ps(P)
                for c in range(3):
                    P.op("pe", lambda e, c=c, h=h, pq=pq: e.matmul(
                        pq[:96, :T], lhsT=Wuq[:, c, h * 192:h * 192 + 96], rhs=cqn[:, c, :],
                        start=(c == 0), stop=(c == 2)), reads=[Wuq.r(c), cqn.r(c)], writes=[pq.r()])
                for c in range(3):
                    P.op("pe", lambda e, c=c, h=h, pqs=pqs: e.matmul(
                        pqs[:96, :T], lhsT=Wuq[:, c, h * 192 + 96:h * 192 + 192], rhs=cqn[:, c, :],
                        start=(c == 0), stop=(c == 2)), reads=[Wuq.r(c), cqn.r(c)], writes=[pqs.r()])
                q = qt[h]
                P.op("act", lambda e, pq=pq, q=q: e.copy(out=q[0:64, :], in_=pq[0:64, :T]), reads=[pq.r()], writes=[q.r()])
                P.op("dve", lambda e, pq=pq: e.tensor_tensor(out=t1[64:96, :], in0=pq[64:96, :T], in1=cs[64:96, 0, :], op=ALU.mult),
                     reads=[pq.r(), cs.r()], writes=[t1.r()])
                P.op("dve", lambda e, pqs=pqs: e.tensor_tensor(out=t2[64:96, :], in0=pqs[64:96, :T], in1=cs[64:96, 1, :], op=ALU.mult),
                     reads=[pqs.r(), cs.r()], writes=[t2.r()])
                P.op("pool", lambda e, q=q: e.tensor_tensor(out=q[64:96, :], in0=t1[64:96, :], in1=t2[64:96, :], op=ALU.add),
                     reads=[t1.r(), t2.r()], writes=[q.r()])
                P.dma(lambda e, h=h, q=q, t0=t0: e.dma_start(out=qTd[h, :, t0:t0 + T], in_=q[:, :]), reads=[q.r()], writes=[qres[h][it]])
                pkn = next_ps(P)
                for c in range(2):
                    P.op("pe", lambda e, c=c, h=h, pkn=pkn: e.matmul(
                        pkn[:64, :T], lhsT=Wukv[:, c, h * 64:(h + 1) * 64], rhs=cqn[:, 3 + c, :],
                        start=(c == 0), stop=(c == 1)), reads=[Wukv.r(c), cqn.r(3 + c)], writes=[pkn.r()])
                P.op("act", lambda e, h=h, pkn=pkn, t0=t0: e.copy(out=kT[h][0:64, t0:t0 + T], in_=pkn[0:64, :T]),
                     reads=[pkn.r()], writes=[kT[h].r(it)])
            for b4 in range(4):
                pv = next_ps(P)
                for c in range(2):
                    P.op("pe", lambda e, c=c, b4=b4, pv=pv: e.matmul(
                        pv[:, :128], lhsT=cqn[:, 3 + c, b4 * 128:(b4 + 1) * 128], rhs=Wukv[:, c, 128:256],
                        start=(c == 0), stop=(c == 1)), reads=[Wukv.r(c), cqn.r(3 + c)], writes=[pv.r()])
                kb = it * 4 + b4
                P.op("act", lambda e, pv=pv, kb=kb: e.copy(
                    out=Va[:, kb, :, 0:64], in_=pv[:, :128].rearrange("p (h d) -> p h d", h=2)),
                    reads=[pv.r()], writes=[Va.r(it)])
        pts = [P.sb(f"pt{i}", [128, T], BF16) for i in range(4)]
        qs = [P.sb(f"qs{i}", [96, T], BF16) for i in range(2)]
        osb = P.sb("osb", [128, T], F32)
        P.op("dve", lambda e: e.memset(osb[:, :], 0.0), writes=[osb.r()])
        rden = P.sb("rden", [64, T], F32)
        on = [P.sb(f"on{i}", [64, T], F32) for i in range(2)]
        po = [P.ps(f"po{i}", [128, 512], F32) for i in range(2)]
        outs = []
        n = 0
        for h in range(2):
            for j in range(NTI):
                q = qs[n % 2]
                pO = po[n % 2]
                o_n = on[n % 2]
                n += 1
                P.dma(lambda e, h=h, q=q, j=j: e.dma_start(out=q[:, :], in_=qTd[h, :, j * T:(j + 1) * T]),
                      reads=[qres[h][j]], writes=[q.r()], q="act")
                nkb = 4 * j + 4
                LA = 2
                pend = []

                def pv(kb, pt, h=h, pO=pO, nkb=nkb):
                    P.op("pe", lambda e, h=h, kb=kb, pt=pt, pO=pO, nkb=nkb: e.matmul(
                        pO[:65, :T], lhsT=Va[:, kb, h, :], rhs=pt[:, :], start=(kb == 0), stop=(kb == nkb - 1)),
                        reads=[Va.r(kb // 4), pt.r()], writes=[pO.r()])

                for kb in range(nkb):
                    ps = next_ps(P)
                    pt = pts[kb % len(pts)]
                    P.op("pe", lambda e, h=h, kb=kb, ps=ps, q=q: e.matmul(
                        ps[:, :T], lhsT=kT[h][:, kb * 128:(kb + 1) * 128], rhs=q[:, :], start=True, stop=True),
                        reads=[kT[h].r(kb // 4), q.r()], writes=[ps.r()])
                    P.op("act", lambda e, ps=ps, pt=pt: e.activation(out=pt[:, :], in_=ps[:, :T], func=AF.Exp, scale=SCALE),
                         reads=[ps.r()], writes=[pt.r()])
                    if kb >= 4 * j:
                        d = kb - 4 * j
                        eng = "dve" if d % 2 == 0 else "pool"
                        P.op(eng, lambda e, pt=pt, d=d: e.tensor_tensor(out=pt[:, :], in0=pt[:, :], in1=cm[:, d, :], op=ALU.mult),
                             reads=[pt.r(), cm.r()], writes=[pt.r()])
                    pend.append((kb, pt))
                    if len(pend) > LA:
                        pv(*pend.pop(0))
                while pend:
                    pv(*pend.pop(0))
                P.op("act", lambda e, pO=pO: e.copy(out=osb[0:65, :], in_=pO[:65, :T]), reads=[pO.r()], writes=[osb.r()])
                pd = next_ps(P)
                P.op("pe", lambda e, pd=pd: e.matmul(pd[:64, :T], lhsT=es[:, :], rhs=osb[:, :], start=True, stop=True),
                     reads=[es.r(), osb.r()], writes=[pd.r()])
                P.op("dve", lambda e, pd=pd: e.reciprocal(out=rden[:, :], in_=pd[:64, :T]), reads=[pd.r()], writes=[rden.r()])
                P.op("dve", lambda e, o_n=o_n: e.tensor_tensor(out=o_n[:, :], in0=osb[0:64, :], in1=rden[:, :], op=ALU.mult),
                     reads=[osb.r(), rden.r()], writes=[o_n.r()])
                outs.append(P.dma(lambda e, h=h, j=j, o_n=o_n: e.dma_start(
                    out=oT[h * 64:(h + 1) * 64, j * T:(j + 1) * T], in_=o_n[:, :]), reads=[o_n.r()]))
        P.emit(final_waits=outs)
    return nc


def build_gdn(L=16384):
    T = 512
    NTI = L // T
    nc = bass.Bass("TRN2", target_bir_lowering=False)
    hT = nc.dram_tensor("hT", [1024, L], F32, kind="ExternalInput").ap()
    g = nc.dram_tensor("g", [1024], F32, kind="ExternalInput").ap()
    wh = nc.dram_tensor("wh", [1024, 640], F32, kind="ExternalInput").ap()
    cw = nc.dram_tensor("cw", [384, 4], F32, kind="ExternalInput").ap()
    sc = nc.dram_tensor("sc", [128, 2], F32, kind="ExternalInput").ap()
    og = nc.dram_tensor("og", [128], F32, kind="ExternalInput").ap()
    cst = nc.dram_tensor("cst", [128, 5, 128], F32, kind="ExternalInput").ap()
    oT = nc.dram_tensor("oT", [128, L], F32, kind="ExternalOutput").ap()
    s_in = nc.dram_tensor("s_in", [128, 128], F32, kind="ExternalInput").ap()
    h_in = nc.dram_tensor("h_in", [128, 3, 3], F32, kind="ExternalInput").ap()
    s_out = nc.dram_tensor("s_out", [128, 128], F32, kind="ExternalOutput").ap()
    h_out = nc.dram_tensor("h_out", [128, 3, 3], F32, kind="ExternalOutput").ap()
    hv = hT.rearrange("(c p) t -> p c t", p=128)
    with ExitStack() as st:
        st.enter_context(nc.allow_low_precision("bf16 matmul operands for the input projection only"))
        P = Prog(nc, st)
        qbanks = [P.ps(f"qb{i}", [128, 512], F32) for i in range(5)]
        qp = [View(qbanks[i], 0, 128, f"qp{i}") for i in range(5)]
        hp = [View(qbanks[i], 0, 256, f"hp{i}") for i in range(5)]
        P.pss = [P.ps(f"fb{i}", [128, 512], F32) for i in range(3)]
        P._psi = 0
        cnt = {"q": 0, "h": 0}

        def nq():
            cnt["q"] += 1
            return qp[cnt["q"] % 5]

        def nh():
            cnt["q"] += 1
            return hp[cnt["q"] % 5]

        C = make_consts(P)
        ones_f = P.sb("ones_f", [128, 128], F32)
        P.op("dve", lambda e: e.memset(ones_f[:, :], 1.0), writes=[ones_f.r()])
        gt = load_vec_fm(P, g, 8, "g")
        ogt = load_vec_fm(P, og, 1, "og")
        cwt = load_vec_fm_2d = P.sb("cwt", [128, 3, 4], F32)
        P.dma(lambda e: e.dma_start(out=cwt[:, :, :], in_=cw.rearrange("(c p) j -> p c j", p=128)), writes=[cwt.r()])
        sct = load_const(P, sc, [128, 2], "sct")
        K = load_const(P, cst, [128, 5, 128], "K")
        ident = K[:, 0, :]
        maskS = K[:, 1, :]
        maskI = K[:, 2, :]
        blk1 = K[:, 3, :]
        cind = K[:, 4, 0:2]
        Wh = load_w_bf16(P, wh, 8, 640, "Wh")
        Whf = P.sb("Whf", [128, 8, 2], F32)
        P.dma(lambda e: e.dma_start(out=Whf[:, :, :], in_=wh.rearrange("(c p) f -> p c f", p=128)[:, :, 512:514]),
              writes=[Whf.r()])
        for c in range(8):
            P.op("dve", lambda e, c=c: e.tensor_scalar(out=Whf[:, c, :], in0=Whf[:, c, :], scalar1=gt[:, c:c + 1],
                                                       scalar2=None, op0=ALU.mult),
                 reads=[Whf.r(), gt.r()], writes=[Whf.r()])
        nA = P.sb("nA", [128, 1], F32)
        P.op("act", lambda e: e.activation(out=nA[:, :], in_=sct[:, 0:1], func=AF.Exp), reads=[sct.r()], writes=[nA.r()])
        P.op("dve", lambda e: e.tensor_scalar(out=nA[:, :], in0=nA[:, :], scalar1=-1.0, scalar2=None, op0=ALU.mult),
             reads=[nA.r()], writes=[nA.r()])
        onec = P.sb("onec", [128, 1], F32)
        P.op("dve", lambda e: e.memset(onec[:, :], 1.0), writes=[onec.r()])
        epsl2 = C.eps
        S = [P.sb(f"S{i}", [128, 128], F32) for i in range(2)]
        P.dma(lambda e: e.dma_start(out=S[0][:, :], in_=s_in), writes=[S[0].r()])
        sidx = [0]
        x = P.sb("x", [128, 8, T], F32)
        sq = P.sb("sq", [128, 8, T], BF16, nsub=8)
        hn = P.sb("hn", [128, 8, T], BF16, nsub=8)
        rstd = P.sb("rstd", [128, T], F32)
        raw = P.sb("raw", [128, 3, T + 3], F32, nsub=3)
        P.dma(lambda e: e.dma_start(out=raw[:, :, 0:3], in_=h_in), writes=raw.all())
        acc = P.sb("acc", [128, 3, T], F32, nsub=3)
        sil = P.sb("sil", [128, 3, T], F32, nsub=3)
        sq2 = P.sb("sq2", [128, 2, T], F32, nsub=2)
        rn = P.sb("rn", [128, 2, T], F32, nsub=2)
        tb = {}

        PERSIST = ("QpT", "O0T", "MT0", "MT1", "B0", "B1", "gateT", "oTt", "osq", "orr")

        def tbuf(par, name, shape=(128, 128)):
            if not name.rstrip("0123456789").endswith(PERSIST) and not any(name.startswith(p) for p in PERSIST):
                par = 0
            key = (par, name)
            if key not in tb:
                tb[key] = P.sb(f"t{par}_{name}", list(shape), F32)
            return tb[key]

        outs = []

        def gen_local(t):
            par = t % 2
            t0 = t * T
            qnT = tbuf(par, "qnT", (128, T))
            knT = tbuf(par, "knT", (128, T))
            vT = tbuf(par, "vT", (128, T))
            gateT = tbuf(par, "gateT", (128, T))
            P.dma(lambda e: e.dma_start(out=x[:, :, :], in_=hv[:, :, t0:t0 + T]), writes=[x.r()])
            norm_tile(P, C, x, gt, sq, hn, rstd, T)
            yield
            for s3 in range(3):
                ps = next_ps(P)
                for c in range(8):
                    P.op("pe", lambda e, c=c, s3=s3, ps=ps: e.matmul(
                        ps[:, :T], lhsT=Wh[:, c, s3 * 128:(s3 + 1) * 128], rhs=hn[:, c, :],
                        start=(c == 0), stop=(c == 7)), reads=[Wh.r(c), hn.r(c)], writes=[ps.r()])
                P.op("act", lambda e, s3=s3, ps=ps: e.copy(out=raw[:, s3, 3:T + 3], in_=ps[:, :T]),
                     reads=[ps.r()], writes=[raw.r(s3)])
            ps = next_ps(P)
            for c in range(8):
                P.op("pe", lambda e, c=c, ps=ps: e.matmul(
                    ps[:, :T], lhsT=Wh[:, c, 384:512], rhs=hn[:, c, :], start=(c == 0), stop=(c == 7)),
                    reads=[Wh.r(c), hn.r(c)], writes=[ps.r()])
            P.op("act", lambda e, ps=ps: e.activation(out=gateT[:, :], in_=ps[:, :T], func=AF.Silu),
                 reads=[ps.r()], writes=[gateT.r()])
            yield
            for s3 in range(3):
                eng = "dve" if s3 != 1 else "pool"
                P.op("dve", lambda e, s3=s3: e.tensor_scalar(
                    out=acc[:, s3, :], in0=raw[:, s3, 3:T + 3], scalar1=cwt[:, s3, 3:4], scalar2=None, op0=ALU.mult),
                    reads=[raw.r(s3), cwt.r()], writes=[acc.r(s3)])
                for j in range(3):
                    P.op("dve", lambda e, s3=s3, j=j: e.scalar_tensor_tensor(
                        out=acc[:, s3, :], in0=raw[:, s3, j:j + T], scalar=cwt[:, s3, j:j + 1], in1=acc[:, s3, :],
                        op0=ALU.mult, op1=ALU.add), reads=[raw.r(s3), cwt.r(), acc.r(s3)], writes=[acc.r(s3)])
                P.op("pool", lambda e, s3=s3: e.tensor_copy(out=raw[:, s3, 0:3], in_=raw[:, s3, T:T + 3]),
                     reads=[raw.r(s3)], writes=[raw.r(s3)])
                dst = vT if s3 == 2 else sil
                if s3 == 2:
                    P.op("act", lambda e: e.activation(out=vT[:, :], in_=acc[:, 2, :], func=AF.Silu),
                         reads=[acc.r(2)], writes=[vT.r()])
                else:
                    P.op("act", lambda e, s3=s3: e.activation(out=sil[:, s3, :], in_=acc[:, s3, :], func=AF.Silu),
                         reads=[acc.r(s3)], writes=[sil.r(s3)])
            yield
            for s2 in range(2):
                P.op("pool", lambda e, s2=s2: e.tensor_tensor(out=sq2[:, s2, :], in0=sil[:, s2, :], in1=sil[:, s2, :], op=ALU.mult),
                     reads=[sil.r(s2)], writes=[sq2.r(s2)])
                ps = next_ps(P)
                P.op("pe", lambda e, s2=s2, ps=ps: e.matmul(ps[:, :T], lhsT=ones_f[:, :], rhs=sq2[:, s2, :], start=True, stop=True),
                     reads=[ones_f.r(), sq2.r(s2)], writes=[ps.r()])
                P.op("act", lambda e, s2=s2, ps=ps: e.activation(out=rn[:, s2, :], in_=ps[:, :T], func=AF.Sqrt,
                                                                bias=epsl2[:, :], scale=1.0),
                     reads=[ps.r(), epsl2.r()], writes=[rn.r(s2)])
                P.op("dve", lambda e, s2=s2: e.reciprocal(out=rn[:, s2, :], in_=rn[:, s2, :]), reads=[rn.r(s2)], writes=[rn.r(s2)])
                dst = qnT if s2 == 0 else knT
                scl = (128.0 ** -0.5) if s2 == 0 else 1.0
                P.op("dve", lambda e, s2=s2, dst=dst, scl=scl: e.scalar_tensor_tensor(
                    out=dst[:, :], in0=sil[:, s2, :], scalar=scl, in1=rn[:, s2, :], op0=ALU.mult, op1=ALU.mult),
                    reads=[sil.r(s2), rn.r(s2)], writes=[dst.r()])
            yield
            B4 = range(4)
            tl = lambda s, name, shape=(128, 128): tbuf(par, f"{name}{s}", shape)
            pKK, pQK, pgc = {}, {}, {}
            for s in B4:
                ts = slice(s * 128, (s + 1) * 128)
                cols = tl(s, "cols", (128, 8))
                tcol = tl(s, "tcol", (128, 8))
                pc = nq()
                for c in range(8):
                    P.op("pe", lambda e, c=c, pc=pc, ts=ts: e.matmul(pc[:, 0:2], lhsT=x[:, c, ts], rhs=Whf[:, c, 0:2],
                                                                   start=(c == 0), stop=(c == 7)),
                         reads=[x.r(), Whf.r()], writes=[pc.r()])
                pss_ = nq()
                for c in range(8):
                    P.op("pe", lambda e, c=c, pss_=pss_, ts=ts: e.matmul(pss_[:, 0:2], lhsT=sq[:, c, ts], rhs=C.ones_bf[:, 0:2],
                                                                       start=(c == 0), stop=(c == 7)),
                         reads=[sq.r(c), C.ones_bf.r()], writes=[pss_.r()])
                P.op("act", lambda e, pss_=pss_, tcol=tcol: e.activation(out=tcol[:, 0:2], in_=pss_[:, 0:2], func=AF.Sqrt,
                                                                       bias=C.eps[:, :], scale=1.0 / 1024),
                     reads=[pss_.r(), C.eps.r()], writes=[tcol.r()])
                P.op("dve", lambda e, tcol=tcol: e.reciprocal(out=tcol[:, 0:2], in_=tcol[:, 0:2]), reads=[tcol.r()], writes=[tcol.r()])
                P.op("dve", lambda e, pc=pc, tcol=tcol: e.tensor_tensor(out=tcol[:, 2:4], in0=pc[:, 0:2], in1=tcol[:, 0:2], op=ALU.mult),
                     reads=[pc.r(), tcol.r()], writes=[tcol.r()])
                P.op("act", lambda e, tcol=tcol, cols=cols: e.activation(out=cols[:, 1:2], in_=tcol[:, 2:3], func=AF.Sigmoid),
                     reads=[tcol.r()], writes=[cols.r()])
                P.op("act", lambda e, tcol=tcol: e.activation(out=tcol[:, 4:5], in_=tcol[:, 3:4], func=AF.Exp, bias=sct[:, 1:2], scale=1.0),
                     reads=[tcol.r(), sct.r()], writes=[tcol.r()])
                P.op("act", lambda e, tcol=tcol: e.activation(out=tcol[:, 5:6], in_=tcol[:, 4:5], func=AF.Ln, bias=onec[:, 0:1], scale=1.0),
                     reads=[tcol.r(), onec.r()], writes=[tcol.r()])
                P.op("dve", lambda e, tcol=tcol, cols=cols: e.tensor_scalar(out=cols[:, 0:1], in0=tcol[:, 5:6], scalar1=nA[:, 0:1], scalar2=None, op0=ALU.mult),
                     reads=[tcol.r(), nA.r()], writes=[cols.r()])
                gB = tl(s, "gB"); bB = tl(s, "bB")
                P.op("dve", lambda e, gB=gB, cols=cols: e.tensor_scalar(out=gB[:, :], in0=ones_f[:, :], scalar1=cols[:, 0:1],
                                                                       scalar2=None, op0=ALU.mult),
                     reads=[ones_f.r(), cols.r()], writes=[gB.r()])
                P.op("pool", lambda e, bB=bB, cols=cols: e.tensor_scalar(out=bB[:, :], in0=ones_f[:, :], scalar1=cols[:, 1:2],
                                                                        scalar2=None, op0=ALU.mult),
                     reads=[ones_f.r(), cols.r()], writes=[bB.r()])
            yield
            for s in B4:
                ts = slice(s * 128, (s + 1) * 128)
                cols = tl(s, "cols", (128, 8)); gB = tl(s, "gB")
                pgc[s] = nq()
                P.op("pe", lambda e, p=pgc[s], gB=gB: e.matmul(p[:, :], lhsT=gB[:, :], rhs=maskI, start=True, stop=True),
                     reads=[gB.r(), K.r()], writes=[pgc[s].r()])
                pm = nq()
                P.op("pe", lambda e, pm=pm, cols=cols: e.matmul(pm[:, 0:2], lhsT=maskI, rhs=cols[:, 0:2], start=True, stop=True),
                     reads=[K.r(), cols.r()], writes=[pm.r()])
                P.op("pe", lambda e, pm=pm, cols=cols: e.matmul(pm[:, 2:4], lhsT=blk1, rhs=cols[:, 0:2], start=True, stop=True),
                     reads=[K.r(), cols.r()], writes=[pm.r()])
                P.op("pe", lambda e, pm=pm, gB=gB: e.matmul(pm[:, 4:6], lhsT=gB[:, :], rhs=cind, start=True, stop=True),
                     reads=[K.r(), gB.r()], writes=[pm.r()])
                P.op("act", lambda e, pm=pm, cols=cols: e.copy(out=cols[:, 2:3], in_=pm[:, 0:1]), reads=[pm.r()], writes=[cols.r()])
                P.op("act", lambda e, pm=pm, cols=cols: e.copy(out=cols[:, 3:4], in_=pm[:, 2:3]), reads=[pm.r()], writes=[cols.r()])
                glb = tl(s, "glb", (128, 2))
                P.op("act", lambda e, pm=pm, glb=glb: e.activation(out=glb[:, :], in_=pm[:, 4:6], func=AF.Exp),
                     reads=[pm.r()], writes=[glb.r()])
                E = tl(s, "E"); egc = tl(s, "egc")
                P.op("dve", lambda e, p=pgc[s], E=E, cols=cols: e.tensor_scalar(
                    out=E[:, :], in0=p[:, :], scalar1=cols[:, 2:3], scalar2=0.0, op0=ALU.subtract, op1=ALU.min),
                    reads=[pgc[s].r(), cols.r()], writes=[E.r()])
                P.op("act", lambda e, p=pgc[s], egc=egc: e.activation(out=egc[:, :], in_=p[:, :], func=AF.Exp),
                     reads=[pgc[s].r()], writes=[egc.r()])
                P.op("act", lambda e, cols=cols: e.activation(out=cols[:, 4:5], in_=cols[:, 2:3], func=AF.Exp),
                     reads=[cols.r()], writes=[cols.r()])
                P.op("dve", lambda e, cols=cols: e.tensor_tensor(out=cols[:, 5:6], in0=cols[:, 4:5], in1=cols[:, 1:2], op=ALU.mult),
                     reads=[cols.r()], writes=[cols.r()])
                P.op("dve", lambda e, cols=cols: e.tensor_tensor(out=cols[:, 6:7], in0=cols[:, 3:4], in1=cols[:, 2:3], op=ALU.subtract),
                     reads=[cols.r()], writes=[cols.r()])
                P.op("act", lambda e, cols=cols: e.activation(out=cols[:, 6:7], in_=cols[:, 6:7], func=AF.Exp),
                     reads=[cols.r()], writes=[cols.r()])
            yield
            for s in B4:
                ts = slice(s * 128, (s + 1) * 128)
                E = tl(s, "E"); EmS = tl(s, "EmS"); EmI = tl(s, "EmI")
                P.op("act", lambda e, E=E: e.activation(out=E[:, :], in_=E[:, :], func=AF.Exp), reads=[E.r()], writes=[E.r()])
                pbb = nq()
                bB = tl(s, "bB")
                P.op("pe", lambda e, pbb=pbb, bB=bB: e.matmul(pbb[:, :], lhsT=bB[:, :], rhs=ident, start=True, stop=True),
                     reads=[bB.r(), K.r()], writes=[pbb.r()])
                P.op("pool", lambda e, E=E, EmS=EmS: e.tensor_tensor(out=EmS[:, :], in0=E[:, :], in1=maskS, op=ALU.mult),
                     reads=[E.r(), K.r()], writes=[EmS.r()])
                P.op("pool", lambda e, E=E, EmI=EmI: e.tensor_tensor(out=EmI[:, :], in0=E[:, :], in1=maskI, op=ALU.mult),
                     reads=[E.r(), K.r()], writes=[EmI.r()])
                P.op("dve", lambda e, pbb=pbb, EmS=EmS: e.tensor_tensor(out=EmS[:, :], in0=pbb[:, :], in1=EmS[:, :], op=ALU.mult),
                     reads=[pbb.r(), EmS.r()], writes=[EmS.r()])
            yield
            for s in B4:
                ts = slice(s * 128, (s + 1) * 128)
                EmS = tl(s, "EmS"); EmI = tl(s, "EmI"); Q0 = tl(s, "Qa"); qkT = tl(s, "qkT")
                pKK[s] = nq()
                P.op("pe", lambda e, p=pKK[s], ts=ts: e.matmul(p[:, :], lhsT=knT[:, ts], rhs=knT[:, ts], start=True, stop=True),
                     reads=[knT.r()], writes=[pKK[s].r()])
                P.op("dve", lambda e, p=pKK[s], EmS=EmS, Q0=Q0: e.scalar_tensor_tensor(
                    out=Q0[:, :], in0=p[:, :], scalar=-1.0, in1=EmS[:, :], op0=ALU.mult, op1=ALU.mult),
                    reads=[pKK[s].r(), EmS.r()], writes=[Q0.r()])
                pQK[s] = nq()
                P.op("pe", lambda e, p=pQK[s], ts=ts: e.matmul(p[:, :], lhsT=knT[:, ts], rhs=qnT[:, ts], start=True, stop=True),
                     reads=[knT.r(), qnT.r()], writes=[pQK[s].r()])
                P.op("dve", lambda e, p=pQK[s], EmI=EmI, qkT=qkT: e.tensor_tensor(out=qkT[:, :], in0=p[:, :], in1=EmI[:, :], op=ALU.mult),
                     reads=[pQK[s].r(), EmI.r()], writes=[qkT.r()])
            yield
            for s in B4:
                Q0 = tl(s, "Qa"); N0 = tl(s, "Na"); R0 = tl(s, "Ra")
                pt = nq()
                P.op("pe", lambda e, pt=pt, Q0=Q0: e.transpose(pt[:, :], Q0[:, :], ident), reads=[Q0.r(), K.r()], writes=[pt.r()])
                P.op("act", lambda e, pt=pt, N0=N0: e.copy(out=N0[:, :], in_=pt[:, :]), reads=[pt.r()], writes=[N0.r()])
                P.op("pool", lambda e, Q0=Q0, R0=R0: e.tensor_tensor(out=R0[:, :], in0=Q0[:, :], in1=ident, op=ALU.add),
                     reads=[Q0.r(), K.r()], writes=[R0.r()])
            yield
            names = ["a", "b"]
            for i in range(1, 6):
                po, pn = names[(i - 1) % 2], names[i % 2]
                for s in B4:
                    Qo = tl(s, "Q" + po); No = tl(s, "N" + po); Ro = tl(s, "R" + po)
                    Qn = tl(s, "Q" + pn); Nn = tl(s, "N" + pn)
                    pN = nq()
                    P.op("pe", lambda e, pN=pN, Qo=Qo, No=No: e.matmul(pN[:, :], lhsT=Qo[:, :], rhs=No[:, :], start=True, stop=True),
                         reads=[Qo.r(), No.r()], writes=[pN.r()])
                    P.op("act", lambda e, pN=pN, Nn=Nn: e.copy(out=Nn[:, :], in_=pN[:, :]), reads=[pN.r()], writes=[Nn.r()])
                    if i < 5:
                        pQ = nq()
                        P.op("pe", lambda e, pQ=pQ, Qo=Qo, No=No: e.matmul(pQ[:, :], lhsT=No[:, :], rhs=Qo[:, :], start=True, stop=True),
                             reads=[Qo.r(), No.r()], writes=[pQ.r()])
                        P.op("dve", lambda e, pQ=pQ, Qn=Qn: e.tensor_copy(out=Qn[:, :], in_=pQ[:, :]), reads=[pQ.r()], writes=[Qn.r()])
                yield
                for s in B4:
                    Nn = tl(s, "N" + pn); Ro = tl(s, "R" + po); Rn = tl(s, "R" + pn)
                    pR = nq()
                    P.op("pe", lambda e, pR=pR, Nn=Nn, Ro=Ro: e.matmul(pR[:, :], lhsT=Nn[:, :], rhs=Ro[:, :], start=True, stop=True),
                         reads=[Nn.r(), Ro.r()], writes=[pR.r()])
                    P.op("dve", lambda e, pR=pR, Ro=Ro, Rn=Rn: e.tensor_tensor(out=Rn[:, :], in0=pR[:, :], in1=Ro[:, :], op=ALU.add),
                         reads=[pR.r(), Ro.r()], writes=[Rn.r()])
                yield
            TTn = names[5 % 2]
            for s in B4:
                ts = slice(s * 128, (s + 1) * 128)
                cols = tl(s, "cols", (128, 8))
                UWin = tl(s, "UWin", (128, 256)); kd = tl(s, "kd")
                pk = nq()
                P.op("pe", lambda e, pk=pk, ts=ts: e.transpose(pk[:, :], knT[:, ts], ident), reads=[knT.r(), K.r()], writes=[pk.r()])
                P.op("dve", lambda e, pk=pk, UWin=UWin, cols=cols: e.tensor_scalar(out=UWin[:, 128:256], in0=pk[:, :], scalar1=cols[:, 5:6], scalar2=None, op0=ALU.mult),
                     reads=[pk.r(), cols.r()], writes=[UWin.r()])
                P.op("dve", lambda e, pk=pk, kd=kd, cols=cols: e.tensor_scalar(out=kd[:, :], in0=pk[:, :], scalar1=cols[:, 6:7], scalar2=None, op0=ALU.mult),
                     reads=[pk.r(), cols.r()], writes=[kd.r()])
                pv = nq()
                P.op("pe", lambda e, pv=pv, ts=ts: e.transpose(pv[:, :], vT[:, ts], ident), reads=[vT.r(), K.r()], writes=[pv.r()])
                P.op("dve", lambda e, pv=pv, UWin=UWin, cols=cols: e.tensor_scalar(out=UWin[:, 0:128], in0=pv[:, :], scalar1=cols[:, 1:2], scalar2=None, op0=ALU.mult),
                     reads=[pv.r(), cols.r()], writes=[UWin.r()])
            yield
            for s in B4:
                ts = slice(s * 128, (s + 1) * 128)
                UWin = tl(s, "UWin", (128, 256)); uw = tl(s, "uw", (128, 256)); TT = tl(s, "R" + TTn)
                egc = tl(s, "egc"); qdT = tl(s, "qdT")
                pu = nh()
                P.op("pe", lambda e, pu=pu, TT=TT, UWin=UWin: e.matmul(pu[:, :], lhsT=TT[:, :], rhs=UWin[:, :], start=True, stop=True),
                     reads=[TT.r(), UWin.r()], writes=[pu.r()])
                P.op("act", lambda e, pu=pu, uw=uw: e.copy(out=uw[:, :], in_=pu[:, :]), reads=[pu.r()], writes=[uw.r()])
                P.op("pool", lambda e, qdT=qdT, egc=egc, ts=ts: e.tensor_tensor(out=qdT[:, :], in0=qnT[:, ts], in1=egc[:, :], op=ALU.mult),
                     reads=[qnT.r(), egc.r()], writes=[qdT.r()])
            yield
            for s in B4:
                uw = tl(s, "uw", (128, 256)); qkT = tl(s, "qkT"); qdT = tl(s, "qdT"); kd = tl(s, "kd")
                QpT = tl(s, "QpT"); O0T = tl(s, "O0T"); glb = tl(s, "glb", (128, 2))
                pw = nq()
                P.op("pe", lambda e, pw=pw, uw=uw, qkT=qkT: e.matmul(pw[:, :], lhsT=uw[:, 128:256], rhs=qkT[:, :], start=True, stop=True),
                     reads=[uw.r(), qkT.r()], writes=[pw.r()])
                P.op("dve", lambda e, pw=pw, qdT=qdT, QpT=QpT: e.tensor_tensor(out=QpT[:, :], in0=qdT[:, :], in1=pw[:, :], op=ALU.subtract),
                     reads=[pw.r(), qdT.r()], writes=[QpT.r()])
                po0 = nq()
                P.op("pe", lambda e, po0=po0, uw=uw, qkT=qkT: e.matmul(po0[:, :], lhsT=uw[:, 0:128], rhs=qkT[:, :], start=True, stop=True),
                     reads=[uw.r(), qkT.r()], writes=[po0.r()])
                P.op("act", lambda e, po0=po0, O0T=O0T: e.copy(out=O0T[:, :], in_=po0[:, :]), reads=[po0.r()], writes=[O0T.r()])
                for c2 in range(2):
                    r = slice(c2 * 64, (c2 + 1) * 64)
                    MT = tl(s, f"MT{c2}"); Bc = tl(s, f"B{c2}")
                    pM = nq()
                    P.op("pe", lambda e, pM=pM, uw=uw, kd=kd, r=r: e.matmul(pM[:, :], lhsT=uw[r, 128:256], rhs=kd[r, :], start=True, stop=True),
                         reads=[uw.r(), kd.r()], writes=[pM.r()])
                    P.op("dve", lambda e, pM=pM, MT=MT, glb=glb, c2=c2: e.scalar_tensor_tensor(
                        out=MT[:, :], in0=ident, scalar=glb[:, c2:c2 + 1], in1=pM[:, :], op0=ALU.mult, op1=ALU.subtract),
                        reads=[pM.r(), glb.r(), K.r()], writes=[MT.r()])
                    pB = nq()
                    P.op("pe", lambda e, pB=pB, uw=uw, kd=kd, r=r: e.matmul(pB[:, :], lhsT=kd[r, :], rhs=uw[r, 0:128], start=True, stop=True),
                         reads=[uw.r(), kd.r()], writes=[pB.r()])
                    P.op("act", lambda e, pB=pB, Bc=Bc: e.copy(out=Bc[:, :], in_=pB[:, :]), reads=[pB.r()], writes=[Bc.r()])
                yield

        def gen_rec(t):
            par = t % 2
            t0 = t * T
            tl = lambda s, name, shape=(128, 128): tbuf(par, f"{name}{s}", shape)
            oTt = tbuf(par, "oTt", (128, T))
            gateT = tbuf(par, "gateT", (128, T))
            for s in range(4):
                QpT = tl(s, "QpT"); O0T = tl(s, "O0T")
                for c2 in range(2):
                    r = slice(c2 * 64, (c2 + 1) * 64)
                    col = slice(s * 128 + c2 * 64, s * 128 + (c2 + 1) * 64)
                    MT = tl(s, f"MT{c2}"); Bc = tl(s, f"B{c2}")
                    So = S[sidx[0] % 2]
                    Sn = S[(sidx[0] + 1) % 2]
                    sidx[0] += 1
                    po = nq()
                    P.op("pe", lambda e, po=po, So=So, QpT=QpT, r=r: e.matmul(po[:, 0:64], lhsT=So[:, :], rhs=QpT[:, r], start=True, stop=True),
                         reads=[So.r(), QpT.r()], writes=[po.r()])
                    pS = nq()
                    P.op("pe", lambda e, pS=pS, So=So, MT=MT: e.matmul(pS[:, :], lhsT=MT[:, :], rhs=So[:, :], start=True, stop=True),
                         reads=[So.r(), MT.r()], writes=[pS.r()])
                    P.op("dve", lambda e, pS=pS, Bc=Bc, Sn=Sn: e.tensor_tensor(out=Sn[:, :], in0=pS[:, :], in1=Bc[:, :], op=ALU.add),
                         reads=[pS.r(), Bc.r()], writes=[Sn.r()])
                    P.op("dve", lambda e, po=po, O0T=O0T, r=r, col=col: e.tensor_tensor(out=oTt[:, col], in0=po[:, 0:64], in1=O0T[:, r], op=ALU.add),
                         reads=[po.r(), O0T.r()], writes=[oTt.r()])
                    yield
            osq = tbuf(par, "osq", (128, T))
            orr = tbuf(par, "orr", (128, T))
            P.op("pool", lambda e: e.tensor_tensor(out=osq[:, :], in0=oTt[:, :], in1=oTt[:, :], op=ALU.mult), reads=[oTt.r()], writes=[osq.r()])
            ps = next_ps(P)
            P.op("pe", lambda e, ps=ps: e.matmul(ps[:, :T], lhsT=ones_f[:, :], rhs=osq[:, :], start=True, stop=True),
                 reads=[ones_f.r(), osq.r()], writes=[ps.r()])
            P.op("act", lambda e, ps=ps: e.activation(out=orr[:, :], in_=ps[:, :T], func=AF.Sqrt, bias=C.eps[:, :], scale=1.0 / 128),
                 reads=[ps.r(), C.eps.r()], writes=[orr.r()])
            P.op("dve", lambda e: e.reciprocal(out=orr[:, :], in_=orr[:, :]), reads=[orr.r()], writes=[orr.r()])
            P.op("dve", lambda e: e.scalar_tensor_tensor(out=osq[:, :], in0=oTt[:, :], scalar=ogt[:, 0:1], in1=orr[:, :],
                                                        op0=ALU.mult, op1=ALU.mult),
                 reads=[oTt.r(), orr.r(), ogt.r()], writes=[osq.r()])
            P.op("pool", lambda e: e.tensor_tensor(out=osq[:, :], in0=osq[:, :], in1=gateT[:, :], op=ALU.mult),
                 reads=[osq.r(), gateT.r()], writes=[osq.r()])
            outs.append(P.dma(lambda e: e.dma_start(out=oT[:, t0:t0 + T], in_=osq[:, :]), reads=[osq.r()]))
            yield

        rec = None
        for t in range(NTI):
            loc = gen_local(t)
            while True:
                a = next(loc, "done")
                if rec is not None:
                    next(rec, None)
                if a == "done":
                    break
            if rec is not None:
                for _ in rec:
                    pass
            rec = gen_rec(t)
        for _ in rec:
            pass
        outs.append(P.dma(lambda e: e.dma_start(out=s_out, in_=S[sidx[0] % 2][:, :]), reads=[S[sidx[0] % 2].r()]))
        outs.append(P.dma(lambda e: e.dma_start(out=h_out, in_=raw[:, :, 0:3]), reads=raw.all()))
        P.emit(final_waits=outs)
    return nc


def build_dsa(L=16384, NIT=18, jset=None):
    T = 512
    NTI = L // T
    NQT = L // 256
    NJ_ALL = NQT // 8
    jset = list(range(NJ_ALL)) if jset is None else list(jset)
    NJ = len(jset)
    NQ = NJ * 256
    SCALE = 128.0 ** -0.5
    WSC = (8.0 ** -0.5) * (64.0 ** -0.5)
    nc = bass.Bass("TRN2", target_bir_lowering=False)
    xT = nc.dram_tensor("xT", [1024, L], F32, kind="ExternalInput").ap()
    xq = nc.dram_tensor("xq", [1024, NQ], F32, kind="ExternalInput").ap()
    g = nc.dram_tensor("g", [1024], F32, kind="ExternalInput").ap()
    win = nc.dram_tensor("win", [1024, 3656], F32, kind="ExternalInput").ap()
    lng = nc.dram_tensor("lng", [64], F32, kind="ExternalInput").ap()
    lnb = nc.dram_tensor("lnb", [64], F32, kind="ExternalInput").ap()
    qrel = nc.dram_tensor("qrel", [128, NJ * 2], F32, kind="ExternalInput").ap()
    kidx = nc.dram_tensor("kidx", [128, 2048], F32, kind="ExternalInput").ap()
    cst = nc.dram_tensor("cst", [128, 5, 128], F32, kind="ExternalInput").ap()
    oT = nc.dram_tensor("oT", [1024, NQ], F32, kind="ExternalOutput").ap()
    kTd = nc.dram_tensor("kTd", [1024, L], BF16, kind="Internal").ap()
    Vd = nc.dram_tensor("Vd", [L, 1024], BF16, kind="Internal").ap()
    qTd = nc.dram_tensor("qTd", [1024, NQ], BF16, kind="Internal").ap()
    qiTd = nc.dram_tensor("qiTd", [64, 8, NQ], F32, kind="Internal").ap()
    kiTd = nc.dram_tensor("kiTd", [64, L], F32, kind="Internal").ap()
    xv = xT.rearrange("(c p) t -> p c t", p=128)
    xqv = xq.rearrange("(c p) t -> p c t", p=128)
    kTv = kTd.rearrange("(h p) t -> p h t", p=128)
    Vv = Vd.rearrange("(n p) d -> p n d", p=128)
    qTv = qTd.rearrange("(h p) t -> p h t", p=128)
    oTv = oT.rearrange("(h p) t -> p h t", p=128)
    wv_ = win.rearrange("(c p) f -> p c f", p=128)
    with ExitStack() as st:
        st.enter_context(nc.allow_low_precision("bf16 matmul operands, fp32 accumulate"))
        P = Prog(nc, st)
        banks = [P.ps(f"b{i}", [128, 512], F32) for i in range(8)]
        P.pss = banks
        P._psi = 0
        C = make_consts(P)
        ones_f = P.sb("ones_f", [128, 128], F32)
        P.op("dve", lambda e: e.memset(ones_f[:, :], 1.0), writes=[ones_f.r()])
        gt = load_vec_fm(P, g, 8, "g")
        lngt = P.sb("lngt", [64, 1], F32)
        lnbt = P.sb("lnbt", [64, 1], F32)
        P.dma(lambda e: e.dma_start(out=lngt[:, :], in_=lng.rearrange("(p o) -> p o", o=1)), writes=[lngt.r()])
        P.dma(lambda e: e.dma_start(out=lnbt[:, :], in_=lnb.rearrange("(p o) -> p o", o=1)), writes=[lnbt.r()])
        qrt = load_const(P, qrel, [128, NJ * 2], "qrt")
        kit = load_const(P, kidx, [128, 2048], "kit")
        Kc = load_const(P, cst, [128, 5, 128], "Kc")
        idb = P.sb("idb", [128, 128], BF16)
        P.op("dve", lambda e: e.tensor_copy(out=idb[:, :], in_=Kc[:, 0, :]), reads=[Kc.r()], writes=[idb.r()])
        selb = P.sb("selb", [128, 2, 512], BF16)
        P.op("dve", lambda e: e.memset(selb[:, :, :], 0.0), writes=[selb.r()])
        for hf in range(2):
            for rep in range(2):
                c0 = rep * 256 + hf * 128
                P.op("dve", lambda e, hf=hf, c0=c0: e.tensor_copy(out=selb[:, hf, c0:c0 + 128], in_=Kc[:, 0, :]),
                     reads=[Kc.r(), selb.r()], writes=[selb.r()])
        wiT = P.sb("wiT", [128, NJ * 2, 8], F32)

        P.push_scope()
        Wq = P.sb("Wq", [128, 8, 1024], BF16, nsub=8)
        Wk = P.sb("Wk", [128, 8, 1024], BF16, nsub=8)
        Wv = P.sb("Wv", [128, 8, 1024], BF16, nsub=8)
        Wi = P.sb("Wi", [128, 8, 584], F32, nsub=8)
        hnf = P.sb("hnf", [128, 8, T], F32, nsub=8)
        for c in range(8):
            P.dma(lambda e, c=c: e.dma_start(out=Wk[:, c, :], in_=wv_[:, c, 1024:2048], max_dma_last_dim=8192), writes=[Wk.r(c)], q="pool")
            P.dma(lambda e, c=c: e.dma_start(out=Wv[:, c, :], in_=wv_[:, c, 2048:3072], max_dma_last_dim=8192), writes=[Wv.r(c)], q="pool")
            P.dma(lambda e, c=c: e.dma_start(out=Wi[:, c, :], in_=wv_[:, c, 3072:3656]), writes=[Wi.r(c)])
            P.dma(lambda e, c=c: e.dma_start(out=Wq[:, c, :], in_=wv_[:, c, 0:1024], max_dma_last_dim=8192), writes=[Wq.r(c)], q="pool")
        x = P.sb("x", [128, 8, T], F32)
        sq = P.sb("sq", [128, 8, T], BF16, nsub=8)
        hn = P.sb("hn", [128, 8, T], BF16, nsub=8)
        rstd = P.sb("rstd", [128, T], F32)
        ktb = [P.sb(f"ktb{i}", [128, 8, T], BF16, nsub=8) for i in range(2)]
        vtb = [P.sb(f"vtb{i}", [128, 4, 1024], BF16, nsub=8) for i in range(2)]
        kraw = P.sb("kraw", [64, T], F32)
        ksq = P.sb("ksq", [64, T], F32)
        kmean = P.sb("kmean", [64, T], F32)
        kvar = P.sb("kvar", [64, T], F32)
        kio = P.sb("kio", [64, T], F32)
        NTI0 = min(NTI, (256 * (8 * max(jset) + 8)) // T)
        for it in range(NTI0):
            t0 = it * T
            kt = ktb[it % 2]
            vt = vtb[it % 2]
            P.dma(lambda e, t0=t0: e.dma_start(out=x[:, :, :], in_=xv[:, :, t0:t0 + T]), writes=[x.r()])
            norm_tile(P, C, x, gt, sq, hn, rstd, T)
            for c in range(8):
                P.op("dve", lambda e, c=c: e.scalar_tensor_tensor(out=hnf[:, c, :], in0=x[:, c, :], scalar=gt[:, c:c + 1], in1=rstd[:, :],
                                                                 op0=ALU.mult, op1=ALU.mult), reads=[x.r(), rstd.r(), gt.r()], writes=[hnf.r(c)])
            for h in range(8):
                ps = next_ps(P)
                for c in range(8):
                    P.op("pe", lambda e, c=c, h=h, ps=ps: e.matmul(ps[:, :T], lhsT=Wk[:, c, h * 128:(h + 1) * 128], rhs=hn[:, c, :],
                                                                  start=(c == 0), stop=(c == 7)),
                         reads=[Wk.r(c), hn.r(c)], writes=[ps.r()])
                if h % 2 == 0:
                    P.op("act", lambda e, h=h, ps=ps, kt=kt: e.copy(out=kt[:, h, :], in_=ps[:, :T]), reads=[ps.r()], writes=[kt.r(h)])
                else:
                    P.op("dve", lambda e, h=h, ps=ps, kt=kt: e.tensor_copy(out=kt[:, h, :], in_=ps[:, :T]), reads=[ps.r()], writes=[kt.r(h)])
            P.dma(lambda e, kt=kt, t0=t0: e.dma_start(out=kTv[:, :, t0:t0 + T], in_=kt[:, :, :]), reads=kt.all())
            for b4 in range(4):
                for half in range(2):
                    ps = next_ps(P)
                    for c in range(8):
                        P.op("pe", lambda e, c=c, b4=b4, half=half, ps=ps: e.matmul(
                            ps[:, :512], lhsT=hn[:, c, b4 * 128:(b4 + 1) * 128], rhs=Wv[:, c, half * 512:(half + 1) * 512],
                            start=(c == 0), stop=(c == 7)), reads=[Wv.r(c), hn.r(c)], writes=[ps.r()])
                    if half == 0:
                        P.op("act", lambda e, b4=b4, half=half, ps=ps, vt=vt: e.copy(out=vt[:, b4, 0:512], in_=ps[:, :512]),
                             reads=[ps.r()], writes=[vt.r(b4 * 2)])
                    else:
                        P.op("dve", lambda e, b4=b4, half=half, ps=ps, vt=vt: e.tensor_copy(out=vt[:, b4, 512:1024], in_=ps[:, :512]),
                             reads=[ps.r()], writes=[vt.r(b4 * 2 + 1)])
            P.dma(lambda e, vt=vt, it=it: e.dma_start(out=Vv[:, it * 4:(it + 1) * 4, :], in_=vt[:, :, :]), reads=vt.all(), q="act")
            ps = next_ps(P)
            for c in range(8):
                P.op("pe", lambda e, c=c, ps=ps: e.matmul(ps[0:64, :T], lhsT=Wi[:, c, 512:576], rhs=hnf[:, c, :],
                                                       start=(c == 0), stop=(c == 7)),
                     reads=[Wi.r(c), hnf.r(c)], writes=[ps.r()])
            P.op("act", lambda e, ps=ps: e.copy(out=kraw[:, :], in_=ps[0:64, :T]), reads=[ps.r()], writes=[kraw.r()])
            P.op("pool", lambda e: e.tensor_tensor(out=ksq[:, :], in0=kraw[:, :], in1=kraw[:, :], op=ALU.mult), reads=[kraw.r()], writes=[ksq.r()])
            p1 = next_ps(P)
            P.op("pe", lambda e, p1=p1: e.matmul(p1[0:64, :T], lhsT=ones_f[0:64, 0:64], rhs=kraw[:, :], start=True, stop=True),
                 reads=[ones_f.r(), kraw.r()], writes=[p1.r()])
            p2 = next_ps(P)
            P.op("pe", lambda e, p2=p2: e.matmul(p2[0:64, :T], lhsT=ones_f[0:64, 0:64], rhs=ksq[:, :], start=True, stop=True),
                 reads=[ones_f.r(), ksq.r()], writes=[p2.r()])
            P.op("act", lambda e, p1=p1: e.activation(out=kmean[:, :], in_=p1[0:64, :T], func=AF.Copy, scale=1.0 / 64), reads=[p1.r()], writes=[kmean.r()])
            P.op("dve", lambda e: e.tensor_tensor(out=ksq[:, :], in0=kmean[:, :], in1=kmean[:, :], op=ALU.mult), reads=[kmean.r(), ksq.r()], writes=[ksq.r()])
            P.op("dve", lambda e, p2=p2: e.scalar_tensor_tensor(out=kvar[:, :], in0=p2[0:64, :T], scalar=1.0 / 64, in1=ksq[:, :],
                                                              op0=ALU.mult, op1=ALU.subtract), reads=[p2.r(), ksq.r()], writes=[kvar.r()])
            P.op("act", lambda e: e.activation(out=kvar[:, :], in_=kvar[:, :], func=AF.Sqrt, bias=C.eps[0:64, :], scale=1.0),
                 reads=[kvar.r(), C.eps.r()], writes=[kvar.r()])
            P.op("dve", lambda e: e.reciprocal(out=kvar[:, :], in_=kvar[:, :]), reads=[kvar.r()], writes=[kvar.r()])
            P.op("pool", lambda e: e.tensor_tensor(out=kraw[:, :], in0=kraw[:, :], in1=kmean[:, :], op=ALU.subtract), reads=[kraw.r(), kmean.r()], writes=[kraw.r()])
            P.op("dve", lambda e: e.scalar_tensor_tensor(out=kraw[:, :], in0=kraw[:, :], scalar=lngt[:, 0:1], in1=kvar[:, :],
                                                        op0=ALU.mult, op1=ALU.mult), reads=[kraw.r(), kvar.r(), lngt.r()], writes=[kraw.r()])
            P.op("act", lambda e: e.activation(out=kio[:, :], in_=kraw[:, :], func=AF.Identity, bias=lnbt[:, 0:1], scale=1.0),
                 reads=[kraw.r(), lnbt.r()], writes=[kio.r()])
            P.dma(lambda e, t0=t0: e.dma_start(out=kiTd[:, t0:t0 + T], in_=kio[:, :]), reads=[kio.r()])
        qtb = P.sb("qtb", [128, 8, 256], BF16, nsub=8)
        qitb = P.sb("qitb", [64, 8, 256], F32, nsub=8)
        for j in range(NJ):
            q0 = j * 256
            P.dma(lambda e, q0=q0: e.dma_start(out=x[:, :, 0:256], in_=xqv[:, :, q0:q0 + 256]), writes=[x.r()])
            norm_tile(P, C, x, gt, sq, hn, rstd, 256)
            for c in range(8):
                P.op("dve", lambda e, c=c: e.scalar_tensor_tensor(out=hnf[:, c, 0:256], in0=x[:, c, 0:256], scalar=gt[:, c:c + 1], in1=rstd[:, 0:256],
                                                                 op0=ALU.mult, op1=ALU.mult), reads=[x.r(), rstd.r(), gt.r()], writes=[hnf.r(c)])
            for h in range(8):
                ps = next_ps(P)
                for c in range(8):
                    P.op("pe", lambda e, c=c, h=h, ps=ps: e.matmul(ps[:, :256], lhsT=Wq[:, c, h * 128:(h + 1) * 128], rhs=hn[:, c, 0:256],
                                                                  start=(c == 0), stop=(c == 7)),
                         reads=[Wq.r(c), hn.r(c)], writes=[ps.r()])
                P.op("act", lambda e, h=h, ps=ps: e.copy(out=qtb[:, h, :], in_=ps[:, :256]), reads=[ps.r()], writes=[qtb.r(h)])
                ps2 = next_ps(P)
                for c in range(8):
                    P.op("pe", lambda e, c=c, h=h, ps2=ps2: e.matmul(ps2[0:64, :256], lhsT=Wi[:, c, h * 64:(h + 1) * 64], rhs=hnf[:, c, 0:256],
                                                                    start=(c == 0), stop=(c == 7)),
                         reads=[Wi.r(c), hnf.r(c)], writes=[ps2.r()])
                P.op("dve", lambda e, h=h, ps2=ps2: e.tensor_copy(out=qitb[:, h, :], in_=ps2[0:64, :256]), reads=[ps2.r()], writes=[qitb.r(h)])
            P.dma(lambda e, q0=q0: e.dma_start(out=qTv[:, :, q0:q0 + 256], in_=qtb[:, :, :]), reads=qtb.all())
            P.dma(lambda e, q0=q0: e.dma_start(out=qiTd[:, :, q0:q0 + 256], in_=qitb[:, :, :]), reads=qitb.all(), q="act")
            for hf in range(2):
                ps = next_ps(P)
                for c in range(8):
                    P.op("pe", lambda e, c=c, hf=hf, ps=ps: e.matmul(ps[:, 0:8], lhsT=hnf[:, c, hf * 128:(hf + 1) * 128], rhs=Wi[:, c, 576:584],
                                                                    start=(c == 0), stop=(c == 7)),
                         reads=[Wi.r(c), hnf.r(c)], writes=[ps.r()])
                P.op("act", lambda e, j=j, hf=hf, ps=ps: e.activation(out=wiT[:, j * 2 + hf, :], in_=ps[:, 0:8], func=AF.Copy, scale=WSC),
                     reads=[ps.r()], writes=[wiT.r()])
        P.pop_scope()

        I = P.sb("I", [128, L], F32)
        Mb = [P.sb(f"Mb{i}", [128, L], BF16) for i in range(2)]
        junk = P.sb("junk", [128, 2048], BF16)
        junkA = P.sb("junkA", [128, 2048], BF16)
        cnA = P.sb("cnA", [128, 8], F32)
        qih = P.sb("qih", [64, 8, 128], F32)
        dg = P.sb("dg", [128, 8, 128], F32)
        rl = [P.sb(f"rl{i}", [128, 512], F32) for i in range(3)]
        kics = [P.sb(f"kic{i}", [64, 512], F32) for i in range(3)]
        tmpb = P.sb("tmpb", [128, 512], F32)
        am = P.sb("am", [128, 32], F32)
        cn = P.sb("cn", [128, 8], F32)
        sm = P.sb("sm", [128, 8], F32)
        qt = P.sb("qt", [128, 4, 256], BF16)
        ktl = [P.sb(f"ktl{i}", [128, 4, 512], BF16) for i in range(2)]
        vtl = [P.sb(f"vtl{i}", [128, 4, 512], BF16) for i in range(2)]
        pts = [P.sb(f"pt{i}", [128, 512], BF16) for i in range(5)]
        rden = P.sb("rden", [128, 512], F32)
        ob = [P.sb(f"ob{i}", [128, 512], F32) for i in range(2)]
        bS = banks[0:3]
        bO = banks[3:5]
        bD = banks[5:7]
        bI = [banks[3], banks[4]]
        outs = []
        rot = {"s": 0, "p": 0, "l": 0, "r": 0, "i": 0, "k": 0}

        def nxt(lst, key):
            rot[key] += 1
            return lst[rot[key] % len(lst)]

        for j in range(NJ):
            Nmax = 256 * (8 * jset[j] + 8)
            ws = Nmax - 2048
            nck = Nmax // 512
            for hf in range(2):
                qi = j * 2 + hf
                M = Mb[hf]
                P.dma(lambda e, j=j, hf=hf: e.dma_start(out=qih[:, :, :], in_=qiTd[:, :, j * 256 + hf * 128:j * 256 + hf * 128 + 128]),
                      writes=[qih.r()])
                for h in range(8):
                    eng = "pool" if h % 2 == 0 else "dve"
                    P.op(eng, lambda e, h=h, qi=qi: e.tensor_scalar(out=dg[:, h, :], in0=Kc[:, 0, :], scalar1=wiT[:, qi, h:h + 1], scalar2=None, op0=ALU.mult),
                         reads=[Kc.r(), wiT.r()], writes=[dg.r()])
                for kc in range(nck):
                    pI = nxt(bI, "i")
                    kic = nxt(kics, "k")
                    P.dma(lambda e, kic=kic, kc=kc: e.dma_start(out=kic[:, :], in_=kiTd[:, kc * 512:(kc + 1) * 512]), writes=[kic.r()], q="act")
                    def score(h, kc=kc, kic=kic):
                        ps = nxt(bS, "s")
                        r = nxt(rl, "r")
                        P.op("pe", lambda e, h=h, ps=ps: e.matmul(ps[:, :512], lhsT=qih[:, h, :], rhs=kic[:, :], start=True, stop=True),
                             reads=[qih.r(), kic.r()], writes=[ps.r()])
                        P.op("act", lambda e, ps=ps, r=r: e.activation(out=r[:, :], in_=ps[:, :512], func=AF.Relu), reads=[ps.r()], writes=[r.r()])
                        return r
                    def accum(h, r, pI=pI):
                        P.op("pe", lambda e, h=h, r=r: e.matmul(pI[:, :512], lhsT=dg[:, h, :], rhs=r[:, :], start=(h == 0), stop=(h == 7)),
                             reads=[dg.r(), r.r()], writes=[pI.r()])
                    prev = score(0)
                    for h in range(1, 8):
                        cur = score(h)
                        accum(h - 1, prev)
                        prev = cur
                    accum(7, prev)
                    P.op("dve", lambda e, pI=pI, kc=kc: e.tensor_reduce(out=am[:, kc:kc + 1], in_=pI[:, :512], axis=AX.X, op=ALU.max, apply_absolute_value=True),
                         reads=[pI.r()], writes=[am.r()])
                    k0 = kc * 512
                    if k0 >= ws:
                        ro = k0 - ws
                        P.op("dve", lambda e, ro=ro, qi=qi: e.tensor_scalar(out=tmpb[:, :], in0=kit[:, ro:ro + 512], scalar1=qrt[:, qi:qi + 1], scalar2=-1e30,
                                                                          op0=ALU.is_gt, op1=ALU.mult), reads=[kit.r(), qrt.r()], writes=[tmpb.r()])
                        P.op("dve", lambda e, pI=pI, k0=k0: e.tensor_tensor(out=I[:, k0:k0 + 512], in0=pI[:, :512], in1=tmpb[:, :], op=ALU.add),
                             reads=[pI.r(), tmpb.r()], writes=[I.r()])
                    else:
                        P.op("dve", lambda e, pI=pI, k0=k0: e.tensor_copy(out=I[:, k0:k0 + 512], in_=pI[:, :512]), reads=[pI.r()], writes=[I.r()])
                P.op("dve", lambda e, nck=nck: e.tensor_reduce(out=sm[:, 0:1], in_=am[:, 0:nck], axis=AX.X, op=ALU.max), reads=[am.r()], writes=[sm.r()])
                P.op("dve", lambda e: e.tensor_scalar(out=sm[:, 1:2], in0=sm[:, 0:1], scalar1=2.0, scalar2=None, op0=ALU.mult), reads=[sm.r()], writes=[sm.r()])
                P.op("dve", lambda e: e.tensor_scalar(out=sm[:, 2:3], in0=sm[:, 0:1], scalar1=-1.0, scalar2=None, op0=ALU.mult), reads=[sm.r()], writes=[sm.r()])
                npc = (Nmax + 2047) // 2048
                for itn in range(NIT):
                    P.op("dve", lambda e, itn=itn: e.tensor_scalar(out=sm[:, 3:4], in0=sm[:, 1:2], scalar1=2.0 ** -(itn + 1), scalar2=None, op0=ALU.mult),
                         reads=[sm.r()], writes=[sm.r()])
                    P.op("dve", lambda e: e.tensor_tensor(out=sm[:, 4:5], in0=sm[:, 2:3], in1=sm[:, 3:4], op=ALU.add), reads=[sm.r()], writes=[sm.r()])
                    dpc = [pc for pc in range(npc) if pc % 2 == 0]
                    apc = [pc for pc in range(npc) if pc % 2 == 1]
                    if apc:
                        P.op("dve", lambda e: e.tensor_scalar(out=sm[:, 7:8], in0=sm[:, 4:5], scalar1=-1.0, scalar2=None, op0=ALU.mult),
                             reads=[sm.r()], writes=[sm.r()])
                    for k, pc in enumerate(dpc):
                        P.op("dve", lambda e, pc=pc, k=k: e.tensor_scalar(out=junk[:, :], in0=I[:, pc * 2048:(pc + 1) * 2048], scalar1=sm[:, 4:5], scalar2=None,
                                                                   op0=ALU.is_ge, op1=ALU.add, accum_out=cn[:, k:k + 1]),
                             reads=[I.r(), sm.r()], writes=[junk.r(), cn.r()])
                    for k, pc in enumerate(apc):
                        P.op("act", lambda e, pc=pc, k=k: e.activation(out=junkA[:, :], in_=I[:, pc * 2048:(pc + 1) * 2048], func=AF.Sign,
                                                                      bias=sm[:, 7:8], scale=1.0, accum_out=cnA[:, k:k + 1]),
                             reads=[I.r(), sm.r()], writes=[junkA.r(), cnA.r()])
                    P.op("dve", lambda e, nd=len(dpc): e.tensor_reduce(out=sm[:, 5:6], in_=cn[:, 0:nd], axis=AX.X, op=ALU.add), reads=[cn.r()], writes=[sm.r()])
                    if apc:
                        na = len(apc)
                        P.op("dve", lambda e, na=na: e.tensor_reduce(out=sm[:, 6:7], in_=cnA[:, 0:na], axis=AX.X, op=ALU.add), reads=[cnA.r()], writes=[sm.r()])
                        P.op("dve", lambda e, na=na: e.tensor_scalar(out=sm[:, 6:7], in0=sm[:, 6:7], scalar1=0.5, scalar2=1024.0 * na, op0=ALU.mult, op1=ALU.add),
                             reads=[sm.r()], writes=[sm.r()])
                        P.op("dve", lambda e: e.tensor_tensor(out=sm[:, 5:6], in0=sm[:, 5:6], in1=sm[:, 6:7], op=ALU.add), reads=[sm.r()], writes=[sm.r()])
                    P.op("dve", lambda e: e.tensor_scalar(out=sm[:, 6:7], in0=sm[:, 5:6], scalar1=255.5, scalar2=sm[:, 3:4], op0=ALU.is_gt, op1=ALU.mult),
                         reads=[sm.r()], writes=[sm.r()])
                    P.op("dve", lambda e: e.tensor_tensor(out=sm[:, 2:3], in0=sm[:, 2:3], in1=sm[:, 6:7], op=ALU.add), reads=[sm.r()], writes=[sm.r()])
                for pc in range(npc):
                    P.op("dve", lambda e, pc=pc, M=M: e.tensor_scalar(out=M[:, pc * 2048:(pc + 1) * 2048], in0=I[:, pc * 2048:(pc + 1) * 2048],
                                                                     scalar1=sm[:, 2:3], scalar2=-30000.0, op0=ALU.is_lt, op1=ALU.mult),
                         reads=[I.r(), sm.r()], writes=[M.r()])
            for pz in range(2):
                P.dma(lambda e, j=j, pz=pz: e.dma_start(out=qt[:, :, :], in_=qTv[:, 4 * pz:4 * pz + 4, j * 256:(j + 1) * 256]), writes=[qt.r()])
                nkb = Nmax // 128
                LA = 2
                pend = []

                def back(kb, pair, pt, vt, kbl, nkb=nkb):
                    for hh in range(2):
                        hl = 2 * pair + hh
                        P.op("pe", lambda e, pair=pair, hh=hh, hl=hl, vt=vt, kbl=kbl, pt=pt, kb=kb, nkb=nkb: e.matmul(
                            bO[pair][:, hh * 256:(hh + 1) * 256], lhsT=vt[:, kbl, hl * 128:(hl + 1) * 128], rhs=pt[:, hh * 256:(hh + 1) * 256],
                            start=(kb == 0 and hh == 0), stop=(kb == nkb - 1 and hh == 1), skip_group_check=True),
                            reads=[vt.r(), pt.r()], writes=[bO[pair].r()])
                    P.op("pe", lambda e, pair=pair, pt=pt, kb=kb, nkb=nkb: e.matmul(
                        bD[pair][:, 0:512], lhsT=C.ones_bf[:, :], rhs=pt[:, :], start=(kb == 0), stop=(kb == nkb - 1)),
                        reads=[C.ones_bf.r(), pt.r()], writes=[bD[pair].r()])

                for kb in range(nkb):
                    kbl = kb % 4
                    if kbl == 0:
                        kt = nxt(ktl, "l")
                        vt = vtl[rot["l"] % len(vtl)]
                        k4 = kb // 4
                        P.dma(lambda e, kt=kt, k4=k4, pz=pz: e.dma_start(out=kt[:, :, :], in_=kTv[:, 4 * pz:4 * pz + 4, k4 * 512:(k4 + 1) * 512]),
                              writes=[kt.r()])
                        P.dma(lambda e, vt=vt, k4=k4, pz=pz: e.dma_start(out=vt[:, :, :], in_=Vv[:, k4 * 4:(k4 + 1) * 4, pz * 512:(pz + 1) * 512]),
                              writes=[vt.r()], q="act")
                    for pair in range(2):
                        pS = nxt(bS, "s")
                        pt = nxt(pts, "p")
                        for hh in range(2):
                            hl = 2 * pair + hh
                            P.op("pe", lambda e, pS=pS, hh=hh, hl=hl, kt=kt, kbl=kbl: e.matmul(
                                pS[:, hh * 256:(hh + 1) * 256], lhsT=kt[:, hl, kbl * 128:(kbl + 1) * 128], rhs=qt[:, hl, :],
                                start=(hh == 0), stop=False, skip_group_check=True), reads=[kt.r(), qt.r()], writes=[pS.r()])
                        for hf in range(2):
                            P.op("pe", lambda e, pS=pS, hf=hf, kb=kb: e.matmul(
                                pS[:, 0:512], lhsT=Mb[hf][:, kb * 128:(kb + 1) * 128], rhs=selb[:, hf, :],
                                start=False, stop=(hf == 1), skip_group_check=True), reads=[Mb[hf].r(), selb.r()], writes=[pS.r()])
                        P.op("act", lambda e, pS=pS, pt=pt: e.activation(out=pt[:, :], in_=pS[:, 0:512], func=AF.Exp, scale=SCALE),
                             reads=[pS.r()], writes=[pt.r()])
                        pend.append((kb, pair, pt, vt, kbl))
                        if len(pend) > LA:
                            back(*pend.pop(0))
                while pend:
                    back(*pend.pop(0))
                for pair in range(2):
                    o = ob[pair]
                    P.op("dve", lambda e, pair=pair: e.reciprocal(out=rden[:, :], in_=bD[pair][:, 0:512]), reads=[bD[pair].r()], writes=[rden.r()])
                    P.op("dve", lambda e, pair=pair, o=o: e.tensor_tensor(out=o[:, :], in0=bO[pair][:, 0:512], in1=rden[:, :], op=ALU.mult),
                         reads=[bO[pair].r(), rden.r()], writes=[o.r()])
                    h0 = 4 * pz + 2 * pair
                    outs.append(P.dma(lambda e, o=o, h0=h0, j=j: e.dma_start(
                        out=oTv[:, h0:h0 + 2, j * 256:(j + 1) * 256], in_=o[:, :].rearrange("p (h q) -> p h q", h=2)), reads=[o.r()]))
        P.emit(final_waits=outs)
    return nc


def _rope_tables(L, dim, theta=10000.0):
    pos = np.arange(L, dtype=np.float32)
    inv = (theta ** (-np.arange(0, dim, 2, dtype=np.float32) / dim)).astype(np.float32)
    ang = pos[:, None] * inv[None, :]
    return np.cos(ang).astype(np.float32), np.sin(ang).astype(np.float32)


def _mla_inputs(hT, g, w_in, gq, w_uq, gkv, w_ukv, core):
    z64 = np.zeros((1024, 64), np.float32)
    kr = w_in[:, 640:672]
    krs = np.concatenate([kr[:, 16:], kr[:, :16]], 1)
    wall = np.concatenate([w_in[:, :640], z64, kr, z64, krs], 1)
    cols = []
    for h in (2 * core, 2 * core + 1):
        wq = w_uq[:, h * 96:(h + 1) * 96]
        wqs = np.concatenate([np.zeros((384, 64), np.float32), wq[:, 80:96], wq[:, 64:80]], 1)
        cols += [wq, wqs]
    wuq = np.concatenate(cols, 1)
    kn = [w_ukv[:, h * 128:h * 128 + 64] for h in (2 * core, 2 * core + 1)]
    vv = [w_ukv[:, h * 128 + 64:h * 128 + 128] for h in (2 * core, 2 * core + 1)]
    wukv = np.concatenate(kn + vv, 1)
    return dict(hT=hT, g=g, wall=np.ascontiguousarray(wall), gq=gq, gkv=gkv, wuq=np.ascontiguousarray(wuq),
                wukv=np.ascontiguousarray(wukv))


def _mla_consts(L):
    cos, sin = _rope_tables(L, 32)
    cos2 = np.zeros((96, L), np.float32)
    sin2 = np.zeros((96, L), np.float32)
    cos2[64:80] = cos.T
    cos2[80:96] = cos.T
    sin2[64:80] = -sin.T
    sin2[80:96] = sin.T
    k = np.arange(128)[:, None, None]
    d = np.arange(4)[None, :, None]
    q = np.arange(512)[None, None, :]
    cmask = ((d * 128 + k) <= q).astype(np.float32)
    esel = np.zeros((65, 64), np.float32)
    esel[64] = 1.0
    return dict(cos2=cos2, sin2=sin2, cmask=np.ascontiguousarray(cmask), esel=esel)


def _gdn_inputs(hT, g, w_in, conv_w, a_log, dt_bias, og, h):
    cols = [w_in[:, h * 128:(h + 1) * 128], w_in[:, 1024 + h * 128:1024 + (h + 1) * 128],
            w_in[:, 2048 + h * 128:2048 + (h + 1) * 128], w_in[:, 3072 + h * 128:3072 + (h + 1) * 128],
            w_in[:, 4096 + h:4097 + h], w_in[:, 4104 + h:4105 + h], np.zeros((1024, 126), np.float32)]
    wh = np.ascontiguousarray(np.concatenate(cols, 1))
    cw = np.concatenate([conv_w[:, h * 128:(h + 1) * 128], conv_w[:, 1024 + h * 128:1024 + (h + 1) * 128],
                         conv_w[:, 2048 + h * 128:2048 + (h + 1) * 128]], 1)
    sc = np.zeros((128, 2), np.float32)
    sc[:, 0] = a_log[h]
    sc[:, 1] = dt_bias[h]
    return dict(hT=hT, g=g, wh=wh, cw=np.ascontiguousarray(cw.T), sc=sc, og=og)


def _gdn_consts():
    j = np.arange(128)[:, None]
    i = np.arange(128)[None, :]
    same = (j // 64) == (i // 64)
    cst = np.zeros((128, 5, 128), np.float32)
    cst[:, 0] = np.eye(128)
    cst[:, 1] = (same & (i > j))
    cst[:, 2] = (same & (i >= j))
    cst[:, 3] = same
    cst[:, 4, 0] = (np.arange(128) < 64)
    cst[:, 4, 1] = (np.arange(128) >= 64)
    return dict(cst=cst)


def _dsa_inputs(xT, g, w_in, lg, lb, core, L, jset=None):
    jset = list(range(L // 256 // 8)) if jset is None else list(jset)
    NJ = len(jset)
    cols = []
    qrel = np.zeros((128, NJ * 2), np.float32)
    for jj, j in enumerate(jset):
        tq = 8 * j + core
        cols.append(xT[:, tq * 256:(tq + 1) * 256])
        ws = 256 * (8 * j + 8) - 2048
        for hf in range(2):
            qrel[:, jj * 2 + hf] = tq * 256 + hf * 128 + np.arange(128) - ws
    return dict(xT=xT, xq=np.ascontiguousarray(np.concatenate(cols, 1)), g=g, win=w_in, lng=lg, lnb=lb, qrel=qrel)


def _dsa_consts():
    kidx = np.tile(np.arange(2048, dtype=np.float32)[None, :], (128, 1))
    cst = np.zeros((128, 5, 128), np.float32)
    cst[:, 0] = np.eye(128)
    return dict(kidx=kidx, cst=cst)


def _dsa_gather(outs, L, jset=None, full=None):
    jset = list(range(L // 256 // 8)) if jset is None else list(jset)
    if full is None:
        full = np.zeros((1024, L), np.float32)
    for c, o in enumerate(outs):
        for jj, j in enumerate(jset):
            tq = 8 * j + c
            full[:, tq * 256:(tq + 1) * 256] = o[:, jj * 256:(jj + 1) * 256]
    return full


_PROGS = {}
DSA_SPLITS = ([0, 1, 2, 3, 4], [5, 6, 7])
GDN_SEG = 4096


def _prog(name, fn):
    if name not in _PROGS:
        _PROGS[name] = fn()
    return _PROGS[name]


def _run(nc, in_maps):
    res = run_bass_kernel_spmd(nc, in_maps, core_ids=list(range(8)))
    return [r["oT"] for r in res.results]


def _split(hT, TOK=2048):
    return [np.ascontiguousarray(hT[:, i * TOK:(i + 1) * TOK]) for i in range(8)]


def kernel(**inp):
    f32 = lambda a: np.ascontiguousarray(np.asarray(a, dtype=np.float32))
    L = 16384
    TOK = L // 8
    x = f32(inp["x"])[0]
    hT = np.ascontiguousarray(x.T)
    ident = np.eye(128, dtype=np.float32)

    def outproj(hT, mT, w):
        nc = _prog("outproj", lambda: build_outproj(TOK, 512))
        hs, ms = _split(hT), _split(mT)
        return np.concatenate(_run(nc, [dict(hT=hs[i], mT=ms[i], w=w) for i in range(8)]), axis=1)

    def mlp(hT, i, final=False):
        hs = _split(hT)
        g, w1, w2 = f32(inp["norm_mlp_g"][i]), f32(inp["mlp_w1"][i]), f32(inp["mlp_w2"][i])
        if final:
            nc = _prog("mlpf", lambda: build_mlp(TOK, 256, True))
            fg = f32(inp["final_g"])
            maps = [dict(hT=hs[c], g=g, w1=w1, w2=w2, fg=fg) for c in range(8)]
        else:
            nc = _prog("mlp", lambda: build_mlp(TOK, 256, False))
            maps = [dict(hT=hs[c], g=g, w1=w1, w2=w2) for c in range(8)]
        return np.concatenate(_run(nc, maps), axis=1)

    cst = _dsa_consts()
    g0, win0 = f32(inp["norm_mix_g"][0]), f32(inp["dsa_w_in"][0])
    lg, lb = f32(inp["dsa_idx_k_g"][0]), f32(inp["dsa_idx_k_b"][0])
    mT = None
    for js in DSA_SPLITS:
        nc = _prog("dsa" + str(js), lambda: build_dsa(L, 18, js))
        outs = _run(nc, [{**_dsa_inputs(hT, g0, win0, lg, lb, c, L, js), **cst} for c in range(8)])
        mT = _dsa_gather(outs, L, js, mT)
    hT = outproj(hT, mT, f32(inp["dsa_w_out"][0]))
    hT = mlp(hT, 0)
    nc = _prog("conv", lambda: build_conv(TOK, 256))
    hp = np.concatenate([np.zeros((1024, 30), np.float32), hT], axis=1)
    cw = dict(g=f32(inp["norm_mix_g"][1]), w1=f32(inp["conv_w_pw1"][0]), b1=f32(inp["conv_b_pw1"][0]),
              wdT=np.ascontiguousarray(f32(inp["conv_w_dw"][0]).T), bd=f32(inp["conv_b_dw"][0]),
              lg=f32(inp["conv_ln_g"][0]), lb=f32(inp["conv_ln_b"][0]), w2=f32(inp["conv_w_pw2"][0]),
              b2=f32(inp["conv_b_pw2"][0]), ident=ident)
    maps = [dict(hT=np.ascontiguousarray(hp[:, c * TOK:c * TOK + TOK + 30]),
                 hs=np.full((128, 1), 0.0 if c == 0 else 1.0, np.float32), **cw) for c in range(8)]
    hT = np.concatenate(_run(nc, maps), axis=1)
    hT = mlp(hT, 1)
    nc = _prog("mla", lambda: build_mla(L))
    cst = _mla_consts(L)
    maps = [{**_mla_inputs(hT, f32(inp["norm_mix_g"][2]), f32(inp["mla_w_in"][0]), f32(inp["mla_q_norm_g"][0]),
                           f32(inp["mla_w_uq"][0]), f32(inp["mla_kv_norm_g"][0]), f32(inp["mla_w_ukv"][0]), c), **cst}
            for c in range(8)]
    mT = np.concatenate(_run(nc, maps), axis=0)
    hT = outproj(hT, mT, f32(inp["mla_w_out"][0]))
    hT = mlp(hT, 2)
    LG = GDN_SEG
    nc = _prog("gdn", lambda: build_gdn(LG))
    cst = _gdn_consts()
    gi = [_gdn_inputs(None, f32(inp["norm_mix_g"][3]), f32(inp["gdn_w_in"][0]), f32(inp["gdn_conv_w"][0]),
                      f32(inp["gdn_a_log"][0]), f32(inp["gdn_dt_bias"][0]), f32(inp["gdn_o_norm_g"][0]), c) for c in range(8)]
    st = [np.zeros((128, 128), np.float32) for _ in range(8)]
    hl = [np.zeros((128, 3, 3), np.float32) for _ in range(8)]
    segs = []
    for s0 in range(0, L, LG):
        hseg = np.ascontiguousarray(hT[:, s0:s0 + LG])
        maps = [{**gi[c], **cst, "hT": hseg, "s_in": st[c], "h_in": hl[c]} for c in range(8)]
        res = run_bass_kernel_spmd(nc, maps, core_ids=list(range(8))).results
        segs.append(np.concatenate([r["oT"] for r in res], axis=0))
        st = [np.ascontiguousarray(r["s_out"]) for r in res]
        hl = [np.ascontiguousarray(r["h_out"]) for r in res]
    mT = np.concatenate(segs, axis=1)
    hT = outproj(hT, mT, f32(inp["gdn_w_out"][0]))
    hT = mlp(hT, 3, final=True)
    return np.ascontiguousarray(hT.T)[None].astype(np.float32)
```

```python
import numpy as np
import concourse.bass as bass
import concourse.mybir as mybir
from contextlib import ExitStack
from concourse.bass_utils import run_bass_kernel_spmd

F32 = mybir.dt.float32
BF16 = mybir.dt.bfloat16
U8 = mybir.dt.uint8
I32 = mybir.dt.int32
ALU = mybir.AluOpType
AF = mybir.ActivationFunctionType
AX = mybir.AxisListType

ENGS = ["pe", "act", "dve", "pool", "sp"]
NDMA = 8


class Res:
    __slots__ = ("name", "lastw", "readers")

    def __init__(self, name):
        self.name = name
        self.lastw = None
        self.readers = []


class Tile:
    def __init__(self, t, name, nsub=1):
        self.t = t
        self.name = name
        self.res = [Res(f"{name}.{i}") for i in range(nsub)]

    def __getitem__(self, idx):
        return self.t[idx]

    def r(self, i=0):
        return self.res[i]

    def all(self):
        return list(self.res)


class View:
    def __init__(self, base, off, width, name, share=True):
        self.base = base
        self.off = off
        self.width = width
        self.res = base.res if share else [Res(name)]

    def __getitem__(self, idx):
        rows, cols = idx
        start = cols.start or 0
        stop = self.width if cols.stop is None else cols.stop
        return self.base.t[rows, self.off + start:self.off + stop]

    def r(self, i=0):
        return self.res[0]

    def all(self):
        return list(self.res)


class Prog:
    def __init__(self, nc, stack):
        self.nc = nc
        self.stack = stack
        self.ops = {e: [] for e in ENGS}
        self.cnt = {e: 0 for e in ENGS}
        self.sem = {e: stack.enter_context(nc.semaphore(f"s_{e}")) for e in ENGS if e != "sp"}
        self.dsem = {q: [stack.enter_context(nc.semaphore(f"d_{q}{i}")) for i in range(NDMA)]
                     for q in ("sp", "pool", "act")}
        self.dcnt = {q: 0 for q in ("sp", "pool", "act")}
        self.dtok = {q: [] for q in ("sp", "pool", "act")}
        self.known = {e: {} for e in ENGS}
        self.nops = 0

    def sb(self, name, shape, dtype, nsub=1):
        t = self.stack.enter_context(self.nc.sbuf_tensor("sb_" + name, list(shape), dtype))
        return Tile(t, name, nsub)

    def ps(self, name, shape, dtype=F32, nsub=1):
        t = self.stack.enter_context(self.nc.psum_tensor("ps_" + name, list(shape), dtype))
        return Tile(t, name, nsub)

    def push_scope(self):
        self._saved_stack = self.stack
        self.stack = ExitStack()
        return self.stack

    def pop_scope(self):
        self.barrier()
        self.stack.close()
        self.stack = self._saved_stack

    def barrier(self):
        toks = []
        for e in ENGS:
            if e != "sp" and self.cnt[e] > 0:
                toks.append((("c", e), self.cnt[e], e))
        for q in self.dtok:
            toks += self.dtok[q][-NDMA:]
        self._pending = {e: list(toks) for e in ENGS}

    def _deps(self, eng, reads, writes):
        deps = {}
        for tok in getattr(self, "_pending", {}).get(eng, []):
            if not (tok[2] == eng and eng == "pe"):
                if deps.get(tok[0], (0,))[0] < tok[1]:
                    deps[tok[0]] = (tok[1], tok)
        if getattr(self, "_pending", None):
            self._pending[eng] = []
        def add(tok):
            if tok is None:
                return
            key, val, teng = tok
            if teng == eng and eng == "pe":
                return
            if deps.get(key, (0,))[0] < val:
                deps[key] = (val, tok)
        for r in reads:
            add(r.lastw)
        for w in writes:
            add(w.lastw)
            for t in w.readers:
                add(t)
        out = []
        kn = self.known[eng]
        for key, (val, tok) in deps.items():
            if kn.get(key, 0) >= val:
                continue
            kn[key] = val
            out.append(tok)
        return out

    def _commit(self, tok, reads, writes):
        for r in reads:
            r.readers.append(tok)
        for w in writes:
            w.lastw = tok
            w.readers = []

    def op(self, eng, fn, reads=(), writes=()):
        waits = self._deps(eng, reads, writes)
        self.cnt[eng] += 1
        tok = (("c", eng), self.cnt[eng], eng)
        self.ops[eng].append((waits, fn, tok))
        self._commit(tok, reads, writes)
        self.nops += 1
        return tok

    def dma(self, fn, reads=(), writes=(), q="sp"):
        eng = q
        waits = self._deps(eng, reads, writes)
        i = self.dcnt[q]
        self.dcnt[q] += 1
        slot = i % NDMA
        val = 16 * (i // NDMA + 1)
        if i >= NDMA:
            prev = self.dtok[q][i - NDMA]
            key, pval, _ = prev
            if self.known[eng].get(key, 0) < pval:
                self.known[eng][key] = pval
                waits.append(prev)
        tok = (("d", q, slot), val, "dma")
        self.dtok[q].append(tok)
        self.ops[eng].append((waits, fn, tok))
        self._commit(tok, reads, writes)
        self.nops += 1
        return tok

    def _semof(self, tok):
        key = tok[0]
        if key[0] == "c":
            return self.sem[key[1]]
        return self.dsem[key[1]][key[2]]

    def emit(self, final_waits=()):
        nc = self.nc
        with nc.Block() as block:
            def run(eng, e):
                for waits, fn, tok in self.ops[eng]:
                    for w in waits:
                        e.wait_ge(self._semof(w), w[1])
                    ins = fn(e)
                    if tok[2] == "dma":
                        ins.then_inc(self._semof(tok), 16)
                    else:
                        ins.then_inc(self._semof(tok), 1)
                if eng == "sp":
                    for w in final_waits:
                        e.wait_ge(self._semof(w), w[1])

            @block.tensor
            def _(e):
                run("pe", e)

            @block.scalar
            def _(e):
                run("act", e)

            @block.vector
            def _(e):
                run("dve", e)

            @block.gpsimd
            def _(e):
                run("pool", e)

            @block.sync
            def _(e):
                run("sp", e)


def load_w_bf16(P, w_ap, KC, Fo, name, q="pool"):
    t = P.sb(name, [128, KC, Fo], BF16, nsub=KC)
    wv = w_ap.rearrange("(c p) f -> p c f", p=128)
    for c in range(KC):
        P.dma(lambda e, c=c: e.dma_start(out=t[:, c, :], in_=wv[:, c, :], max_dma_last_dim=8192),
              writes=[t.r(c)], q=q)
    return t


def load_vec_fm(P, v_ap, KC, name):
    t = P.sb(name, [128, KC], F32)
    vv = v_ap.rearrange("(c p) -> p c", p=128)
    P.dma(lambda e: e.dma_start(out=t[:, :], in_=vv, allow_slow_non_contiguous=True), writes=[t.r()])
    return t


class Ctx:
    pass


def make_consts(P):
    C = Ctx()
    C.ones_bf = P.sb("ones_bf", [128, 128], BF16)
    P.op("dve", lambda e: e.memset(C.ones_bf[:, :], 1.0), writes=[C.ones_bf.r()])
    C.eps = P.sb("eps_t", [128, 1], F32)
    P.op("dve", lambda e: e.memset(C.eps[:, :], 1e-6), writes=[C.eps.r()])
    return C


def rms_rstd(P, C, x, KC, T, psum, sq, rstd, dim):
    for c in range(KC):
        P.op("act", lambda e, c=c: e.activation(out=sq[:, c, :], in_=x[:, c, :], func=AF.Square),
             reads=[x.r()], writes=[sq.r(c)])
    for c in range(KC):
        P.op("pe", lambda e, c=c: e.matmul(psum[:, :T], lhsT=C.ones_bf[:, :], rhs=sq[:, c, :],
                                          start=(c == 0), stop=(c == KC - 1)),
             reads=[sq.r(c), C.ones_bf.r()], writes=[psum.r()])
    P.op("act", lambda e: e.activation(out=rstd[:, :], in_=psum[:, :T], func=AF.Sqrt,
                                       bias=C.eps[:, :], scale=1.0 / dim),
         reads=[psum.r(), C.eps.r()], writes=[rstd.r()])
    P.op("dve", lambda e: e.reciprocal(out=rstd[:, :], in_=rstd[:, :]), reads=[rstd.r()], writes=[rstd.r()])


def build_mlp(TOK=2048, T=256, final=False, proj=False):
    nc = bass.Bass("TRN2", target_bir_lowering=False)
    hT = nc.dram_tensor("hT", [1024, TOK], F32, kind="ExternalInput").ap()
    g = nc.dram_tensor("g", [1024], F32, kind="ExternalInput").ap()
    w1 = nc.dram_tensor("w1", [1024, 4096], F32, kind="ExternalInput").ap()
    w2 = nc.dram_tensor("w2", [4096, 1024], F32, kind="ExternalInput").ap()
    oT = nc.dram_tensor("oT", [1024, TOK], F32, kind="ExternalOutput").ap()
    with ExitStack() as st:
        st.enter_context(nc.allow_low_precision("bf16 matmul operands, fp32 accumulate"))
        P = Prog(nc, st)
        C = make_consts(P)
        gt = load_vec_fm(P, g, 8, "g")
        W1 = load_w_bf16(P, w1, 8, 4096, "W1")
        W2 = load_w_bf16(P, w2, 32, 1024, "W2")
        fgt = None
        if final:
            fg = nc.dram_tensor("fg", [1024], F32, kind="ExternalInput").ap()
            fgt = load_vec_fm(P, fg, 8, "fg")
        pj = None
        if proj:
            mT = nc.dram_tensor("mT", [1024, TOK], F32, kind="ExternalInput").ap()
            wo = nc.dram_tensor("wo", [1024, 1024], F32, kind="ExternalInput").ap()
            pj = (mT, load_w_bf16(P, wo, 8, 1024, "Wo"))
        mlp_body(P, C, hT, oT, gt, W1, W2, TOK, T, fgt, pj)
        P.emit(final_waits=P.out_toks)
    return nc


def mlp_body(P, C, hT, oT, gt, W1, W2, TOK, T, fgt=None, pj=None):
    hv = hT.rearrange("(c p) t -> p c t", p=128)
    ov = oT.rearrange("(c p) t -> p c t", p=128)
    NB = 2
    xs = [P.sb(f"x{i}", [128, 8, T], F32) for i in range(NB)]
    nb1 = 1 if pj is not None else NB
    sqs = [P.sb(f"sq{i}", [128, 8, T], BF16, nsub=8) for i in range(nb1)] * (NB // nb1)
    hns = [P.sb(f"hn{i}", [128, 8, T], BF16, nsub=8) for i in range(nb1)] * (NB // nb1)
    rstds = [P.sb(f"rstd{i}", [128, T], F32) for i in range(NB)]
    aT = P.sb("aT", [128, 32, T], BF16, nsub=32)
    rl = [P.sb(f"rl{i}", [128, T], BF16) for i in range(2)]
    ys = [P.sb(f"y{i}", [128, 8, T], F32, nsub=8) for i in range(1 if pj is not None else NB)] * (2 if pj is not None else 1)
    pss = [P.ps(f"ps{i}", [128, 512], F32) for i in range(8)]
    P.out_toks = []
    pi = 0
    if pj is not None:
        mv = pj[0].rearrange("(c p) t -> p c t", p=128)
        Wo = pj[1]
        mt = P.sb("mt", [128, 8, T], F32)
        mbb = P.sb("mbb", [128, 8, T], BF16, nsub=8)
    for it in range(TOK // T):
        b = it % NB
        x, sq, hn, rstd, y = xs[b], sqs[b], hns[b], rstds[b], ys[b]
        t0 = it * T
        P.dma(lambda e, x=x, t0=t0: e.dma_start(out=x[:, :, :], in_=hv[:, :, t0:t0 + T]), writes=[x.r()])
        if pj is not None:
            P.dma(lambda e, t0=t0: e.dma_start(out=mt[:, :, :], in_=mv[:, :, t0:t0 + T]), writes=[mt.r()], q="act")
            for c in range(8):
                if c % 2 == 0:
                    P.op("act", lambda e, c=c: e.copy(out=mbb[:, c, :], in_=mt[:, c, :]), reads=[mt.r()], writes=[mbb.r(c)])
                else:
                    P.op("pool", lambda e, c=c: e.tensor_copy(out=mbb[:, c, :], in_=mt[:, c, :]), reads=[mt.r()], writes=[mbb.r(c)])
            for o in range(8):
                ps = pss[pi % 8]; pi += 1
                for c in range(8):
                    P.op("pe", lambda e, o=o, c=c, ps=ps: e.matmul(
                        ps[:, :T], lhsT=Wo[:, c, o * 128:(o + 1) * 128], rhs=mbb[:, c, :],
                        start=(c == 0), stop=(c == 7)), reads=[Wo.r(c), mbb.r(c)], writes=[ps.r()])
                P.op("dve", lambda e, o=o, ps=ps, x=x: e.tensor_tensor(
                    out=x[:, o, :], in0=ps[:, :T], in1=x[:, o, :], op=ALU.add), reads=[ps.r(), x.r()], writes=[x.r()])
        ps = pss[pi % 8]; pi += 1
        rms_rstd(P, C, x, 8, T, ps, sq, rstd, 1024.0)
        for c in range(8):
            P.op("dve", lambda e, c=c, x=x, hn=hn, rstd=rstd: e.scalar_tensor_tensor(
                out=hn[:, c, :], in0=x[:, c, :], scalar=gt[:, c:c + 1], in1=rstd[:, :],
                op0=ALU.mult, op1=ALU.mult), reads=[x.r(), rstd.r(), gt.r()], writes=[hn.r(c)])
        for f in range(32):
            ps = pss[pi % 8]; pi += 1
            for c in range(8):
                P.op("pe", lambda e, c=c, f=f, ps=ps, hn=hn: e.matmul(
                    ps[:, :T], lhsT=W1[:, c, f * 128:(f + 1) * 128], rhs=hn[:, c, :],
                    start=(c == 0), stop=(c == 7)), reads=[W1.r(c), hn.r(c)], writes=[ps.r()])
            r = rl[f % 2]
            P.op("act", lambda e, ps=ps, r=r: e.activation(out=r[:, :], in_=ps[:, :T], func=AF.Relu),
                 reads=[ps.r()], writes=[r.r()])
            eng = "pool" if f % 2 == 0 else "dve"
            P.op(eng, lambda e, f=f, r=r: e.tensor_tensor(out=aT[:, f, :], in0=r[:, :], in1=r[:, :], op=ALU.mult),
                 reads=[r.r()], writes=[aT.r(f)])
        for o in range(8):
            ps = pss[pi % 8]; pi += 1
            for f in range(32):
                P.op("pe", lambda e, o=o, f=f, ps=ps: e.matmul(
                    ps[:, :T], lhsT=W2[:, f, o * 128:(o + 1) * 128], rhs=aT[:, f, :],
                    start=(f == 0), stop=(f == 31)), reads=[W2.r(f), aT.r(f)], writes=[ps.r()])
            P.op("dve", lambda e, o=o, ps=ps, x=x, y=y: e.tensor_tensor(
                out=y[:, o, :], in0=ps[:, :T], in1=x[:, o, :], op=ALU.add),
                reads=[ps.r(), x.r()], writes=[y.r(o)])
        if fgt is not None:
            ps = pss[pi % 8]; pi += 1
            for c in range(8):
                P.op("act", lambda e, c=c, y=y, sq=sq: e.activation(out=sq[:, c, :], in_=y[:, c, :], func=AF.Square),
                     reads=[y.r(c)], writes=[sq.r(c)])
            for c in range(8):
                P.op("pe", lambda e, c=c, ps=ps, sq=sq: e.matmul(ps[:, :T], lhsT=C.ones_bf[:, :], rhs=sq[:, c, :],
                                                              start=(c == 0), stop=(c == 7)),
                     reads=[sq.r(c), C.ones_bf.r()], writes=[ps.r()])
            P.op("act", lambda e, ps=ps, rstd=rstd: e.activation(out=rstd[:, :], in_=ps[:, :T], func=AF.Sqrt,
                                                               bias=C.eps[:, :], scale=1.0 / 1024),
                 reads=[ps.r(), C.eps.r()], writes=[rstd.r()])
            P.op("dve", lambda e, rstd=rstd: e.reciprocal(out=rstd[:, :], in_=rstd[:, :]), reads=[rstd.r()], writes=[rstd.r()])
            for c in range(8):
                P.op("dve", lambda e, c=c, y=y, rstd=rstd: e.scalar_tensor_tensor(
                    out=y[:, c, :], in0=y[:, c, :], scalar=fgt[:, c:c + 1], in1=rstd[:, :], op0=ALU.mult, op1=ALU.mult),
                    reads=[y.r(c), rstd.r(), fgt.r()], writes=[y.r(c)])
        tok = P.dma(lambda e, y=y, t0=t0: e.dma_start(out=ov[:, :, t0:t0 + T], in_=y[:, :, :]),
                    reads=y.all())
        P.out_toks.append(tok)


def next_ps(P):
    i = getattr(P, "_psi", 0)
    P._psi = i + 1
    return P.pss[i % len(P.pss)]


def alloc_ps(P, n=8):
    P.pss = [P.ps(f"ps{i}", [128, 512], F32) for i in range(n)]
    P._psi = 0


def load_const(P, ap, shape, name, dtype=F32, q="sp"):
    t = P.sb(name, shape, dtype)
    P.dma(lambda e: e.dma_start(out=t[tuple(slice(None) for _ in shape)], in_=ap), writes=[t.r()], q=q)
    return t


def build_outproj(TOK=2048, T=512):
    nc = bass.Bass("TRN2", target_bir_lowering=False)
    hT = nc.dram_tensor("hT", [1024, TOK], F32, kind="ExternalInput").ap()
    mT = nc.dram_tensor("mT", [1024, TOK], F32, kind="ExternalInput").ap()
    w = nc.dram_tensor("w", [1024, 1024], F32, kind="ExternalInput").ap()
    oT = nc.dram_tensor("oT", [1024, TOK], F32, kind="ExternalOutput").ap()
    hv = hT.rearrange("(c p) t -> p c t", p=128)
    mv = mT.rearrange("(c p) t -> p c t", p=128)
    ov = oT.rearrange("(c p) t -> p c t", p=128)
    with ExitStack() as st:
        st.enter_context(nc.allow_low_precision("bf16 matmul operands, fp32 accumulate"))
        P = Prog(nc, st)
        alloc_ps(P)
        W = load_w_bf16(P, w, 8, 1024, "W")
        NB = 2
        xs = [P.sb(f"x{i}", [128, 8, T], F32) for i in range(NB)]
        ms = [P.sb(f"m{i}", [128, 8, T], F32) for i in range(NB)]
        mb = [P.sb(f"mb{i}", [128, 8, T], BF16, nsub=8) for i in range(NB)]
        ys = [P.sb(f"y{i}", [128, 8, T], F32, nsub=8) for i in range(NB)]
        outs = []
        for it in range(TOK // T):
            b = it % NB
            x, m, mbb, y = xs[b], ms[b], mb[b], ys[b]
            t0 = it * T
            P.dma(lambda e, x=x, t0=t0: e.dma_start(out=x[:, :, :], in_=hv[:, :, t0:t0 + T]), writes=[x.r()])
            P.dma(lambda e, m=m, t0=t0: e.dma_start(out=m[:, :, :], in_=mv[:, :, t0:t0 + T]), writes=[m.r()], q="act")
            for c in range(8):
                eng = "act" if c % 2 == 0 else "pool"
                if eng == "act":
                    P.op("act", lambda e, c=c, m=m, mbb=mbb: e.copy(out=mbb[:, c, :], in_=m[:, c, :]),
                         reads=[m.r()], writes=[mbb.r(c)])
                else:
                    P.op("pool", lambda e, c=c, m=m, mbb=mbb: e.tensor_copy(out=mbb[:, c, :], in_=m[:, c, :]),
                         reads=[m.r()], writes=[mbb.r(c)])
            for o in range(8):
                ps = next_ps(P)
                for c in range(8):
                    P.op("pe", lambda e, o=o, c=c, ps=ps, mbb=mbb: e.matmul(
                        ps[:, :T], lhsT=W[:, c, o * 128:(o + 1) * 128], rhs=mbb[:, c, :],
                        start=(c == 0), stop=(c == 7)), reads=[W.r(c), mbb.r(c)], writes=[ps.r()])
                P.op("dve", lambda e, o=o, ps=ps, x=x, y=y: e.tensor_tensor(
                    out=y[:, o, :], in0=ps[:, :T], in1=x[:, o, :], op=ALU.add),
                    reads=[ps.r(), x.r()], writes=[y.r(o)])
            outs.append(P.dma(lambda e, y=y, t0=t0: e.dma_start(out=ov[:, :, t0:t0 + T], in_=y[:, :, :]),
                              reads=y.all()))
        P.emit(final_waits=outs)
    return nc


def build_conv(TOK=2048, T=256):
    HALO = 30
    NT = TOK + HALO
    nc = bass.Bass("TRN2", target_bir_lowering=False)
    hT = nc.dram_tensor("hT", [1024, NT], F32, kind="ExternalInput").ap()
    g = nc.dram_tensor("g", [1024], F32, kind="ExternalInput").ap()
    w1 = nc.dram_tensor("w1", [1024, 2048], F32, kind="ExternalInput").ap()
    b1 = nc.dram_tensor("b1", [2048], F32, kind="ExternalInput").ap()
    wdT = nc.dram_tensor("wdT", [1024, 31], F32, kind="ExternalInput").ap()
    bd = nc.dram_tensor("bd", [1024], F32, kind="ExternalInput").ap()
    lg = nc.dram_tensor("lg", [1024], F32, kind="ExternalInput").ap()
    lb = nc.dram_tensor("lb", [1024], F32, kind="ExternalInput").ap()
    w2 = nc.dram_tensor("w2", [1024, 1024], F32, kind="ExternalInput").ap()
    b2 = nc.dram_tensor("b2", [1024], F32, kind="ExternalInput").ap()
    hs = nc.dram_tensor("hs", [128, 1], F32, kind="ExternalInput").ap()
    ident = nc.dram_tensor("ident", [128, 128], F32, kind="ExternalInput").ap()
    oT = nc.dram_tensor("oT", [1024, TOK], F32, kind="ExternalOutput").ap()
    hv = hT.rearrange("(c p) t -> p c t", p=128)
    ov = oT.rearrange("(c p) t -> p c t", p=128)
    with ExitStack() as st:
        st.enter_context(nc.allow_low_precision("bf16 matmul operands, fp32 accumulate"))
        P = Prog(nc, st)
        alloc_ps(P)
        C = make_consts(P)
        ones_f = P.sb("ones_f", [128, 128], F32)
        P.op("dve", lambda e: e.memset(ones_f[:, :], 1.0), writes=[ones_f.r()])
        gt = load_vec_fm(P, g, 8, "g")
        b1t = load_vec_fm(P, b1, 16, "b1")
        bdt = load_vec_fm(P, bd, 8, "bd")
        lgt = load_vec_fm(P, lg, 8, "lg")
        lbt = load_vec_fm(P, lb, 8, "lb")
        b2t = load_vec_fm(P, b2, 8, "b2")
        hst = load_const(P, hs, [128, 1], "hs")
        idf = load_const(P, ident, [128, 128], "idf")
        wd = P.sb("wd", [128, 8, 31], F32)
        P.dma(lambda e: e.dma_start(out=wd[:, :, :], in_=wdT.rearrange("(c p) j -> p c j", p=128)), writes=[wd.r()])
        W1 = load_w_bf16(P, w1, 8, 2048, "W1")
        W2 = load_w_bf16(P, w2, 8, 1024, "W2")
        diag = P.sb("diag", [128, 8 * 31, 128], BF16, nsub=8)
        for c in range(8):
            for j in range(31):
                eng = "pool" if (j % 2 == 0) else "dve"
                P.op(eng, lambda e, c=c, j=j: e.tensor_scalar(
                    out=diag[:, c * 31 + j, :], in0=idf[:, :], scalar1=wd[:, c, j:j + 1], scalar2=None,
                    op0=ALU.mult), reads=[idf.r(), wd.r()], writes=[diag.r(c)])
        uT = P.sb("uT", [128, 8, NT], BF16, nsub=8)
        NB = 2
        xs = [P.sb(f"x{i}", [128, 8, T], F32) for i in range(NB)]
        sqs = [P.sb(f"sq{i}", [128, 8, T], BF16, nsub=8) for i in range(1)] * 2
        hns = [P.sb(f"hn{i}", [128, 8, T], BF16, nsub=8) for i in range(1)] * 2
        rstds = [P.sb(f"rstd{i}", [128, T], F32) for i in range(1)] * 2
        sig = [P.sb(f"sig{i}", [128, T], F32) for i in range(2)]
        segs = [(0, HALO)] + [(HALO + k * T, T) for k in range(TOK // T)]
        for it, (s0, L) in enumerate(segs):
            b = it % NB
            x, sq, hn, rstd = xs[b], sqs[b], hns[b], rstds[b]
            P.dma(lambda e, x=x, s0=s0, L=L: e.dma_start(out=x[:, :, :L], in_=hv[:, :, s0:s0 + L]), writes=[x.r()])
            ps = next_ps(P)
            for c in range(8):
                P.op("act", lambda e, c=c, x=x, sq=sq, L=L: e.activation(out=sq[:, c, :L], in_=x[:, c, :L], func=AF.Square),
                     reads=[x.r()], writes=[sq.r(c)])
            for c in range(8):
                P.op("pe", lambda e, c=c, ps=ps, sq=sq, L=L: e.matmul(ps[:, :L], lhsT=C.ones_bf[:, :], rhs=sq[:, c, :L],
                                                                  start=(c == 0), stop=(c == 7)),
                     reads=[sq.r(c), C.ones_bf.r()], writes=[ps.r()])
            P.op("act", lambda e, ps=ps, rstd=rstd, L=L: e.activation(out=rstd[:, :L], in_=ps[:, :L], func=AF.Sqrt,
                                                               bias=C.eps[:, :], scale=1.0 / 1024),
                 reads=[ps.r(), C.eps.r()], writes=[rstd.r()])
            P.op("dve", lambda e, rstd=rstd, L=L: e.reciprocal(out=rstd[:, :L], in_=rstd[:, :L]),
                 reads=[rstd.r()], writes=[rstd.r()])
            for c in range(8):
                P.op("dve", lambda e, c=c, x=x, hn=hn, rstd=rstd, L=L: e.scalar_tensor_tensor(
                    out=hn[:, c, :L], in0=x[:, c, :L], scalar=gt[:, c:c + 1], in1=rstd[:, :L],
                    op0=ALU.mult, op1=ALU.mult), reads=[x.r(), rstd.r(), gt.r()], writes=[hn.r(c)])
            for j in range(8):
                psa = next_ps(P)
                psg = next_ps(P)
                for c in range(8):
                    P.op("pe", lambda e, c=c, j=j, psa=psa, hn=hn, L=L: e.matmul(
                        psa[:, :L], lhsT=W1[:, c, j * 128:(j + 1) * 128], rhs=hn[:, c, :L],
                        start=(c == 0), stop=(c == 7)), reads=[W1.r(c), hn.r(c)], writes=[psa.r()])
                for c in range(8):
                    P.op("pe", lambda e, c=c, j=j, psg=psg, hn=hn, L=L: e.matmul(
                        psg[:, :L], lhsT=W1[:, c, 1024 + j * 128:1024 + (j + 1) * 128], rhs=hn[:, c, :L],
                        start=(c == 0), stop=(c == 7)), reads=[W1.r(c), hn.r(c)], writes=[psg.r()])
                sg = sig[j % 2]
                P.op("act", lambda e, j=j, psg=psg, sg=sg, L=L: e.activation(
                    out=sg[:, :L], in_=psg[:, :L], func=AF.Sigmoid, bias=b1t[:, 8 + j:9 + j], scale=1.0),
                    reads=[psg.r(), b1t.r()], writes=[sg.r()])
                P.op("dve", lambda e, j=j, psa=psa, sg=sg, s0=s0, L=L: e.scalar_tensor_tensor(
                    out=uT[:, j, s0:s0 + L], in0=psa[:, :L], scalar=b1t[:, j:j + 1], in1=sg[:, :L],
                    op0=ALU.add, op1=ALU.mult), reads=[psa.r(), sg.r(), b1t.r()], writes=[uT.r(j)])
            if it == 0:
                for j in range(8):
                    P.op("dve", lambda e, j=j: e.tensor_scalar(
                        out=uT[:, j, 0:HALO], in0=uT[:, j, 0:HALO], scalar1=hst[:, 0:1], scalar2=None, op0=ALU.mult),
                        reads=[uT.r(j), hst.r()], writes=[uT.r(j)])
        vs = [P.sb(f"v{i}", [128, 8, T], F32, nsub=8) for i in range(1)] * 2
        zs = [P.sb(f"z{i}", [128, 8, T], BF16, nsub=8) for i in range(1)] * 2
        ys = [P.sb(f"y{i}", [128, 8, T], F32, nsub=8) for i in range(1)] * 2
        v2 = P.sb("v2", [128, 8, T], F32, nsub=8)
        mean = P.sb("mean", [128, T], F32)
        msq = P.sb("msq", [128, T], F32)
        lrstd = P.sb("lrstd", [128, T], F32)
        dd = [P.sb(f"dd{i}", [128, T], F32) for i in range(2)]
        outs = []
        for it in range(TOK // T):
            b = it % NB
            x, v, z, y = xs[b], vs[b], zs[b], ys[b]
            tl = it * T
            P.dma(lambda e, x=x, tl=tl: e.dma_start(out=x[:, :, :], in_=hv[:, :, HALO + tl:HALO + tl + T]), writes=[x.r()])
            for c in range(8):
                ps = next_ps(P)
                for j in range(31):
                    P.op("pe", lambda e, c=c, j=j, ps=ps, tl=tl: e.matmul(
                        ps[:, :T], lhsT=diag[:, c * 31 + j, :], rhs=uT[:, c, tl + j:tl + j + T],
                        start=(j == 0), stop=(j == 30)), reads=[diag.r(c), uT.r(c)], writes=[ps.r()])
                P.op("act", lambda e, c=c, ps=ps, v=v: e.activation(
                    out=v[:, c, :], in_=ps[:, :T], func=AF.Identity, bias=bdt[:, c:c + 1], scale=1.0),
                    reads=[ps.r(), bdt.r()], writes=[v.r(c)])
                P.op("pool", lambda e, c=c, v=v: e.tensor_tensor(out=v2[:, c, :], in0=v[:, c, :], in1=v[:, c, :], op=ALU.mult),
                     reads=[v.r(c)], writes=[v2.r(c)])
            ps1 = next_ps(P)
            ps2 = next_ps(P)
            for c in range(8):
                P.op("pe", lambda e, c=c, ps1=ps1, v=v: e.matmul(ps1[:, :T], lhsT=ones_f[:, :], rhs=v[:, c, :],
                                                              start=(c == 0), stop=(c == 7)),
                     reads=[ones_f.r(), v.r(c)], writes=[ps1.r()])
            for c in range(8):
                P.op("pe", lambda e, c=c, ps2=ps2: e.matmul(ps2[:, :T], lhsT=ones_f[:, :], rhs=v2[:, c, :],
                                                         start=(c == 0), stop=(c == 7)),
                     reads=[ones_f.r(), v2.r(c)], writes=[ps2.r()])
            P.op("act", lambda e, ps1=ps1: e.activation(out=mean[:, :], in_=ps1[:, :T], func=AF.Copy, scale=1.0 / 1024),
                 reads=[ps1.r()], writes=[mean.r()])
            P.op("dve", lambda e: e.tensor_tensor(out=msq[:, :], in0=mean[:, :], in1=mean[:, :], op=ALU.mult),
                 reads=[mean.r()], writes=[msq.r()])
            P.op("dve", lambda e, ps2=ps2: e.scalar_tensor_tensor(
                out=lrstd[:, :], in0=ps2[:, :T], scalar=1.0 / 1024, in1=msq[:, :], op0=ALU.mult, op1=ALU.subtract),
                reads=[ps2.r(), msq.r()], writes=[lrstd.r()])
            P.op("act", lambda e: e.activation(out=lrstd[:, :], in_=lrstd[:, :], func=AF.Sqrt, bias=C.eps[:, :], scale=1.0),
                 reads=[lrstd.r(), C.eps.r()], writes=[lrstd.r()])
            P.op("dve", lambda e: e.reciprocal(out=lrstd[:, :], in_=lrstd[:, :]), reads=[lrstd.r()], writes=[lrstd.r()])
            for c in range(8):
                d = dd[c % 2]
                P.op("pool", lambda e, c=c, d=d, v=v: e.tensor_tensor(out=d[:, :], in0=v[:, c, :], in1=mean[:, :], op=ALU.subtract),
                     reads=[v.r(c), mean.r()], writes=[d.r()])
                P.op("dve", lambda e, c=c, d=d: e.scalar_tensor_tensor(
                    out=d[:, :], in0=d[:, :], scalar=lgt[:, c:c + 1], in1=lrstd[:, :], op0=ALU.mult, op1=ALU.mult),
                    reads=[d.r(), lrstd.r(), lgt.r()], writes=[d.r()])
                P.op("act", lambda e, c=c, d=d, z=z: e.activation(
                    out=z[:, c, :], in_=d[:, :], func=AF.Silu, bias=lbt[:, c:c + 1], scale=1.0),
                    reads=[d.r(), lbt.r()], writes=[z.r(c)])
            for o in range(8):
                ps = next_ps(P)
                for c in range(8):
                    P.op("pe", lambda e, o=o, c=c, ps=ps, z=z: e.matmul(
                        ps[:, :T], lhsT=W2[:, c, o * 128:(o + 1) * 128], rhs=z[:, c, :],
                        start=(c == 0), stop=(c == 7)), reads=[W2.r(c), z.r(c)], writes=[ps.r()])
                P.op("dve", lambda e, o=o, ps=ps, x=x, y=y: e.scalar_tensor_tensor(
                    out=y[:, o, :], in0=ps[:, :T], scalar=b2t[:, o:o + 1], in1=x[:, o, :], op0=ALU.add, op1=ALU.add),
                    reads=[ps.r(), x.r(), b2t.r()], writes=[y.r(o)])
            outs.append(P.dma(lambda e, y=y, tl=tl: e.dma_start(out=ov[:, :, tl:tl + T], in_=y[:, :, :]),
                              reads=y.all()))
        P.emit(final_waits=outs)
    return nc


def norm_tile(P, C, x, gt, sq, hn, rstd, L, dim=1024.0, KC=8):
    ps = next_ps(P)
    for c in range(KC):
        P.op("act", lambda e, c=c: e.activation(out=sq[:, c, :L], in_=x[:, c, :L], func=AF.Square),
             reads=[x.r()], writes=[sq.r(c)])
    for c in range(KC):
        P.op("pe", lambda e, c=c: e.matmul(ps[:, :L], lhsT=C.ones_bf[:, :], rhs=sq[:, c, :L],
                                          start=(c == 0), stop=(c == KC - 1)),
             reads=[sq.r(c), C.ones_bf.r()], writes=[ps.r()])
    P.op("act", lambda e: e.activation(out=rstd[:, :L], in_=ps[:, :L], func=AF.Sqrt,
                                       bias=C.eps[:, :], scale=1.0 / dim),
         reads=[ps.r(), C.eps.r()], writes=[rstd.r()])
    P.op("dve", lambda e: e.reciprocal(out=rstd[:, :L], in_=rstd[:, :L]), reads=[rstd.r()], writes=[rstd.r()])
    for c in range(KC):
        P.op("dve", lambda e, c=c: e.scalar_tensor_tensor(
            out=hn[:, c, :L], in0=x[:, c, :L], scalar=gt[:, c:c + 1], in1=rstd[:, :L],
            op0=ALU.mult, op1=ALU.mult), reads=[x.r(), rstd.r(), gt.r()], writes=[hn.r(c)])


def build_mla(L=16384):
    T = 512
    NTI = L // T
    NKB = L // 128
    SCALE = 96.0 ** -0.5
    nc = bass.Bass("TRN2", target_bir_lowering=False)
    hT = nc.dram_tensor("hT", [1024, L], F32, kind="ExternalInput").ap()
    g = nc.dram_tensor("g", [1024], F32, kind="ExternalInput").ap()
    wall = nc.dram_tensor("wall", [1024, 832], F32, kind="ExternalInput").ap()
    gq = nc.dram_tensor("gq", [384], F32, kind="ExternalInput").ap()
    gkv = nc.dram_tensor("gkv", [256], F32, kind="ExternalInput").ap()
    wuq = nc.dram_tensor("wuq", [384, 384], F32, kind="ExternalInput").ap()
    wukv = nc.dram_tensor("wukv", [256, 256], F32, kind="ExternalInput").ap()
    cos2 = nc.dram_tensor("cos2", [96, L], F32, kind="ExternalInput").ap()
    sin2 = nc.dram_tensor("sin2", [96, L], F32, kind="ExternalInput").ap()
    cmask = nc.dram_tensor("cmask", [128, 4, 512], F32, kind="ExternalInput").ap()
    esel = nc.dram_tensor("esel", [65, 64], F32, kind="ExternalInput").ap()
    oT = nc.dram_tensor("oT", [128, L], F32, kind="ExternalOutput").ap()
    qTd = nc.dram_tensor("qTd", [2, 96, L], BF16, kind="Internal").ap()
    hv = hT.rearrange("(c p) t -> p c t", p=128)
    with ExitStack() as st:
        st.enter_context(nc.allow_low_precision("bf16 matmul operands, fp32 accumulate"))
        P = Prog(nc, st)
        alloc_ps(P, 6)
        C = make_consts(P)
        gt = load_vec_fm(P, g, 8, "g")
        gqt = load_vec_fm(P, gq, 3, "gq")
        gkvt = load_vec_fm(P, gkv, 2, "gkv")
        Wall = load_w_bf16(P, wall, 8, 832, "Wall")
        Wuq = load_w_bf16(P, wuq, 3, 384, "Wuq")
        Wukv = load_w_bf16(P, wukv, 2, 256, "Wukv")
        cm_f = load_const(P, cmask, [128, 4, 512], "cm_f")
        cm = P.sb("cm", [128, 4, 512], BF16)
        P.op("dve", lambda e: e.tensor_copy(out=cm[:, :, :], in_=cm_f[:, :, :]), reads=[cm_f.r()], writes=[cm.r()])
        es = P.sb("es", [128, 64], F32)
        P.op("dve", lambda e: e.memset(es[:, :], 0.0), writes=[es.r()])
        P.dma(lambda e: e.dma_start(out=es[0:65, :], in_=esel), reads=[es.r()], writes=[es.r()])
        kT = [P.sb(f"kT{h}", [96, L], BF16, nsub=NTI) for h in range(2)]
        qres = [[Res(f"qTd{h}_{i}") for i in range(NTI)] for h in range(2)]
        Va = P.sb("Va", [128, NKB, 2, 65], BF16, nsub=NTI)
        P.op("pool", lambda e: e.memset(Va[:, :, :, :], 1.0), writes=Va.all())
        x = P.sb("x", [128, 8, T], F32)
        sq = P.sb("sq", [128, 8, T], BF16, nsub=8)
        hn = P.sb("hn", [128, 8, T], BF16, nsub=8)
        rstd = P.sb("rstd", [128, T], F32)
        cq = P.sb("cq", [128, 5, T], F32, nsub=5)
        cqs = P.sb("cqs", [128, 5, T], BF16, nsub=5)
        cqn = P.sb("cqn", [128, 5, T], BF16, nsub=5)
        rq = P.sb("rq", [128, T], F32)
        rkv = P.sb("rkv", [128, T], F32)
        cs = P.sb("cs", [96, 2, T], F32)
        t1 = P.sb("t1", [96, T], F32)
        t2 = P.sb("t2", [96, T], F32)
        qt = [P.sb(f"qt{h}", [96, T], BF16) for h in range(2)]
        for it in range(NTI):
            t0 = it * T
            P.dma(lambda e, t0=t0: e.dma_start(out=x[:, :, :], in_=hv[:, :, t0:t0 + T]), writes=[x.r()])
            P.dma(lambda e, t0=t0: e.dma_start(out=cs[:, 0, :], in_=cos2[:, t0:t0 + T]), writes=[cs.r()], q="act")
            P.dma(lambda e, t0=t0: e.dma_start(out=cs[:, 1, :], in_=sin2[:, t0:t0 + T]), writes=[cs.r()], q="act")
            norm_tile(P, C, x, gt, sq, hn, rstd, T)
            for j in range(5):
                ps = next_ps(P)
                for c in range(8):
                    P.op("pe", lambda e, c=c, j=j, ps=ps: e.matmul(
                        ps[:, :T], lhsT=Wall[:, c, j * 128:(j + 1) * 128], rhs=hn[:, c, :],
                        start=(c == 0), stop=(c == 7)), reads=[Wall.r(c), hn.r(c)], writes=[ps.r()])
                P.op("act", lambda e, j=j, ps=ps: e.copy(out=cq[:, j, :], in_=ps[:, :T]), reads=[ps.r()], writes=[cq.r(j)])
                P.op("pool", lambda e, j=j: e.tensor_tensor(out=cqs[:, j, :], in0=cq[:, j, :], in1=cq[:, j, :], op=ALU.mult),
                     reads=[cq.r(j)], writes=[cqs.r(j)])
            for (lo, hi, rr, dim, gg) in ((0, 3, rq, 384.0, gqt), (3, 5, rkv, 256.0, gkvt)):
                ps = next_ps(P)
                for j in range(lo, hi):
                    P.op("pe", lambda e, j=j, ps=ps, lo=lo, hi=hi: e.matmul(
                        ps[:, :T], lhsT=C.ones_bf[:, :], rhs=cqs[:, j, :], start=(j == lo), stop=(j == hi - 1)),
                        reads=[cqs.r(j), C.ones_bf.r()], writes=[ps.r()])
                P.op("act", lambda e, ps=ps, rr=rr, dim=dim: e.activation(
                    out=rr[:, :], in_=ps[:, :T], func=AF.Sqrt, bias=C.eps[:, :], scale=1.0 / dim),
                    reads=[ps.r(), C.eps.r()], writes=[rr.r()])
                P.op("dve", lambda e, rr=rr: e.reciprocal(out=rr[:, :], in_=rr[:, :]), reads=[rr.r()], writes=[rr.r()])
                for j in range(lo, hi):
                    P.op("dve", lambda e, j=j, rr=rr, gg=gg, lo=lo: e.scalar_tensor_tensor(
                        out=cqn[:, j, :], in0=cq[:, j, :], scalar=gg[:, j - lo:j - lo + 1], in1=rr[:, :],
                        op0=ALU.mult, op1=ALU.mult), reads=[cq.r(j), rr.r(), gg.r()], writes=[cqn.r(j)])
            pk = next_ps(P)
            pks = next_ps(P)
            for c in range(8):
                P.op("pe", lambda e, c=c, pk=pk: e.matmul(pk[:96, :T], lhsT=Wall[:, c, 640:736], rhs=hn[:, c, :],
                                                       start=(c == 0), stop=(c == 7)),
                     reads=[Wall.r(c), hn.r(c)], writes=[pk.r()])
            for c in range(8):
                P.op("pe", lambda e, c=c, pks=pks: e.matmul(pks[:96, :T], lhsT=Wall[:, c, 736:832], rhs=hn[:, c, :],
                                                         start=(c == 0), stop=(c == 7)),
                     reads=[Wall.r(c), hn.r(c)], writes=[pks.r()])
            P.op("dve", lambda e, pk=pk: e.tensor_tensor(out=t1[64:96, :], in0=pk[64:96, :T], in1=cs[64:96, 0, :], op=ALU.mult),
                 reads=[pk.r(), cs.r()], writes=[t1.r()])
            P.op("dve", lambda e, pks=pks: e.tensor_tensor(out=t2[64:96, :], in0=pks[64:96, :T], in1=cs[64:96, 1, :], op=ALU.mult),
                 reads=[pks.r(), cs.r()], writes=[t2.r()])
            for h in range(2):
                P.op("pool", lambda e, h=h, t0=t0: e.tensor_tensor(out=kT[h][64:96, t0:t0 + T], in0=t1[64:96, :], in1=t2[64:96, :], op=ALU.add),
                     reads=[t1.r(), t2.r()], writes=[kT[h].r(it)])
            for h in range(2):
                pq = next_ps(P)
                pqs = next_ps(P)
                for c in range(3):
                    P.op("pe", lambda e, c=c, h=h, pq=pq: e.matmul(
                        pq[:96, :T], lhsT=Wuq[:, c, h * 192:h * 192 + 96], rhs=cqn[:, c, :],
                        start=(c == 0), stop=(c == 2)), reads=[Wuq.r(c), cqn.r(c)], writes=[pq.r()])
                for c in range(3):
                    P.op("pe", lambda e, c=c, h=h, pqs=pqs: e.matmul(
                        pqs[:96, :T], lhsT=Wuq[:, c, h * 192 + 96:h * 192 + 192], rhs=cqn[:, c, :],
                        start=(c == 0), stop=(c == 2)), reads=[Wuq.r(c), cqn.r(c)], writes=[pqs.r()])
                q = qt[h]
                P.op("act", lambda e, pq=pq, q=q: e.copy(out=q[0:64, :], in_=pq[0:64, :T]), reads=[pq.r()], writes=[q.r()])
                P.op("dve", lambda e, pq=pq: e.tensor_tensor(out=t1[64:96, :], in0=pq[64:96, :T], in1=cs[64:96, 0, :], op=ALU.mult),
                     reads=[pq.r(), cs.r()], writes=[t1.r()])
                P.op("dve", lambda e, pqs=pqs: e.tensor_tensor(out=t2[64:96, :], in0=pqs[64:96, :T], in1=cs[64:96, 1, :], op=ALU.mult),
                     reads=[pqs.r(), cs.r()], writes=[t2.r()])
                P.op("pool", lambda e, q=q: e.tensor_tensor(out=q[64:96, :], in0=t1[64:96, :], in1=t2[64:96, :], op=ALU.add),
                     reads=[t1.r(), t2.r()], writes=[q.r()])
                P.dma(lambda e, h=h, q=q, t0=t0: e.dma_start(out=qTd[h, :, t0:t0 + T], in_=q[:, :]), reads=[q.r()], writes=[qres[h][it]])
                pkn = next_ps(P)
                for c in range(2):
                    P.op("pe", lambda e, c=c, h=h, pkn=pkn: e.matmul(
                        pkn[:64, :T], lhsT=Wukv[:, c, h * 64:(h + 1) * 64], rhs=cqn[:, 3 + c, :],
                        start=(c == 0), stop=(c == 1)), reads=[Wukv.r(c), cqn.r(3 + c)], writes=[pkn.r()])
                P.op("act", lambda e, h=h, pkn=pkn, t0=t0: e.copy(out=kT[h][0:64, t0:t0 + T], in_=pkn[0:64, :T]),
                     reads=[pkn.r()], writes=[kT[h].r(it)])
            for b4 in range(4):
                pv = next_ps(P)
                for c in range(2):
                    P.op("pe", lambda e, c=c, b4=b4, pv=pv: e.matmul(
                        pv[:, :128], lhsT=cqn[:, 3 + c, b4 * 128:(b4 + 1) * 128], rhs=Wukv[:, c, 128:256],
                        start=(c == 0), stop=(c == 1)), reads=[Wukv.r(c), cqn.r(3 + c)], writes=[pv.r()])
                kb = it * 4 + b4
                P.op("act", lambda e, pv=pv, kb=kb: e.copy(
                    out=Va[:, kb, :, 0:64], in_=pv[:, :128].rearrange("p (h d) -> p h d", h=2)),
                    reads=[pv.r()], writes=[Va.r(it)])
        pts = [P.sb(f"pt{i}", [128, T], BF16) for i in range(4)]
        qs = [P.sb(f"qs{i}", [96, T], BF16) for i in range(2)]
        osb = P.sb("osb", [128, T], F32)
        P.op("dve", lambda e: e.memset(osb[:, :], 0.0), writes=[osb.r()])
        rden = P.sb("rden", [64, T], F32)
        on = [P.sb(f"on{i}", [64, T], F32) for i in range(2)]
        po = [P.ps(f"po{i}", [128, 512], F32) for i in range(2)]
        outs = []
        n = 0
        for h in range(2):
            for j in range(NTI):
                q = qs[n % 2]
                pO = po[n % 2]
                o_n = on[n % 2]
                n += 1
                P.dma(lambda e, h=h, q=q, j=j: e.dma_start(out=q[:, :], in_=qTd[h, :, j * T:(j + 1) * T]),
                      reads=[qres[h][j]], writes=[q.r()], q="act")
                nkb = 4 * j + 4
                LA = 2
                pend = []

                def pv(kb, pt, h=h, pO=pO, nkb=nkb):
                    P.op("pe", lambda e, h=h, kb=kb, pt=pt, pO=pO, nkb=nkb: e.matmul(
                        pO[:65, :T], lhsT=Va[:, kb, h, :], rhs=pt[:, :], start=(kb == 0), stop=(kb == nkb - 1)),
                        reads=[Va.r(kb // 4), pt.r()], writes=[pO.r()])

                for kb in range(nkb):
                    ps = next_ps(P)
                    pt = pts[kb % len(pts)]
                    P.op("pe", lambda e, h=h, kb=kb, ps=ps, q=q: e.matmul(
                        ps[:, :T], lhsT=kT[h][:, kb * 128:(kb + 1) * 128], rhs=q[:, :], start=True, stop=True),
                        reads=[kT[h].r(kb // 4), q.r()], writes=[ps.r()])
                    P.op("act", lambda e, ps=ps, pt=pt: e.activation(out=pt[:, :], in_=ps[:, :T], func=AF.Exp, scale=SCALE),
                         reads=[ps.r()], writes=[pt.r()])
                    if kb >= 4 * j:
                        d = kb - 4 * j
                        eng = "dve" if d % 2 == 0 else "pool"
                        P.op(eng, lambda e, pt=pt, d=d: e.tensor_tensor(out=pt[:, :], in0=pt[:, :], in1=cm[:, d, :], op=ALU.mult),
                             reads=[pt.r(), cm.r()], writes=[pt.r()])
                    pend.append((kb, pt))
                    if len(pend) > LA:
                        pv(*pend.pop(0))
                while pend:
                    pv(*pend.pop(0))
                P.op("act", lambda e, pO=pO: e.copy(out=osb[0:65, :], in_=pO[:65, :T]), reads=[pO.r()], writes=[osb.r()])
                pd = next_ps(P)
                P.op("pe", lambda e, pd=pd: e.matmul(pd[:64, :T], lhsT=es[:, :], rhs=osb[:, :], start=True, stop=True),
                     reads=[es.r(), osb.r()], writes=[pd.r()])
                P.op("dve", lambda e, pd=pd: e.reciprocal(out=rden[:, :], in_=pd[:64, :T]), reads=[pd.r()], writes=[rden.r()])
                P.op("dve", lambda e, o_n=o_n: e.tensor_tensor(out=o_n[:, :], in0=osb[0:64, :], in1=rden[:, :], op=ALU.mult),
                     reads=[osb.r(), rden.r()], writes=[o_n.r()])
                outs.append(P.dma(lambda e, h=h, j=j, o_n=o_n: e.dma_start(
                    out=oT[h * 64:(h + 1) * 64, j * T:(j + 1) * T], in_=o_n[:, :]), reads=[o_n.r()]))
        P.emit(final_waits=outs)
    return nc


def build_gdn(L=16384):
    T = 512
    NTI = L // T
    nc = bass.Bass("TRN2", target_bir_lowering=False)
    hT = nc.dram_tensor("hT", [1024, L], F32, kind="ExternalInput").ap()
    g = nc.dram_tensor("g", [1024], F32, kind="ExternalInput").ap()
    wh = nc.dram_tensor("wh", [1024, 640], F32, kind="ExternalInput").ap()
    cw = nc.dram_tensor("cw", [384, 4], F32, kind="ExternalInput").ap()
    sc = nc.dram_tensor("sc", [128, 2], F32, kind="ExternalInput").ap()
    og = nc.dram_tensor("og", [128], F32, kind="ExternalInput").ap()
    cst = nc.dram_tensor("cst", [128, 5, 128], F32, kind="ExternalInput").ap()
    oT = nc.dram_tensor("oT", [128, L], F32, kind="ExternalOutput").ap()
    s_in = nc.dram_tensor("s_in", [128, 128], F32, kind="ExternalInput").ap()
    h_in = nc.dram_tensor("h_in", [128, 3, 3], F32, kind="ExternalInput").ap()
    s_out = nc.dram_tensor("s_out", [128, 128], F32, kind="ExternalOutput").ap()
    h_out = nc.dram_tensor("h_out", [128, 3, 3], F32, kind="ExternalOutput").ap()
    hv = hT.rearrange("(c p) t -> p c t", p=128)
    with ExitStack() as st:
        st.enter_context(nc.allow_low_precision("bf16 matmul operands for the input projection only"))
        P = Prog(nc, st)
        qbanks = [P.ps(f"qb{i}", [128, 512], F32) for i in range(5)]
        qp = [View(qbanks[i], 0, 128, f"qp{i}") for i in range(5)]
        hp = [View(qbanks[i], 0, 256, f"hp{i}") for i in range(5)]
        P.pss = [P.ps(f"fb{i}", [128, 512], F32) for i in range(3)]
        P._psi = 0
        cnt = {"q": 0, "h": 0}

        def nq():
            cnt["q"] += 1
            return qp[cnt["q"] % 5]

        def nh():
            cnt["q"] += 1
            return hp[cnt["q"] % 5]

        C = make_consts(P)
        ones_f = P.sb("ones_f", [128, 128], F32)
        P.op("dve", lambda e: e.memset(ones_f[:, :], 1.0), writes=[ones_f.r()])
        gt = load_vec_fm(P, g, 8, "g")
        ogt = load_vec_fm(P, og, 1, "og")
        cwt = load_vec_fm_2d = P.sb("cwt", [128, 3, 4], F32)
        P.dma(lambda e: e.dma_start(out=cwt[:, :, :], in_=cw.rearrange("(c p) j -> p c j", p=128)), writes=[cwt.r()])
        sct = load_const(P, sc, [128, 2], "sct")
        K = load_const(P, cst, [128, 5, 128], "K")
        ident = K[:, 0, :]
        maskS = K[:, 1, :]
        maskI = K[:, 2, :]
        blk1 = K[:, 3, :]
        cind = K[:, 4, 0:2]
        Wh = load_w_bf16(P, wh, 8, 640, "Wh")
        Whf = P.sb("Whf", [128, 8, 2], F32)
        P.dma(lambda e: e.dma_start(out=Whf[:, :, :], in_=wh.rearrange("(c p) f -> p c f", p=128)[:, :, 512:514]),
              writes=[Whf.r()])
        for c in range(8):
            P.op("dve", lambda e, c=c: e.tensor_scalar(out=Whf[:, c, :], in0=Whf[:, c, :], scalar1=gt[:, c:c + 1],
                                                       scalar2=None, op0=ALU.mult),
                 reads=[Whf.r(), gt.r()], writes=[Whf.r()])
        nA = P.sb("nA", [128, 1], F32)
        P.op("act", lambda e: e.activation(out=nA[:, :], in_=sct[:, 0:1], func=AF.Exp), reads=[sct.r()], writes=[nA.r()])
        P.op("dve", lambda e: e.tensor_scalar(out=nA[:, :], in0=nA[:, :], scalar1=-1.0, scalar2=None, op0=ALU.mult),
             reads=[nA.r()], writes=[nA.r()])
        onec = P.sb("onec", [128, 1], F32)
        P.op("dve", lambda e: e.memset(onec[:, :], 1.0), writes=[onec.r()])
        epsl2 = C.eps
        S = [P.sb(f"S{i}", [128, 128], F32) for i in range(2)]
        P.dma(lambda e: e.dma_start(out=S[0][:, :], in_=s_in), writes=[S[0].r()])
        sidx = [0]
        x = P.sb("x", [128, 8, T], F32)
        sq = P.sb("sq", [128, 8, T], BF16, nsub=8)
        hn = P.sb("hn", [128, 8, T], BF16, nsub=8)
        rstd = P.sb("rstd", [128, T], F32)
        raw = P.sb("raw", [128, 3, T + 3], F32, nsub=3)
        P.dma(lambda e: e.dma_start(out=raw[:, :, 0:3], in_=h_in), writes=raw.all())
        acc = P.sb("acc", [128, 3, T], F32, nsub=3)
        sil = P.sb("sil", [128, 3, T], F32, nsub=3)
        sq2 = P.sb("sq2", [128, 2, T], F32, nsub=2)
        rn = P.sb("rn", [128, 2, T], F32, nsub=2)
        tb = {}

        PERSIST = ("QpT", "O0T", "MT0", "MT1", "B0", "B1", "gateT", "oTt", "osq", "orr")

        def tbuf(par, name, shape=(128, 128)):
            if not name.rstrip("0123456789").endswith(PERSIST) and not any(name.startswith(p) for p in PERSIST):
                par = 0
            key = (par, name)
            if key not in tb:
                tb[key] = P.sb(f"t{par}_{name}", list(shape), F32)
            return tb[key]

        outs = []

        def gen_local(t):
            par = t % 2
            t0 = t * T
            qnT = tbuf(par, "qnT", (128, T))
            knT = tbuf(par, "knT", (128, T))
            vT = tbuf(par, "vT", (128, T))
            gateT = tbuf(par, "gateT", (128, T))
            P.dma(lambda e: e.dma_start(out=x[:, :, :], in_=hv[:, :, t0:t0 + T]), writes=[x.r()])
            norm_tile(P, C, x, gt, sq, hn, rstd, T)
            yield
            for s3 in range(3):
                ps = next_ps(P)
                for c in range(8):
                    P.op("pe", lambda e, c=c, s3=s3, ps=ps: e.matmul(
                        ps[:, :T], lhsT=Wh[:, c, s3 * 128:(s3 + 1) * 128], rhs=hn[:, c, :],
                        start=(c == 0), stop=(c == 7)), reads=[Wh.r(c), hn.r(c)], writes=[ps.r()])
                P.op("act", lambda e, s3=s3, ps=ps: e.copy(out=raw[:, s3, 3:T + 3], in_=ps[:, :T]),
                     reads=[ps.r()], writes=[raw.r(s3)])
            ps = next_ps(P)
            for c in range(8):
                P.op("pe", lambda e, c=c, ps=ps: e.matmul(
                    ps[:, :T], lhsT=Wh[:, c, 384:512], rhs=hn[:, c, :], start=(c == 0), stop=(c == 7)),
                    reads=[Wh.r(c), hn.r(c)], writes=[ps.r()])
            P.op("act", lambda e, ps=ps: e.activation(out=gateT[:, :], in_=ps[:, :T], func=AF.Silu),
                 reads=[ps.r()], writes=[gateT.r()])
            yield
            for s3 in range(3):
                eng = "dve" if s3 != 1 else "pool"
                P.op("dve", lambda e, s3=s3: e.tensor_scalar(
                    out=acc[:, s3, :], in0=raw[:, s3, 3:T + 3], scalar1=cwt[:, s3, 3:4], scalar2=None, op0=ALU.mult),
                    reads=[raw.r(s3), cwt.r()], writes=[acc.r(s3)])
                for j in range(3):
                    P.op("dve", lambda e, s3=s3, j=j: e.scalar_tensor_tensor(
                        out=acc[:, s3, :], in0=raw[:, s3, j:j + T], scalar=cwt[:, s3, j:j + 1], in1=acc[:, s3, :],
                        op0=ALU.mult, op1=ALU.add), reads=[raw.r(s3), cwt.r(), acc.r(s3)], writes=[acc.r(s3)])
                P.op("pool", lambda e, s3=s3: e.tensor_copy(out=raw[:, s3, 0:3], in_=raw[:, s3, T:T + 3]),
                     reads=[raw.r(s3)], writes=[raw.r(s3)])
                dst = vT if s3 == 2 else sil
                if s3 == 2:
                    P.op("act", lambda e: e.activation(out=vT[:, :], in_=acc[:, 2, :], func=AF.Silu),
                         reads=[acc.r(2)], writes=[vT.r()])
                else:
                    P.op("act", lambda e, s3=s3: e.activation(out=sil[:, s3, :], in_=acc[:, s3, :], func=AF.Silu),
                         reads=[acc.r(s3)], writes=[sil.r(s3)])
            yield
            for s2 in range(2):
                P.op("pool", lambda e, s2=s2: e.tensor_tensor(out=sq2[:, s2, :], in0=sil[:, s2, :], in1=sil[:, s2, :], op=ALU.mult),
                     reads=[sil.r(s2)], writes=[sq2.r(s2)])
                ps = next_ps(P)
                P.op("pe", lambda e, s2=s2, ps=ps: e.matmul(ps[:, :T], lhsT=ones_f[:, :], rhs=sq2[:, s2, :], start=True, stop=True),
                     reads=[ones_f.r(), sq2.r(s2)], writes=[ps.r()])
                P.op("act", lambda e, s2=s2, ps=ps: e.activation(out=rn[:, s2, :], in_=ps[:, :T], func=AF.Sqrt,
                                                                bias=epsl2[:, :], scale=1.0),
                     reads=[ps.r(), epsl2.r()], writes=[rn.r(s2)])
                P.op("dve", lambda e, s2=s2: e.reciprocal(out=rn[:, s2, :], in_=rn[:, s2, :]), reads=[rn.r(s2)], writes=[rn.r(s2)])
                dst = qnT if s2 == 0 else knT
                scl = (128.0 ** -0.5) if s2 == 0 else 1.0
                P.op("dve", lambda e, s2=s2, dst=dst, scl=scl: e.scalar_tensor_tensor(
                    out=dst[:, :], in0=sil[:, s2, :], scalar=scl, in1=rn[:, s2, :], op0=ALU.mult, op1=ALU.mult),
                    reads=[sil.r(s2), rn.r(s2)], writes=[dst.r()])
            yield
            B4 = range(4)
            tl = lambda s, name, shape=(128, 128): tbuf(par, f"{name}{s}", shape)
            pKK, pQK, pgc = {}, {}, {}
            for s in B4:
                ts = slice(s * 128, (s + 1) * 128)
                cols = tl(s, "cols", (128, 8))
                tcol = tl(s, "tcol", (128, 8))
                pc = nq()
                for c in range(8):
                    P.op("pe", lambda e, c=c, pc=pc, ts=ts: e.matmul(pc[:, 0:2], lhsT=x[:, c, ts], rhs=Whf[:, c, 0:2],
                                                                   start=(c == 0), stop=(c == 7)),
                         reads=[x.r(), Whf.r()], writes=[pc.r()])
                pss_ = nq()
                for c in range(8):
                    P.op("pe", lambda e, c=c, pss_=pss_, ts=ts: e.matmul(pss_[:, 0:2], lhsT=sq[:, c, ts], rhs=C.ones_bf[:, 0:2],
                                                                       start=(c == 0), stop=(c == 7)),
                         reads=[sq.r(c), C.ones_bf.r()], writes=[pss_.r()])
                P.op("act", lambda e, pss_=pss_, tcol=tcol: e.activation(out=tcol[:, 0:2], in_=pss_[:, 0:2], func=AF.Sqrt,
                                                                       bias=C.eps[:, :], scale=1.0 / 1024),
                     reads=[pss_.r(), C.eps.r()], writes=[tcol.r()])
                P.op("dve", lambda e, tcol=tcol: e.reciprocal(out=tcol[:, 0:2], in_=tcol[:, 0:2]), reads=[tcol.r()], writes=[tcol.r()])
                P.op("dve", lambda e, pc=pc, tcol=tcol: e.tensor_tensor(out=tcol[:, 2:4], in0=pc[:, 0:2], in1=tcol[:, 0:2], op=ALU.mult),
                     reads=[pc.r(), tcol.r()], writes=[tcol.r()])
                P.op("act", lambda e, tcol=tcol, cols=cols: e.activation(out=cols[:, 1:2], in_=tcol[:, 2:3], func=AF.Sigmoid),
                     reads=[tcol.r()], writes=[cols.r()])
                P.op("act", lambda e, tcol=tcol: e.activation(out=tcol[:, 4:5], in_=tcol[:, 3:4], func=AF.Exp, bias=sct[:, 1:2], scale=1.0),
                     reads=[tcol.r(), sct.r()], writes=[tcol.r()])
                P.op("act", lambda e, tcol=tcol: e.activation(out=tcol[:, 5:6], in_=tcol[:, 4:5], func=AF.Ln, bias=onec[:, 0:1], scale=1.0),
                     reads=[tcol.r(), onec.r()], writes=[tcol.r()])
                P.op("dve", lambda e, tcol=tcol, cols=cols: e.tensor_scalar(out=cols[:, 0:1], in0=tcol[:, 5:6], scalar1=nA[:, 0:1], scalar2=None, op0=ALU.mult),
                     reads=[tcol.r(), nA.r()], writes=[cols.r()])
                gB = tl(s, "gB"); bB = tl(s, "bB")
                P.op("dve", lambda e, gB=gB, cols=cols: e.tensor_scalar(out=gB[:, :], in0=ones_f[:, :], scalar1=cols[:, 0:1],
                                                                       scalar2=None, op0=ALU.mult),
                     reads=[ones_f.r(), cols.r()], writes=[gB.r()])
                P.op("pool", lambda e, bB=bB, cols=cols: e.tensor_scalar(out=bB[:, :], in0=ones_f[:, :], scalar1=cols[:, 1:2],
                                                                        scalar2=None, op0=ALU.mult),
                     reads=[ones_f.r(), cols.r()], writes=[bB.r()])
            yield
            for s in B4:
                ts = slice(s * 128, (s + 1) * 128)
                cols = tl(s, "cols", (128, 8)); gB = tl(s, "gB")
                pgc[s] = nq()
                P.op("pe", lambda e, p=pgc[s], gB=gB: e.matmul(p[:, :], lhsT=gB[:, :], rhs=maskI, start=True, stop=True),
                     reads=[gB.r(), K.r()], writes=[pgc[s].r()])
                pm = nq()
                P.op("pe", lambda e, pm=pm, cols=cols: e.matmul(pm[:, 0:2], lhsT=maskI, rhs=cols[:, 0:2], start=True, stop=True),
                     reads=[K.r(), cols.r()], writes=[pm.r()])
                P.op("pe", lambda e, pm=pm, cols=cols: e.matmul(pm[:, 2:4], lhsT=blk1, rhs=cols[:, 0:2], start=True, stop=True),
                     reads=[K.r(), cols.r()], writes=[pm.r()])
                P.op("pe", lambda e, pm=pm, gB=gB: e.matmul(pm[:, 4:6], lhsT=gB[:, :], rhs=cind, start=True, stop=True),
                     reads=[K.r(), gB.r()], writes=[pm.r()])
                P.op("act", lambda e, pm=pm, cols=cols: e.copy(out=cols[:, 2:3], in_=pm[:, 0:1]), reads=[pm.r()], writes=[cols.r()])
                P.op("act", lambda e, pm=pm, cols=cols: e.copy(out=cols[:, 3:4], in_=pm[:, 2:3]), reads=[pm.r()], writes=[cols.r()])
                glb = tl(s, "glb", (128, 2))
                P.op("act", lambda e, pm=pm, glb=glb: e.activation(out=glb[:, :], in_=pm[:, 4:6], func=AF.Exp),
                     reads=[pm.r()], writes=[glb.r()])
                E = tl(s, "E"); egc = tl(s, "egc")
                P.op("dve", lambda e, p=pgc[s], E=E, cols=cols: e.tensor_scalar(
                    out=E[:, :], in0=p[:, :], scalar1=cols[:, 2:3], scalar2=0.0, op0=ALU.subtract, op1=ALU.min),
                    reads=[pgc[s].r(), cols.r()], writes=[E.r()])
                P.op("act", lambda e, p=pgc[s], egc=egc: e.activation(out=egc[:, :], in_=p[:, :], func=AF.Exp),
                     reads=[pgc[s].r()], writes=[egc.r()])
                P.op("act", lambda e, cols=cols: e.activation(out=cols[:, 4:5], in_=cols[:, 2:3], func=AF.Exp),
                     reads=[cols.r()], writes=[cols.r()])
                P.op("dve", lambda e, cols=cols: e.tensor_tensor(out=cols[:, 5:6], in0=cols[:, 4:5], in1=cols[:, 1:2], op=ALU.mult),
                     reads=[cols.r()], writes=[cols.r()])
                P.op("dve", lambda e, cols=cols: e.tensor_tensor(out=cols[:, 6:7], in0=cols[:, 3:4], in1=cols[:, 2:3], op=ALU.subtract),
                     reads=[cols.r()], writes=[cols.r()])
                P.op("act", lambda e, cols=cols: e.activation(out=cols[:, 6:7], in_=cols[:, 6:7], func=AF.Exp),
                     reads=[cols.r()], writes=[cols.r()])
            yield
            for s in B4:
                ts = slice(s * 128, (s + 1) * 128)
                E = tl(s, "E"); EmS = tl(s, "EmS"); EmI = tl(s, "EmI")
                P.op("act", lambda e, E=E: e.activation(out=E[:, :], in_=E[:, :], func=AF.Exp), reads=[E.r()], writes=[E.r()])
                pbb = nq()
                bB = tl(s, "bB")
                P.op("pe", lambda e, pbb=pbb, bB=bB: e.matmul(pbb[:, :], lhsT=bB[:, :], rhs=ident, start=True, stop=True),
                     reads=[bB.r(), K.r()], writes=[pbb.r()])
                P.op("pool", lambda e, E=E, EmS=EmS: e.tensor_tensor(out=EmS[:, :], in0=E[:, :], in1=maskS, op=ALU.mult),
                     reads=[E.r(), K.r()], writes=[EmS.r()])
                P.op("pool", lambda e, E=E, EmI=EmI: e.tensor_tensor(out=EmI[:, :], in0=E[:, :], in1=maskI, op=ALU.mult),
                     reads=[E.r(), K.r()], writes=[EmI.r()])
                P.op("dve", lambda e, pbb=pbb, EmS=EmS: e.tensor_tensor(out=EmS[:, :], in0=pbb[:, :], in1=EmS[:, :], op=ALU.mult),
                     reads=[pbb.r(), EmS.r()], writes=[EmS.r()])
            yield
            for s in B4:
                ts = slice(s * 128, (s + 1) * 128)
                EmS = tl(s, "EmS"); EmI = tl(s, "EmI"); Q0 = tl(s, "Qa"); qkT = tl(s, "qkT")
                pKK[s] = nq()
                P.op("pe", lambda e, p=pKK[s], ts=ts: e.matmul(p[:, :], lhsT=knT[:, ts], rhs=knT[:, ts], start=True, stop=True),
                     reads=[knT.r()], writes=[pKK[s].r()])
                P.op("dve", lambda e, p=pKK[s], EmS=EmS, Q0=Q0: e.scalar_tensor_tensor(
                    out=Q0[:, :], in0=p[:, :], scalar=-1.0, in1=EmS[:, :], op0=ALU.mult, op1=ALU.mult),
                    reads=[pKK[s].r(), EmS.r()], writes=[Q0.r()])
                pQK[s] = nq()
                P.op("pe", lambda e, p=pQK[s], ts=ts: e.matmul(p[:, :], lhsT=knT[:, ts], rhs=qnT[:, ts], start=True, stop=True),
                     reads=[knT.r(), qnT.r()], writes=[pQK[s].r()])
                P.op("dve", lambda e, p=pQK[s], EmI=EmI, qkT=qkT: e.tensor_tensor(out=qkT[:, :], in0=p[:, :], in1=EmI[:, :], op=ALU.mult),
                     reads=[pQK[s].r(), EmI.r()], writes=[qkT.r()])
            yield
            for s in B4:
                Q0 = tl(s, "Qa"); N0 = tl(s, "Na"); R0 = tl(s, "Ra")
                pt = nq()
                P.op("pe", lambda e, pt=pt, Q0=Q0: e.transpose(pt[:, :], Q0[:, :], ident), reads=[Q0.r(), K.r()], writes=[pt.r()])
                P.op("act", lambda e, pt=pt, N0=N0: e.copy(out=N0[:, :], in_=pt[:, :]), reads=[pt.r()], writes=[N0.r()])
                P.op("pool", lambda e, Q0=Q0, R0=R0: e.tensor_tensor(out=R0[:, :], in0=Q0[:, :], in1=ident, op=ALU.add),
                     reads=[Q0.r(), K.r()], writes=[R0.r()])
            yield
            names = ["a", "b"]
            for i in range(1, 6):
                po, pn = names[(i - 1) % 2], names[i % 2]
                for s in B4:
                    Qo = tl(s, "Q" + po); No = tl(s, "N" + po); Ro = tl(s, "R" + po)
                    Qn = tl(s, "Q" + pn); Nn = tl(s, "N" + pn)
                    pN = nq()
                    P.op("pe", lambda e, pN=pN, Qo=Qo, No=No: e.matmul(pN[:, :], lhsT=Qo[:, :], rhs=No[:, :], start=True, stop=True),
                         reads=[Qo.r(), No.r()], writes=[pN.r()])
                    P.op("act", lambda e, pN=pN, Nn=Nn: e.copy(out=Nn[:, :], in_=pN[:, :]), reads=[pN.r()], writes=[Nn.r()])
                    if i < 5:
                        pQ = nq()
                        P.op("pe", lambda e, pQ=pQ, Qo=Qo, No=No: e.matmul(pQ[:, :], lhsT=No[:, :], rhs=Qo[:, :], start=True, stop=True),
                             reads=[Qo.r(), No.r()], writes=[pQ.r()])
                        P.op("dve", lambda e, pQ=pQ, Qn=Qn: e.tensor_copy(out=Qn[:, :], in_=pQ[:, :]), reads=[pQ.r()], writes=[Qn.r()])
                yield
                for s in B4:
                    Nn = tl(s, "N" + pn); Ro = tl(s, "R" + po); Rn = tl(s, "R" + pn)
                    pR = nq()
                    P.op("pe", lambda e, pR=pR, Nn=Nn, Ro=Ro: e.matmul(pR[:, :], lhsT=Nn[:, :], rhs=Ro[:, :], start=True, stop=True),
                         reads=[Nn.r(), Ro.r()], writes=[pR.r()])
                    P.op("dve", lambda e, pR=pR, Ro=Ro, Rn=Rn: e.tensor_tensor(out=Rn[:, :], in0=pR[:, :], in1=Ro[:, :], op=ALU.add),
                         reads=[pR.r(), Ro.r()], writes=[Rn.r()])
                yield
            TTn = names[5 % 2]
            for s in B4:
                ts = slice(s * 128, (s + 1) * 128)
                cols = tl(s, "cols", (128, 8))
                UWin = tl(s, "UWin", (128, 256)); kd = tl(s, "kd")
                pk = nq()
                P.op("pe", lambda e, pk=pk, ts=ts: e.transpose(pk[:, :], knT[:, ts], ident), reads=[knT.r(), K.r()], writes=[pk.r()])
                P.op("dve", lambda e, pk=pk, UWin=UWin, cols=cols: e.tensor_scalar(out=UWin[:, 128:256], in0=pk[:, :], scalar1=cols[:, 5:6], scalar2=None, op0=ALU.mult),
                     reads=[pk.r(), cols.r()], writes=[UWin.r()])
                P.op("dve", lambda e, pk=pk, kd=kd, cols=cols: e.tensor_scalar(out=kd[:, :], in0=pk[:, :], scalar1=cols[:, 6:7], scalar2=None, op0=ALU.mult),
                     reads=[pk.r(), cols.r()], writes=[kd.r()])
                pv = nq()
                P.op("pe", lambda e, pv=pv, ts=ts: e.transpose(pv[:, :], vT[:, ts], ident), reads=[vT.r(), K.r()], writes=[pv.r()])
                P.op("dve", lambda e, pv=pv, UWin=UWin, cols=cols: e.tensor_scalar(out=UWin[:, 0:128], in0=pv[:, :], scalar1=cols[:, 1:2], scalar2=None, op0=ALU.mult),
                     reads=[pv.r(), cols.r()], writes=[UWin.r()])
            yield
            for s in B4:
                ts = slice(s * 128, (s + 1) * 128)
                UWin = tl(s, "UWin", (128, 256)); uw = tl(s, "uw", (128, 256)); TT = tl(s, "R" + TTn)
                egc = tl(s, "egc"); qdT = tl(s, "qdT")
                pu = nh()
                P.op("pe", lambda e, pu=pu, TT=TT, UWin=UWin: e.matmul(pu[:, :], lhsT=TT[:, :], rhs=UWin[:, :], start=True, stop=True),
                     reads=[TT.r(), UWin.r()], writes=[pu.r()])
                P.op("act", lambda e, pu=pu, uw=uw: e.copy(out=uw[:, :], in_=pu[:, :]), reads=[pu.r()], writes=[uw.r()])
                P.op("pool", lambda e, qdT=qdT, egc=egc, ts=ts: e.tensor_tensor(out=qdT[:, :], in0=qnT[:, ts], in1=egc[:, :], op=ALU.mult),
                     reads=[qnT.r(), egc.r()], writes=[qdT.r()])
            yield
            for s in B4:
                uw = tl(s, "uw", (128, 256)); qkT = tl(s, "qkT"); qdT = tl(s, "qdT"); kd = tl(s, "kd")
                QpT = tl(s, "QpT"); O0T = tl(s, "O0T"); glb = tl(s, "glb", (128, 2))
                pw = nq()
                P.op("pe", lambda e, pw=pw, uw=uw, qkT=qkT: e.matmul(pw[:, :], lhsT=uw[:, 128:256], rhs=qkT[:, :], start=True, stop=True),
                     reads=[uw.r(), qkT.r()], writes=[pw.r()])
                P.op("dve", lambda e, pw=pw, qdT=qdT, QpT=QpT: e.tensor_tensor(out=QpT[:, :], in0=qdT[:, :], in1=pw[:, :], op=ALU.subtract),
                     reads=[pw.r(), qdT.r()], writes=[QpT.r()])
                po0 = nq()
                P.op("pe", lambda e, po0=po0, uw=uw, qkT=qkT: e.matmul(po0[:, :], lhsT=uw[:, 0:128], rhs=qkT[:, :], start=True, stop=True),
                     reads=[uw.r(), qkT.r()], writes=[po0.r()])
                P.op("act", lambda e, po0=po0, O0T=O0T: e.copy(out=O0T[:, :], in_=po0[:, :]), reads=[po0.r()], writes=[O0T.r()])
                for c2 in range(2):
                    r = slice(c2 * 64, (c2 + 1) * 64)
                    MT = tl(s, f"MT{c2}"); Bc = tl(s, f"B{c2}")
                    pM = nq()
                    P.op("pe", lambda e, pM=pM, uw=uw, kd=kd, r=r: e.matmul(pM[:, :], lhsT=uw[r, 128:256], rhs=kd[r, :], start=True, stop=True),
                         reads=[uw.r(), kd.r()], writes=[pM.r()])
                    P.op("dve", lambda e, pM=pM, MT=MT, glb=glb, c2=c2: e.scalar_tensor_tensor(
                        out=MT[:, :], in0=ident, scalar=glb[:, c2:c2 + 1], in1=pM[:, :], op0=ALU.mult, op1=ALU.subtract),
                        reads=[pM.r(), glb.r(), K.r()], writes=[MT.r()])
                    pB = nq()
                    P.op("pe", lambda e, pB=pB, uw=uw, kd=kd, r=r: e.matmul(pB[:, :], lhsT=kd[r, :], rhs=uw[r, 0:128], start=True, stop=True),
                         reads=[uw.r(), kd.r()], writes=[pB.r()])
                    P.op("act", lambda e, pB=pB, Bc=Bc: e.copy(out=Bc[:, :], in_=pB[:, :]), reads=[pB.r()], writes=[Bc.r()])
                yield

        def gen_rec(t):
            par = t % 2
            t0 = t * T
            tl = lambda s, name, shape=(128, 128): tbuf(par, f"{name}{s}", shape)
            oTt = tbuf(par, "oTt", (128, T))
            gateT = tbuf(par, "gateT", (128, T))
            for s in range(4):
                QpT = tl(s, "QpT"); O0T = tl(s, "O0T")
                for c2 in range(2):
                    r = slice(c2 * 64, (c2 + 1) * 64)
                    col = slice(s * 128 + c2 * 64, s * 128 + (c2 + 1) * 64)
                    MT = tl(s, f"MT{c2}"); Bc = tl(s, f"B{c2}")
                    So = S[sidx[0] % 2]
                    Sn = S[(sidx[0] + 1) % 2]
                    sidx[0] += 1
                    po = nq()
                    P.op("pe", lambda e, po=po, So=So, QpT=QpT, r=r: e.matmul(po[:, 0:64], lhsT=So[:, :], rhs=QpT[:, r], start=True, stop=True),
                         reads=[So.r(), QpT.r()], writes=[po.r()])
                    pS = nq()
                    P.op("pe", lambda e, pS=pS, So=So, MT=MT: e.matmul(pS[:, :], lhsT=MT[:, :], rhs=So[:, :], start=True, stop=True),
                         reads=[So.r(), MT.r()], writes=[pS.r()])
                    P.op("dve", lambda e, pS=pS, Bc=Bc, Sn=Sn: e.tensor_tensor(out=Sn[:, :], in0=pS[:, :], in1=Bc[:, :], op=ALU.add),
                         reads=[pS.r(), Bc.r()], writes=[Sn.r()])
                    P.op("dve", lambda e, po=po, O0T=O0T, r=r, col=col: e.tensor_tensor(out=oTt[:, col], in0=po[:, 0:64], in1=O0T[:, r], op=ALU.add),
                         reads=[po.r(), O0T.r()], writes=[oTt.r()])
                    yield
            osq = tbuf(par, "osq", (128, T))
            orr = tbuf(par, "orr", (128, T))
            P.op("pool", lambda e: e.tensor_tensor(out=osq[:, :], in0=oTt[:, :], in1=oTt[:, :], op=ALU.mult), reads=[oTt.r()], writes=[osq.r()])
            ps = next_ps(P)
            P.op("pe", lambda e, ps=ps: e.matmul(ps[:, :T], lhsT=ones_f[:, :], rhs=osq[:, :], start=True, stop=True),
                 reads=[ones_f.r(), osq.r()], writes=[ps.r()])
            P.op("act", lambda e, ps=ps: e.activation(out=orr[:, :], in_=ps[:, :T], func=AF.Sqrt, bias=C.eps[:, :], scale=1.0 / 128),
                 reads=[ps.r(), C.eps.r()], writes=[orr.r()])
            P.op("dve", lambda e: e.reciprocal(out=orr[:, :], in_=orr[:, :]), reads=[orr.r()], writes=[orr.r()])
            P.op("dve", lambda e: e.scalar_tensor_tensor(out=osq[:, :], in0=oTt[:, :], scalar=ogt[:, 0:1], in1=orr[:, :],
                                                        op0=ALU.mult, op1=ALU.mult),
                 reads=[oTt.r(), orr.r(), ogt.r()], writes=[osq.r()])
            P.op("pool", lambda e: e.tensor_tensor(out=osq[:, :], in0=osq[:, :], in1=gateT[:, :], op=ALU.mult),
                 reads=[osq.r(), gateT.r()], writes=[osq.r()])
            outs.append(P.dma(lambda e: e.dma_start(out=oT[:, t0:t0 + T], in_=osq[:, :]), reads=[osq.r()]))
            yield

        rec = None
        for t in range(NTI):
            loc = gen_local(t)
            while True:
                a = next(loc, "done")
                if rec is not None:
                    next(rec, None)
                if a == "done":
                    break
            if rec is not None:
                for _ in rec:
                    pass
            rec = gen_rec(t)
        for _ in rec:
            pass
        outs.append(P.dma(lambda e: e.dma_start(out=s_out, in_=S[sidx[0] % 2][:, :]), reads=[S[sidx[0] % 2].r()]))
        outs.append(P.dma(lambda e: e.dma_start(out=h_out, in_=raw[:, :, 0:3]), reads=raw.all()))
        P.emit(final_waits=outs)
    return nc


def build_dsa(L=16384, NIT=18, jset=None):
    T = 512
    NTI = L // T
    NQT = L // 256
    NJ_ALL = NQT // 8
    jset = list(range(NJ_ALL)) if jset is None else list(jset)
    NJ = len(jset)
    NQ = NJ * 256
    SCALE = 128.0 ** -0.5
    WSC = (8.0 ** -0.5) * (64.0 ** -0.5)
    nc = bass.Bass("TRN2", target_bir_lowering=False)
    xT = nc.dram_tensor("xT", [1024, L], F32, kind="ExternalInput").ap()
    xq = nc.dram_tensor("xq", [1024, NQ], F32, kind="ExternalInput").ap()
    g = nc.dram_tensor("g", [1024], F32, kind="ExternalInput").ap()
    win = nc.dram_tensor("win", [1024, 3656], F32, kind="ExternalInput").ap()
    lng = nc.dram_tensor("lng", [64], F32, kind="ExternalInput").ap()
    lnb = nc.dram_tensor("lnb", [64], F32, kind="ExternalInput").ap()
    qrel = nc.dram_tensor("qrel", [128, NJ * 2], F32, kind="ExternalInput").ap()
    kidx = nc.dram_tensor("kidx", [128, 2048], F32, kind="ExternalInput").ap()
    cst = nc.dram_tensor("cst", [128, 5, 128], F32, kind="ExternalInput").ap()
    oT = nc.dram_tensor("oT", [1024, NQ], F32, kind="ExternalOutput").ap()
    kTd = nc.dram_tensor("kTd", [1024, L], BF16, kind="Internal").ap()
    Vd = nc.dram_tensor("Vd", [L, 1024], BF16, kind="Internal").ap()
    qTd = nc.dram_tensor("qTd", [1024, NQ], BF16, kind="Internal").ap()
    qiTd = nc.dram_tensor("qiTd", [64, 8, NQ], F32, kind="Internal").ap()
    kiTd = nc.dram_tensor("kiTd", [64, L], F32, kind="Internal").ap()
    xv = xT.rearrange("(c p) t -> p c t", p=128)
    xqv = xq.rearrange("(c p) t -> p c t", p=128)
    kTv = kTd.rearrange("(h p) t -> p h t", p=128)
    Vv = Vd.rearrange("(n p) d -> p n d", p=128)
    qTv = qTd.rearrange("(h p) t -> p h t", p=128)
    oTv = oT.rearrange("(h p) t -> p h t", p=128)
    wv_ = win.rearrange("(c p) f -> p c f", p=128)
    with ExitStack() as st:
        st.enter_context(nc.allow_low_precision("bf16 matmul operands, fp32 accumulate"))
        P = Prog(nc, st)
        banks = [P.ps(f"b{i}", [128, 512], F32) for i in range(8)]
        P.pss = banks
        P._psi = 0
        C = make_consts(P)
        ones_f = P.sb("ones_f", [128, 128], F32)
        P.op("dve", lambda e: e.memset(ones_f[:, :], 1.0), writes=[ones_f.r()])
        gt = load_vec_fm(P, g, 8, "g")
        lngt = P.sb("lngt", [64, 1], F32)
        lnbt = P.sb("lnbt", [64, 1], F32)
        P.dma(lambda e: e.dma_start(out=lngt[:, :], in_=lng.rearrange("(p o) -> p o", o=1)), writes=[lngt.r()])
        P.dma(lambda e: e.dma_start(out=lnbt[:, :], in_=lnb.rearrange("(p o) -> p o", o=1)), writes=[lnbt.r()])
        qrt = load_const(P, qrel, [128, NJ * 2], "qrt")
        kit = load_const(P, kidx, [128, 2048], "kit")
        Kc = load_const(P, cst, [128, 5, 128], "Kc")
        idb = P.sb("idb", [128, 128], BF16)
        P.op("dve", lambda e: e.tensor_copy(out=idb[:, :], in_=Kc[:, 0, :]), reads=[Kc.r()], writes=[idb.r()])
        selb = P.sb("selb", [128, 2, 512], BF16)
        P.op("dve", lambda e: e.memset(selb[:, :, :], 0.0), writes=[selb.r()])
        for hf in range(2):
            for rep in range(2):
                c0 = rep * 256 + hf * 128
                P.op("dve", lambda e, hf=hf, c0=c0: e.tensor_copy(out=selb[:, hf, c0:c0 + 128], in_=Kc[:, 0, :]),
                     reads=[Kc.r(), selb.r()], writes=[selb.r()])
        wiT = P.sb("wiT", [128, NJ * 2, 8], F32)

        P.push_scope()
        Wq = P.sb("Wq", [128, 8, 1024], BF16, nsub=8)
        Wk = P.sb("Wk", [128, 8, 1024], BF16, nsub=8)
        Wv = P.sb("Wv", [128, 8, 1024], BF16, nsub=8)
        Wi = P.sb("Wi", [128, 8, 584], F32, nsub=8)
        hnf = P.sb("hnf", [128, 8, T], F32, nsub=8)
        for c in range(8):
            P.dma(lambda e, c=c: e.dma_start(out=Wk[:, c, :], in_=wv_[:, c, 1024:2048], max_dma_last_dim=8192), writes=[Wk.r(c)], q="pool")
            P.dma(lambda e, c=c: e.dma_start(out=Wv[:, c, :], in_=wv_[:, c, 2048:3072], max_dma_last_dim=8192), writes=[Wv.r(c)], q="pool")
            P.dma(lambda e, c=c: e.dma_start(out=Wi[:, c, :], in_=wv_[:, c, 3072:3656]), writes=[Wi.r(c)])
            P.dma(lambda e, c=c: e.dma_start(out=Wq[:, c, :], in_=wv_[:, c, 0:1024], max_dma_last_dim=8192), writes=[Wq.r(c)], q="pool")
        x = P.sb("x", [128, 8, T], F32)
        sq = P.sb("sq", [128, 8, T], BF16, nsub=8)
        hn = P.sb("hn", [128, 8, T], BF16, nsub=8)
        rstd = P.sb("rstd", [128, T], F32)
        ktb = [P.sb(f"ktb{i}", [128, 8, T], BF16, nsub=8) for i in range(2)]
        vtb = [P.sb(f"vtb{i}", [128, 4, 1024], BF16, nsub=8) for i in range(2)]
        kraw = P.sb("kraw", [64, T], F32)
        ksq = P.sb("ksq", [64, T], F32)
        kmean = P.sb("kmean", [64, T], F32)
        kvar = P.sb("kvar", [64, T], F32)
        kio = P.sb("kio", [64, T], F32)
        NTI0 = min(NTI, (256 * (8 * max(jset) + 8)) // T)
        for it in range(NTI0):
            t0 = it * T
            kt = ktb[it % 2]
            vt = vtb[it % 2]
            P.dma(lambda e, t0=t0: e.dma_start(out=x[:, :, :], in_=xv[:, :, t0:t0 + T]), writes=[x.r()])
            norm_tile(P, C, x, gt, sq, hn, rstd, T)
            for c in range(8):
                P.op("dve", lambda e, c=c: e.scalar_tensor_tensor(out=hnf[:, c, :], in0=x[:, c, :], scalar=gt[:, c:c + 1], in1=rstd[:, :],
                                                                 op0=ALU.mult, op1=ALU.mult), reads=[x.r(), rstd.r(), gt.r()], writes=[hnf.r(c)])
            for h in range(8):
                ps = next_ps(P)
                for c in range(8):
                    P.op("pe", lambda e, c=c, h=h, ps=ps: e.matmul(ps[:, :T], lhsT=Wk[:, c, h * 128:(h + 1) * 128], rhs=hn[:, c, :],
                                                                  start=(c == 0), stop=(c == 7)),
                         reads=[Wk.r(c), hn.r(c)], writes=[ps.r()])
                if h % 2 == 0:
                    P.op("act", lambda e, h=h, ps=ps, kt=kt: e.copy(out=kt[:, h, :], in_=ps[:, :T]), reads=[ps.r()], writes=[kt.r(h)])
                else:
                    P.op("dve", lambda e, h=h, ps=ps, kt=kt: e.tensor_copy(out=kt[:, h, :], in_=ps[:, :T]), reads=[ps.r()], writes=[kt.r(h)])
            P.dma(lambda e, kt=kt, t0=t0: e.dma_start(out=kTv[:, :, t0:t0 + T], in_=kt[:, :, :]), reads=kt.all())
            for b4 in range(4):
                for half in range(2):
                    ps = next_ps(P)
                    for c in range(8):
                        P.op("pe", lambda e, c=c, b4=b4, half=half, ps=ps: e.matmul(
                            ps[:, :512], lhsT=hn[:, c, b4 * 128:(b4 + 1) * 128], rhs=Wv[:, c, half * 512:(half + 1) * 512],
                            start=(c == 0), stop=(c == 7)), reads=[Wv.r(c), hn.r(c)], writes=[ps.r()])
                    if half == 0:
                        P.op("act", lambda e, b4=b4, half=half, ps=ps, vt=vt: e.copy(out=vt[:, b4, 0:512], in_=ps[:, :512]),
                             reads=[ps.r()], writes=[vt.r(b4 * 2)])
                    else:
                        P.op("dve", lambda e, b4=b4, half=half, ps=ps, vt=vt: e.tensor_copy(out=vt[:, b4, 512:1024], in_=ps[:, :512]),
                             reads=[ps.r()], writes=[vt.r(b4 * 2 + 1)])
            P.dma(lambda e, vt=vt, it=it: e.dma_start(out=Vv[:, it * 4:(it + 1) * 4, :], in_=vt[:, :, :]), reads=vt.all(), q="act")
            ps = next_ps(P)
            for c in range(8):
                P.op("pe", lambda e, c=c, ps=ps: e.matmul(ps[0:64, :T], lhsT=Wi[:, c, 512:576], rhs=hnf[:, c, :],
                                                       start=(c == 0), stop=(c == 7)),
                     reads=[Wi.r(c), hnf.r(c)], writes=[ps.r()])
            P.op("act", lambda e, ps=ps: e.copy(out=kraw[:, :], in_=ps[0:64, :T]), reads=[ps.r()], writes=[kraw.r()])
            P.op("pool", lambda e: e.tensor_tensor(out=ksq[:, :], in0=kraw[:, :], in1=kraw[:, :], op=ALU.mult), reads=[kraw.r()], writes=[ksq.r()])
            p1 = next_ps(P)
            P.op("pe", lambda e, p1=p1: e.matmul(p1[0:64, :T], lhsT=ones_f[0:64, 0:64], rhs=kraw[:, :], start=True, stop=True),
                 reads=[ones_f.r(), kraw.r()], writes=[p1.r()])
            p2 = next_ps(P)
            P.op("pe", lambda e, p2=p2: e.matmul(p2[0:64, :T], lhsT=ones_f[0:64, 0:64], rhs=ksq[:, :], start=True, stop=True),
                 reads=[ones_f.r(), ksq.r()], writes=[p2.r()])
            P.op("act", lambda e, p1=p1: e.activation(out=kmean[:, :], in_=p1[0:64, :T], func=AF.Copy, scale=1.0 / 64), reads=[p1.r()], writes=[kmean.r()])
            P.op("dve", lambda e: e.tensor_tensor(out=ksq[:, :], in0=kmean[:, :], in1=kmean[:, :], op=ALU.mult), reads=[kmean.r(), ksq.r()], writes=[ksq.r()])
            P.op("dve", lambda e, p2=p2: e.scalar_tensor_tensor(out=kvar[:, :], in0=p2[0:64, :T], scalar=1.0 / 64, in1=ksq[:, :],
                                                              op0=ALU.mult, op1=ALU.subtract), reads=[p2.r(), ksq.r()], writes=[kvar.r()])
            P.op("act", lambda e: e.activation(out=kvar[:, :], in_=kvar[:, :], func=AF.Sqrt, bias=C.eps[0:64, :], scale=1.0),
                 reads=[kvar.r(), C.eps.r()], writes=[kvar.r()])
            P.op("dve", lambda e: e.reciprocal(out=kvar[:, :], in_=kvar[:, :]), reads=[kvar.r()], writes=[kvar.r()])
            P.op("pool", lambda e: e.tensor_tensor(out=kraw[:, :], in0=kraw[:, :], in1=kmean[:, :], op=ALU.subtract), reads=[kraw.r(), kmean.r()], writes=[kraw.r()])
            P.op("dve", lambda e: e.scalar_tensor_tensor(out=kraw[:, :], in0=kraw[:, :], scalar=lngt[:, 0:1], in1=kvar[:, :],
                                                        op0=ALU.mult, op1=ALU.mult), reads=[kraw.r(), kvar.r(), lngt.r()], writes=[kraw.r()])
            P.op("act", lambda e: e.activation(out=kio[:, :], in_=kraw[:, :], func=AF.Identity, bias=lnbt[:, 0:1], scale=1.0),
                 reads=[kraw.r(), lnbt.r()], writes=[kio.r()])
            P.dma(lambda e, t0=t0: e.dma_start(out=kiTd[:, t0:t0 + T], in_=kio[:, :]), reads=[kio.r()])
        qtb = P.sb("qtb", [128, 8, 256], BF16, nsub=8)
        qitb = P.sb("qitb", [64, 8, 256], F32, nsub=8)
        for j in range(NJ):
            q0 = j * 256
            P.dma(lambda e, q0=q0: e.dma_start(out=x[:, :, 0:256], in_=xqv[:, :, q0:q0 + 256]), writes=[x.r()])
            norm_tile(P, C, x, gt, sq, hn, rstd, 256)
            for c in range(8):
                P.op("dve", lambda e, c=c: e.scalar_tensor_tensor(out=hnf[:, c, 0:256], in0=x[:, c, 0:256], scalar=gt[:, c:c + 1], in1=rstd[:, 0:256],
                                                                 op0=ALU.mult, op1=ALU.mult), reads=[x.r(), rstd.r(), gt.r()], writes=[hnf.r(c)])
            for h in range(8):
                ps = next_ps(P)
                for c in range(8):
                    P.op("pe", lambda e, c=c, h=h, ps=ps: e.matmul(ps[:, :256], lhsT=Wq[:, c, h * 128:(h + 1) * 128], rhs=hn[:, c, 0:256],
                                                                  start=(c == 0), stop=(c == 7)),
                         reads=[Wq.r(c), hn.r(c)], writes=[ps.r()])
                P.op("act", lambda e, h=h, ps=ps: e.copy(out=qtb[:, h, :], in_=ps[:, :256]), reads=[ps.r()], writes=[qtb.r(h)])
                ps2 = next_ps(P)
                for c in range(8):
                    P.op("pe", lambda e, c=c, h=h, ps2=ps2: e.matmul(ps2[0:64, :256], lhsT=Wi[:, c, h * 64:(h + 1) * 64], rhs=hnf[:, c, 0:256],
                                                                    start=(c == 0), stop=(c == 7)),
                         reads=[Wi.r(c), hnf.r(c)], writes=[ps2.r()])
                P.op("dve", lambda e, h=h, ps2=ps2: e.tensor_copy(out=qitb[:, h, :], in_=ps2[0:64, :256]), reads=[ps2.r()], writes=[qitb.r(h)])
            P.dma(lambda e, q0=q0: e.dma_start(out=qTv[:, :, q0:q0 + 256], in_=qtb[:, :, :]), reads=qtb.all())
            P.dma(lambda e, q0=q0: e.dma_start(out=qiTd[:, :, q0:q0 + 256], in_=qitb[:, :, :]), reads=qitb.all(), q="act")
            for hf in range(2):
                ps = next_ps(P)
                for c in range(8):
                    P.op("pe", lambda e, c=c, hf=hf, ps=ps: e.matmul(ps[:, 0:8], lhsT=hnf[:, c, hf * 128:(hf + 1) * 128], rhs=Wi[:, c, 576:584],
                                                                    start=(c == 0), stop=(c == 7)),
                         reads=[Wi.r(c), hnf.r(c)], writes=[ps.r()])
                P.op("act", lambda e, j=j, hf=hf, ps=ps: e.activation(out=wiT[:, j * 2 + hf, :], in_=ps[:, 0:8], func=AF.Copy, scale=WSC),
                     reads=[ps.r()], writes=[wiT.r()])
        P.pop_scope()

        I = P.sb("I", [128, L], F32)
        Mb = [P.sb(f"Mb{i}", [128, L], BF16) for i in range(2)]
        junk = P.sb("junk", [128, 2048], BF16)
        junkA = P.sb("junkA", [128, 2048], BF16)
        cnA = P.sb("cnA", [128, 8], F32)
        qih = P.sb("qih", [64, 8, 128], F32)
        dg = P.sb("dg", [128, 8, 128], F32)
        rl = [P.sb(f"rl{i}", [128, 512], F32) for i in range(3)]
        kics = [P.sb(f"kic{i}", [64, 512], F32) for i in range(3)]
        tmpb = P.sb("tmpb", [128, 512], F32)
        am = P.sb("am", [128, 32], F32)
        cn = P.sb("cn", [128, 8], F32)
        sm = P.sb("sm", [128, 8], F32)
        qt = P.sb("qt", [128, 4, 256], BF16)
        ktl = [P.sb(f"ktl{i}", [128, 4, 512], BF16) for i in range(2)]
        vtl = [P.sb(f"vtl{i}", [128, 4, 512], BF16) for i in range(2)]
        pts = [P.sb(f"pt{i}", [128, 512], BF16) for i in range(5)]
        rden = P.sb("rden", [128, 512], F32)
        ob = [P.sb(f"ob{i}", [128, 512], F32) for i in range(2)]
        bS = banks[0:3]
        bO = banks[3:5]
        bD = banks[5:7]
        bI = [banks[3], banks[4]]
        outs = []
        rot = {"s": 0, "p": 0, "l": 0, "r": 0, "i": 0, "k": 0}

        def nxt(lst, key):
            rot[key] += 1
            return lst[rot[key] % len(lst)]

        for j in range(NJ):
            Nmax = 256 * (8 * jset[j] + 8)
            ws = Nmax - 2048
            nck = Nmax // 512
            for hf in range(2):
                qi = j * 2 + hf
                M = Mb[hf]
                P.dma(lambda e, j=j, hf=hf: e.dma_start(out=qih[:, :, :], in_=qiTd[:, :, j * 256 + hf * 128:j * 256 + hf * 128 + 128]),
                      writes=[qih.r()])
                for h in range(8):
                    eng = "pool" if h % 2 == 0 else "dve"
                    P.op(eng, lambda e, h=h, qi=qi: e.tensor_scalar(out=dg[:, h, :], in0=Kc[:, 0, :], scalar1=wiT[:, qi, h:h + 1], scalar2=None, op0=ALU.mult),
                         reads=[Kc.r(), wiT.r()], writes=[dg.r()])
                for kc in range(nck):
                    pI = nxt(bI, "i")
                    kic = nxt(kics, "k")
                    P.dma(lambda e, kic=kic, kc=kc: e.dma_start(out=kic[:, :], in_=kiTd[:, kc * 512:(kc + 1) * 512]), writes=[kic.r()], q="act")
                    def score(h, kc=kc, kic=kic):
                        ps = nxt(bS, "s")
                        r = nxt(rl, "r")
                        P.op("pe", lambda e, h=h, ps=ps: e.matmul(ps[:, :512], lhsT=qih[:, h, :], rhs=kic[:, :], start=True, stop=True),
                             reads=[qih.r(), kic.r()], writes=[ps.r()])
                        P.op("act", lambda e, ps=ps, r=r: e.activation(out=r[:, :], in_=ps[:, :512], func=AF.Relu), reads=[ps.r()], writes=[r.r()])
                        return r
                    def accum(h, r, pI=pI):
                        P.op("pe", lambda e, h=h, r=r: e.matmul(pI[:, :512], lhsT=dg[:, h, :], rhs=r[:, :], start=(h == 0), stop=(h == 7)),
                             reads=[dg.r(), r.r()], writes=[pI.r()])
                    prev = score(0)
                    for h in range(1, 8):
                        cur = score(h)
                        accum(h - 1, prev)
                        prev = cur
                    accum(7, prev)
                    P.op("dve", lambda e, pI=pI, kc=kc: e.tensor_reduce(out=am[:, kc:kc + 1], in_=pI[:, :512], axis=AX.X, op=ALU.max, apply_absolute_value=True),
                         reads=[pI.r()], writes=[am.r()])
                    k0 = kc * 512
                    if k0 >= ws:
                        ro = k0 - ws
                        P.op("dve", lambda e, ro=ro, qi=qi: e.tensor_scalar(out=tmpb[:, :], in0=kit[:, ro:ro + 512], scalar1=qrt[:, qi:qi + 1], scalar2=-1e30,
                                                                          op0=ALU.is_gt, op1=ALU.mult), reads=[kit.r(), qrt.r()], writes=[tmpb.r()])
                        P.op("dve", lambda e, pI=pI, k0=k0: e.tensor_tensor(out=I[:, k0:k0 + 512], in0=pI[:, :512], in1=tmpb[:, :], op=ALU.add),
                             reads=[pI.r(), tmpb.r()], writes=[I.r()])
                    else:
                        P.op("dve", lambda e, pI=pI, k0=k0: e.tensor_copy(out=I[:, k0:k0 + 512], in_=pI[:, :512]), reads=[pI.r()], writes=[I.r()])
                P.op("dve", lambda e, nck=nck: e.tensor_reduce(out=sm[:, 0:1], in_=am[:, 0:nck], axis=AX.X, op=ALU.max), reads=[am.r()], writes=[sm.r()])
                P.op("dve", lambda e: e.tensor_scalar(out=sm[:, 1:2], in0=sm[:, 0:1], scalar1=2.0, scalar2=None, op0=ALU.mult), reads=[sm.r()], writes=[sm.r()])
                P.op("dve", lambda e: e.tensor_scalar(out=sm[:, 2:3], in0=sm[:, 0:1], scalar1=-1.0, scalar2=None, op0=ALU.mult), reads=[sm.r()], writes=[sm.r()])
                npc = (Nmax + 2047) // 2048
                for itn in range(NIT):
                    P.op("dve", lambda e, itn=itn: e.tensor_scalar(out=sm[:, 3:4], in0=sm[:, 1:2], scalar1=2.0 ** -(itn + 1), scalar2=None, op0=ALU.mult),
                         reads=[sm.r()], writes=[sm.r()])
                    P.op("dve", lambda e: e.tensor_tensor(out=sm[:, 4:5], in0=sm[:, 2:3], in1=sm[:, 3:4], op=ALU.add), reads=[sm.r()], writes=[sm.r()])
                    dpc = [pc for pc in range(npc) if pc % 2 == 0]
                    apc = [pc for pc in range(npc) if pc % 2 == 1]
                    if apc:
                        P.op("dve", lambda e: e.tensor_scalar(out=sm[:, 7:8], in0=sm[:, 4:5], scalar1=-1.0, scalar2=None, op0=ALU.mult),
                             reads=[sm.r()], writes=[sm.r()])
                    for k, pc in enumerate(dpc):
                        P.op("dve", lambda e, pc=pc, k=k: e.tensor_scalar(out=junk[:, :], in0=I[:, pc * 2048:(pc + 1) * 2048], scalar1=sm[:, 4:5], scalar2=None,
                                                                   op0=ALU.is_ge, op1=ALU.add, accum_out=cn[:, k:k + 1]),
                             reads=[I.r(), sm.r()], writes=[junk.r(), cn.r()])
                    for k, pc in enumerate(apc):
                        P.op("act", lambda e, pc=pc, k=k: e.activation(out=junkA[:, :], in_=I[:, pc * 2048:(pc + 1) * 2048], func=AF.Sign,
                                                                      bias=sm[:, 7:8], scale=1.0, accum_out=cnA[:, k:k + 1]),
                             reads=[I.r(), sm.r()], writes=[junkA.r(), cnA.r()])
                    P.op("dve", lambda e, nd=len(dpc): e.tensor_reduce(out=sm[:, 5:6], in_=cn[:, 0:nd], axis=AX.X, op=ALU.add), reads=[cn.r()], writes=[sm.r()])
                    if apc:
                        na = len(apc)
                        P.op("dve", lambda e, na=na: e.tensor_reduce(out=sm[:, 6:7], in_=cnA[:, 0:na], axis=AX.X, op=ALU.add), reads=[cnA.r()], writes=[sm.r()])
                        P.op("dve", lambda e, na=na: e.tensor_scalar(out=sm[:, 6:7], in0=sm[:, 6:7], scalar1=0.5, scalar2=1024.0 * na, op0=ALU.mult, op1=ALU.add),
                             reads=[sm.r()], writes=[sm.r()])
                        P.op("dve", lambda e: e.tensor_tensor(out=sm[:, 5:6], in0=sm[:, 5:6], in1=sm[:, 6:7], op=ALU.add), reads=[sm.r()], writes=[sm.r()])
                    P.op("dve", lambda e: e.tensor_scalar(out=sm[:, 6:7], in0=sm[:, 5:6], scalar1=255.5, scalar2=sm[:, 3:4], op0=ALU.is_gt, op1=ALU.mult),
                         reads=[sm.r()], writes=[sm.r()])
                    P.op("dve", lambda e: e.tensor_tensor(out=sm[:, 2:3], in0=sm[:, 2:3], in1=sm[:, 6:7], op=ALU.add), reads=[sm.r()], writes=[sm.r()])
                for pc in range(npc):
                    P.op("dve", lambda e, pc=pc, M=M: e.tensor_scalar(out=M[:, pc * 2048:(pc + 1) * 2048], in0=I[:, pc * 2048:(pc + 1) * 2048],
                                                                     scalar1=sm[:, 2:3], scalar2=-30000.0, op0=ALU.is_lt, op1=ALU.mult),
                         reads=[I.r(), sm.r()], writes=[M.r()])
            for pz in range(2):
                P.dma(lambda e, j=j, pz=pz: e.dma_start(out=qt[:, :, :], in_=qTv[:, 4 * pz:4 * pz + 4, j * 256:(j + 1) * 256]), writes=[qt.r()])
                nkb = Nmax // 128
                LA = 2
                pend = []

                def back(kb, pair, pt, vt, kbl, nkb=nkb):
                    for hh in range(2):
                        hl = 2 * pair + hh
                        P.op("pe", lambda e, pair=pair, hh=hh, hl=hl, vt=vt, kbl=kbl, pt=pt, kb=kb, nkb=nkb: e.matmul(
                            bO[pair][:, hh * 256:(hh + 1) * 256], lhsT=vt[:, kbl, hl * 128:(hl + 1) * 128], rhs=pt[:, hh * 256:(hh + 1) * 256],
                            start=(kb == 0 and hh == 0), stop=(kb == nkb - 1 and hh == 1), skip_group_check=True),
                            reads=[vt.r(), pt.r()], writes=[bO[pair].r()])
                    P.op("pe", lambda e, pair=pair, pt=pt, kb=kb, nkb=nkb: e.matmul(
                        bD[pair][:, 0:512], lhsT=C.ones_bf[:, :], rhs=pt[:, :], start=(kb == 0), stop=(kb == nkb - 1)),
                        reads=[C.ones_bf.r(), pt.r()], writes=[bD[pair].r()])

                for kb in range(nkb):
                    kbl = kb % 4
                    if kbl == 0:
                        kt = nxt(ktl, "l")
                        vt = vtl[rot["l"] % len(vtl)]
                        k4 = kb // 4
                        P.dma(lambda e, kt=kt, k4=k4, pz=pz: e.dma_start(out=kt[:, :, :], in_=kTv[:, 4 * pz:4 * pz + 4, k4 * 512:(k4 + 1) * 512]),
                              writes=[kt.r()])
                        P.dma(lambda e, vt=vt, k4=k4, pz=pz: e.dma_start(out=vt[:, :, :], in_=Vv[:, k4 * 4:(k4 + 1) * 4, pz * 512:(pz + 1) * 512]),
                              writes=[vt.r()], q="act")
                    for pair in range(2):
                        pS = nxt(bS, "s")
                        pt = nxt(pts, "p")
                        for hh in range(2):
                            hl = 2 * pair + hh
                            P.op("pe", lambda e, pS=pS, hh=hh, hl=hl, kt=kt, kbl=kbl: e.matmul(
                                pS[:, hh * 256:(hh + 1) * 256], lhsT=kt[:, hl, kbl * 128:(kbl + 1) * 128], rhs=qt[:, hl, :],
                                start=(hh == 0), stop=False, skip_group_check=True), reads=[kt.r(), qt.r()], writes=[pS.r()])
                        for hf in range(2):
                            P.op("pe", lambda e, pS=pS, hf=hf, kb=kb: e.matmul(
                                pS[:, 0:512], lhsT=Mb[hf][:, kb * 128:(kb + 1) * 128], rhs=selb[:, hf, :],
                                start=False, stop=(hf == 1), skip_group_check=True), reads=[Mb[hf].r(), selb.r()], writes=[pS.r()])
                        P.op("act", lambda e, pS=pS, pt=pt: e.activation(out=pt[:, :], in_=pS[:, 0:512], func=AF.Exp, scale=SCALE),
                             reads=[pS.r()], writes=[pt.r()])
                        pend.append((kb, pair, pt, vt, kbl))
                        if len(pend) > LA:
                            back(*pend.pop(0))
                while pend:
                    back(*pend.pop(0))
                for pair in range(2):
                    o = ob[pair]
                    P.op("dve", lambda e, pair=pair: e.reciprocal(out=rden[:, :], in_=bD[pair][:, 0:512]), reads=[bD[pair].r()], writes=[rden.r()])
                    P.op("dve", lambda e, pair=pair, o=o: e.tensor_tensor(out=o[:, :], in0=bO[pair][:, 0:512], in1=rden[:, :], op=ALU.mult),
                         reads=[bO[pair].r(), rden.r()], writes=[o.r()])
                    h0 = 4 * pz + 2 * pair
                    outs.append(P.dma(lambda e, o=o, h0=h0, j=j: e.dma_start(
                        out=oTv[:, h0:h0 + 2, j * 256:(j + 1) * 256], in_=o[:, :].rearrange("p (h q) -> p h q", h=2)), reads=[o.r()]))
        P.emit(final_waits=outs)
    return nc


def _rope_tables(L, dim, theta=10000.0):
    pos = np.arange(L, dtype=np.float32)
    inv = (theta ** (-np.arange(0, dim, 2, dtype=np.float32) / dim)).astype(np.float32)
    ang = pos[:, None] * inv[None, :]
    return np.cos(ang).astype(np.float32), np.sin(ang).astype(np.float32)


def _mla_inputs(hT, g, w_in, gq, w_uq, gkv, w_ukv, core):
    z64 = np.zeros((1024, 64), np.float32)
    kr = w_in[:, 640:672]
    krs = np.concatenate([kr[:, 16:], kr[:, :16]], 1)
    wall = np.concatenate([w_in[:, :640], z64, kr, z64, krs], 1)
    cols = []
    for h in (2 * core, 2 * core + 1):
        wq = w_uq[:, h * 96:(h + 1) * 96]
        wqs = np.concatenate([np.zeros((384, 64), np.float32), wq[:, 80:96], wq[:, 64:80]], 1)
        cols += [wq, wqs]
    wuq = np.concatenate(cols, 1)
    kn = [w_ukv[:, h * 128:h * 128 + 64] for h in (2 * core, 2 * core + 1)]
    vv = [w_ukv[:, h * 128 + 64:h * 128 + 128] for h in (2 * core, 2 * core + 1)]
    wukv = np.concatenate(kn + vv, 1)
    return dict(hT=hT, g=g, wall=np.ascontiguousarray(wall), gq=gq, gkv=gkv, wuq=np.ascontiguousarray(wuq),
                wukv=np.ascontiguousarray(wukv))


def _mla_consts(L):
    cos, sin = _rope_tables(L, 32)
    cos2 = np.zeros((96, L), np.float32)
    sin2 = np.zeros((96, L), np.float32)
    cos2[64:80] = cos.T
    cos2[80:96] = cos.T
    sin2[64:80] = -sin.T
    sin2[80:96] = sin.T
    k = np.arange(128)[:, None, None]
    d = np.arange(4)[None, :, None]
    q = np.arange(512)[None, None, :]
    cmask = ((d * 128 + k) <= q).astype(np.float32)
    esel = np.zeros((65, 64), np.float32)
    esel[64] = 1.0
    return dict(cos2=cos2, sin2=sin2, cmask=np.ascontiguousarray(cmask), esel=esel)


def _gdn_inputs(hT, g, w_in, conv_w, a_log, dt_bias, og, h):
    cols = [w_in[:, h * 128:(h + 1) * 128], w_in[:, 1024 + h * 128:1024 + (h + 1) * 128],
            w_in[:, 2048 + h * 128:2048 + (h + 1) * 128], w_in[:, 3072 + h * 128:3072 + (h + 1) * 128],
            w_in[:, 4096 + h:4097 + h], w_in[:, 4104 + h:4105 + h], np.zeros((1024, 126), np.float32)]
    wh = np.ascontiguousarray(np.concatenate(cols, 1))
    cw = np.concatenate([conv_w[:, h * 128:(h + 1) * 128], conv_w[:, 1024 + h * 128:1024 + (h + 1) * 128],
                         conv_w[:, 2048 + h * 128:2048 + (h + 1) * 128]], 1)
    sc = np.zeros((128, 2), np.float32)
    sc[:, 0] = a_log[h]
    sc[:, 1] = dt_bias[h]
    return dict(hT=hT, g=g, wh=wh, cw=np.ascontiguousarray(cw.T), sc=sc, og=og)


def _gdn_consts():
    j = np.arange(128)[:, None]
    i = np.arange(128)[None, :]
    same = (j // 64) == (i // 64)
    cst = np.zeros((128, 5, 128), np.float32)
    cst[:, 0] = np.eye(128)
    cst[:, 1] = (same & (i > j))
    cst[:, 2] = (same & (i >= j))
    cst[:, 3] = same
    cst[:, 4, 0] = (np.arange(128) < 64)
    cst[:, 4, 1] = (np.arange(128) >= 64)
    return dict(cst=cst)


def _dsa_inputs(xT, g, w_in, lg, lb, core, L, jset=None):
    jset = list(range(L // 256 // 8)) if jset is None else list(jset)
    NJ = len(jset)
    cols = []
    qrel = np.zeros((128, NJ * 2), np.float32)
    for jj, j in enumerate(jset):
        tq = 8 * j + core
        cols.append(xT[:, tq * 256:(tq + 1) * 256])
        ws = 256 * (8 * j + 8) - 2048
        for hf in range(2):
            qrel[:, jj * 2 + hf] = tq * 256 + hf * 128 + np.arange(128) - ws
    return dict(xT=xT, xq=np.ascontiguousarray(np.concatenate(cols, 1)), g=g, win=w_in, lng=lg, lnb=lb, qrel=qrel)


def _dsa_consts():
    kidx = np.tile(np.arange(2048, dtype=np.float32)[None, :], (128, 1))
    cst = np.zeros((128, 5, 128), np.float32)
    cst[:, 0] = np.eye(128)
    return dict(kidx=kidx, cst=cst)


def _dsa_gather(outs, L, jset=None, full=None):
    jset = list(range(L // 256 // 8)) if jset is None else list(jset)
    if full is None:
        full = np.zeros((1024, L), np.float32)
    for c, o in enumerate(outs):
        for jj, j in enumerate(jset):
            tq = 8 * j + c
            full[:, tq * 256:(tq + 1) * 256] = o[:, jj * 256:(jj + 1) * 256]
    return full


_PROGS = {}
DSA_SPLITS = ([0, 1, 2, 3, 4], [5, 6, 7])
GDN_SEG = 4096


def _prog(name, fn):
    if name not in _PROGS:
        _PROGS[name] = fn()
    return _PROGS[name]


def _run(nc, in_maps):
    res = run_bass_kernel_spmd(nc, in_maps, core_ids=list(range(8)))
    return [r["oT"] for r in res.results]


def _split(hT, TOK=2048):
    return [np.ascontiguousarray(hT[:, i * TOK:(i + 1) * TOK]) for i in range(8)]


def kernel(**inp):
    f32 = lambda a: np.ascontiguousarray(np.asarray(a, dtype=np.float32))
    L = 16384
    TOK = L // 8
    x = f32(inp["x"])[0]
    hT = np.ascontiguousarray(x.T)
    ident = np.eye(128, dtype=np.float32)

    def outproj(hT, mT, w):
        nc = _prog("outproj", lambda: build_outproj(TOK, 512))
        hs, ms = _split(hT), _split(mT)
        return np.concatenate(_run(nc, [dict(hT=hs[i], mT=ms[i], w=w) for i in range(8)]), axis=1)

    def mlp(hT, i, final=False, mT=None, wo=None):
        hs = _split(hT)
        g, w1, w2 = f32(inp["norm_mlp_g"][i]), f32(inp["mlp_w1"][i]), f32(inp["mlp_w2"][i])
        proj = mT is not None
        nc = _prog("mlp%d%d" % (final, proj), lambda: build_mlp(TOK, 256, final, proj))
        maps = [dict(hT=hs[c], g=g, w1=w1, w2=w2) for c in range(8)]
        if final:
            fg = f32(inp["final_g"])
            for m in maps:
                m["fg"] = fg
        if proj:
            ms = _split(mT)
            for c, m in enumerate(maps):
                m["mT"] = ms[c]
                m["wo"] = wo
        return np.concatenate(_run(nc, maps), axis=1)

    cst = _dsa_consts()
    g0, win0 = f32(inp["norm_mix_g"][0]), f32(inp["dsa_w_in"][0])
    lg, lb = f32(inp["dsa_idx_k_g"][0]), f32(inp["dsa_idx_k_b"][0])
    mT = None
    for js in DSA_SPLITS:
        nc = _prog("dsa" + str(js), lambda: build_dsa(L, 18, js))
        outs = _run(nc, [{**_dsa_inputs(hT, g0, win0, lg, lb, c, L, js), **cst} for c in range(8)])
        mT = _dsa_gather(outs, L, js, mT)
    hT = mlp(hT, 0, False, mT, f32(inp["dsa_w_out"][0]))
    nc = _prog("conv", lambda: build_conv(TOK, 256))
    hp = np.concatenate([np.zeros((1024, 30), np.float32), hT], axis=1)
    cw = dict(g=f32(inp["norm_mix_g"][1]), w1=f32(inp["conv_w_pw1"][0]), b1=f32(inp["conv_b_pw1"][0]),
              wdT=np.ascontiguousarray(f32(inp["conv_w_dw"][0]).T), bd=f32(inp["conv_b_dw"][0]),
              lg=f32(inp["conv_ln_g"][0]), lb=f32(inp["conv_ln_b"][0]), w2=f32(inp["conv_w_pw2"][0]),
              b2=f32(inp["conv_b_pw2"][0]), ident=ident)
    maps = [dict(hT=np.ascontiguousarray(hp[:, c * TOK:c * TOK + TOK + 30]),
                 hs=np.full((128, 1), 0.0 if c == 0 else 1.0, np.float32), **cw) for c in range(8)]
    hT = np.concatenate(_run(nc, maps), axis=1)
    hT = mlp(hT, 1)
    nc = _prog("mla", lambda: build_mla(L))
    cst = _mla_consts(L)
    maps = [{**_mla_inputs(hT, f32(inp["norm_mix_g"][2]), f32(inp["mla_w_in"][0]), f32(inp["mla_q_norm_g"][0]),
                           f32(inp["mla_w_uq"][0]), f32(inp["mla_kv_norm_g"][0]), f32(inp["mla_w_ukv"][0]), c), **cst}
            for c in range(8)]
    mT = np.concatenate(_run(nc, maps), axis=0)
    hT = mlp(hT, 2, False, mT, f32(inp["mla_w_out"][0]))
    LG = GDN_SEG
    nc = _prog("gdn", lambda: build_gdn(LG))
    cst = _gdn_consts()
    gi = [_gdn_inputs(None, f32(inp["norm_mix_g"][3]), f32(inp["gdn_w_in"][0]), f32(inp["gdn_conv_w"][0]),
                      f32(inp["gdn_a_log"][0]), f32(inp["gdn_dt_bias"][0]), f32(inp["gdn_o_norm_g"][0]), c) for c in range(8)]
    st = [np.zeros((128, 128), np.float32) for _ in range(8)]
    hl = [np.zeros((128, 3, 3), np.float32) for _ in range(8)]
    segs = []
    for s0 in range(0, L, LG):
        hseg = np.ascontiguousarray(hT[:, s0:s0 + LG])
        maps = [{**gi[c], **cst, "hT": hseg, "s_in": st[c], "h_in": hl[c]} for c in range(8)]
        res = run_bass_kernel_spmd(nc, maps, core_ids=list(range(8))).results
        segs.append(np.concatenate([r["oT"] for r in res], axis=0))
        st = [np.ascontiguousarray(r["s_out"]) for r in res]
        hl = [np.ascontiguousarray(r["h_out"]) for r in res]
    mT = np.concatenate(segs, axis=1)
    hT = mlp(hT, 3, True, mT, f32(inp["gdn_w_out"][0]))
    return np.ascontiguousarray(hT.T)[None].astype(np.float32)
```
